# Optimizing a Trainium2 kernel written in Bass

```python
import jax, jax.numpy as jnp
from jax import lax
import numpy as np

D_MODEL = 1024
BATCH = 16
SEQ = 4096
DEPTH = 1
DEC_BATCH = 16
DEC_SEQ = 2048
PAST_LEN = 128

POOL_WIDTH = D_MODEL // 2
POOL_GROUPS = 4
POOL_GROUP_DIM = POOL_WIDTH // POOL_GROUPS
POOL_WINDOWS = (2, 4, 8, 16)
ATTN_HEADS = 4
QK_HEAD_DIM = 64
V_HEAD_DIM = 2 * QK_HEAD_DIM
ATTN_WIDTH = ATTN_HEADS * V_HEAD_DIM
MIX_WIDTH = POOL_WIDTH + ATTN_WIDTH
QK_WIDTH = ATTN_HEADS * 2 * QK_HEAD_DIM
IN_WIDTH = POOL_WIDTH + 2 * QK_WIDTH + ATTN_WIDTH
ROPE_THETA = 10000.0
Q_BLOCK = 128
MOE_GROUPS = 4
EXPERTS_PER_GROUP = 8
N_EXPERTS = MOE_GROUPS * EXPERTS_PER_GROUP
TOP_K = 2
D_FF_EXPERT = 512
ROW_BLOCK = 128
EPS = 1e-6

kernel_name = "hymba_pool_diffattn_hmoe_encoder"


def rmsnorm(x, g):
    xf = x.astype(jnp.float32)
    y = xf * lax.rsqrt(jnp.mean(xf * xf, axis=-1, keepdims=True) + EPS)
    return (y * g.astype(jnp.float32)).astype(x.dtype)


def rope_tables(n):
    inv_freq = ROPE_THETA ** (-jnp.arange(0, QK_HEAD_DIM, 2, dtype=jnp.float32) / QK_HEAD_DIM)
    ang = jnp.arange(n, dtype=jnp.float32)[:, None] * inv_freq[None, :]
    return jnp.cos(ang), jnp.sin(ang)


def apply_rope(x, cos, sin):
    xf = x.astype(jnp.float32)
    c = cos[None, :, None, None, :]
    s = sin[None, :, None, None, :]
    x1, x2 = jnp.split(xf, 2, axis=-1)
    return jnp.concatenate([x1 * c - x2 * s, x2 * c + x1 * s], axis=-1).astype(x.dtype)


def pool_mixer(z, w_pool, pool_scale):
    B, N, _ = z.shape
    zg = z.reshape(B, N, POOL_GROUPS, POOL_GROUP_DIM).astype(jnp.float32)
    cs = jnp.concatenate([jnp.zeros((B, 1, POOL_GROUPS, POOL_GROUP_DIM), jnp.float32),
                          jnp.cumsum(zg, axis=1)], axis=1)
    t = jnp.arange(N)[:, None]
    half = jnp.array(POOL_WINDOWS, dtype=jnp.int32)[None, :] // 2
    lo = jnp.clip(t - half, 0, N)
    hi = jnp.clip(t + half, 0, N)
    gi = jnp.arange(POOL_GROUPS)
    win_sum = cs[:, hi, gi] - cs[:, lo, gi]
    cnt = (hi - lo).astype(jnp.float32)[None, :, :, None]
    pooled = (win_sum / cnt - zg).astype(z.dtype)
    out = jnp.einsum('bngc,gce->bnge', pooled, w_pool)
    return out.reshape(B, N, POOL_WIDTH) * pool_scale


def diff_attention(q, k, v, lam, subln_g, lam_init):
    B, N = q.shape[0], q.shape[1]
    nb = N // Q_BLOCK
    qb = q.reshape(B, nb, Q_BLOCK, ATTN_HEADS, 2, QK_HEAD_DIM).transpose(1, 0, 2, 3, 4, 5)
    scale = QK_HEAD_DIM ** -0.5

    def block(qblk):
        s = jnp.einsum('bqhcd,bkhcd->bhcqk', qblk, k, preferred_element_type=jnp.float32) * scale
        p = jax.nn.softmax(s, axis=-1)
        w = p[:, :, 0] - lam * p[:, :, 1]
        o = jnp.einsum('bhqk,bkhe->bqhe', w.astype(v.dtype), v, preferred_element_type=jnp.float32)
        return o.astype(v.dtype)

    o = lax.map(block, qb)
    o = o.transpose(1, 0, 2, 3, 4).reshape(B, N, ATTN_HEADS, V_HEAD_DIM)
    o = rmsnorm(o, subln_g) * (1.0 - lam_init)
    return o.reshape(B, N, ATTN_WIDTH)


def hier_moe(h, w_rg, b_rg, w_re, b_re, w_gate, w_up, w_down):
    B, N, D = h.shape
    T = B * N
    xt = h.reshape(T, D)
    pg = jax.nn.softmax((xt @ w_rg).astype(jnp.float32) + b_rg.astype(jnp.float32), axis=-1)
    g_top, g_idx = lax.top_k(pg, 1)
    le = ((xt @ w_re).astype(jnp.float32) + b_re.astype(jnp.float32)).reshape(T, MOE_GROUPS, EXPERTS_PER_GROUP)
    le_sel = le[jnp.arange(T), g_idx[:, 0]]
    pe = jax.nn.softmax(le_sel, axis=-1)
    e_top, e_loc = lax.top_k(pe, TOP_K)
    gates = g_top * (e_top / jnp.sum(e_top, axis=-1, keepdims=True))
    eid = g_idx * EXPERTS_PER_GROUP + e_loc

    A = T * TOP_K
    flat_e = eid.reshape(A)
    flat_g = gates.reshape(A)
    flat_t = jnp.repeat(jnp.arange(T, dtype=jnp.int32), TOP_K)
    order = jnp.argsort(flat_e)
    se = flat_e[order]
    counts = jnp.bincount(flat_e, length=N_EXPERTS)
    padded = (counts + ROW_BLOCK - 1) // ROW_BLOCK * ROW_BLOCK
    start = jnp.cumsum(counts) - counts
    pad_end = jnp.cumsum(padded)
    pad_start = pad_end - padded
    dest = pad_start[se] + (jnp.arange(A) - start[se])
    P = -(-A // ROW_BLOCK) * ROW_BLOCK + N_EXPERTS * ROW_BLOCK
    n_blk = P // ROW_BLOCK
    row_tok = jnp.full((P,), T, jnp.int32).at[dest].set(flat_t[order])
    row_gate = jnp.zeros((P,), jnp.float32).at[dest].set(flat_g[order])
    blk_e = jnp.minimum(jnp.searchsorted(pad_end, jnp.arange(n_blk) * ROW_BLOCK, side='right'),
                        N_EXPERTS - 1)
    xpad = jnp.concatenate([xt, jnp.zeros((1, D), xt.dtype)], axis=0)
    xb = xpad[row_tok].reshape(n_blk, ROW_BLOCK, D)

    def expert_block(args):
        xblk, e = args
        hid = jax.nn.silu(xblk @ w_gate[e]) * (xblk @ w_up[e])
        return hid @ w_down[e]

    yb = lax.map(expert_block, (xb, blk_e)).reshape(P, D)
    y = jax.ops.segment_sum(yb * row_gate[:, None].astype(yb.dtype), row_tok, num_segments=T + 1)[:T]
    return y.reshape(B, N, D).astype(h.dtype)


def encoder_layer(x, cos, sin, layer, lam_init, g_mix, w_in, w_pool, pool_scale,
                  lambda_q1, lambda_k1, lambda_q2, lambda_k2, subln_g, w_out, g_ffn,
                  w_router_group, b_router_group, w_router_expert, b_router_expert,
                  w_gate, w_up, w_down):
    B, N, _ = x.shape
    l = layer
    xn = rmsnorm(x, g_mix[l])
    z = xn @ w_in[l]
    z_pool = z[..., :POOL_WIDTH]
    q = z[..., POOL_WIDTH:POOL_WIDTH + QK_WIDTH].reshape(B, N, ATTN_HEADS, 2, QK_HEAD_DIM)
    k = z[..., POOL_WIDTH + QK_WIDTH:POOL_WIDTH + 2 * QK_WIDTH].reshape(B, N, ATTN_HEADS, 2, QK_HEAD_DIM)
    v = z[..., POOL_WIDTH + 2 * QK_WIDTH:].reshape(B, N, ATTN_HEADS, V_HEAD_DIM)
    pool_out = pool_mixer(z_pool, w_pool[l], pool_scale[l])
    q = apply_rope(q, cos, sin)
    k = apply_rope(k, cos, sin)
    f32 = jnp.float32
    lam = (jnp.exp(jnp.sum(lambda_q1[l].astype(f32) * lambda_k1[l].astype(f32)))
           - jnp.exp(jnp.sum(lambda_q2[l].astype(f32) * lambda_k2[l].astype(f32))) + lam_init)
    attn_out = diff_attention(q, k, v, lam, subln_g[l], lam_init)
    h = x + jnp.concatenate([pool_out, attn_out], axis=-1) @ w_out[l]
    ff = hier_moe(rmsnorm(h, g_ffn[l]), w_router_group[l], b_router_group[l],
                  w_router_expert[l], b_router_expert[l], w_gate[l], w_up[l], w_down[l])
    return h + ff


def run_trunk(x, g_mix, w_in, w_pool, pool_scale, lambda_q1, lambda_k1, lambda_q2, lambda_k2,
              subln_g, w_out, g_ffn, w_router_group, b_router_group, w_router_expert,
              b_router_expert, w_gate, w_up, w_down, g_final):
    cos, sin = rope_tables(x.shape[1])
    for layer in range(DEPTH):
        lam_init = 0.8 - 0.6 * float(np.exp(-0.3 * layer))
        x = encoder_layer(x, cos, sin, layer, lam_init, g_mix, w_in, w_pool, pool_scale,
                          lambda_q1, lambda_k1, lambda_q2, lambda_k2, subln_g, w_out, g_ffn,
                          w_router_group, b_router_group, w_router_expert, b_router_expert,
                          w_gate, w_up, w_down)
    return rmsnorm(x, g_final)


def setup_inputs(seed: int = 0) -> dict:
    key = jax.random.key(seed)
    ks = jax.random.split(key, 24)
    nrm = lambda k, shape, s: jax.random.normal(k, shape, jnp.float32) * s
    L, D = DEPTH, D_MODEL
    return {
        "x_prompt": nrm(ks[0], (BATCH, SEQ, D), 1.0),
        "x_sample": nrm(ks[1], (DEC_BATCH, DEC_SEQ, D), 1.0),
        "g_mix": 1.0 + nrm(ks[2], (L, D), 0.02),
        "w_in": nrm(ks[3], (L, D, IN_WIDTH), D ** -0.5),
        "w_pool": nrm(ks[4], (L, POOL_GROUPS, POOL_GROUP_DIM, POOL_GROUP_DIM), POOL_GROUP_DIM ** -0.5),
        "pool_scale": 1.0 + nrm(ks[5], (L, POOL_WIDTH), 0.02),
        "lambda_q1": nrm(ks[6], (L, QK_HEAD_DIM), 0.1),
        "lambda_k1": nrm(ks[7], (L, QK_HEAD_DIM), 0.1),
        "lambda_q2": nrm(ks[8], (L, QK_HEAD_DIM), 0.1),
        "lambda_k2": nrm(ks[9], (L, QK_HEAD_DIM), 0.1),
        "subln_g": 1.0 + nrm(ks[10], (L, V_HEAD_DIM), 0.02),
        "w_out": nrm(ks[11], (L, MIX_WIDTH, D), MIX_WIDTH ** -0.5),
        "g_ffn": 1.0 + nrm(ks[12], (L, D), 0.02),
        "w_router_group": nrm(ks[13], (L, D, MOE_GROUPS), D ** -0.5),
        "b_router_group": nrm(ks[14], (L, MOE_GROUPS), 0.01),
        "w_router_expert": nrm(ks[15], (L, D, N_EXPERTS), D ** -0.5),
        "b_router_expert": nrm(ks[16], (L, N_EXPERTS), 0.01),
        "w_gate": nrm(ks[17], (L, N_EXPERTS, D, D_FF_EXPERT), D ** -0.5),
        "w_up": nrm(ks[18], (L, N_EXPERTS, D, D_FF_EXPERT), D ** -0.5),
        "w_down": nrm(ks[19], (L, N_EXPERTS, D_FF_EXPERT, D), D_FF_EXPERT ** -0.5),
        "g_final": 1.0 + nrm(ks[20], (D,), 0.02),
    }


def reference(x_prompt, x_sample, g_mix, w_in, w_pool, pool_scale, lambda_q1, lambda_k1,
              lambda_q2, lambda_k2, subln_g, w_out, g_ffn, w_router_group, b_router_group,
              w_router_expert, b_router_expert, w_gate, w_up, w_down, g_final):
    y_prompt = run_trunk(x_prompt, g_mix, w_in, w_pool, pool_scale, lambda_q1, lambda_k1,
                         lambda_q2, lambda_k2, subln_g, w_out, g_ffn, w_router_group,
                         b_router_group, w_router_expert, b_router_expert, w_gate, w_up,
                         w_down, g_final)
    y_sample = run_trunk(x_sample, g_mix, w_in, w_pool, pool_scale, lambda_q1, lambda_k1,
                         lambda_q2, lambda_k2, subln_g, w_out, g_ffn, w_router_group,
                         b_router_group, w_router_expert, b_router_expert, w_gate, w_up,
                         w_down, g_final)
    return (y_prompt, y_sample)
```

```python
import contextlib
import numpy as np
import ml_dtypes
import concourse.bass as bass
import concourse.mybir as mybir
from concourse.bass_utils import run_bass_kernel_spmd

F32 = mybir.dt.float32
BF16 = mybir.dt.bfloat16
ALU = mybir.AluOpType
AF = mybir.ActivationFunctionType
AX = mybir.AxisListType

D = 1024
NE = 32
DFF = 512
EPS = 1e-6
SEM_WRAP = 30000
LAM_INIT = 0.2


class Res:
    __slots__ = ("name", "last_w", "readers", "dma_sem", "dma_cnt")

    def __init__(self, name):
        self.name = name
        self.last_w = None
        self.readers = []
        self.dma_sem = None
        self.dma_cnt = 0


class Eng:
    def __init__(self, fw, name, obj, is_pe=False):
        self.fw = fw
        self.name = name
        self.obj = obj
        self.is_pe = is_pe
        self.n = 0
        self.sems = []
        self.waited = {}

    def cur_sem(self):
        idx = self.n // SEM_WRAP
        while len(self.sems) <= idx:
            self.sems.append(self.fw.new_sem(f"tl_{self.name}_{len(self.sems)}"))
        return self.sems[idx]

    def wait_tokens(self, toks):
        best = {}
        for (sk, v) in toks:
            if v > best.get(sk, 0):
                best[sk] = v
        for sk, v in best.items():
            if self.waited.get(sk, 0) >= v:
                continue
            self.waited[sk] = v
            self.obj.wait_ge(self.fw.sem_by_key[sk], v)


class FW:
    def __init__(self, nc, stack):
        self.nc = nc
        self.sem_by_key = {}
        self._stack = stack
        self.all_res = []
        self.dma_reg = {}
        self.pe = Eng(self, "pe", nc.tensor, is_pe=True)
        self.act = Eng(self, "act", nc.scalar)
        self.dve = Eng(self, "dve", nc.vector)
        self.pool = Eng(self, "pool", nc.gpsimd)
        self.sp = Eng(self, "sp", nc.sync)
        self.engs = [self.pe, self.act, self.dve, self.pool, self.sp]

    def res(self, name):
        r = Res(name)
        self.all_res.append(r)
        return r

    def new_sem(self, name):
        h = self._stack.enter_context(self.nc.semaphore(name))
        key = len(self.sem_by_key)
        self.sem_by_key[key] = h
        return key

    def op(self, eng, fn, reads=(), writes=()):
        deps = []
        own = set(eng.sems)
        for r in reads:
            if r.last_w is not None:
                deps.append(r.last_w)
        for w in writes:
            if w.last_w is not None:
                deps.append(w.last_w)
            for t in w.readers:
                if t[0] in own:
                    continue
                deps.append(t)
        if eng.is_pe:
            deps = [t for t in deps if t[0] not in own]
        eng.wait_tokens(deps)
        ins = fn(eng.obj)
        sk = eng.cur_sem()
        val = (eng.n % SEM_WRAP) + 1
        eng.n += 1
        ins.then_inc(self.sem_by_key[sk], 1)
        tok = (sk, val)
        for r in reads:
            r.readers.append(tok)
        for w in writes:
            w.last_w = tok
            w.readers = []
        return tok

    def dma(self, q, out, in_, sb_res, reads=(), writes=()):
        deps = []
        for r in reads:
            if r.last_w is not None:
                deps.append(r.last_w)
        for w in writes:
            if w.last_w is not None:
                deps.append(w.last_w)
            deps.extend(w.readers)
        q.wait_tokens(deps)
        ent = self.dma_reg.setdefault(sb_res.name, [None, 0])
        if ent[0] is None:
            ent[0] = self.new_sem(f"dma_{sb_res.name}")
        ins = q.obj.dma_start(out=out, in_=in_)
        ent[1] += 16
        ins.then_inc(self.sem_by_key[ent[0]], 16)
        tok = (ent[0], ent[1])
        for r in reads:
            r.readers.append(tok)
        for w in writes:
            w.last_w = tok
            w.readers = []
        return tok

    def all_tokens(self):
        toks = []
        for r in self.all_res:
            if r.last_w is not None:
                toks.append(r.last_w)
            toks.extend(r.readers)
        for e in self.engs:
            if e.n > 0:
                idx = (e.n - 1) // SEM_WRAP
                toks.append((e.sems[idx], ((e.n - 1) % SEM_WRAP) + 1))
        return toks

    def barrier(self):
        toks = self.all_tokens()
        for e in self.engs:
            e.wait_tokens(toks)


import os as _os
_DBG = _os.environ.get("KDBG", "full")


def build_program(seq_lens, sg_tok):
    T = sum(seq_lens)
    NT = T // 128
    NG = T // 512
    max_nt = max(seq_lens) // 128
    assert all(n % 512 == 0 for n in seq_lens)
    assert T % sg_tok == 0 and sg_tok % 512 == 0
    nc = bass.Bass("TRN2", target_bir_lowering=False)

    def din(name, shape, dt=F32):
        return nc.dram_tensor(name, list(shape), dt, kind="ExternalInput").ap()

    x = din("x", [T, D])
    w_in = din("w_in", [D, 2048])
    w_out = din("w_out", [D, D])
    w_pool = din("w_pool", [4, 128, 128])
    w_gate = din("w_gate", [NE * 128, 8 * DFF])
    w_up = din("w_up", [NE * 128, 8 * DFF])
    w_down = din("w_down", [NE * 128, 4 * D])
    w_r = din("w_r", [D, 36])
    gmix_t = din("gmix_t", [128, 8])
    wos_t = din("wos_t", [128, 8])
    gffn_b = din("gffn_b", [128, D])
    gfin_b = din("gfin_b", [128, D])
    bias_b = din("bias_b", [128, 36])
    lam_b = din("lam_b", [128, 4, 64])
    ident_bf_d = din("ident_bf", [128, 128], BF16)
    ident_f_d = din("ident_f", [128, 128])
    cs_tab_d = din("cs_tab", [128, max_nt, 2, 32])
    invc_d = din("invc", [128, 3, 4, 128])
    y = nc.dram_tensor("y", [T, D], F32, kind="ExternalOutput").ap()
    qd = nc.dram_tensor("qd", [NG, 128, 4, 512], BF16, kind="Internal").ap()
    pd = nc.dram_tensor("pd", [NG, 128, 4, 4, 128], BF16, kind="Internal").ap()
    h_d = nc.dram_tensor("h_d", [NT, 128, D], F32, kind="Internal").ap()
    hn_d = nc.dram_tensor("hn_d", [NT, 128, D], BF16, kind="Internal").ap()
    NB = (2 * T) // 128 + NE
    PR = NB * 128
    xg = nc.dram_tensor("xg", [PR, D], BF16, kind="Internal").ap()
    yg = nc.dram_tensor("yg", [PR, D], F32, kind="Internal").ap()
    tri_d = din("tri_bf", [128, 128], BF16)
    bstart_d = din("bstart", [128, NB])
    pidx_d = din("pidx", [128, 1])

    with contextlib.ExitStack() as st0:
        fw = FW(nc, st0)
        R = fw.res

        sfx = [""]

        def sb(st, name, shape, dt):
            return st.enter_context(nc.sbuf_tensor("s_" + name + sfx[0], list(shape), dt))

        lg_all = sb(st0, "lg_all", [128, NT, 36], F32)
        ones_bf = sb(st0, "ones_bf", [128, 128], BF16)
        ones_f = sb(st0, "ones_f", [128, 128], F32)
        r_gates = [R(f"gates{i}") for i in range(NT)]
        r_gfin = R("gfin")
        r_ones = R("ones")
        fw.op(fw.dve, lambda e: e.memset(ones_f[:], 1.0), writes=[r_ones])
        fw.op(fw.dve, lambda e: e.tensor_copy(out=ones_bf[:], in_=ones_f[:]), reads=[r_ones], writes=[r_ones])

        with contextlib.ExitStack() as st:
            S0 = st.enter_context(nc.psum_tensor("S0", [128, 2, 512], F32))
            S1 = st.enter_context(nc.psum_tensor("S1", [128, 2, 512], F32))
            PO = st.enter_context(nc.psum_tensor("PO", [128, 2, 512], F32))
            PL = st.enter_context(nc.psum_tensor("PL", [128, 2, 512], F32))
            r_S0, r_S1, r_PO, r_PL = R("S0"), R("S1"), R("PO"), R("PL")
            r_S0a, r_S0b, r_S1a, r_S1b = R("S0a"), R("S0b"), R("S1a"), R("S1b")
            r_POa, r_POb, r_PLa, r_PLb = R("POa"), R("POb"), R("PLa"), R("PLb")
            r_psr = [R("psr0"), R("psr1")]

            Win = sb(st, "Win", [128, 8, 2048], BF16)
            Wout = sb(st, "Wout", [128, 8, 1024], BF16)
            Wp = sb(st, "Wp", [128, 4, 128], BF16)
            Wr = sb(st, "Wr", [128, 8, 36], F32)
            gmix = sb(st, "gmix", [128, 8], F32)
            wos = sb(st, "wos", [128, 8], F32)
            gffn = sb(st, "gffn", [128, D], F32)
            biasb = sb(st, "biasb", [128, 36], F32)
            lamt = sb(st, "lamt", [128, 4, 64], F32)
            lamp = sb(st, "lamp", [128, 2, 64], F32)
            lams = sb(st, "lams", [128, 2], F32)
            neglam = sb(st, "neglam", [128, 1], F32)
            ident_bf = sb(st, "ident_bf", [128, 128], BF16)
            ident_f = sb(st, "ident_f", [128, 128], F32)
            invc = sb(st, "invc", [128, 3, 4, 128], F32)
            st_w = contextlib.ExitStack()
            wstage = sb(st_w, "wstage", [128, 2048], F32)
            r_Win, r_Wout, r_Wp, r_Wr, r_wstage = R("Win"), R("Wout"), R("Wp"), R("Wr"), R("wstage")
            r_c = R("consts")
            r_lam = R("lam")
            for (t_sb, t_dr) in ((gmix, gmix_t), (wos, wos_t), (gffn, gffn_b), (biasb, bias_b),
                                 (ident_bf, ident_bf_d), (ident_f, ident_f_d), (invc, invc_d)):
                fw.dma(fw.sp, t_sb[:], t_dr, r_c, writes=[r_c])
            fw.dma(fw.sp, lamt[:], lam_b, r_lam, writes=[r_lam])
            fw.dma(fw.sp, Wr[:], w_r.rearrange("(c p) j -> p c j", p=128), r_Wr, writes=[r_Wr])
            fw.dma(fw.pool, Wp[:], w_pool.rearrange("g c e -> c g e"), r_Wp, writes=[r_Wp])
            for c in range(8):
                fw.dma(fw.sp, wstage[:], w_in[c * 128:(c + 1) * 128, :], r_wstage, writes=[r_wstage])
                fw.op(fw.act, lambda e, c=c: e.activation(out=Win[:, c, :], in_=wstage[:], func=AF.Copy,
                                                          scale=gmix[:, c:c + 1]),
                      reads=[r_wstage, r_c], writes=[r_Win])
            for c in range(8):
                fw.dma(fw.sp, wstage[:, 0:1024], w_out[c * 128:(c + 1) * 128, :], r_wstage, writes=[r_wstage])
                fw.op(fw.act, lambda e, c=c: e.activation(out=Wout[:, c, :], in_=wstage[:, 0:1024], func=AF.Copy,
                                                          scale=wos[:, c:c + 1]),
                      reads=[r_wstage, r_c], writes=[r_Wout])
            lam4 = lamt[:].rearrange("p (a b) d -> p a b d", b=2)
            fw.op(fw.dve, lambda e: e.tensor_tensor(out=lamp[:], in0=lam4[:, :, 0, :], in1=lam4[:, :, 1, :], op=ALU.mult),
                  reads=[r_lam], writes=[r_lam])
            fw.op(fw.dve, lambda e: e.tensor_reduce(out=lams[:], in_=lamp[:], axis=AX.X, op=ALU.add),
                  reads=[r_lam], writes=[r_lam])
            fw.op(fw.act, lambda e: e.activation(out=lams[:], in_=lams[:], func=AF.Exp), reads=[r_lam], writes=[r_lam])
            fw.op(fw.dve, lambda e: e.tensor_tensor(out=neglam[:], in0=lams[:, 1:2], in1=lams[:, 0:1], op=ALU.subtract),
                  reads=[r_lam], writes=[r_lam])
            fw.op(fw.dve, lambda e: e.tensor_scalar(out=neglam[:], in0=neglam[:], scalar1=-LAM_INIT, scalar2=None,
                                                    op0=ALU.add), reads=[r_lam], writes=[r_lam])

            fw.barrier()
            st_w.close()
            max_n = max(seq_lens)
            kT = sb(st, "kT", [128, 4, max_n], BF16)
            V = sb(st, "V", [128, max_n // 128, 512], BF16)
            r_kT, r_V = R("kT"), R("V")


            def rstd_chain(src, dst, res_list, inv_n):
                fw.op(fw.dve, lambda e: e.tensor_scalar(out=dst, in0=src, scalar1=inv_n, scalar2=EPS,
                                                        op0=ALU.mult, op1=ALU.add), reads=res_list, writes=res_list)
                fw.op(fw.act, lambda e: e.activation(out=dst, in_=dst, func=AF.Ln), reads=res_list, writes=res_list)
                fw.op(fw.act, lambda e: e.activation(out=dst, in_=dst, func=AF.Exp, scale=-0.5),
                      reads=res_list, writes=res_list)

            psT = PO[:, 0, :].bitcast(BF16).rearrange("p (c t) -> p c t", c=8)
            psT2 = PO[:, 1, :].bitcast(BF16).rearrange("p (c t) -> p c t", c=8)
            ps_qk = S0[:].rearrange("p a (b d) -> p (a b) d", d=32)
            ps_qk4 = S0[:].rearrange("p a (b h d) -> p (a b) h d", h=2, d=32)
            ps_v = S1[:, 0, :]
            ps_p = S1[:, 1, :].rearrange("p (g t) -> p g t", g=4)
            ps_po = PL[:, 0, :].rearrange("p (g t) -> p g t", g=4)

            tok0 = 0
            g0 = 0
            for si, N in enumerate(seq_lens):
                nt = N // 128
                ng = N // 512

                sfx[0] = f"_s{si}"
                st1 = contextlib.ExitStack()
                cs_tab = sb(st1, "cs_tab", [128, max_nt, 2, 32], F32)
                r_cs = R("cs_tab")
                fw.dma(fw.sp, cs_tab[:], cs_tab_d, r_cs, writes=[r_cs])
                xt = [sb(st1, f"xt{i}", [128, D], F32) for i in range(2)]
                r_xt = [R(f"xt{i}") for i in range(2)]
                junk = sb(st1, "junk", [128, D], BF16)
                r_junk = R("junk")
                ss = [sb(st1, f"ss{i}", [128, 1], F32) for i in range(2)]
                r_ss = [R(f"ss{i}") for i in range(2)]
                xs = [sb(st1, f"xs{i}", [128, D], BF16) for i in range(2)]
                r_xs = [R(f"xs{i}") for i in range(2)]
                xnT = [sb(st1, f"xnT{i}", [128, 8, 128], BF16) for i in range(2)]
                r_xnT = [R(f"xnT{i}") for i in range(2)]
                rp = [sb(st1, f"rp{i}", [128, 16, 32], F32) for i in range(4)]
                r_rp = R("rp")
                qkr = sb(st1, "qkr", [128, 16, 2, 32], BF16)
                r_qkr = R("qkr")
                qst = [sb(st1, f"qst{i}", [128, 4, 128], BF16) for i in range(2)]
                r_qst = [R(f"qst{i}") for i in range(2)]
                pst = [sb(st1, f"pst{i}", [128, 4, 128], BF16) for i in range(2)]
                r_pst = [R(f"pst{i}") for i in range(2)]
                zpt = [sb(st1, f"zpt{i}", [128, 4, 128], F32) for i in range(3)]
                r_zpt = [R(f"zpt{i}") for i in range(3)]
                ZW = sb(st1, "ZW", [128, 4, 144], F32)
                za = sb(st1, "za", [128, 4, 144], F32)
                zb = sb(st1, "zb", [128, 4, 144], F32)
                zc = sb(st1, "zc", [128, 4, 144], F32)
                zd = sb(st1, "zd", [128, 4, 144], F32)
                pw = sb(st1, "pw", [128, 4, 128], F32)
                pooled = sb(st1, "pooled", [128, 4, 128], BF16)
                r_pm = R("poolmix")
                r_pooled = R("pooled")

                def pool_stage(i):
                    cur = zpt[i % 3]
                    deps_r = [r_zpt[i % 3]]
                    if i > 0:
                        prev = zpt[(i - 1) % 3]
                        deps_r.append(r_zpt[(i - 1) % 3])
                        fw.op(fw.pool, lambda e: e.tensor_copy(out=ZW[:, :, 0:8], in_=prev[:, :, 120:128]),
                              reads=deps_r, writes=[r_pm])
                    else:
                        fw.op(fw.pool, lambda e: e.memset(ZW[:, :, 0:8], 0.0), writes=[r_pm])
                    fw.op(fw.pool, lambda e: e.tensor_copy(out=ZW[:, :, 8:136], in_=cur[:]), reads=deps_r, writes=[r_pm])
                    if i < nt - 1:
                        nxt = zpt[(i + 1) % 3]
                        fw.op(fw.pool, lambda e: e.tensor_copy(out=ZW[:, :, 136:144], in_=nxt[:, :, 0:8]),
                              reads=[r_zpt[(i + 1) % 3]], writes=[r_pm])
                    else:
                        fw.op(fw.pool, lambda e: e.memset(ZW[:, :, 136:144], 0.0), writes=[r_pm])
                    P = fw.pool
                    fw.op(P, lambda e: e.tensor_tensor(out=za[:, :, 0:143], in0=ZW[:, :, 0:143], in1=ZW[:, :, 1:144], op=ALU.add),
                          reads=[r_pm], writes=[r_pm])
                    fw.op(P, lambda e: e.tensor_tensor(out=zb[:, :, 0:141], in0=za[:, :, 0:141], in1=za[:, :, 2:143], op=ALU.add),
                          reads=[r_pm], writes=[r_pm])
                    fw.op(P, lambda e: e.tensor_tensor(out=zc[:, :, 0:137], in0=zb[:, :, 0:137], in1=zb[:, :, 4:141], op=ALU.add),
                          reads=[r_pm], writes=[r_pm])
                    fw.op(P, lambda e: e.tensor_tensor(out=zd[:, :, 0:129], in0=zc[:, :, 0:129], in1=zc[:, :, 8:137], op=ALU.add),
                          reads=[r_pm], writes=[r_pm])
                    kind = 0 if i == 0 else (2 if i == nt - 1 else 1)
                    srcs = [za[:, 0, 7:135], zb[:, 1, 6:134], zc[:, 2, 4:132], zd[:, 3, 0:128]]
                    for g in range(4):
                        fw.op(P, lambda e, g=g: e.tensor_tensor(out=pw[:, g, :], in0=srcs[g], in1=invc[:, kind, g, :], op=ALU.mult),
                              reads=[r_pm, r_c], writes=[r_pm])
                    fw.op(P, lambda e: e.tensor_tensor(out=pooled[:], in0=pw[:], in1=cur[:], op=ALU.subtract),
                          reads=[r_pm, r_zpt[i % 3]], writes=[r_pooled])

                    def mm_pool(e):
                        ins = None
                        for g in range(4):
                            ins = e.matmul(ps_po[:, g, :], Wp[:, g, :], pooled[:, g, :], start=True, stop=True)
                        return ins
                    fw.op(fw.pe, mm_pool, reads=[r_pooled, r_Wp], writes=[r_PLa])
                    sl = i % 2
                    fw.op(fw.act, lambda e: e.activation(out=pst[sl][:], in_=ps_po, func=AF.Copy),
                          reads=[r_PLa], writes=[r_pst[sl]])
                    g_idx = g0 + i // 4
                    fw.dma(fw.act, pd[g_idx, :, i % 4, :, :], pst[sl][:], r_pst[sl], reads=[r_pst[sl]])

                def stageA(i):
                    sl = i % 2
                    fw.dma(fw.sp, xt[sl][:], x[tok0 + i * 128: tok0 + (i + 1) * 128, :], r_xt[sl], writes=[r_xt[sl]])
                    fw.op(fw.act, lambda e: e.activation(out=junk[:], in_=xt[sl][:], func=AF.Square, accum_out=ss[sl][:]),
                          reads=[r_xt[sl]], writes=[r_junk, r_ss[sl]])
                    rstd_chain(ss[sl][:], ss[sl][:], [r_ss[sl]], 1.0 / D)
                    fw.op(fw.act, lambda e: e.activation(out=xs[sl][:], in_=xt[sl][:], func=AF.Copy, scale=ss[sl][:]),
                          reads=[r_xt[sl], r_ss[sl]], writes=[r_xs[sl]])

                    def tr1(e):
                        ins = None
                        for c in range(8):
                            ins = e.transpose(psT[:, c, :], xs[sl][:, c * 128:(c + 1) * 128], ident_bf[:])
                        return ins
                    fw.op(fw.pe, tr1, reads=[r_xs[sl], r_c], writes=[r_POa])
                    fw.op(fw.dve, lambda e: e.tensor_copy(out=xnT[sl][:], in_=psT), reads=[r_POa], writes=[r_xnT[sl]])

                def stageB(i):
                    sl = i % 2

                    def mm_qkv(e):
                        ins = None
                        for j, dst in enumerate((S0[:, 0, :], S0[:, 1, :], ps_v)):
                            for c in range(8):
                                ins = e.matmul(dst, xnT[sl][:, c, :], Win[:, c, 512 + j * 512: 1024 + j * 512],
                                               start=(c == 0), stop=(c == 7))
                        for g in range(4):
                            for c in range(8):
                                ins = e.matmul(ps_p[:, g, :], Win[:, c, g * 128:(g + 1) * 128], xnT[sl][:, c, :],
                                               start=(c == 0), stop=(c == 7))
                        return ins
                    fw.op(fw.pe, mm_qkv, reads=[r_xnT[sl], r_Win], writes=[r_S0a, r_S0b, r_S1a, r_S1b])
                    fw.op(fw.act, lambda e: e.activation(out=V[:, i, :], in_=ps_v, func=AF.Copy), reads=[r_S1a], writes=[r_V])
                    fw.op(fw.act, lambda e: e.activation(out=zpt[i % 3][:], in_=ps_p, func=AF.Copy),
                          reads=[r_S1b], writes=[r_zpt[i % 3]])
                    cosb = cs_tab[:, i, 0, :].unsqueeze(1).broadcast_to([128, 16, 32])
                    sinb = cs_tab[:, i, 1, :].unsqueeze(1).broadcast_to([128, 16, 32])
                    x1 = ps_qk4[:, :, 0, :]
                    x2 = ps_qk4[:, :, 1, :]
                    fw.op(fw.dve, lambda e: e.tensor_tensor(out=rp[0][:], in0=x1, in1=cosb, op=ALU.mult), reads=[r_S0a, r_S0b, r_cs], writes=[r_rp])
                    fw.op(fw.dve, lambda e: e.tensor_tensor(out=rp[1][:], in0=x2, in1=sinb, op=ALU.mult), reads=[r_S0a, r_S0b, r_cs], writes=[r_rp])
                    fw.op(fw.dve, lambda e: e.tensor_tensor(out=rp[2][:], in0=x2, in1=cosb, op=ALU.mult), reads=[r_S0a, r_S0b, r_cs], writes=[r_rp])
                    fw.op(fw.dve, lambda e: e.tensor_tensor(out=rp[3][:], in0=x1, in1=sinb, op=ALU.mult), reads=[r_S0a, r_S0b, r_cs], writes=[r_rp])
                    fw.op(fw.dve, lambda e: e.tensor_tensor(out=qkr[:, :, 0, :], in0=rp[0][:], in1=rp[1][:], op=ALU.subtract), reads=[r_rp], writes=[r_qkr])
                    fw.op(fw.dve, lambda e: e.tensor_tensor(out=qkr[:, :, 1, :], in0=rp[2][:], in1=rp[3][:], op=ALU.add), reads=[r_rp], writes=[r_qkr])
                    qkr_f = qkr[:].rearrange("p a h d -> p (a h d)")

                    def tr2(e):
                        ins = None
                        for c in range(8):
                            ins = e.transpose(psT2[:, c, :], qkr_f[:, c * 128:(c + 1) * 128], ident_bf[:])
                        return ins
                    fw.op(fw.pe, tr2, reads=[r_qkr, r_c], writes=[r_POb])
                    fw.op(fw.dve, lambda e: e.tensor_copy(out=kT[:, :, i * 128:(i + 1) * 128], in_=psT2[:, 4:8, :]),
                          reads=[r_POb], writes=[r_kT])
                    fw.op(fw.dve, lambda e: e.tensor_copy(out=qst[sl][:], in_=psT2[:, 0:4, :]), reads=[r_POb], writes=[r_qst[sl]])
                    fw.dma(fw.act, qd[g0 + i // 4, :, :, (i % 4) * 128:(i % 4 + 1) * 128], qst[sl][:], r_qst[sl], reads=[r_qst[sl]])

                stageA(0)
                for i in range(nt):
                    if i + 1 < nt:
                        stageA(i + 1)
                    stageB(i)
                    if i >= 1:
                        pool_stage(i - 1)
                pool_stage(nt - 1)
                r_qd = R("qd_seq")
                fw.barrier()

                st1.close()
                st2 = contextlib.ExitStack()
                qpad = sb(st2, "qpad", [128, 4, 2, 512], BF16)
                r_qpad = R("qpad")
                poolT = sb(st2, "poolT", [128, 4, 4, 128], BF16)
                r_poolT = R("poolT")
                pT = [sb(st2, f"pT{i}", [128, 2, 512], BF16) for i in range(4)]
                r_pT = [R(f"pT{i}") for i in range(4)]
                pTs = [sb(st2, f"pTs{i}", [128, 2, 512], BF16) for i in range(2)]
                r_pTs = [R(f"pTs{i}") for i in range(2)]
                rl = sb(st2, "rl", [128, 2, 512], F32)
                on = rl
                od = sb(st2, "od", [128, 512], F32)
                osq = sb(st2, "osq", [128, 512], F32)
                rn = sb(st2, "rn", [128, 512], F32)
                r_ep = R("epi")
                attnT = sb(st2, "attnT", [128, 4, 512], BF16)
                r_attnT = R("attnT")
                xr = [sb(st2, f"xr{i}", [128, D], F32) for i in range(2)]
                r_xr = [R(f"xr{i}") for i in range(2)]
                ht = [sb(st2, f"ht{i}", [128, D], F32) for i in range(2)]
                r_ht = [R(f"ht{i}") for i in range(2)]
                hnf = sb(st2, "hnf", [128, D], F32)
                r_hnf = R("hnf")
                hnTf = sb(st2, "hnTf", [128, 8, 128], F32)
                r_hnTf = R("hnTf")
                hnTb = [sb(st2, f"hnTb{i}", [128, D], BF16) for i in range(2)]
                r_hnTb = [R(f"hnTb{i}") for i in range(2)]
                ss2 = sb(st2, "ss2", [128, 1], F32)
                r_ss2 = R("ss2")

                fw.op(fw.pool, lambda e: e.memset(qpad[:], 0.0), writes=[r_qpad])
                nk = nt
                for j in range(ng if _DBG not in ("p1",) else 0):
                    gi = g0 + j
                    for m in range(2):
                        dst = qpad[m * 64:(m + 1) * 64, :, m, :]
                        fw.dma(fw.sp, dst, qd[gi, m * 64:(m + 1) * 64, :, :], r_qpad, writes=[r_qpad])
                    fw.dma(fw.sp, poolT[:], pd[gi], r_poolT, writes=[r_poolT])
                    units = [(hh, kk) for hh in range(4) for kk in range(nk)]

                    def emit_qk(ui):
                        hh, kk = units[ui]
                        bb = ui % 2
                        Sb = S0 if bb == 0 else S1
                        r_Sb = [r_S0a, r_S0b] if bb == 0 else [r_S1a, r_S1b]

                        def mm_s(e):
                            e.matmul(Sb[:, 0, :], kT[:, hh, kk * 128:(kk + 1) * 128], qpad[:, hh, 0, :], start=True, stop=True)
                            return e.matmul(Sb[:, 1, :], kT[:, hh, kk * 128:(kk + 1) * 128], qpad[:, hh, 1, :], start=True, stop=True)
                        fw.op(fw.pe, mm_s, reads=[r_kT, r_qpad], writes=r_Sb)

                    def emit_exp_pv(ui):
                        hh, kk = units[ui]
                        bb = ui % 2
                        p4 = ui % 4
                        Sb = S0 if bb == 0 else S1
                        r_Sb = [r_S0a, r_S0b] if bb == 0 else [r_S1a, r_S1b]
                        fw.op(fw.act, lambda e: e.activation(out=pT[p4][:], in_=Sb[:], func=AF.Exp, scale=0.125),
                              reads=r_Sb, writes=[r_pT[p4]])
                        if ui + 2 < len(units):
                            emit_qk(ui + 2)

                        def mm_pv(e):
                            ins = None
                            for m in range(2):
                                ins = e.matmul(PO[:, m, :], V[:, kk, hh * 128:(hh + 1) * 128], pT[p4][:, m, :],
                                               start=(kk == 0), stop=(kk == nk - 1))
                            return ins
                        fw.op(fw.pe, mm_pv, reads=[r_V, r_pT[p4]], writes=[r_POa, r_POb])
                        def mm_l_for(ps2_, kk_):
                            def mm_l(e):
                                ins = None
                                for m in range(2):
                                    ins = e.matmul(PL[:, m, :], ones_bf[:], pTs[ps2_][:, m, :],
                                                   start=(kk_ == 1), stop=(kk_ == nk - 1))
                                return ins
                            fw.op(fw.pe, mm_l, reads=[r_pTs[ps2_], r_ones], writes=[r_PLa, r_PLb])
                        if kk % 2 == 0 and pend_l:
                            mm_l_for(*pend_l.pop())
                        if kk % 2 == 1:
                            ps2 = (ui // 2) % 2
                            pprev = (ui - 1) % 4
                            fw.op(fw.dve, lambda e: e.tensor_tensor(out=pTs[ps2][:], in0=pT[pprev][:], in1=pT[p4][:], op=ALU.add),
                                  reads=[r_pT[pprev], r_pT[p4]], writes=[r_pTs[ps2]])
                            if kk == nk - 1:
                                mm_l_for(ps2, kk)
                            else:
                                pend_l.append((ps2, kk))

                    pend_l = []
                    emit_qk(0)
                    if len(units) > 1:
                        emit_qk(1)
                    for ui in range(len(units)):
                        h, kc = units[ui]
                        emit_exp_pv(ui)
                        if kc != nk - 1:
                            continue
                        fw.op(fw.dve, lambda e: e.reciprocal(out=rl[:], in_=PL[:]), reads=[r_PLa, r_PLb], writes=[r_ep])
                        fw.op(fw.dve, lambda e: e.tensor_tensor(out=on[:], in0=PO[:], in1=rl[:], op=ALU.mult),
                              reads=[r_POa, r_POb, r_ep], writes=[r_ep])
                        fw.op(fw.dve, lambda e: e.scalar_tensor_tensor(out=od[:], in0=on[:, 1, :], scalar=neglam[:], in1=on[:, 0, :],
                                                                       op0=ALU.mult, op1=ALU.add),
                              reads=[r_ep, r_lam], writes=[r_ep])
                        fw.op(fw.act, lambda e: e.activation(out=osq[:], in_=od[:], func=AF.Square), reads=[r_ep], writes=[r_ep])
                        fw.op(fw.pe, lambda e: e.matmul(PL[:, 0, :], ones_f[:], osq[:], start=True, stop=True),
                              reads=[r_ep, r_ones], writes=[r_PLa])
                        fw.op(fw.dve, lambda e: e.tensor_scalar(out=rn[:], in0=PL[:, 0, :], scalar1=1.0 / 128, scalar2=EPS,
                                                                op0=ALU.mult, op1=ALU.add), reads=[r_PLa], writes=[r_ep])
                        fw.op(fw.act, lambda e: e.activation(out=rn[:], in_=rn[:], func=AF.Ln), reads=[r_ep], writes=[r_ep])
                        fw.op(fw.act, lambda e: e.activation(out=rn[:], in_=rn[:], func=AF.Exp, scale=-0.5), reads=[r_ep], writes=[r_ep])
                        fw.op(fw.dve, lambda e, h=h: e.scalar_tensor_tensor(out=attnT[:, h, :], in0=od[:], scalar=1.0 - LAM_INIT, in1=rn[:],
                                                                            op0=ALU.mult, op1=ALU.mult),
                              reads=[r_ep], writes=[r_attnT])
                    def stage2A(ti):
                        tg = (tok0 // 128) + j * 4 + ti
                        sl = ti % 2
                        Hb = S0 if sl == 0 else S1
                        r_Hb = [r_S0a, r_S0b] if sl == 0 else [r_S1a, r_S1b]
                        fw.dma(fw.sp, xr[sl][:], x[tg * 128:(tg + 1) * 128, :], r_xr[sl], writes=[r_xr[sl]])

                        def mm_o(e, ti=ti, Hb=Hb):
                            ins = None
                            for cc in range(2):
                                for c in range(8):
                                    lhsT = poolT[:, ti, c, :] if c < 4 else attnT[:, c - 4, ti * 128:(ti + 1) * 128]
                                    ins = e.matmul(Hb[:, cc, :], lhsT, Wout[:, c, cc * 512:(cc + 1) * 512],
                                                   start=(c == 0), stop=(c == 7))
                            return ins
                        fw.op(fw.pe, mm_o, reads=[r_poolT, r_attnT, r_Wout], writes=r_Hb)
                        Hf = Hb[:].rearrange("p a b -> p (a b)")
                        fw.op(fw.dve, lambda e, Hf=Hf, sl=sl: e.tensor_tensor(out=ht[sl][:], in0=Hf, in1=xr[sl][:], op=ALU.add),
                              reads=r_Hb + [r_xr[sl]], writes=[r_ht[sl]])
                        fw.dma(fw.act, h_d[tg], ht[sl][:], r_ht[sl], reads=[r_ht[sl]])
                        fw.op(fw.act, lambda e, sl=sl: e.activation(out=hnTb[sl][:], in_=ht[sl][:], func=AF.Square, accum_out=ss2[:]),
                              reads=[r_ht[sl]], writes=[r_hnTb[sl], r_ss2])
                        rstd_chain(ss2[:], ss2[:], [r_ss2], 1.0 / D)
                        fw.op(fw.dve, lambda e, sl=sl: e.scalar_tensor_tensor(out=hnf[:], in0=ht[sl][:], scalar=ss2[:], in1=gffn[:],
                                                                              op0=ALU.mult, op1=ALU.mult),
                              reads=[r_ht[sl], r_ss2, r_c], writes=[r_hnf])
                        psTf = PO[:].rearrange("p a (c t) -> p (a c) t", t=128)

                        def tr3(e):
                            ins = None
                            for c in range(8):
                                ins = e.matmul(psTf[:, c, :], hnf[:, c * 128:(c + 1) * 128], ident_f[:], start=True, stop=True)
                            return ins
                        fw.op(fw.pe, tr3, reads=[r_hnf, r_c], writes=[r_POa, r_POb])
                        fw.op(fw.dve, lambda e: e.tensor_copy(out=hnTf[:], in_=psTf), reads=[r_POa, r_POb], writes=[r_hnTf])
                        fw.op(fw.pool, lambda e, sl=sl: e.tensor_copy(out=hnTb[sl][:], in_=hnf[:]),
                              reads=[r_hnf], writes=[r_hnTb[sl]])
                        fw.dma(fw.pool, hn_d[tg], hnTb[sl][:], r_hnTb[sl], reads=[r_hnTb[sl]])
                        ps_r = PL[:, 1, (ti % 2) * 64:(ti % 2) * 64 + 36]

                        def mm_r(e):
                            ins = None
                            for c in range(8):
                                ins = e.matmul(ps_r, hnTf[:, c, :], Wr[:, c, :], start=(c == 0), stop=(c == 7))
                            return ins
                        fw.op(fw.pe, mm_r, reads=[r_hnTf, r_Wr], writes=[r_psr[ti % 2], r_PLb])

                    def stage2B(ti):
                        tg = (tok0 // 128) + j * 4 + ti
                        ps_r = PL[:, 1, (ti % 2) * 64:(ti % 2) * 64 + 36]
                        fw.op(fw.dve, lambda e: e.tensor_tensor(out=lg_all[:, tg, :], in0=ps_r, in1=biasb[:], op=ALU.add),
                              reads=[r_psr[ti % 2], r_PLb, r_c], writes=[r_gates[tg]])

                    stage2A(0)
                    for ti in range(4):
                        if ti + 1 < 4:
                            stage2A(ti + 1)
                        stage2B(ti)
                tok0 += N
                g0 += ng
                fw.barrier()
                st2.close()
        fw.barrier()
        I32 = mybir.dt.int32
        with contextlib.ExitStack() as st:
            OH1 = sb(st, "OH1", [128, NT, 32], BF16)
            OH2 = sb(st, "OH2", [128, NT, 32], BF16)
            wts = sb(st, "wts", [128, NT, 2], F32)
            d1i = sb(st, "d1i", [128, NT], I32)
            d2i = sb(st, "d2i", [128, NT], I32)
            idxw = sb(st, "idxw", [128, NB], I32)
            r_rt3 = R("route3")
            TA = st.enter_context(nc.psum_tensor("TA", [128, 512], F32))
            TB = st.enter_context(nc.psum_tensor("TB", [128, 512], F32))
            G0 = st.enter_context(nc.psum_tensor("G0", [128, 2, 512], F32))
            G1 = st.enter_context(nc.psum_tensor("G1", [128, 2, 512], F32))
            Y0 = st.enter_context(nc.psum_tensor("Y0", [128, 2, 512], F32))
            r_TA, r_TB, r_G0, r_G1, r_Y0 = R("TA"), R("TB"), R("G0p"), R("G1p"), R("Y0p")
            with contextlib.ExitStack() as s0:
                r_r0 = R("router0")
                gq = sb(s0, "gq", [128, 8, NT], F32)
                mg = sb(s0, "mg", [128, NT, 4], F32)
                eg = sb(s0, "eg", [128, NT, 4], F32)
                t48 = sb(s0, "t48", [128, NT, 4, 8], F32)
                les = sb(s0, "les", [128, NT, 8], F32)
                les2 = sb(s0, "les2", [128, NT, 8], F32)
                k1 = sb(s0, "k1", [128, NT, 8], F32)
                k2 = sb(s0, "k2", [128, NT, 8], F32)
                gmax, gsum, m1, m2, ex, w1, w2 = (gq[:, i, :] for i in range(7))
                lg = lg_all[:, :, 0:4]
                le4 = lg_all[:, :, 4:36].rearrange("p t (g j) -> p t g j", g=4)
                rr0 = [r_r0]

                def d0(fn, extra=()):
                    fw.op(fw.dve, fn, reads=rr0 + list(extra), writes=rr0)

                def bc3(ap2, k):
                    return ap2.unsqueeze(2).broadcast_to([128, NT, k])
                d0(lambda e: e.tensor_reduce(out=gmax, in_=lg, axis=AX.X, op=ALU.max), r_gates)
                d0(lambda e: e.tensor_tensor(out=mg[:], in0=lg, in1=bc3(gmax, 4), op=ALU.is_equal), r_gates)
                d0(lambda e: e.tensor_tensor(out=eg[:], in0=lg, in1=bc3(gmax, 4), op=ALU.subtract), r_gates)
                fw.op(fw.act, lambda e: e.activation(out=eg[:], in_=eg[:], func=AF.Exp), reads=rr0, writes=rr0)
                d0(lambda e: e.tensor_reduce(out=gsum, in_=eg[:], axis=AX.X, op=ALU.add))
                d0(lambda e: e.reciprocal(out=gsum, in_=gsum))
                d0(lambda e: e.tensor_tensor(out=t48[:], in0=le4, in1=mg[:].unsqueeze(3).broadcast_to([128, NT, 4, 8]), op=ALU.mult), r_gates)
                d0(lambda e: e.tensor_reduce(out=les[:], in_=t48[:].rearrange("p t g j -> p t j g"), axis=AX.X, op=ALU.add))
                d0(lambda e: e.tensor_reduce(out=m1, in_=les[:], axis=AX.X, op=ALU.max))
                d0(lambda e: e.tensor_tensor(out=k1[:], in0=les[:], in1=bc3(m1, 8), op=ALU.is_equal))
                d0(lambda e: e.scalar_tensor_tensor(out=les2[:], in0=k1[:], scalar=-1e30, in1=les[:], op0=ALU.mult, op1=ALU.add))
                d0(lambda e: e.tensor_reduce(out=m2, in_=les2[:], axis=AX.X, op=ALU.max))
                d0(lambda e: e.tensor_tensor(out=k2[:], in0=les2[:], in1=bc3(m2, 8), op=ALU.is_equal))
                d0(lambda e: e.tensor_tensor(out=ex, in0=m2, in1=m1, op=ALU.subtract))
                fw.op(fw.act, lambda e: e.activation(out=ex, in_=ex, func=AF.Exp), reads=rr0, writes=rr0)
                d0(lambda e: e.tensor_scalar(out=w1, in0=ex, scalar1=1.0, scalar2=None, op0=ALU.add))
                d0(lambda e: e.reciprocal(out=w1, in_=w1))
                d0(lambda e: e.tensor_tensor(out=w1, in0=w1, in1=gsum, op=ALU.mult))
                d0(lambda e: e.tensor_tensor(out=w2, in0=w1, in1=ex, op=ALU.mult))
                mgb4 = mg[:].unsqueeze(3).broadcast_to([128, NT, 4, 8])
                o1v = OH1[:].rearrange("p t (g j) -> p t g j", g=4)
                o2v = OH2[:].rearrange("p t (g j) -> p t g j", g=4)
                fw.op(fw.dve, lambda e: e.tensor_tensor(out=o1v, in0=k1[:].unsqueeze(2).broadcast_to([128, NT, 4, 8]), in1=mgb4, op=ALU.mult),
                      reads=rr0, writes=r_gates)
                fw.op(fw.dve, lambda e: e.tensor_tensor(out=o2v, in0=k2[:].unsqueeze(2).broadcast_to([128, NT, 4, 8]), in1=mgb4, op=ALU.mult),
                      reads=rr0, writes=r_gates)
                fw.op(fw.dve, lambda e: e.tensor_copy(out=wts[:, :, 0], in_=w1), reads=rr0, writes=r_gates)
                fw.op(fw.dve, lambda e: e.tensor_copy(out=wts[:, :, 1], in_=w2), reads=rr0, writes=r_gates)
                fw.barrier()
            with contextlib.ExitStack() as sa:
                tri = sb(sa, "tri", [128, 128], BF16)
                bstart = sb(sa, "bstart", [128, NB], F32)
                pidx = sb(sa, "pidx", [128, 1], F32)
                r_ca = R("constA")
                fw.dma(fw.sp, tri[:], tri_d, r_ca, writes=[r_ca])
                fw.dma(fw.sp, bstart[:], bstart_d, r_ca, writes=[r_ca])
                fw.dma(fw.sp, pidx[:], pidx_d, r_ca, writes=[r_ca])
                NC3 = NT * 32
                Mb = sb(sa, "Mb", [128, NC3], BF16)
                rank = sb(sa, "rank", [128, NT, 32], F32)
                cnt = sb(sa, "cnt", [128, NT, 32], F32)
                sA = sb(sa, "sA", [128, NT, 32], F32)
                sB = sb(sa, "sB", [128, NT, 32], F32)
                cmp = sb(sa, "cmp", [128, NB, 32], F32)
                sm = sb(sa, "sm", [128, 8, 32], F32)
                ebf = sb(sa, "ebf", [128, NB], F32)
                eb2 = sb(sa, "eb2", [128, NB], F32)
                dfl = sb(sa, "dfl", [128, 2, NT], F32)
                Dv = fw.dve
                rr = [r_rt3]

                def dv(fn, extra=()):
                    fw.op(Dv, fn, reads=rr + list(extra), writes=rr)
                OH1f = OH1[:].rearrange("p t e -> p (t e)")
                OH2f = OH2[:].rearrange("p t e -> p (t e)")
                dv(lambda e: e.tensor_tensor(out=Mb[:], in0=OH1f, in1=OH2f, op=ALU.add), r_gates)
                rankf = rank[:].rearrange("p t e -> p (t e)")
                cntf = cnt[:].rearrange("p t e -> p (t e)")
                nch = (NC3 + 511) // 512
                for ch in range(nch):
                    c0 = ch * 512
                    c1 = min(NC3, c0 + 512)
                    w_ = c1 - c0
                    fw.op(fw.pe, lambda e: e.matmul(TA[:, 0:w_], tri[:], Mb[:, c0:c1], start=True, stop=True),
                          reads=[r_rt3, r_ca], writes=[r_TA])
                    fw.op(fw.pe, lambda e: e.matmul(TB[:, 0:w_], ones_bf[:], Mb[:, c0:c1], start=True, stop=True),
                          reads=[r_rt3, r_ones], writes=[r_TB])
                    fw.op(Dv, lambda e: e.tensor_copy(out=rankf[:, c0:c1], in_=TA[:, 0:w_]), reads=[r_TA], writes=rr)
                    fw.op(Dv, lambda e: e.tensor_copy(out=cntf[:, c0:c1], in_=TB[:, 0:w_]), reads=[r_TB], writes=rr)
                dv(lambda e: e.tensor_copy(out=sA[:], in_=cnt[:]))
                src, dst = sA, sB
                sft = 1
                while sft < NT:
                    dv(lambda e: e.tensor_tensor(out=dst[:, sft:, :], in0=src[:, sft:, :], in1=src[:, 0:NT - sft, :], op=ALU.add))
                    dv(lambda e: e.tensor_copy(out=dst[:, 0:sft, :], in_=src[:, 0:sft, :]))
                    src, dst = dst, src
                    sft *= 2
                incl = src
                other = dst
                tot = sm[:, 0, :]
                xm = sm[:, 1, :]
                padded = sm[:, 2, :]
                pA = sm[:, 3, :]
                pB = sm[:, 4, :]
                pstart = sm[:, 5, :]
                dv(lambda e: e.tensor_copy(out=tot, in_=incl[:, NT - 1, :]))
                dv(lambda e: e.tensor_scalar(out=xm, in0=tot, scalar1=1.0 / 128, scalar2=0.49609375, op0=ALU.mult, op1=ALU.add))
                dv(lambda e: e.tensor_scalar(out=xm, in0=xm, scalar1=8388608.0, scalar2=None, op0=ALU.add))
                dv(lambda e: e.tensor_scalar(out=xm, in0=xm, scalar1=-8388608.0, scalar2=None, op0=ALU.add))
                dv(lambda e: e.tensor_scalar(out=padded, in0=xm, scalar1=128.0, scalar2=None, op0=ALU.mult))
                dv(lambda e: e.tensor_copy(out=pA, in_=padded))
                ps_, pd_ = pA, pB
                sft = 1
                while sft < 32:
                    dv(lambda e: e.tensor_tensor(out=pd_[:, sft:], in0=ps_[:, sft:], in1=ps_[:, 0:32 - sft], op=ALU.add))
                    dv(lambda e: e.tensor_copy(out=pd_[:, 0:sft], in_=ps_[:, 0:sft]))
                    ps_, pd_ = pd_, ps_
                    sft *= 2
                pend = ps_
                dv(lambda e: e.tensor_tensor(out=pstart, in0=pend, in1=padded, op=ALU.subtract))
                dv(lambda e: e.tensor_tensor(out=other[:], in0=incl[:], in1=cnt[:], op=ALU.subtract))
                dv(lambda e: e.tensor_tensor(out=other[:], in0=other[:], in1=rank[:], op=ALU.add))
                dv(lambda e: e.tensor_tensor(out=other[:], in0=other[:], in1=pstart.unsqueeze(1).broadcast_to([128, NT, 32]), op=ALU.add))
                dest = other
                dv(lambda e: e.tensor_tensor(out=incl[:], in0=dest[:], in1=OH1[:], op=ALU.mult), r_gates)
                dv(lambda e: e.tensor_reduce(out=dfl[:, 0, :], in_=incl[:], axis=AX.X, op=ALU.add))
                dv(lambda e: e.tensor_tensor(out=incl[:], in0=dest[:], in1=OH2[:], op=ALU.mult), r_gates)
                dv(lambda e: e.tensor_reduce(out=dfl[:, 1, :], in_=incl[:], axis=AX.X, op=ALU.add))
                dv(lambda e: e.tensor_copy(out=d1i[:], in_=dfl[:, 0, :]))
                dv(lambda e: e.tensor_copy(out=d2i[:], in_=dfl[:, 1, :]))
                dv(lambda e: e.tensor_tensor(out=cmp[:], in0=pend.unsqueeze(1).broadcast_to([128, NB, 32]),
                                             in1=bstart[:].unsqueeze(2).broadcast_to([128, NB, 32]), op=ALU.is_le), [r_ca])
                dv(lambda e: e.tensor_reduce(out=ebf[:], in_=cmp[:], axis=AX.X, op=ALU.add))
                dv(lambda e: e.tensor_scalar(out=ebf[:], in0=ebf[:], scalar1=float(NE - 1), scalar2=None, op0=ALU.min))
                BIG = 1.0e6
                dv(lambda e: e.memset(eb2[:], 1.0))
                dv(lambda e: e.tensor_tensor(out=eb2[:, 2:], in0=ebf[:, 2:], in1=ebf[:, 0:NB - 2], op=ALU.not_equal))
                dv(lambda e: e.tensor_scalar(out=ebf[:], in0=ebf[:], scalar1=128.0, scalar2=pidx[:], op0=ALU.mult, op1=ALU.add), [r_ca])
                dv(lambda e: e.tensor_scalar(out=ebf[:], in0=ebf[:], scalar1=-BIG, scalar2=None, op0=ALU.add))
                dv(lambda e: e.tensor_tensor(out=ebf[:], in0=ebf[:], in1=eb2[:], op=ALU.mult))
                dv(lambda e: e.tensor_scalar(out=ebf[:], in0=ebf[:], scalar1=BIG, scalar2=None, op0=ALU.add))
                dv(lambda e: e.tensor_copy(out=idxw[:], in_=ebf[:]))
                fw.barrier()

            _bregs = {}

            def _breg(bound):
                if bound not in _bregs:
                    rg = nc.gpsimd.alloc_register(f"bnd{len(_bregs)}")
                    nc.gpsimd.reg_mov(rg, int(bound))
                    _bregs[bound] = rg
                return _bregs[bound]

            def indirect(out, out_off, in_, in_off, bound, sb_res, reads, writes):
                deps = []
                for r_ in reads:
                    if r_.last_w is not None:
                        deps.append(r_.last_w)
                for w_r in writes:
                    if w_r.last_w is not None:
                        deps.append(w_r.last_w)
                    deps.extend(w_r.readers)
                fw.pool.wait_tokens(deps)
                ent = fw.dma_reg.setdefault(sb_res.name, [None, 0])
                if ent[0] is None:
                    ent[0] = fw.new_sem(f"dma_{sb_res.name}")
                ins = nc.gpsimd.indirect_dma_start(
                    out=out, out_offset=(bass.IndirectOffsetOnAxis(ap=out_off, axis=0) if out_off is not None else None),
                    in_=in_, in_offset=(bass.IndirectOffsetOnAxis(ap=in_off, axis=0) if in_off is not None else None),
                    bounds_check=_breg(bound), oob_is_err=False)
                ent[1] += 16
                ins.then_inc(fw.sem_by_key[ent[0]], 16)
                tok = (ent[0], ent[1])
                for r_ in reads:
                    r_.readers.append(tok)
                for w_r in writes:
                    w_r.last_w = tok
                    w_r.readers = []

            with contextlib.ExitStack() as sbk:
                hb = [sb(sbk, f"hb{i}", [128, D], BF16) for i in range(3)]
                r_hb = [R(f"hb{i}") for i in range(3)]
                for i in range(NT):
                    s3 = i % 3
                    fw.dma(fw.sp, hb[s3][:], hn_d[i], r_hb[s3], writes=[r_hb[s3]])
                    indirect(xg[:, :], d1i[:, i:i + 1], hb[s3][:, :], None, PR - 1, r_hb[s3], [r_hb[s3], r_rt3], [])
                    indirect(xg[:, :], d2i[:, i:i + 1], hb[s3][:, :], None, PR - 1, r_hb[s3], [r_hb[s3], r_rt3], [])
                fw.barrier()
            wgv = w_gate[:, :]
            wuv = w_up[:, :]
            wdv = w_down[:, :]
            with contextlib.ExitStack() as sc:
                identb = sb(sc, "identb", [128, 128], BF16)
                r_idb = R("identb")
                fw.dma(fw.sp, identb[:], ident_bf_d, r_idb, writes=[r_idb])
                wg = [sb(sc, f"wg{i}", [128, 8 * DFF], BF16) for i in range(2)]
                wu = [sb(sc, f"wu{i}", [128, 8 * DFF], BF16) for i in range(2)]
                wd = [sb(sc, f"wd{i}", [128, 4 * D], BF16) for i in range(2)]
                r_wg = [R("wg0"), R("wg1")]
                r_wu = [R("wu0"), R("wu1")]
                r_wd = [R("wd0"), R("wd1")]
                xb = [sb(sc, f"xb{i}", [128, D], BF16) for i in range(2)]
                r_xb = [R("xb0"), R("xb1")]
                xgT = [sb(sc, f"xgT{i}", [128, 8, 128], BF16) for i in range(2)]
                r_xgT = [R("xgT0"), R("xgT1")]
                sgl = sb(sc, "sgl", [128, 512], F32)
                r_sgl = R("sgl")
                hidT = [sb(sc, f"hidT{i}", [128, 4, 128], BF16) for i in range(2)]
                r_hid = [R("hid0"), R("hid1")]
                yb = [sb(sc, f"yb{i}", [128, D], F32) for i in range(2)]
                r_yb = [R("yb0"), R("yb1")]
                Ts = [TA, TB]
                r_Ts = [r_TA, r_TB]
                Gs = [G0, G1]
                r_Gs = [r_G0, r_G1]
                for ws in range(2):
                    pass
                hidtok = [sb(sc, f"hidtok{i}", [128, DFF], BF16) for i in range(2)]
                r_hidtok = [R("hidtok0"), R("hidtok1")]
                sgl2 = [sgl, sb(sc, "sglb", [128, 512], F32)]
                r_sgl2 = [r_sgl, R("sglb")]
                xb3 = xb + [sb(sc, "xb2", [128, D], BF16)]
                r_xb3 = r_xb + [R("xb2")]
                xgT3 = xgT + [sb(sc, "xgT2", [128, 8, 128], BF16)]
                r_xgT3 = r_xgT + [R("xgT2")]
                psT3 = TA[:].bitcast(BF16).rearrange("p (c t) -> p c t", c=8)
                psH = TB[:, 0:256].bitcast(BF16).rearrange("p (c t) -> p c t", c=4)

                def gath_gu(b):
                    ws = b % 2
                    indirect(wg[ws][:, :], None, wgv, idxw[:, b:b + 1], NE * 128 - 1, r_wg[ws], [r_rt3], [r_wg[ws]])
                    indirect(wu[ws][:, :], None, wuv, idxw[:, b:b + 1], NE * 128 - 1, r_wu[ws], [r_rt3], [r_wu[ws]])

                def gath_d(b):
                    ws = b % 2
                    indirect(wd[ws][:, :], None, wdv, idxw[:, b:b + 1], NE * 128 - 1, r_wd[ws], [r_rt3], [r_wd[ws]])

                def preC(b):
                    s3 = b % 3
                    fw.dma(fw.sp, xb3[s3][:], xg[b * 128:(b + 1) * 128, :], r_xb3[s3], writes=[r_xb3[s3]])
                    xbv = xb3[s3][:].rearrange("p (q c) -> p c q", c=8)

                    def tr4(e):
                        ins = None
                        for c in range(8):
                            ins = e.transpose(psT3[:, c, :], xbv[:, c, :], identb[:])
                        return ins
                    fw.op(fw.pe, tr4, reads=[r_xb3[s3], r_idb], writes=[r_TA])
                    fw.op(fw.dve, lambda e: e.tensor_copy(out=xgT3[s3][:], in_=psT3), reads=[r_TA], writes=[r_xgT3[s3]])

                def stageG(b):
                    ws = b % 2
                    s3 = b % 3
                    Gb = Gs[ws]
                    wgs = wg[ws][:].rearrange("p (c f) -> p c f", c=8)
                    wus = wu[ws][:].rearrange("p (c f) -> p c f", c=8)

                    def mm_gu(e):
                        ins = None
                        for which, wv in ((0, wgs), (1, wus)):
                            for c in range(8):
                                ins = e.matmul(Gb[:, which, :], xgT3[s3][:, c, :], wv[:, c, :], start=(c == 0), stop=(c == 7))
                        return ins
                    fw.op(fw.pe, mm_gu, reads=[r_xgT3[s3], r_wg[ws], r_wu[ws]], writes=[r_Gs[ws]])
                    fw.op(fw.act, lambda e: e.activation(out=sgl2[ws][:], in_=Gb[:, 0, :], func=AF.Silu), reads=[r_Gs[ws]], writes=[r_sgl2[ws]])
                    fw.op(fw.dve, lambda e: e.tensor_tensor(out=hidtok[ws][:], in0=sgl2[ws][:], in1=Gb[:, 1, :], op=ALU.mult),
                          reads=[r_sgl2[ws], r_Gs[ws]], writes=[r_hidtok[ws]])

                def stageH(b):
                    ws = b % 2
                    wds = wd[ws][:].rearrange("p (c d) -> p c d", c=4)
                    hv = hidtok[ws][:].rearrange("p (q c) -> p c q", c=4)

                    def trH(e):
                        ins = None
                        for c in range(4):
                            ins = e.transpose(psH[:, c, :], hv[:, c, :], identb[:])
                        return ins
                    fw.op(fw.pe, trH, reads=[r_hidtok[ws], r_idb], writes=[r_TB])
                    fw.op(fw.dve, lambda e: e.tensor_copy(out=hidT[ws][:], in_=psH), reads=[r_TB], writes=[r_hid[ws]])

                def stageH2(b):
                    ws = b % 2
                    wds = wd[ws][:].rearrange("p (c d) -> p c d", c=4)

                    def mm_d(e):
                        ins = None
                        for cc in range(2):
                            for fc in range(4):
                                ins = e.matmul(Y0[:, cc, :], hidT[ws][:, fc, :], wds[:, fc, cc * 512:(cc + 1) * 512],
                                               start=(fc == 0), stop=(fc == 3))
                        return ins
                    fw.op(fw.pe, mm_d, reads=[r_hid[ws], r_wd[ws]], writes=[r_Y0])
                    fw.op(fw.act, lambda e: e.activation(out=yb[ws][:], in_=Y0[:].rearrange("p a b -> p (a b)"), func=AF.Copy),
                          reads=[r_Y0], writes=[r_yb[ws]])
                    fw.dma(fw.sp, yg[b * 128:(b + 1) * 128, :], yb[ws][:], r_yb[ws], reads=[r_yb[ws]])

                for bb in (0, 1):
                    if bb < NB:
                        gath_gu(bb)
                        gath_d(bb)
                        preC(bb)
                stageG(0)
                for b in range(NB):
                    stageH(b)
                    if b + 2 < NB:
                        gath_gu(b + 2)
                        preC(b + 2)
                    stageH2(b)
                    if b + 1 < NB:
                        stageG(b + 1)
                    if b + 2 < NB:
                        gath_d(b + 2)
                fw.barrier()
            with contextlib.ExitStack() as sd:
                gfin = sb(sd, "gfin", [128, D], F32)
                fw.dma(fw.sp, gfin[:], gfin_b, r_gfin, writes=[r_gfin])
                y1 = [sb(sd, f"y1{i}", [128, D], F32) for i in range(2)]
                y2 = [sb(sd, f"y2{i}", [128, D], F32) for i in range(2)]
                r_y1 = [R("y10"), R("y11")]
                r_y2 = [R("y20"), R("y21")]
                hr = [sb(sd, f"hr{i}", [128, D], F32) for i in range(2)]
                r_hr = [R("hr0"), R("hr1")]
                yt = [sb(sd, f"yt{i}", [128, D], F32) for i in range(2)]
                r_yt = [R("yt0"), R("yt1")]
                junk3 = sb(sd, "junk3", [128, D], BF16)
                r_junk3 = R("junk3")
                ss3 = [sb(sd, f"ss3{i}", [128, 1], F32) for i in range(2)]
                r_ss3 = [R("ss30"), R("ss31")]
                def loadD(tg):
                    sl = tg % 2
                    indirect(y1[sl][:, :], None, yg[:, :], d1i[:, tg:tg + 1], PR - 1, r_y1[sl], [r_rt3], [r_y1[sl]])
                    indirect(y2[sl][:, :], None, yg[:, :], d2i[:, tg:tg + 1], PR - 1, r_y2[sl], [r_rt3], [r_y2[sl]])
                    fw.dma(fw.sp, hr[sl][:], h_d[tg], r_hr[sl], writes=[r_hr[sl]])

                def compD(tg):
                    sl = tg % 2
                    fw.op(fw.dve, lambda e: e.scalar_tensor_tensor(out=hr[sl][:], in0=y1[sl][:], scalar=wts[:, tg, 0:1], in1=hr[sl][:],
                                                                   op0=ALU.mult, op1=ALU.add),
                          reads=[r_y1[sl], r_hr[sl], r_gates[tg]], writes=[r_hr[sl]])
                    fw.op(fw.dve, lambda e: e.scalar_tensor_tensor(out=hr[sl][:], in0=y2[sl][:], scalar=wts[:, tg, 1:2], in1=hr[sl][:],
                                                                   op0=ALU.mult, op1=ALU.add),
                          reads=[r_y2[sl], r_hr[sl], r_gates[tg]], writes=[r_hr[sl]])
                    fw.op(fw.act, lambda e: e.activation(out=junk3[:], in_=hr[sl][:], func=AF.Square, accum_out=ss3[sl][:]),
                          reads=[r_hr[sl]], writes=[r_junk3, r_ss3[sl]])
                    fw.op(fw.dve, lambda e: e.tensor_scalar(out=ss3[sl][:], in0=ss3[sl][:], scalar1=1.0 / D, scalar2=EPS, op0=ALU.mult, op1=ALU.add),
                          reads=[r_ss3[sl]], writes=[r_ss3[sl]])
                    fw.op(fw.act, lambda e: e.activation(out=ss3[sl][:], in_=ss3[sl][:], func=AF.Ln), reads=[r_ss3[sl]], writes=[r_ss3[sl]])
                    fw.op(fw.act, lambda e: e.activation(out=ss3[sl][:], in_=ss3[sl][:], func=AF.Exp, scale=-0.5), reads=[r_ss3[sl]], writes=[r_ss3[sl]])
                    fw.op(fw.dve, lambda e: e.scalar_tensor_tensor(out=yt[sl][:], in0=hr[sl][:], scalar=ss3[sl][:], in1=gfin[:],
                                                                   op0=ALU.mult, op1=ALU.mult),
                          reads=[r_hr[sl], r_ss3[sl], r_gfin], writes=[r_yt[sl]])
                    fw.dma(fw.sp, y[tg * 128:(tg + 1) * 128, :], yt[sl][:], r_yt[sl], reads=[r_yt[sl]])

                loadD(0)
                for tg in range(NT):
                    if tg + 1 < NT:
                        loadD(tg + 1)
                    compD(tg)
                fw.barrier()
    return nc


def _const_tables(max_n):
    max_nt = max_n // 128
    inv_freq = (np.float32(10000.0) ** (-np.arange(0, 64, 2, dtype=np.float32) / np.float32(64))).astype(np.float32)
    pos = np.arange(max_n, dtype=np.float32)
    ang = (pos[:, None] * inv_freq[None, :]).astype(np.float32)
    cos = np.cos(ang).astype(np.float32).reshape(max_nt, 128, 32)
    sin = np.sin(ang).astype(np.float32).reshape(max_nt, 128, 32)
    cs = np.stack([cos, sin], axis=2)
    cs = np.ascontiguousarray(cs.transpose(1, 0, 2, 3))
    invc = np.zeros((3, 4, 128), np.float32)
    for g, w in enumerate((2, 4, 8, 16)):
        half = w // 2
        t = np.arange(128)
        invc[0, g] = 1.0 / (np.minimum(t + half, 10 ** 9) - np.maximum(t - half, 0))
        invc[1, g] = 1.0 / w
        invc[2, g] = 1.0 / (np.minimum(t + half, 128) - (t - half))
    invc = np.ascontiguousarray(np.broadcast_to(invc[None], (128, 3, 4, 128))).astype(np.float32)
    return cs, invc


_PROG_CACHE = {}


def run_cores(core_seqs, weights, n_cores, sg_tok):
    seq_lens = tuple(int(s.shape[0]) for s in core_seqs[0])
    key = (seq_lens, sg_tok)
    if key not in _PROG_CACHE:
        _PROG_CACHE[key] = build_program(list(seq_lens), sg_tok)
    nc = _PROG_CACHE[key]
    f32 = np.float32
    W = {k: np.asarray(v, dtype=f32) for k, v in weights.items()}
    cs, invc = _const_tables(max(seq_lens))
    NBh = (2 * sum(seq_lens)) // 128 + NE
    shared = {
        "w_in": np.ascontiguousarray(W["w_in"][0]),
        "w_out": np.ascontiguousarray(W["w_out"][0]),
        "w_pool": np.ascontiguousarray(W["w_pool"][0]),
        "w_gate": np.ascontiguousarray(W["w_gate"][0]).reshape(NE * 128, 8 * DFF),
        "w_up": np.ascontiguousarray(W["w_up"][0]).reshape(NE * 128, 8 * DFF),
        "w_down": np.ascontiguousarray(W["w_down"][0]).reshape(NE * 128, 4 * D),
        "w_r": np.ascontiguousarray(np.concatenate([W["w_router_group"][0], W["w_router_expert"][0]], axis=1)),
        "gmix_t": np.ascontiguousarray(W["g_mix"][0].reshape(8, 128).T),
        "wos_t": np.ascontiguousarray(np.concatenate([W["pool_scale"][0].reshape(4, 128).T,
                                                      np.repeat(W["subln_g"][0][:, None], 4, axis=1)], axis=1)),
        "gffn_b": np.ascontiguousarray(np.broadcast_to(W["g_ffn"][0][None, :], (128, D))),
        "gfin_b": np.ascontiguousarray(np.broadcast_to(W["g_final"][None, :], (128, D))),
        "bias_b": np.ascontiguousarray(np.broadcast_to(
            np.concatenate([W["b_router_group"][0], W["b_router_expert"][0]])[None, :], (128, 36))),
        "lam_b": np.ascontiguousarray(np.broadcast_to(
            np.stack([W["lambda_q1"][0], W["lambda_k1"][0], W["lambda_q2"][0], W["lambda_k2"][0]])[None], (128, 4, 64))),
        "ident_bf": np.eye(128, dtype=f32).astype(ml_dtypes.bfloat16),
        "ident_f": np.eye(128, dtype=f32),
        "cs_tab": cs,
        "invc": invc,
        "tri_bf": np.triu(np.ones((128, 128), f32), k=1).astype(ml_dtypes.bfloat16),
        "bstart": np.ascontiguousarray(np.broadcast_to((np.arange(NBh, dtype=f32) * 128.0)[None, :], (128, NBh))),
        "pidx": np.arange(128, dtype=f32).reshape(128, 1),
    }
    in_maps = []
    for c in range(n_cores):
        m = dict(shared)
        m["x"] = np.ascontiguousarray(np.concatenate([np.asarray(s, dtype=f32) for s in core_seqs[c]], axis=0))
        in_maps.append(m)
    res = run_bass_kernel_spmd(nc, in_maps, core_ids=list(range(n_cores)))
    outs = []
    for c in range(n_cores):
        yc = np.asarray(res.results[c]["y"])
        o = []
        off = 0
        for n in seq_lens:
            o.append(yc[off:off + n])
            off += n
        outs.append(o)
    return outs


def kernel(x_prompt, x_sample, **weights):
    n_cores = 8
    xp = np.asarray(x_prompt)
    xsm = np.asarray(x_sample)
    pp = xp.shape[0] // n_cores
    ps = xsm.shape[0] // n_cores
    core_seqs = []
    for c in range(n_cores):
        core_seqs.append([xp[c * pp + i] for i in range(pp)] + [xsm[c * ps + i] for i in range(ps)])
    outs = run_cores(core_seqs, weights, n_cores, sg_tok=2048)
    yp = np.empty(xp.shape, np.float32)
    ys = np.empty(xsm.shape, np.float32)
    for c in range(n_cores):
        for i in range(pp):
            yp[c * pp + i] = outs[c][i]
        for i in range(ps):
            ys[c * ps + i] = outs[c][pp + i]
    return (yp, ys)
```

```python
import contextlib
import numpy as np
import ml_dtypes
import concourse.bass as bass
import concourse.mybir as mybir
from concourse.bass_utils import run_bass_kernel_spmd

F32 = mybir.dt.float32
BF16 = mybir.dt.bfloat16
ALU = mybir.AluOpType
AF = mybir.ActivationFunctionType
AX = mybir.AxisListType

D = 1024
NE = 32
DFF = 512
EPS = 1e-6
SEM_WRAP = 30000
LAM_INIT = 0.2


class Res:
    __slots__ = ("name", "last_w", "readers", "dma_sem", "dma_cnt")

    def __init__(self, name):
        self.name = name
        self.last_w = None
        self.readers = []
        self.dma_sem = None
        self.dma_cnt = 0


class Eng:
    def __init__(self, fw, name, obj, is_pe=False):
        self.fw = fw
        self.name = name
        self.obj = obj
        self.is_pe = is_pe
        self.n = 0
        self.sems = []
        self.waited = {}

    def cur_sem(self):
        idx = self.n // SEM_WRAP
        while len(self.sems) <= idx:
            self.sems.append(self.fw.new_sem(f"tl_{self.name}_{len(self.sems)}"))
        return self.sems[idx]

    def wait_tokens(self, toks):
        best = {}
        for (sk, v) in toks:
            if v > best.get(sk, 0):
                best[sk] = v
        for sk, v in best.items():
            if self.waited.get(sk, 0) >= v:
                continue
            self.waited[sk] = v
            self.obj.wait_ge(self.fw.sem_by_key[sk], v)


class FW:
    def __init__(self, nc, stack):
        self.nc = nc
        self.sem_by_key = {}
        self._stack = stack
        self.all_res = []
        self.dma_reg = {}
        self.pe = Eng(self, "pe", nc.tensor, is_pe=True)
        self.act = Eng(self, "act", nc.scalar)
        self.dve = Eng(self, "dve", nc.vector)
        self.pool = Eng(self, "pool", nc.gpsimd)
        self.sp = Eng(self, "sp", nc.sync)
        self.engs = [self.pe, self.act, self.dve, self.pool, self.sp]

    def res(self, name):
        r = Res(name)
        self.all_res.append(r)
        return r

    def new_sem(self, name):
        h = self._stack.enter_context(self.nc.semaphore(name))
        key = len(self.sem_by_key)
        self.sem_by_key[key] = h
        return key

    def op(self, eng, fn, reads=(), writes=()):
        deps = []
        own = set(eng.sems)
        for r in reads:
            if r.last_w is not None:
                deps.append(r.last_w)
        for w in writes:
            if w.last_w is not None:
                deps.append(w.last_w)
            for t in w.readers:
                if t[0] in own:
                    continue
                deps.append(t)
        if eng.is_pe:
            deps = [t for t in deps if t[0] not in own]
        eng.wait_tokens(deps)
        ins = fn(eng.obj)
        sk = eng.cur_sem()
        val = (eng.n % SEM_WRAP) + 1
        eng.n += 1
        ins.then_inc(self.sem_by_key[sk], 1)
        tok = (sk, val)
        for r in reads:
            r.readers.append(tok)
        for w in writes:
            w.last_w = tok
            w.readers = []
        return tok

    def dma(self, q, out, in_, sb_res, reads=(), writes=()):
        deps = []
        for r in reads:
            if r.last_w is not None:
                deps.append(r.last_w)
        for w in writes:
            if w.last_w is not None:
                deps.append(w.last_w)
            deps.extend(w.readers)
        q.wait_tokens(deps)
        ent = self.dma_reg.setdefault(sb_res.name, [None, 0])
        if ent[0] is None:
            ent[0] = self.new_sem(f"dma_{sb_res.name}")
        ins = q.obj.dma_start(out=out, in_=in_)
        ent[1] += 16
        ins.then_inc(self.sem_by_key[ent[0]], 16)
        tok = (ent[0], ent[1])
        for r in reads:
            r.readers.append(tok)
        for w in writes:
            w.last_w = tok
            w.readers = []
        return tok

    def all_tokens(self):
        toks = []
        for r in self.all_res:
            if r.last_w is not None:
                toks.append(r.last_w)
            toks.extend(r.readers)
        for e in self.engs:
            if e.n > 0:
                idx = (e.n - 1) // SEM_WRAP
                toks.append((e.sems[idx], ((e.n - 1) % SEM_WRAP) + 1))
        return toks

    def barrier(self):
        toks = self.all_tokens()
        for e in self.engs:
            e.wait_tokens(toks)


import os as _os
_DBG = _os.environ.get("KDBG", "full")


def build_program(seq_lens, sg_tok):
    T = sum(seq_lens)
    NT = T // 128
    NG = T // 512
    max_nt = max(seq_lens) // 128
    assert all(n % 512 == 0 for n in seq_lens)
    assert T % sg_tok == 0 and sg_tok % 512 == 0
    nc = bass.Bass("TRN2", target_bir_lowering=False)

    def din(name, shape, dt=F32):
        return nc.dram_tensor(name, list(shape), dt, kind="ExternalInput").ap()

    x = din("x", [T, D])
    w_in = din("w_in", [D, 2048])
    w_out = din("w_out", [D, D])
    w_pool = din("w_pool", [4, 128, 128])
    w_gate = din("w_gate", [NE * 128, 8 * DFF])
    w_up = din("w_up", [NE * 128, 8 * DFF])
    w_down = din("w_down", [NE * 128, 4 * D])
    w_r = din("w_r", [D, 36])
    gmix_t = din("gmix_t", [128, 8])
    wos_t = din("wos_t", [128, 8])
    gffn_b = din("gffn_b", [128, D])
    gfin_b = din("gfin_b", [128, D])
    bias_b = din("bias_b", [128, 36])
    lam_b = din("lam_b", [128, 4, 64])
    ident_bf_d = din("ident_bf", [128, 128], BF16)
    ident_f_d = din("ident_f", [128, 128])
    cs_tab_d = din("cs_tab", [128, max_nt, 2, 32])
    invc_d = din("invc", [128, 3, 4, 128])
    y = nc.dram_tensor("y", [T, D], F32, kind="ExternalOutput").ap()
    qd = nc.dram_tensor("qd", [NG, 128, 4, 512], BF16, kind="Internal").ap()
    pd = nc.dram_tensor("pd", [NG, 128, 4, 4, 128], BF16, kind="Internal").ap()
    h_d = nc.dram_tensor("h_d", [NT, 128, D], F32, kind="Internal").ap()
    hn_d = nc.dram_tensor("hn_d", [NT, 128, D], BF16, kind="Internal").ap()
    NB = (2 * T) // 128 + NE
    PR = NB * 128
    xg = nc.dram_tensor("xg", [PR, D], BF16, kind="Internal").ap()
    yg = nc.dram_tensor("yg", [PR, D], F32, kind="Internal").ap()
    tri_d = din("tri_bf", [128, 128], BF16)
    bstart_d = din("bstart", [128, NB])
    pidx_d = din("pidx", [128, 1])

    with contextlib.ExitStack() as st0:
        fw = FW(nc, st0)
        R = fw.res

        sfx = [""]

        def sb(st, name, shape, dt):
            return st.enter_context(nc.sbuf_tensor("s_" + name + sfx[0], list(shape), dt))

        lg_all = sb(st0, "lg_all", [128, NT, 36], F32)
        ones_bf = sb(st0, "ones_bf", [128, 128], BF16)
        ones_f = sb(st0, "ones_f", [128, 128], F32)
        r_gates = [R(f"gates{i}") for i in range(NT)]
        r_gfin = R("gfin")
        r_ones = R("ones")
        fw.op(fw.dve, lambda e: e.memset(ones_f[:], 1.0), writes=[r_ones])
        fw.op(fw.dve, lambda e: e.tensor_copy(out=ones_bf[:], in_=ones_f[:]), reads=[r_ones], writes=[r_ones])

        with contextlib.ExitStack() as st:
            S0 = st.enter_context(nc.psum_tensor("S0", [128, 2, 512], F32))
            S1 = st.enter_context(nc.psum_tensor("S1", [128, 2, 512], F32))
            PO = st.enter_context(nc.psum_tensor("PO", [128, 2, 512], F32))
            PL = st.enter_context(nc.psum_tensor("PL", [128, 2, 512], F32))
            r_S0, r_S1, r_PO, r_PL = R("S0"), R("S1"), R("PO"), R("PL")
            r_S0a, r_S0b, r_S1a, r_S1b = R("S0a"), R("S0b"), R("S1a"), R("S1b")
            r_POa, r_POb, r_PLa, r_PLb = R("POa"), R("POb"), R("PLa"), R("PLb")
            r_psr = [R("psr0"), R("psr1")]

            Win = sb(st, "Win", [128, 8, 2048], BF16)
            Wout = sb(st, "Wout", [128, 8, 1024], BF16)
            Wp = sb(st, "Wp", [128, 4, 128], BF16)
            Wr = sb(st, "Wr", [128, 8, 36], F32)
            gmix = sb(st, "gmix", [128, 8], F32)
            wos = sb(st, "wos", [128, 8], F32)
            gffn = sb(st, "gffn", [128, D], F32)
            biasb = sb(st, "biasb", [128, 36], F32)
            lamt = sb(st, "lamt", [128, 4, 64], F32)
            lamp = sb(st, "lamp", [128, 2, 64], F32)
            lams = sb(st, "lams", [128, 2], F32)
            neglam = sb(st, "neglam", [128, 1], F32)
            ident_bf = sb(st, "ident_bf", [128, 128], BF16)
            ident_f = sb(st, "ident_f", [128, 128], F32)
            invc = sb(st, "invc", [128, 3, 4, 128], F32)
            st_w = contextlib.ExitStack()
            wstage = sb(st_w, "wstage", [128, 2048], F32)
            r_Win, r_Wout, r_Wp, r_Wr, r_wstage = R("Win"), R("Wout"), R("Wp"), R("Wr"), R("wstage")
            r_c = R("consts")
            r_lam = R("lam")
            for (t_sb, t_dr) in ((gmix, gmix_t), (wos, wos_t), (gffn, gffn_b), (biasb, bias_b),
                                 (ident_bf, ident_bf_d), (ident_f, ident_f_d), (invc, invc_d)):
                fw.dma(fw.sp, t_sb[:], t_dr, r_c, writes=[r_c])
            fw.dma(fw.sp, lamt[:], lam_b, r_lam, writes=[r_lam])
            fw.dma(fw.sp, Wr[:], w_r.rearrange("(c p) j -> p c j", p=128), r_Wr, writes=[r_Wr])
            fw.dma(fw.pool, Wp[:], w_pool.rearrange("g c e -> c g e"), r_Wp, writes=[r_Wp])
            for c in range(8):
                fw.dma(fw.sp, wstage[:], w_in[c * 128:(c + 1) * 128, :], r_wstage, writes=[r_wstage])
                fw.op(fw.act, lambda e, c=c: e.activation(out=Win[:, c, :], in_=wstage[:], func=AF.Copy,
                                                          scale=gmix[:, c:c + 1]),
                      reads=[r_wstage, r_c], writes=[r_Win])
            for c in range(8):
                fw.dma(fw.sp, wstage[:, 0:1024], w_out[c * 128:(c + 1) * 128, :], r_wstage, writes=[r_wstage])
                fw.op(fw.act, lambda e, c=c: e.activation(out=Wout[:, c, :], in_=wstage[:, 0:1024], func=AF.Copy,
                                                          scale=wos[:, c:c + 1]),
                      reads=[r_wstage, r_c], writes=[r_Wout])
            lam4 = lamt[:].rearrange("p (a b) d -> p a b d", b=2)
            fw.op(fw.dve, lambda e: e.tensor_tensor(out=lamp[:], in0=lam4[:, :, 0, :], in1=lam4[:, :, 1, :], op=ALU.mult),
                  reads=[r_lam], writes=[r_lam])
            fw.op(fw.dve, lambda e: e.tensor_reduce(out=lams[:], in_=lamp[:], axis=AX.X, op=ALU.add),
                  reads=[r_lam], writes=[r_lam])
            fw.op(fw.act, lambda e: e.activation(out=lams[:], in_=lams[:], func=AF.Exp), reads=[r_lam], writes=[r_lam])
            fw.op(fw.dve, lambda e: e.tensor_tensor(out=neglam[:], in0=lams[:, 1:2], in1=lams[:, 0:1], op=ALU.subtract),
                  reads=[r_lam], writes=[r_lam])
            fw.op(fw.dve, lambda e: e.tensor_scalar(out=neglam[:], in0=neglam[:], scalar1=-LAM_INIT, scalar2=None,
                                                    op0=ALU.add), reads=[r_lam], writes=[r_lam])

            fw.barrier()
            st_w.close()
            max_n = max(seq_lens)
            kT = sb(st, "kT", [128, 4, max_n], BF16)
            V = sb(st, "V", [128, max_n // 128, 512], BF16)
            r_kT, r_V = R("kT"), R("V")


            def rstd_chain(src, dst, res_list, inv_n):
                fw.op(fw.dve, lambda e: e.tensor_scalar(out=dst, in0=src, scalar1=inv_n, scalar2=EPS,
                                                        op0=ALU.mult, op1=ALU.add), reads=res_list, writes=res_list)
                fw.op(fw.act, lambda e: e.activation(out=dst, in_=dst, func=AF.Ln), reads=res_list, writes=res_list)
                fw.op(fw.act, lambda e: e.activation(out=dst, in_=dst, func=AF.Exp, scale=-0.5),
                      reads=res_list, writes=res_list)

            psT = PO[:, 0, :].bitcast(BF16).rearrange("p (c t) -> p c t", c=8)
            psT2 = PO[:, 1, :].bitcast(BF16).rearrange("p (c t) -> p c t", c=8)
            ps_qk = S0[:].rearrange("p a (b d) -> p (a b) d", d=32)
            ps_qk4 = S0[:].rearrange("p a (b h d) -> p (a b) h d", h=2, d=32)
            ps_v = S1[:, 0, :]
            ps_p = S1[:, 1, :].rearrange("p (g t) -> p g t", g=4)
            ps_po = PL[:, 0, :].rearrange("p (g t) -> p g t", g=4)

            tok0 = 0
            g0 = 0
            for si, N in enumerate(seq_lens):
                nt = N // 128
                ng = N // 512

                sfx[0] = f"_s{si}"
                st1 = contextlib.ExitStack()
                cs_tab = sb(st1, "cs_tab", [128, max_nt, 2, 32], F32)
                r_cs = R("cs_tab")
                fw.dma(fw.sp, cs_tab[:], cs_tab_d, r_cs, writes=[r_cs])
                xt = [sb(st1, f"xt{i}", [128, D], F32) for i in range(2)]
                r_xt = [R(f"xt{i}") for i in range(2)]
                junk = sb(st1, "junk", [128, D], BF16)
                r_junk = R("junk")
                ss = [sb(st1, f"ss{i}", [128, 1], F32) for i in range(2)]
                r_ss = [R(f"ss{i}") for i in range(2)]
                xs = [sb(st1, f"xs{i}", [128, D], BF16) for i in range(2)]
                r_xs = [R(f"xs{i}") for i in range(2)]
                xnT = [sb(st1, f"xnT{i}", [128, 8, 128], BF16) for i in range(2)]
                r_xnT = [R(f"xnT{i}") for i in range(2)]
                rp = [sb(st1, f"rp{i}", [128, 16, 32], F32) for i in range(4)]
                r_rp = R("rp")
                qkr = sb(st1, "qkr", [128, 16, 2, 32], BF16)
                r_qkr = R("qkr")
                qst = [sb(st1, f"qst{i}", [128, 4, 128], BF16) for i in range(2)]
                r_qst = [R(f"qst{i}") for i in range(2)]
                pst = [sb(st1, f"pst{i}", [128, 4, 128], BF16) for i in range(2)]
                r_pst = [R(f"pst{i}") for i in range(2)]
                zpt = [sb(st1, f"zpt{i}", [128, 4, 128], F32) for i in range(3)]
                r_zpt = [R(f"zpt{i}") for i in range(3)]
                ZW = sb(st1, "ZW", [128, 4, 144], F32)
                za = sb(st1, "za", [128, 4, 144], F32)
                zb = sb(st1, "zb", [128, 4, 144], F32)
                zc = sb(st1, "zc", [128, 4, 144], F32)
                zd = sb(st1, "zd", [128, 4, 144], F32)
                pw = sb(st1, "pw", [128, 4, 128], F32)
                pooled = sb(st1, "pooled", [128, 4, 128], BF16)
                r_pm = R("poolmix")
                r_pooled = R("pooled")

                def pool_stage(i):
                    cur = zpt[i % 3]
                    deps_r = [r_zpt[i % 3]]
                    if i > 0:
                        prev = zpt[(i - 1) % 3]
                        deps_r.append(r_zpt[(i - 1) % 3])
                        fw.op(fw.pool, lambda e: e.tensor_copy(out=ZW[:, :, 0:8], in_=prev[:, :, 120:128]),
                              reads=deps_r, writes=[r_pm])
                    else:
                        fw.op(fw.pool, lambda e: e.memset(ZW[:, :, 0:8], 0.0), writes=[r_pm])
                    fw.op(fw.pool, lambda e: e.tensor_copy(out=ZW[:, :, 8:136], in_=cur[:]), reads=deps_r, writes=[r_pm])
                    if i < nt - 1:
                        nxt = zpt[(i + 1) % 3]
                        fw.op(fw.pool, lambda e: e.tensor_copy(out=ZW[:, :, 136:144], in_=nxt[:, :, 0:8]),
                              reads=[r_zpt[(i + 1) % 3]], writes=[r_pm])
                    else:
                        fw.op(fw.pool, lambda e: e.memset(ZW[:, :, 136:144], 0.0), writes=[r_pm])
                    P = fw.pool
                    fw.op(P, lambda e: e.tensor_tensor(out=za[:, :, 0:143], in0=ZW[:, :, 0:143], in1=ZW[:, :, 1:144], op=ALU.add),
                          reads=[r_pm], writes=[r_pm])
                    fw.op(P, lambda e: e.tensor_tensor(out=zb[:, :, 0:141], in0=za[:, :, 0:141], in1=za[:, :, 2:143], op=ALU.add),
                          reads=[r_pm], writes=[r_pm])
                    fw.op(P, lambda e: e.tensor_tensor(out=zc[:, :, 0:137], in0=zb[:, :, 0:137], in1=zb[:, :, 4:141], op=ALU.add),
                          reads=[r_pm], writes=[r_pm])
                    fw.op(P, lambda e: e.tensor_tensor(out=zd[:, :, 0:129], in0=zc[:, :, 0:129], in1=zc[:, :, 8:137], op=ALU.add),
                          reads=[r_pm], writes=[r_pm])
                    kind = 0 if i == 0 else (2 if i == nt - 1 else 1)
                    srcs = [za[:, 0, 7:135], zb[:, 1, 6:134], zc[:, 2, 4:132], zd[:, 3, 0:128]]
                    for g in range(4):
                        fw.op(P, lambda e, g=g: e.tensor_tensor(out=pw[:, g, :], in0=srcs[g], in1=invc[:, kind, g, :], op=ALU.mult),
                              reads=[r_pm, r_c], writes=[r_pm])
                    fw.op(P, lambda e: e.tensor_tensor(out=pooled[:], in0=pw[:], in1=cur[:], op=ALU.subtract),
                          reads=[r_pm, r_zpt[i % 3]], writes=[r_pooled])

                    def mm_pool(e):
                        ins = None
                        for g in range(4):
                            ins = e.matmul(ps_po[:, g, :], Wp[:, g, :], pooled[:, g, :], start=True, stop=True)
                        return ins
                    fw.op(fw.pe, mm_pool, reads=[r_pooled, r_Wp], writes=[r_PLa])
                    sl = i % 2
                    fw.op(fw.act, lambda e: e.activation(out=pst[sl][:], in_=ps_po, func=AF.Copy),
                          reads=[r_PLa], writes=[r_pst[sl]])
                    g_idx = g0 + i // 4
                    fw.dma(fw.sp, pd[g_idx, :, i % 4, :, :], pst[sl][:], r_pst[sl], reads=[r_pst[sl]])

                def stageA_load(i):
                    sl = i % 2
                    fw.dma(fw.sp, xt[sl][:], x[tok0 + i * 128: tok0 + (i + 1) * 128, :], r_xt[sl], writes=[r_xt[sl]])

                def stageA(i):
                    sl = i % 2
                    fw.op(fw.act, lambda e: e.activation(out=junk[:], in_=xt[sl][:], func=AF.Square, accum_out=ss[sl][:]),
                          reads=[r_xt[sl]], writes=[r_junk, r_ss[sl]])
                    rstd_chain(ss[sl][:], ss[sl][:], [r_ss[sl]], 1.0 / D)
                    fw.op(fw.act, lambda e: e.activation(out=xs[sl][:], in_=xt[sl][:], func=AF.Copy, scale=ss[sl][:]),
                          reads=[r_xt[sl], r_ss[sl]], writes=[r_xs[sl]])

                    def tr1(e):
                        ins = None
                        for c in range(8):
                            ins = e.transpose(psT[:, c, :], xs[sl][:, c * 128:(c + 1) * 128], ident_bf[:])
                        return ins
                    fw.op(fw.pe, tr1, reads=[r_xs[sl], r_c], writes=[r_POa])
                    fw.op(fw.dve, lambda e: e.tensor_copy(out=xnT[sl][:], in_=psT), reads=[r_POa], writes=[r_xnT[sl]])

                def stageB(i):
                    sl = i % 2

                    def mm_qkv(e):
                        ins = None
                        for j, dst in enumerate((S0[:, 0, :], S0[:, 1, :], ps_v)):
                            for c in range(8):
                                ins = e.matmul(dst, xnT[sl][:, c, :], Win[:, c, 512 + j * 512: 1024 + j * 512],
                                               start=(c == 0), stop=(c == 7))
                        for g in range(4):
                            for c in range(8):
                                ins = e.matmul(ps_p[:, g, :], Win[:, c, g * 128:(g + 1) * 128], xnT[sl][:, c, :],
                                               start=(c == 0), stop=(c == 7))
                        return ins
                    fw.op(fw.pe, mm_qkv, reads=[r_xnT[sl], r_Win], writes=[r_S0a, r_S0b, r_S1a, r_S1b])
                    fw.op(fw.act, lambda e: e.activation(out=V[:, i, :], in_=ps_v, func=AF.Copy), reads=[r_S1a], writes=[r_V])
                    fw.op(fw.act, lambda e: e.activation(out=zpt[i % 3][:], in_=ps_p, func=AF.Copy),
                          reads=[r_S1b], writes=[r_zpt[i % 3]])
                    cosb = cs_tab[:, i, 0, :].unsqueeze(1).broadcast_to([128, 16, 32])
                    sinb = cs_tab[:, i, 1, :].unsqueeze(1).broadcast_to([128, 16, 32])
                    x1 = ps_qk4[:, :, 0, :]
                    x2 = ps_qk4[:, :, 1, :]
                    fw.op(fw.dve, lambda e: e.tensor_tensor(out=rp[0][:], in0=x1, in1=cosb, op=ALU.mult), reads=[r_S0a, r_S0b, r_cs], writes=[r_rp])
                    fw.op(fw.dve, lambda e: e.tensor_tensor(out=rp[1][:], in0=x2, in1=sinb, op=ALU.mult), reads=[r_S0a, r_S0b, r_cs], writes=[r_rp])
                    fw.op(fw.dve, lambda e: e.tensor_tensor(out=rp[2][:], in0=x2, in1=cosb, op=ALU.mult), reads=[r_S0a, r_S0b, r_cs], writes=[r_rp])
                    fw.op(fw.dve, lambda e: e.tensor_tensor(out=rp[3][:], in0=x1, in1=sinb, op=ALU.mult), reads=[r_S0a, r_S0b, r_cs], writes=[r_rp])
                    fw.op(fw.dve, lambda e: e.tensor_tensor(out=qkr[:, :, 0, :], in0=rp[0][:], in1=rp[1][:], op=ALU.subtract), reads=[r_rp], writes=[r_qkr])
                    fw.op(fw.dve, lambda e: e.tensor_tensor(out=qkr[:, :, 1, :], in0=rp[2][:], in1=rp[3][:], op=ALU.add), reads=[r_rp], writes=[r_qkr])

                def stageB2(i):
                    sl = i % 2
                    qkr_f = qkr[:].rearrange("p a h d -> p (a h d)")

                    def tr2(e):
                        ins = None
                        for c in range(8):
                            ins = e.transpose(psT2[:, c, :], qkr_f[:, c * 128:(c + 1) * 128], ident_bf[:])
                        return ins
                    fw.op(fw.pe, tr2, reads=[r_qkr, r_c], writes=[r_POb])
                    fw.op(fw.dve, lambda e: e.tensor_copy(out=kT[:, :, i * 128:(i + 1) * 128], in_=psT2[:, 4:8, :]),
                          reads=[r_POb], writes=[r_kT])
                    fw.op(fw.dve, lambda e: e.tensor_copy(out=qst[sl][:], in_=psT2[:, 0:4, :]), reads=[r_POb], writes=[r_qst[sl]])
                    fw.dma(fw.sp, qd[g0 + i // 4, :, :, (i % 4) * 128:(i % 4 + 1) * 128], qst[sl][:], r_qst[sl], reads=[r_qst[sl]])

                stageA_load(0)
                stageA_load(1)
                stageA(0)
                stageA(1)
                for i in range(nt):
                    if i + 2 < nt:
                        stageA_load(i + 2)
                    stageB(i)
                    if i + 2 < nt:
                        stageA(i + 2)
                    stageB2(i)
                    if i >= 1:
                        pool_stage(i - 1)
                pool_stage(nt - 1)
                r_qd = R("qd_seq")
                fw.barrier()

                st1.close()
                st2 = contextlib.ExitStack()
                qpad = sb(st2, "qpad", [128, 4, 2, 512], BF16)
                r_qpad = R("qpad")
                poolT = sb(st2, "poolT", [128, 4, 4, 128], BF16)
                r_poolT = R("poolT")
                pT = [sb(st2, f"pT{i}", [128, 2, 512], BF16) for i in range(4)]
                r_pT = [R(f"pT{i}") for i in range(4)]
                pTs = [sb(st2, f"pTs{i}", [128, 2, 512], BF16) for i in range(2)]
                r_pTs = [R(f"pTs{i}") for i in range(2)]
                rl = sb(st2, "rl", [128, 2, 512], F32)
                on = rl
                od = sb(st2, "od", [128, 512], F32)
                osq = sb(st2, "osq", [128, 512], F32)
                rn = sb(st2, "rn", [128, 512], F32)
                r_ep = R("epi")
                attnT = sb(st2, "attnT", [128, 4, 512], BF16)
                r_attnT = R("attnT")
                xr = [sb(st2, f"xr{i}", [128, D], F32) for i in range(2)]
                r_xr = [R(f"xr{i}") for i in range(2)]
                ht = [sb(st2, f"ht{i}", [128, D], F32) for i in range(2)]
                r_ht = [R(f"ht{i}") for i in range(2)]
                hnf = sb(st2, "hnf", [128, D], F32)
                r_hnf = R("hnf")
                hnTf = sb(st2, "hnTf", [128, 8, 128], F32)
                r_hnTf = R("hnTf")
                hnTb = [sb(st2, f"hnTb{i}", [128, D], BF16) for i in range(2)]
                r_hnTb = [R(f"hnTb{i}") for i in range(2)]
                ss2 = sb(st2, "ss2", [128, 1], F32)
                r_ss2 = R("ss2")

                fw.op(fw.pool, lambda e: e.memset(qpad[:], 0.0), writes=[r_qpad])
                nk = nt
                for j in range(ng if _DBG not in ("p1",) else 0):
                    gi = g0 + j
                    for m in range(2):
                        dst = qpad[m * 64:(m + 1) * 64, :, m, :]
                        fw.dma(fw.sp, dst, qd[gi, m * 64:(m + 1) * 64, :, :], r_qpad, writes=[r_qpad])
                    fw.dma(fw.sp, poolT[:], pd[gi], r_poolT, writes=[r_poolT])
                    units = [(hh, kk) for hh in range(4) for kk in range(nk)]

                    def emit_qk(ui):
                        hh, kk = units[ui]
                        bb = ui % 2
                        Sb = S0 if bb == 0 else S1
                        r_Sb = [r_S0a, r_S0b] if bb == 0 else [r_S1a, r_S1b]

                        def mm_s(e):
                            e.matmul(Sb[:, 0, :], kT[:, hh, kk * 128:(kk + 1) * 128], qpad[:, hh, 0, :], start=True, stop=True)
                            return e.matmul(Sb[:, 1, :], kT[:, hh, kk * 128:(kk + 1) * 128], qpad[:, hh, 1, :], start=True, stop=True)
                        fw.op(fw.pe, mm_s, reads=[r_kT, r_qpad], writes=r_Sb)

                    def emit_exp_pv(ui):
                        hh, kk = units[ui]
                        bb = ui % 2
                        p4 = ui % 4
                        Sb = S0 if bb == 0 else S1
                        r_Sb = [r_S0a, r_S0b] if bb == 0 else [r_S1a, r_S1b]
                        fw.op(fw.act, lambda e: e.activation(out=pT[p4][:], in_=Sb[:], func=AF.Exp, scale=0.125),
                              reads=r_Sb, writes=[r_pT[p4]])
                        if ui + 2 < len(units):
                            emit_qk(ui + 2)

                        def mm_pv(e):
                            ins = None
                            for m in range(2):
                                ins = e.matmul(PO[:, m, :], V[:, kk, hh * 128:(hh + 1) * 128], pT[p4][:, m, :],
                                               start=(kk == 0), stop=(kk == nk - 1))
                            return ins
                        fw.op(fw.pe, mm_pv, reads=[r_V, r_pT[p4]], writes=[r_POa, r_POb])
                        def mm_l_for(ps2_, kk_):
                            def mm_l(e):
                                ins = None
                                for m in range(2):
                                    ins = e.matmul(PL[:, m, :], ones_bf[:], pTs[ps2_][:, m, :],
                                                   start=(kk_ == 1), stop=(kk_ == nk - 1))
                                return ins
                            fw.op(fw.pe, mm_l, reads=[r_pTs[ps2_], r_ones], writes=[r_PLa, r_PLb])
                        if kk % 2 == 0 and pend_l:
                            mm_l_for(*pend_l.pop())
                        if kk % 2 == 1:
                            ps2 = (ui // 2) % 2
                            pprev = (ui - 1) % 4
                            fw.op(fw.dve, lambda e: e.tensor_tensor(out=pTs[ps2][:], in0=pT[pprev][:], in1=pT[p4][:], op=ALU.add),
                                  reads=[r_pT[pprev], r_pT[p4]], writes=[r_pTs[ps2]])
                            if kk == nk - 1:
                                mm_l_for(ps2, kk)
                            else:
                                pend_l.append((ps2, kk))

                    pend_l = []
                    emit_qk(0)
                    if len(units) > 1:
                        emit_qk(1)
                    for ui in range(len(units)):
                        h, kc = units[ui]
                        emit_exp_pv(ui)
                        if kc != nk - 1:
                            continue
                        fw.op(fw.dve, lambda e: e.reciprocal(out=rl[:], in_=PL[:]), reads=[r_PLa, r_PLb], writes=[r_ep])
                        fw.op(fw.dve, lambda e: e.tensor_tensor(out=on[:], in0=PO[:], in1=rl[:], op=ALU.mult),
                              reads=[r_POa, r_POb, r_ep], writes=[r_ep])
                        fw.op(fw.dve, lambda e: e.scalar_tensor_tensor(out=od[:], in0=on[:, 1, :], scalar=neglam[:], in1=on[:, 0, :],
                                                                       op0=ALU.mult, op1=ALU.add),
                              reads=[r_ep, r_lam], writes=[r_ep])
                        fw.op(fw.act, lambda e: e.activation(out=osq[:], in_=od[:], func=AF.Square), reads=[r_ep], writes=[r_ep])
                        fw.op(fw.pe, lambda e: e.matmul(PL[:, 0, :], ones_f[:], osq[:], start=True, stop=True),
                              reads=[r_ep, r_ones], writes=[r_PLa])
                        fw.op(fw.dve, lambda e: e.tensor_scalar(out=rn[:], in0=PL[:, 0, :], scalar1=1.0 / 128, scalar2=EPS,
                                                                op0=ALU.mult, op1=ALU.add), reads=[r_PLa], writes=[r_ep])
                        fw.op(fw.act, lambda e: e.activation(out=rn[:], in_=rn[:], func=AF.Ln), reads=[r_ep], writes=[r_ep])
                        fw.op(fw.act, lambda e: e.activation(out=rn[:], in_=rn[:], func=AF.Exp, scale=-0.5), reads=[r_ep], writes=[r_ep])
                        fw.op(fw.dve, lambda e, h=h: e.scalar_tensor_tensor(out=attnT[:, h, :], in0=od[:], scalar=1.0 - LAM_INIT, in1=rn[:],
                                                                            op0=ALU.mult, op1=ALU.mult),
                              reads=[r_ep], writes=[r_attnT])
                    def stage2A(ti):
                        tg = (tok0 // 128) + j * 4 + ti
                        sl = ti % 2
                        Hb = S0 if sl == 0 else S1
                        r_Hb = [r_S0a, r_S0b] if sl == 0 else [r_S1a, r_S1b]
                        fw.dma(fw.sp, xr[sl][:], x[tg * 128:(tg + 1) * 128, :], r_xr[sl], writes=[r_xr[sl]])

                        def mm_o(e, ti=ti, Hb=Hb):
                            ins = None
                            for cc in range(2):
                                for c in range(8):
                                    lhsT = poolT[:, ti, c, :] if c < 4 else attnT[:, c - 4, ti * 128:(ti + 1) * 128]
                                    ins = e.matmul(Hb[:, cc, :], lhsT, Wout[:, c, cc * 512:(cc + 1) * 512],
                                                   start=(c == 0), stop=(c == 7))
                            return ins
                        fw.op(fw.pe, mm_o, reads=[r_poolT, r_attnT, r_Wout], writes=r_Hb)
                        Hf = Hb[:].rearrange("p a b -> p (a b)")
                        fw.op(fw.dve, lambda e, Hf=Hf, sl=sl: e.tensor_tensor(out=ht[sl][:], in0=Hf, in1=xr[sl][:], op=ALU.add),
                              reads=r_Hb + [r_xr[sl]], writes=[r_ht[sl]])
                        fw.dma(fw.act, h_d[tg], ht[sl][:], r_ht[sl], reads=[r_ht[sl]])
                        fw.op(fw.act, lambda e, sl=sl: e.activation(out=hnTb[sl][:], in_=ht[sl][:], func=AF.Square, accum_out=ss2[:]),
                              reads=[r_ht[sl]], writes=[r_hnTb[sl], r_ss2])
                        rstd_chain(ss2[:], ss2[:], [r_ss2], 1.0 / D)
                        fw.op(fw.dve, lambda e, sl=sl: e.scalar_tensor_tensor(out=hnf[:], in0=ht[sl][:], scalar=ss2[:], in1=gffn[:],
                                                                              op0=ALU.mult, op1=ALU.mult),
                              reads=[r_ht[sl], r_ss2, r_c], writes=[r_hnf])
                        psTf = PO[:].rearrange("p a (c t) -> p (a c) t", t=128)

                        def tr3(e):
                            ins = None
                            for c in range(8):
                                ins = e.matmul(psTf[:, c, :], hnf[:, c * 128:(c + 1) * 128], ident_f[:], start=True, stop=True)
                            return ins
                        fw.op(fw.pe, tr3, reads=[r_hnf, r_c], writes=[r_POa, r_POb])
                        fw.op(fw.dve, lambda e: e.tensor_copy(out=hnTf[:], in_=psTf), reads=[r_POa, r_POb], writes=[r_hnTf])
                        fw.op(fw.pool, lambda e, sl=sl: e.tensor_copy(out=hnTb[sl][:], in_=hnf[:]),
                              reads=[r_hnf], writes=[r_hnTb[sl]])
                        fw.dma(fw.pool, hn_d[tg], hnTb[sl][:], r_hnTb[sl], reads=[r_hnTb[sl]])
                        ps_r = PL[:, 1, (ti % 2) * 64:(ti % 2) * 64 + 36]

                        def mm_r(e):
                            ins = None
                            for c in range(8):
                                ins = e.matmul(ps_r, hnTf[:, c, :], Wr[:, c, :], start=(c == 0), stop=(c == 7))
                            return ins
                        fw.op(fw.pe, mm_r, reads=[r_hnTf, r_Wr], writes=[r_psr[ti % 2], r_PLb])

                    def stage2B(ti):
                        tg = (tok0 // 128) + j * 4 + ti
                        ps_r = PL[:, 1, (ti % 2) * 64:(ti % 2) * 64 + 36]
                        fw.op(fw.dve, lambda e: e.tensor_tensor(out=lg_all[:, tg, :], in0=ps_r, in1=biasb[:], op=ALU.add),
                              reads=[r_psr[ti % 2], r_PLb, r_c], writes=[r_gates[tg]])

                    stage2A(0)
                    for ti in range(4):
                        if ti + 1 < 4:
                            stage2A(ti + 1)
                        stage2B(ti)
                tok0 += N
                g0 += ng
                fw.barrier()
                st2.close()
        fw.barrier()
        I32 = mybir.dt.int32
        with contextlib.ExitStack() as st:
            OH1 = sb(st, "OH1", [128, NT, 32], BF16)
            OH2 = sb(st, "OH2", [128, NT, 32], BF16)
            wts = sb(st, "wts", [128, NT, 2], F32)
            d1i = sb(st, "d1i", [128, NT], I32)
            d2i = sb(st, "d2i", [128, NT], I32)
            idxw = sb(st, "idxw", [128, NB], I32)
            r_rt3 = R("route3")
            TA = st.enter_context(nc.psum_tensor("TA", [128, 512], F32))
            TB = st.enter_context(nc.psum_tensor("TB", [128, 512], F32))
            G0 = st.enter_context(nc.psum_tensor("G0", [128, 2, 512], F32))
            G1 = st.enter_context(nc.psum_tensor("G1", [128, 2, 512], F32))
            Y0 = st.enter_context(nc.psum_tensor("Y0", [128, 2, 512], F32))
            r_TA, r_TB, r_G0, r_G1, r_Y0 = R("TA"), R("TB"), R("G0p"), R("G1p"), R("Y0p")
            with contextlib.ExitStack() as s0:
                r_r0 = R("router0")
                gq = sb(s0, "gq", [128, 8, NT], F32)
                mg = sb(s0, "mg", [128, NT, 4], F32)
                eg = sb(s0, "eg", [128, NT, 4], F32)
                t48 = sb(s0, "t48", [128, NT, 4, 8], F32)
                les = sb(s0, "les", [128, NT, 8], F32)
                les2 = sb(s0, "les2", [128, NT, 8], F32)
                k1 = sb(s0, "k1", [128, NT, 8], F32)
                k2 = sb(s0, "k2", [128, NT, 8], F32)
                gmax, gsum, m1, m2, ex, w1, w2 = (gq[:, i, :] for i in range(7))
                lg = lg_all[:, :, 0:4]
                le4 = lg_all[:, :, 4:36].rearrange("p t (g j) -> p t g j", g=4)
                rr0 = [r_r0]

                def d0(fn, extra=()):
                    fw.op(fw.dve, fn, reads=rr0 + list(extra), writes=rr0)

                def bc3(ap2, k):
                    return ap2.unsqueeze(2).broadcast_to([128, NT, k])
                d0(lambda e: e.tensor_reduce(out=gmax, in_=lg, axis=AX.X, op=ALU.max), r_gates)
                d0(lambda e: e.tensor_tensor(out=mg[:], in0=lg, in1=bc3(gmax, 4), op=ALU.is_equal), r_gates)
                d0(lambda e: e.tensor_tensor(out=eg[:], in0=lg, in1=bc3(gmax, 4), op=ALU.subtract), r_gates)
                fw.op(fw.act, lambda e: e.activation(out=eg[:], in_=eg[:], func=AF.Exp), reads=rr0, writes=rr0)
                d0(lambda e: e.tensor_reduce(out=gsum, in_=eg[:], axis=AX.X, op=ALU.add))
                d0(lambda e: e.reciprocal(out=gsum, in_=gsum))
                d0(lambda e: e.tensor_tensor(out=t48[:], in0=le4, in1=mg[:].unsqueeze(3).broadcast_to([128, NT, 4, 8]), op=ALU.mult), r_gates)
                d0(lambda e: e.tensor_reduce(out=les[:], in_=t48[:].rearrange("p t g j -> p t j g"), axis=AX.X, op=ALU.add))
                d0(lambda e: e.tensor_reduce(out=m1, in_=les[:], axis=AX.X, op=ALU.max))
                d0(lambda e: e.tensor_tensor(out=k1[:], in0=les[:], in1=bc3(m1, 8), op=ALU.is_equal))
                d0(lambda e: e.scalar_tensor_tensor(out=les2[:], in0=k1[:], scalar=-1e30, in1=les[:], op0=ALU.mult, op1=ALU.add))
                d0(lambda e: e.tensor_reduce(out=m2, in_=les2[:], axis=AX.X, op=ALU.max))
                d0(lambda e: e.tensor_tensor(out=k2[:], in0=les2[:], in1=bc3(m2, 8), op=ALU.is_equal))
                d0(lambda e: e.tensor_tensor(out=ex, in0=m2, in1=m1, op=ALU.subtract))
                fw.op(fw.act, lambda e: e.activation(out=ex, in_=ex, func=AF.Exp), reads=rr0, writes=rr0)
                d0(lambda e: e.tensor_scalar(out=w1, in0=ex, scalar1=1.0, scalar2=None, op0=ALU.add))
                d0(lambda e: e.reciprocal(out=w1, in_=w1))
                d0(lambda e: e.tensor_tensor(out=w1, in0=w1, in1=gsum, op=ALU.mult))
                d0(lambda e: e.tensor_tensor(out=w2, in0=w1, in1=ex, op=ALU.mult))
                mgb4 = mg[:].unsqueeze(3).broadcast_to([128, NT, 4, 8])
                o1v = OH1[:].rearrange("p t (g j) -> p t g j", g=4)
                o2v = OH2[:].rearrange("p t (g j) -> p t g j", g=4)
                fw.op(fw.dve, lambda e: e.tensor_tensor(out=o1v, in0=k1[:].unsqueeze(2).broadcast_to([128, NT, 4, 8]), in1=mgb4, op=ALU.mult),
                      reads=rr0, writes=r_gates)
                fw.op(fw.dve, lambda e: e.tensor_tensor(out=o2v, in0=k2[:].unsqueeze(2).broadcast_to([128, NT, 4, 8]), in1=mgb4, op=ALU.mult),
                      reads=rr0, writes=r_gates)
                fw.op(fw.dve, lambda e: e.tensor_copy(out=wts[:, :, 0], in_=w1), reads=rr0, writes=r_gates)
                fw.op(fw.dve, lambda e: e.tensor_copy(out=wts[:, :, 1], in_=w2), reads=rr0, writes=r_gates)
                fw.barrier()
            with contextlib.ExitStack() as sa:
                tri = sb(sa, "tri", [128, 128], BF16)
                bstart = sb(sa, "bstart", [128, NB], F32)
                pidx = sb(sa, "pidx", [128, 1], F32)
                r_ca = R("constA")
                fw.dma(fw.sp, tri[:], tri_d, r_ca, writes=[r_ca])
                fw.dma(fw.sp, bstart[:], bstart_d, r_ca, writes=[r_ca])
                fw.dma(fw.sp, pidx[:], pidx_d, r_ca, writes=[r_ca])
                NC3 = NT * 32
                Mb = sb(sa, "Mb", [128, NC3], BF16)
                rank = sb(sa, "rank", [128, NT, 32], F32)
                cnt = sb(sa, "cnt", [128, NT, 32], F32)
                sA = sb(sa, "sA", [128, NT, 32], F32)
                sB = sb(sa, "sB", [128, NT, 32], F32)
                cmp = sb(sa, "cmp", [128, NB, 32], F32)
                sm = sb(sa, "sm", [128, 8, 32], F32)
                ebf = sb(sa, "ebf", [128, NB], F32)
                eb2 = sb(sa, "eb2", [128, NB], F32)
                dfl = sb(sa, "dfl", [128, 2, NT], F32)
                Dv = fw.dve
                rr = [r_rt3]

                def dv(fn, extra=()):
                    fw.op(Dv, fn, reads=rr + list(extra), writes=rr)
                OH1f = OH1[:].rearrange("p t e -> p (t e)")
                OH2f = OH2[:].rearrange("p t e -> p (t e)")
                dv(lambda e: e.tensor_tensor(out=Mb[:], in0=OH1f, in1=OH2f, op=ALU.add), r_gates)
                rankf = rank[:].rearrange("p t e -> p (t e)")
                cntf = cnt[:].rearrange("p t e -> p (t e)")
                nch = (NC3 + 511) // 512
                for ch in range(nch):
                    c0 = ch * 512
                    c1 = min(NC3, c0 + 512)
                    w_ = c1 - c0
                    fw.op(fw.pe, lambda e: e.matmul(TA[:, 0:w_], tri[:], Mb[:, c0:c1], start=True, stop=True),
                          reads=[r_rt3, r_ca], writes=[r_TA])
                    fw.op(fw.pe, lambda e: e.matmul(TB[:, 0:w_], ones_bf[:], Mb[:, c0:c1], start=True, stop=True),
                          reads=[r_rt3, r_ones], writes=[r_TB])
                    fw.op(Dv, lambda e: e.tensor_copy(out=rankf[:, c0:c1], in_=TA[:, 0:w_]), reads=[r_TA], writes=rr)
                    fw.op(Dv, lambda e: e.tensor_copy(out=cntf[:, c0:c1], in_=TB[:, 0:w_]), reads=[r_TB], writes=rr)
                dv(lambda e: e.tensor_copy(out=sA[:], in_=cnt[:]))
                src, dst = sA, sB
                sft = 1
                while sft < NT:
                    dv(lambda e: e.tensor_tensor(out=dst[:, sft:, :], in0=src[:, sft:, :], in1=src[:, 0:NT - sft, :], op=ALU.add))
                    dv(lambda e: e.tensor_copy(out=dst[:, 0:sft, :], in_=src[:, 0:sft, :]))
                    src, dst = dst, src
                    sft *= 2
                incl = src
                other = dst
                tot = sm[:, 0, :]
                xm = sm[:, 1, :]
                padded = sm[:, 2, :]
                pA = sm[:, 3, :]
                pB = sm[:, 4, :]
                pstart = sm[:, 5, :]
                dv(lambda e: e.tensor_copy(out=tot, in_=incl[:, NT - 1, :]))
                dv(lambda e: e.tensor_scalar(out=xm, in0=tot, scalar1=1.0 / 128, scalar2=0.49609375, op0=ALU.mult, op1=ALU.add))
                dv(lambda e: e.tensor_scalar(out=xm, in0=xm, scalar1=8388608.0, scalar2=None, op0=ALU.add))
                dv(lambda e: e.tensor_scalar(out=xm, in0=xm, scalar1=-8388608.0, scalar2=None, op0=ALU.add))
                dv(lambda e: e.tensor_scalar(out=padded, in0=xm, scalar1=128.0, scalar2=None, op0=ALU.mult))
                dv(lambda e: e.tensor_copy(out=pA, in_=padded))
                ps_, pd_ = pA, pB
                sft = 1
                while sft < 32:
                    dv(lambda e: e.tensor_tensor(out=pd_[:, sft:], in0=ps_[:, sft:], in1=ps_[:, 0:32 - sft], op=ALU.add))
                    dv(lambda e: e.tensor_copy(out=pd_[:, 0:sft], in_=ps_[:, 0:sft]))
                    ps_, pd_ = pd_, ps_
                    sft *= 2
                pend = ps_
                dv(lambda e: e.tensor_tensor(out=pstart, in0=pend, in1=padded, op=ALU.subtract))
                dv(lambda e: e.tensor_tensor(out=other[:], in0=incl[:], in1=cnt[:], op=ALU.subtract))
                dv(lambda e: e.tensor_tensor(out=other[:], in0=other[:], in1=rank[:], op=ALU.add))
                dv(lambda e: e.tensor_tensor(out=other[:], in0=other[:], in1=pstart.unsqueeze(1).broadcast_to([128, NT, 32]), op=ALU.add))
                dest = other
                dv(lambda e: e.tensor_tensor(out=incl[:], in0=dest[:], in1=OH1[:], op=ALU.mult), r_gates)
                dv(lambda e: e.tensor_reduce(out=dfl[:, 0, :], in_=incl[:], axis=AX.X, op=ALU.add))
                dv(lambda e: e.tensor_tensor(out=incl[:], in0=dest[:], in1=OH2[:], op=ALU.mult), r_gates)
                dv(lambda e: e.tensor_reduce(out=dfl[:, 1, :], in_=incl[:], axis=AX.X, op=ALU.add))
                dv(lambda e: e.tensor_copy(out=d1i[:], in_=dfl[:, 0, :]))
                dv(lambda e: e.tensor_copy(out=d2i[:], in_=dfl[:, 1, :]))
                dv(lambda e: e.tensor_tensor(out=cmp[:], in0=pend.unsqueeze(1).broadcast_to([128, NB, 32]),
                                             in1=bstart[:].unsqueeze(2).broadcast_to([128, NB, 32]), op=ALU.is_le), [r_ca])
                dv(lambda e: e.tensor_reduce(out=ebf[:], in_=cmp[:], axis=AX.X, op=ALU.add))
                dv(lambda e: e.tensor_scalar(out=ebf[:], in0=ebf[:], scalar1=float(NE - 1), scalar2=None, op0=ALU.min))
                BIG = 1.0e6
                dv(lambda e: e.memset(eb2[:], 1.0))
                dv(lambda e: e.tensor_tensor(out=eb2[:, 2:], in0=ebf[:, 2:], in1=ebf[:, 0:NB - 2], op=ALU.not_equal))
                dv(lambda e: e.tensor_scalar(out=ebf[:], in0=ebf[:], scalar1=128.0, scalar2=pidx[:], op0=ALU.mult, op1=ALU.add), [r_ca])
                dv(lambda e: e.tensor_scalar(out=ebf[:], in0=ebf[:], scalar1=-BIG, scalar2=None, op0=ALU.add))
                dv(lambda e: e.tensor_tensor(out=ebf[:], in0=ebf[:], in1=eb2[:], op=ALU.mult))
                dv(lambda e: e.tensor_scalar(out=ebf[:], in0=ebf[:], scalar1=BIG, scalar2=None, op0=ALU.add))
                dv(lambda e: e.tensor_copy(out=idxw[:], in_=ebf[:]))
                fw.barrier()

            _bregs = {}

            def _breg(bound):
                if bound not in _bregs:
                    rg = nc.gpsimd.alloc_register(f"bnd{len(_bregs)}")
                    nc.gpsimd.reg_mov(rg, int(bound))
                    _bregs[bound] = rg
                return _bregs[bound]

            def indirect(out, out_off, in_, in_off, bound, sb_res, reads, writes):
                deps = []
                for r_ in reads:
                    if r_.last_w is not None:
                        deps.append(r_.last_w)
                for w_r in writes:
                    if w_r.last_w is not None:
                        deps.append(w_r.last_w)
                    deps.extend(w_r.readers)
                fw.pool.wait_tokens(deps)
                ent = fw.dma_reg.setdefault(sb_res.name, [None, 0])
                if ent[0] is None:
                    ent[0] = fw.new_sem(f"dma_{sb_res.name}")
                ins = nc.gpsimd.indirect_dma_start(
                    out=out, out_offset=(bass.IndirectOffsetOnAxis(ap=out_off, axis=0) if out_off is not None else None),
                    in_=in_, in_offset=(bass.IndirectOffsetOnAxis(ap=in_off, axis=0) if in_off is not None else None),
                    bounds_check=_breg(bound), oob_is_err=False)
                ent[1] += 16
                ins.then_inc(fw.sem_by_key[ent[0]], 16)
                tok = (ent[0], ent[1])
                for r_ in reads:
                    r_.readers.append(tok)
                for w_r in writes:
                    w_r.last_w = tok
                    w_r.readers = []

            with contextlib.ExitStack() as sbk:
                hb = [sb(sbk, f"hb{i}", [128, D], BF16) for i in range(3)]
                r_hb = [R(f"hb{i}") for i in range(3)]
                for i in range(NT):
                    s3 = i % 3
                    fw.dma(fw.sp, hb[s3][:], hn_d[i], r_hb[s3], writes=[r_hb[s3]])
                    indirect(xg[:, :], d1i[:, i:i + 1], hb[s3][:, :], None, PR - 1, r_hb[s3], [r_hb[s3], r_rt3], [])
                    indirect(xg[:, :], d2i[:, i:i + 1], hb[s3][:, :], None, PR - 1, r_hb[s3], [r_hb[s3], r_rt3], [])
                fw.barrier()
            wgv = w_gate[:, :]
            wuv = w_up[:, :]
            wdv = w_down[:, :]
            with contextlib.ExitStack() as sc:
                identb = sb(sc, "identb", [128, 128], BF16)
                r_idb = R("identb")
                fw.dma(fw.sp, identb[:], ident_bf_d, r_idb, writes=[r_idb])
                wg = [sb(sc, f"wg{i}", [128, 8 * DFF], BF16) for i in range(2)]
                wu = [sb(sc, f"wu{i}", [128, 8 * DFF], BF16) for i in range(2)]
                wd = [sb(sc, f"wd{i}", [128, 4 * D], BF16) for i in range(2)]
                r_wg = [R("wg0"), R("wg1")]
                r_wu = [R("wu0"), R("wu1")]
                r_wd = [R("wd0"), R("wd1")]
                xb = [sb(sc, f"xb{i}", [128, D], BF16) for i in range(2)]
                r_xb = [R("xb0"), R("xb1")]
                xgT = [sb(sc, f"xgT{i}", [128, 8, 128], BF16) for i in range(2)]
                r_xgT = [R("xgT0"), R("xgT1")]
                sgl = sb(sc, "sgl", [128, 512], F32)
                r_sgl = R("sgl")
                hidT = [sb(sc, f"hidT{i}", [128, 4, 128], BF16) for i in range(2)]
                r_hid = [R("hid0"), R("hid1")]
                yb = [sb(sc, f"yb{i}", [128, D], F32) for i in range(2)]
                r_yb = [R("yb0"), R("yb1")]
                Ts = [TA, TB]
                r_Ts = [r_TA, r_TB]
                Gs = [G0, G1]
                r_Gs = [r_G0, r_G1]
                for ws in range(2):
                    pass
                hidtok = [sb(sc, f"hidtok{i}", [128, DFF], BF16) for i in range(2)]
                r_hidtok = [R("hidtok0"), R("hidtok1")]
                sgl2 = [sgl, sb(sc, "sglb", [128, 512], F32)]
                r_sgl2 = [r_sgl, R("sglb")]
                xb3 = xb + [sb(sc, "xb2", [128, D], BF16)]
                r_xb3 = r_xb + [R("xb2")]
                xgT3 = xgT + [sb(sc, "xgT2", [128, 8, 128], BF16)]
                r_xgT3 = r_xgT + [R("xgT2")]
                psT3 = TA[:].bitcast(BF16).rearrange("p (c t) -> p c t", c=8)
                psH = TB[:, 0:256].bitcast(BF16).rearrange("p (c t) -> p c t", c=4)

                def gath_gu(b):
                    ws = b % 2
                    indirect(wg[ws][:, :], None, wgv, idxw[:, b:b + 1], NE * 128 - 1, r_wg[ws], [r_rt3], [r_wg[ws]])
                    indirect(wu[ws][:, :], None, wuv, idxw[:, b:b + 1], NE * 128 - 1, r_wu[ws], [r_rt3], [r_wu[ws]])

                def gath_d(b):
                    ws = b % 2
                    indirect(wd[ws][:, :], None, wdv, idxw[:, b:b + 1], NE * 128 - 1, r_wd[ws], [r_rt3], [r_wd[ws]])

                def preC(b):
                    s3 = b % 3
                    fw.dma(fw.sp, xb3[s3][:], xg[b * 128:(b + 1) * 128, :], r_xb3[s3], writes=[r_xb3[s3]])
                    xbv = xb3[s3][:].rearrange("p (q c) -> p c q", c=8)

                    def tr4(e):
                        ins = None
                        for c in range(8):
                            ins = e.transpose(psT3[:, c, :], xbv[:, c, :], identb[:])
                        return ins
                    fw.op(fw.pe, tr4, reads=[r_xb3[s3], r_idb], writes=[r_TA])
                    fw.op(fw.dve, lambda e: e.tensor_copy(out=xgT3[s3][:], in_=psT3), reads=[r_TA], writes=[r_xgT3[s3]])

                def stageG(b):
                    ws = b % 2
                    s3 = b % 3
                    Gb = Gs[ws]
                    wgs = wg[ws][:].rearrange("p (c f) -> p c f", c=8)
                    wus = wu[ws][:].rearrange("p (c f) -> p c f", c=8)

                    def mm_gu(e):
                        ins = None
                        for which, wv in ((0, wgs), (1, wus)):
                            for c in range(8):
                                ins = e.matmul(Gb[:, which, :], xgT3[s3][:, c, :], wv[:, c, :], start=(c == 0), stop=(c == 7))
                        return ins
                    fw.op(fw.pe, mm_gu, reads=[r_xgT3[s3], r_wg[ws], r_wu[ws]], writes=[r_Gs[ws]])
                    fw.op(fw.act, lambda e: e.activation(out=sgl2[ws][:], in_=Gb[:, 0, :], func=AF.Silu), reads=[r_Gs[ws]], writes=[r_sgl2[ws]])
                    fw.op(fw.dve, lambda e: e.tensor_tensor(out=hidtok[ws][:], in0=sgl2[ws][:], in1=Gb[:, 1, :], op=ALU.mult),
                          reads=[r_sgl2[ws], r_Gs[ws]], writes=[r_hidtok[ws]])

                def stageH(b):
                    ws = b % 2
                    wds = wd[ws][:].rearrange("p (c d) -> p c d", c=4)
                    hv = hidtok[ws][:].rearrange("p (q c) -> p c q", c=4)

                    def trH(e):
                        ins = None
                        for c in range(4):
                            ins = e.transpose(psH[:, c, :], hv[:, c, :], identb[:])
                        return ins
                    fw.op(fw.pe, trH, reads=[r_hidtok[ws], r_idb], writes=[r_TB])
                    fw.op(fw.dve, lambda e: e.tensor_copy(out=hidT[ws][:], in_=psH), reads=[r_TB], writes=[r_hid[ws]])

                def stageH2(b):
                    ws = b % 2
                    wds = wd[ws][:].rearrange("p (c d) -> p c d", c=4)

                    def mm_d(e):
                        ins = None
                        for cc in range(2):
                            for fc in range(4):
                                ins = e.matmul(Y0[:, cc, :], hidT[ws][:, fc, :], wds[:, fc, cc * 512:(cc + 1) * 512],
                                               start=(fc == 0), stop=(fc == 3))
                        return ins
                    fw.op(fw.pe, mm_d, reads=[r_hid[ws], r_wd[ws]], writes=[r_Y0])
                    fw.op(fw.act, lambda e: e.activation(out=yb[ws][:], in_=Y0[:].rearrange("p a b -> p (a b)"), func=AF.Copy),
                          reads=[r_Y0], writes=[r_yb[ws]])
                    fw.dma(fw.sp, yg[b * 128:(b + 1) * 128, :], yb[ws][:], r_yb[ws], reads=[r_yb[ws]])

                for bb in (0, 1):
                    if bb < NB:
                        gath_gu(bb)
                        gath_d(bb)
                        preC(bb)
                stageG(0)
                for b in range(NB):
                    stageH(b)
                    if b + 2 < NB:
                        gath_gu(b + 2)
                        preC(b + 2)
                    stageH2(b)
                    if b + 1 < NB:
                        stageG(b + 1)
                    if b + 2 < NB:
                        gath_d(b + 2)
                fw.barrier()
            with contextlib.ExitStack() as sd:
                gfin = sb(sd, "gfin", [128, D], F32)
                fw.dma(fw.sp, gfin[:], gfin_b, r_gfin, writes=[r_gfin])
                y1 = [sb(sd, f"y1{i}", [128, D], F32) for i in range(2)]
                y2 = [sb(sd, f"y2{i}", [128, D], F32) for i in range(2)]
                r_y1 = [R("y10"), R("y11")]
                r_y2 = [R("y20"), R("y21")]
                hr = [sb(sd, f"hr{i}", [128, D], F32) for i in range(2)]
                r_hr = [R("hr0"), R("hr1")]
                yt = [sb(sd, f"yt{i}", [128, D], F32) for i in range(2)]
                r_yt = [R("yt0"), R("yt1")]
                junk3 = sb(sd, "junk3", [128, D], BF16)
                r_junk3 = R("junk3")
                ss3 = [sb(sd, f"ss3{i}", [128, 1], F32) for i in range(2)]
                r_ss3 = [R("ss30"), R("ss31")]
                def loadD(tg):
                    sl = tg % 2
                    indirect(y1[sl][:, :], None, yg[:, :], d1i[:, tg:tg + 1], PR - 1, r_y1[sl], [r_rt3], [r_y1[sl]])
                    indirect(y2[sl][:, :], None, yg[:, :], d2i[:, tg:tg + 1], PR - 1, r_y2[sl], [r_rt3], [r_y2[sl]])
                    fw.dma(fw.sp, hr[sl][:], h_d[tg], r_hr[sl], writes=[r_hr[sl]])

                def compD(tg):
                    sl = tg % 2
                    fw.op(fw.dve, lambda e: e.scalar_tensor_tensor(out=hr[sl][:], in0=y1[sl][:], scalar=wts[:, tg, 0:1], in1=hr[sl][:],
                                                                   op0=ALU.mult, op1=ALU.add),
                          reads=[r_y1[sl], r_hr[sl], r_gates[tg]], writes=[r_hr[sl]])
                    fw.op(fw.dve, lambda e: e.scalar_tensor_tensor(out=hr[sl][:], in0=y2[sl][:], scalar=wts[:, tg, 1:2], in1=hr[sl][:],
                                                                   op0=ALU.mult, op1=ALU.add),
                          reads=[r_y2[sl], r_hr[sl], r_gates[tg]], writes=[r_hr[sl]])
                    fw.op(fw.act, lambda e: e.activation(out=junk3[:], in_=hr[sl][:], func=AF.Square, accum_out=ss3[sl][:]),
                          reads=[r_hr[sl]], writes=[r_junk3, r_ss3[sl]])
                    fw.op(fw.dve, lambda e: e.tensor_scalar(out=ss3[sl][:], in0=ss3[sl][:], scalar1=1.0 / D, scalar2=EPS, op0=ALU.mult, op1=ALU.add),
                          reads=[r_ss3[sl]], writes=[r_ss3[sl]])
                    fw.op(fw.act, lambda e: e.activation(out=ss3[sl][:], in_=ss3[sl][:], func=AF.Ln), reads=[r_ss3[sl]], writes=[r_ss3[sl]])
                    fw.op(fw.act, lambda e: e.activation(out=ss3[sl][:], in_=ss3[sl][:], func=AF.Exp, scale=-0.5), reads=[r_ss3[sl]], writes=[r_ss3[sl]])
                    fw.op(fw.dve, lambda e: e.scalar_tensor_tensor(out=yt[sl][:], in0=hr[sl][:], scalar=ss3[sl][:], in1=gfin[:],
                                                                   op0=ALU.mult, op1=ALU.mult),
                          reads=[r_hr[sl], r_ss3[sl], r_gfin], writes=[r_yt[sl]])
                    fw.dma(fw.sp, y[tg * 128:(tg + 1) * 128, :], yt[sl][:], r_yt[sl], reads=[r_yt[sl]])

                loadD(0)
                for tg in range(NT):
                    if tg + 1 < NT:
                        loadD(tg + 1)
                    compD(tg)
                fw.barrier()
    return nc


def _const_tables(max_n):
    max_nt = max_n // 128
    inv_freq = (np.float32(10000.0) ** (-np.arange(0, 64, 2, dtype=np.float32) / np.float32(64))).astype(np.float32)
    pos = np.arange(max_n, dtype=np.float32)
    ang = (pos[:, None] * inv_freq[None, :]).astype(np.float32)
    cos = np.cos(ang).astype(np.float32).reshape(max_nt, 128, 32)
    sin = np.sin(ang).astype(np.float32).reshape(max_nt, 128, 32)
    cs = np.stack([cos, sin], axis=2)
    cs = np.ascontiguousarray(cs.transpose(1, 0, 2, 3))
    invc = np.zeros((3, 4, 128), np.float32)
    for g, w in enumerate((2, 4, 8, 16)):
        half = w // 2
        t = np.arange(128)
        invc[0, g] = 1.0 / (np.minimum(t + half, 10 ** 9) - np.maximum(t - half, 0))
        invc[1, g] = 1.0 / w
        invc[2, g] = 1.0 / (np.minimum(t + half, 128) - (t - half))
    invc = np.ascontiguousarray(np.broadcast_to(invc[None], (128, 3, 4, 128))).astype(np.float32)
    return cs, invc


_PROG_CACHE = {}


def run_cores(core_seqs, weights, n_cores, sg_tok):
    seq_lens = tuple(int(s.shape[0]) for s in core_seqs[0])
    key = (seq_lens, sg_tok)
    if key not in _PROG_CACHE:
        _PROG_CACHE[key] = build_program(list(seq_lens), sg_tok)
    nc = _PROG_CACHE[key]
    f32 = np.float32
    W = {k: np.asarray(v, dtype=f32) for k, v in weights.items()}
    cs, invc = _const_tables(max(seq_lens))
    NBh = (2 * sum(seq_lens)) // 128 + NE
    shared = {
        "w_in": np.ascontiguousarray(W["w_in"][0]),
        "w_out": np.ascontiguousarray(W["w_out"][0]),
        "w_pool": np.ascontiguousarray(W["w_pool"][0]),
        "w_gate": np.ascontiguousarray(W["w_gate"][0]).reshape(NE * 128, 8 * DFF),
        "w_up": np.ascontiguousarray(W["w_up"][0]).reshape(NE * 128, 8 * DFF),
        "w_down": np.ascontiguousarray(W["w_down"][0]).reshape(NE * 128, 4 * D),
        "w_r": np.ascontiguousarray(np.concatenate([W["w_router_group"][0], W["w_router_expert"][0]], axis=1)),
        "gmix_t": np.ascontiguousarray(W["g_mix"][0].reshape(8, 128).T),
        "wos_t": np.ascontiguousarray(np.concatenate([W["pool_scale"][0].reshape(4, 128).T,
                                                      np.repeat(W["subln_g"][0][:, None], 4, axis=1)], axis=1)),
        "gffn_b": np.ascontiguousarray(np.broadcast_to(W["g_ffn"][0][None, :], (128, D))),
        "gfin_b": np.ascontiguousarray(np.broadcast_to(W["g_final"][None, :], (128, D))),
        "bias_b": np.ascontiguousarray(np.broadcast_to(
            np.concatenate([W["b_router_group"][0], W["b_router_expert"][0]])[None, :], (128, 36))),
        "lam_b": np.ascontiguousarray(np.broadcast_to(
            np.stack([W["lambda_q1"][0], W["lambda_k1"][0], W["lambda_q2"][0], W["lambda_k2"][0]])[None], (128, 4, 64))),
        "ident_bf": np.eye(128, dtype=f32).astype(ml_dtypes.bfloat16),
        "ident_f": np.eye(128, dtype=f32),
        "cs_tab": cs,
        "invc": invc,
        "tri_bf": np.triu(np.ones((128, 128), f32), k=1).astype(ml_dtypes.bfloat16),
        "bstart": np.ascontiguousarray(np.broadcast_to((np.arange(NBh, dtype=f32) * 128.0)[None, :], (128, NBh))),
        "pidx": np.arange(128, dtype=f32).reshape(128, 1),
    }
    in_maps = []
    for c in range(n_cores):
        m = dict(shared)
        m["x"] = np.ascontiguousarray(np.concatenate([np.asarray(s, dtype=f32) for s in core_seqs[c]], axis=0))
        in_maps.append(m)
    res = run_bass_kernel_spmd(nc, in_maps, core_ids=list(range(n_cores)))
    outs = []
    for c in range(n_cores):
        yc = np.asarray(res.results[c]["y"])
        o = []
        off = 0
        for n in seq_lens:
            o.append(yc[off:off + n])
            off += n
        outs.append(o)
    return outs


def kernel(x_prompt, x_sample, **weights):
    n_cores = 8
    xp = np.asarray(x_prompt)
    xsm = np.asarray(x_sample)
    pp = xp.shape[0] // n_cores
    ps = xsm.shape[0] // n_cores
    core_seqs = []
    for c in range(n_cores):
        core_seqs.append([xp[c * pp + i] for i in range(pp)] + [xsm[c * ps + i] for i in range(ps)])
    outs = run_cores(core_seqs, weights, n_cores, sg_tok=2048)
    yp = np.empty(xp.shape, np.float32)
    ys = np.empty(xsm.shape, np.float32)
    for c in range(n_cores):
        for i in range(pp):
            yp[c * pp + i] = outs[c][i]
        for i in range(ps):
            ys[c * ps + i] = outs[c][pp + i]
    return (yp, ys)
```

```python
import contextlib
import numpy as np
import ml_dtypes
import concourse.bass as bass
import concourse.mybir as mybir
from concourse.bass_utils import run_bass_kernel_spmd

F32 = mybir.dt.float32
BF16 = mybir.dt.bfloat16
ALU = mybir.AluOpType
AF = mybir.ActivationFunctionType
AX = mybir.AxisListType

D = 1024
NE = 32
DFF = 512
EPS = 1e-6
SEM_WRAP = 30000
LAM_INIT = 0.2


class Res:
    __slots__ = ("name", "last_w", "readers", "dma_sem", "dma_cnt")

    def __init__(self, name):
        self.name = name
        self.last_w = None
        self.readers = []
        self.dma_sem = None
        self.dma_cnt = 0


class Eng:
    def __init__(self, fw, name, obj, is_pe=False):
        self.fw = fw
        self.name = name
        self.obj = obj
        self.is_pe = is_pe
        self.n = 0
        self.sems = []
        self.waited = {}

    def cur_sem(self):
        idx = self.n // SEM_WRAP
        while len(self.sems) <= idx:
            self.sems.append(self.fw.new_sem(f"tl_{self.name}_{len(self.sems)}"))
        return self.sems[idx]

    def wait_tokens(self, toks):
        best = {}
        for (sk, v) in toks:
            if v > best.get(sk, 0):
                best[sk] = v
        for sk, v in best.items():
            if self.waited.get(sk, 0) >= v:
                continue
            self.waited[sk] = v
            self.obj.wait_ge(self.fw.sem_by_key[sk], v)


class FW:
    def __init__(self, nc, stack):
        self.nc = nc
        self.sem_by_key = {}
        self._stack = stack
        self.all_res = []
        self.dma_reg = {}
        self.pe = Eng(self, "pe", nc.tensor, is_pe=True)
        self.act = Eng(self, "act", nc.scalar)
        self.dve = Eng(self, "dve", nc.vector)
        self.pool = Eng(self, "pool", nc.gpsimd)
        self.sp = Eng(self, "sp", nc.sync)
        self.engs = [self.pe, self.act, self.dve, self.pool, self.sp]

    def res(self, name):
        r = Res(name)
        self.all_res.append(r)
        return r

    def new_sem(self, name):
        h = self._stack.enter_context(self.nc.semaphore(name))
        key = len(self.sem_by_key)
        self.sem_by_key[key] = h
        return key

    def op(self, eng, fn, reads=(), writes=()):
        deps = []
        own = set(eng.sems)
        for r in reads:
            if r.last_w is not None:
                deps.append(r.last_w)
        for w in writes:
            if w.last_w is not None:
                deps.append(w.last_w)
            for t in w.readers:
                if t[0] in own:
                    continue
                deps.append(t)
        if eng.is_pe:
            deps = [t for t in deps if t[0] not in own]
        eng.wait_tokens(deps)
        ins = fn(eng.obj)
        sk = eng.cur_sem()
        val = (eng.n % SEM_WRAP) + 1
        eng.n += 1
        ins.then_inc(self.sem_by_key[sk], 1)
        tok = (sk, val)
        for r in reads:
            r.readers.append(tok)
        for w in writes:
            w.last_w = tok
            w.readers = []
        return tok

    def dma(self, q, out, in_, sb_res, reads=(), writes=()):
        deps = []
        for r in reads:
            if r.last_w is not None:
                deps.append(r.last_w)
        for w in writes:
            if w.last_w is not None:
                deps.append(w.last_w)
            deps.extend(w.readers)
        q.wait_tokens(deps)
        ent = self.dma_reg.setdefault(sb_res.name, [None, 0])
        if ent[0] is None:
            ent[0] = self.new_sem(f"dma_{sb_res.name}")
        ins = q.obj.dma_start(out=out, in_=in_)
        ent[1] += 16
        ins.then_inc(self.sem_by_key[ent[0]], 16)
        tok = (ent[0], ent[1])
        for r in reads:
            r.readers.append(tok)
        for w in writes:
            w.last_w = tok
            w.readers = []
        return tok

    def all_tokens(self):
        toks = []
        for r in self.all_res:
            if r.last_w is not None:
                toks.append(r.last_w)
            toks.extend(r.readers)
        for e in self.engs:
            if e.n > 0:
                idx = (e.n - 1) // SEM_WRAP
                toks.append((e.sems[idx], ((e.n - 1) % SEM_WRAP) + 1))
        return toks

    def barrier(self):
        toks = self.all_tokens()
        for e in self.engs:
            e.wait_tokens(toks)


import os as _os
_DBG = _os.environ.get("KDBG", "full")


def build_program(seq_lens, sg_tok):
    T = sum(seq_lens)
    NT = T // 128
    NG = T // 512
    max_nt = max(seq_lens) // 128
    assert all(n % 512 == 0 for n in seq_lens)
    assert T % sg_tok == 0 and sg_tok % 512 == 0
    nc = bass.Bass("TRN2", target_bir_lowering=False)

    def din(name, shape, dt=F32):
        return nc.dram_tensor(name, list(shape), dt, kind="ExternalInput").ap()

    x = din("x", [T, D])
    w_in = din("w_in", [D, 2048])
    w_out = din("w_out", [D, D])
    w_pool = din("w_pool", [4, 128, 128])
    w_gate = din("w_gate", [NE * 128, 8 * DFF])
    w_up = din("w_up", [NE * 128, 8 * DFF])
    w_down = din("w_down", [NE * 128, 4 * D])
    w_r = din("w_r", [D, 36])
    gmix_t = din("gmix_t", [128, 8])
    wos_t = din("wos_t", [128, 8])
    gffn_b = din("gffn_b", [128, D])
    gfin_b = din("gfin_b", [128, D])
    bias_b = din("bias_b", [128, 36])
    lam_b = din("lam_b", [128, 4, 64])
    ident_bf_d = din("ident_bf", [128, 128], BF16)
    ident_f_d = din("ident_f", [128, 128])
    cs_tab_d = din("cs_tab", [128, max_nt, 2, 32])
    invc_d = din("invc", [128, 3, 4, 128])
    y = nc.dram_tensor("y", [T, D], F32, kind="ExternalOutput").ap()
    qd = nc.dram_tensor("qd", [NG, 128, 4, 512], BF16, kind="Internal").ap()
    pd = nc.dram_tensor("pd", [NG, 128, 4, 4, 128], BF16, kind="Internal").ap()
    h_d = nc.dram_tensor("h_d", [NT, 128, D], F32, kind="Internal").ap()
    hn_d = nc.dram_tensor("hn_d", [NT, 128, D], BF16, kind="Internal").ap()
    NB = (2 * T) // 128 + NE
    PR = NB * 128
    xg = nc.dram_tensor("xg", [PR, D], BF16, kind="Internal").ap()
    yg = nc.dram_tensor("yg", [PR, D], F32, kind="Internal").ap()
    tri_d = din("tri_bf", [128, 128], BF16)
    bstart_d = din("bstart", [128, NB])
    pidx_d = din("pidx", [128, 1])

    with contextlib.ExitStack() as st0:
        fw = FW(nc, st0)
        R = fw.res

        sfx = [""]

        def sb(st, name, shape, dt):
            return st.enter_context(nc.sbuf_tensor("s_" + name + sfx[0], list(shape), dt))

        lg_all = sb(st0, "lg_all", [128, NT, 36], F32)
        ones_bf = sb(st0, "ones_bf", [128, 128], BF16)
        ones_f = sb(st0, "ones_f", [128, 128], F32)
        r_gates = [R(f"gates{i}") for i in range(NT)]
        r_gfin = R("gfin")
        r_ones = R("ones")
        fw.op(fw.dve, lambda e: e.memset(ones_f[:], 1.0), writes=[r_ones])
        fw.op(fw.dve, lambda e: e.tensor_copy(out=ones_bf[:], in_=ones_f[:]), reads=[r_ones], writes=[r_ones])

        with contextlib.ExitStack() as st:
            S0 = st.enter_context(nc.psum_tensor("S0", [128, 2, 512], F32))
            S1 = st.enter_context(nc.psum_tensor("S1", [128, 2, 512], F32))
            PO = st.enter_context(nc.psum_tensor("PO", [128, 2, 512], F32))
            PL = st.enter_context(nc.psum_tensor("PL", [128, 2, 512], F32))
            r_S0, r_S1, r_PO, r_PL = R("S0"), R("S1"), R("PO"), R("PL")
            r_S0a, r_S0b, r_S1a, r_S1b = R("S0a"), R("S0b"), R("S1a"), R("S1b")
            r_POa, r_POb, r_PLa, r_PLb = R("POa"), R("POb"), R("PLa"), R("PLb")
            r_psr = [R("psr0"), R("psr1")]

            Win = sb(st, "Win", [128, 8, 2048], BF16)
            Wout = sb(st, "Wout", [128, 8, 1024], BF16)
            Wp = sb(st, "Wp", [128, 4, 128], BF16)
            Wr = sb(st, "Wr", [128, 8, 36], F32)
            gmix = sb(st, "gmix", [128, 8], F32)
            wos = sb(st, "wos", [128, 8], F32)
            gffn = sb(st, "gffn", [128, D], F32)
            biasb = sb(st, "biasb", [128, 36], F32)
            lamt = sb(st, "lamt", [128, 4, 64], F32)
            lamp = sb(st, "lamp", [128, 2, 64], F32)
            lams = sb(st, "lams", [128, 2], F32)
            neglam = sb(st, "neglam", [128, 1], F32)
            ident_bf = sb(st, "ident_bf", [128, 128], BF16)
            ident_f = sb(st, "ident_f", [128, 128], F32)
            invc = sb(st, "invc", [128, 3, 4, 128], F32)
            st_w = contextlib.ExitStack()
            wstage = sb(st_w, "wstage", [128, 2048], F32)
            r_Win, r_Wout, r_Wp, r_Wr, r_wstage = R("Win"), R("Wout"), R("Wp"), R("Wr"), R("wstage")
            r_c = R("consts")
            r_lam = R("lam")
            for (t_sb, t_dr) in ((gmix, gmix_t), (wos, wos_t), (gffn, gffn_b), (biasb, bias_b),
                                 (ident_bf, ident_bf_d), (ident_f, ident_f_d), (invc, invc_d)):
                fw.dma(fw.sp, t_sb[:], t_dr, r_c, writes=[r_c])
            fw.dma(fw.sp, lamt[:], lam_b, r_lam, writes=[r_lam])
            fw.dma(fw.sp, Wr[:], w_r.rearrange("(c p) j -> p c j", p=128), r_Wr, writes=[r_Wr])
            fw.dma(fw.pool, Wp[:], w_pool.rearrange("g c e -> c g e"), r_Wp, writes=[r_Wp])
            for c in range(8):
                fw.dma(fw.sp, wstage[:], w_in[c * 128:(c + 1) * 128, :], r_wstage, writes=[r_wstage])
                fw.op(fw.act, lambda e, c=c: e.activation(out=Win[:, c, :], in_=wstage[:], func=AF.Copy,
                                                          scale=gmix[:, c:c + 1]),
                      reads=[r_wstage, r_c], writes=[r_Win])
            for c in range(8):
                fw.dma(fw.sp, wstage[:, 0:1024], w_out[c * 128:(c + 1) * 128, :], r_wstage, writes=[r_wstage])
                fw.op(fw.act, lambda e, c=c: e.activation(out=Wout[:, c, :], in_=wstage[:, 0:1024], func=AF.Copy,
                                                          scale=wos[:, c:c + 1]),
                      reads=[r_wstage, r_c], writes=[r_Wout])
            lam4 = lamt[:].rearrange("p (a b) d -> p a b d", b=2)
            fw.op(fw.dve, lambda e: e.tensor_tensor(out=lamp[:], in0=lam4[:, :, 0, :], in1=lam4[:, :, 1, :], op=ALU.mult),
                  reads=[r_lam], writes=[r_lam])
            fw.op(fw.dve, lambda e: e.tensor_reduce(out=lams[:], in_=lamp[:], axis=AX.X, op=ALU.add),
                  reads=[r_lam], writes=[r_lam])
            fw.op(fw.act, lambda e: e.activation(out=lams[:], in_=lams[:], func=AF.Exp), reads=[r_lam], writes=[r_lam])
            fw.op(fw.dve, lambda e: e.tensor_tensor(out=neglam[:], in0=lams[:, 1:2], in1=lams[:, 0:1], op=ALU.subtract),
                  reads=[r_lam], writes=[r_lam])
            fw.op(fw.dve, lambda e: e.tensor_scalar(out=neglam[:], in0=neglam[:], scalar1=-LAM_INIT, scalar2=None,
                                                    op0=ALU.add), reads=[r_lam], writes=[r_lam])

            fw.barrier()
            st_w.close()
            max_n = max(seq_lens)
            kT = sb(st, "kT", [128, 4, max_n], BF16)
            V = sb(st, "V", [128, max_n // 128, 512], BF16)
            r_kT, r_V = R("kT"), R("V")


            def rstd_chain(src, dst, res_list, inv_n):
                fw.op(fw.dve, lambda e: e.tensor_scalar(out=dst, in0=src, scalar1=inv_n, scalar2=EPS,
                                                        op0=ALU.mult, op1=ALU.add), reads=res_list, writes=res_list)
                fw.op(fw.act, lambda e: e.activation(out=dst, in_=dst, func=AF.Ln), reads=res_list, writes=res_list)
                fw.op(fw.act, lambda e: e.activation(out=dst, in_=dst, func=AF.Exp, scale=-0.5),
                      reads=res_list, writes=res_list)

            psT = PO[:, 0, :].bitcast(BF16).rearrange("p (c t) -> p c t", c=8)
            psT2 = PO[:, 1, :].bitcast(BF16).rearrange("p (c t) -> p c t", c=8)
            ps_qk = S0[:].rearrange("p a (b d) -> p (a b) d", d=32)
            ps_qk4 = S0[:].rearrange("p a (b h d) -> p (a b) h d", h=2, d=32)
            ps_v = S1[:, 0, :]
            ps_p = S1[:, 1, :].rearrange("p (g t) -> p g t", g=4)
            ps_po = PL[:, 0, :].rearrange("p (g t) -> p g t", g=4)

            tok0 = 0
            g0 = 0
            for si, N in enumerate(seq_lens):
                nt = N // 128
                ng = N // 512

                sfx[0] = f"_s{si}"
                st1 = contextlib.ExitStack()
                cs_tab = sb(st1, "cs_tab", [128, max_nt, 2, 32], F32)
                r_cs = R("cs_tab")
                fw.dma(fw.sp, cs_tab[:], cs_tab_d, r_cs, writes=[r_cs])
                xt = [sb(st1, f"xt{i}", [128, D], F32) for i in range(2)]
                r_xt = [R(f"xt{i}") for i in range(2)]
                junk = sb(st1, "junk", [128, D], BF16)
                r_junk = R("junk")
                ss = [sb(st1, f"ss{i}", [128, 1], F32) for i in range(2)]
                r_ss = [R(f"ss{i}") for i in range(2)]
                xs = [sb(st1, f"xs{i}", [128, D], BF16) for i in range(2)]
                r_xs = [R(f"xs{i}") for i in range(2)]
                xnT = [sb(st1, f"xnT{i}", [128, 8, 128], BF16) for i in range(2)]
                r_xnT = [R(f"xnT{i}") for i in range(2)]
                rp = [sb(st1, f"rp{i}", [128, 16, 32], F32) for i in range(4)]
                r_rp = R("rp")
                qkr = sb(st1, "qkr", [128, 16, 2, 32], BF16)
                r_qkr = R("qkr")
                qst = [sb(st1, f"qst{i}", [128, 4, 128], BF16) for i in range(2)]
                r_qst = [R(f"qst{i}") for i in range(2)]
                pst = [sb(st1, f"pst{i}", [128, 4, 128], BF16) for i in range(2)]
                r_pst = [R(f"pst{i}") for i in range(2)]
                zpt = [sb(st1, f"zpt{i}", [128, 4, 128], F32) for i in range(3)]
                r_zpt = [R(f"zpt{i}") for i in range(3)]
                ZW = sb(st1, "ZW", [128, 4, 144], F32)
                za = sb(st1, "za", [128, 4, 144], F32)
                zb = sb(st1, "zb", [128, 4, 144], F32)
                zc = sb(st1, "zc", [128, 4, 144], F32)
                zd = sb(st1, "zd", [128, 4, 144], F32)
                pw = sb(st1, "pw", [128, 4, 128], F32)
                pooled = sb(st1, "pooled", [128, 4, 128], BF16)
                r_pm = R("poolmix")
                r_pooled = R("pooled")

                def pool_stage(i):
                    cur = zpt[i % 3]
                    deps_r = [r_zpt[i % 3]]
                    if i > 0:
                        prev = zpt[(i - 1) % 3]
                        deps_r.append(r_zpt[(i - 1) % 3])
                        fw.op(fw.pool, lambda e: e.tensor_copy(out=ZW[:, :, 0:8], in_=prev[:, :, 120:128]),
                              reads=deps_r, writes=[r_pm])
                    else:
                        fw.op(fw.pool, lambda e: e.memset(ZW[:, :, 0:8], 0.0), writes=[r_pm])
                    fw.op(fw.pool, lambda e: e.tensor_copy(out=ZW[:, :, 8:136], in_=cur[:]), reads=deps_r, writes=[r_pm])
                    if i < nt - 1:
                        nxt = zpt[(i + 1) % 3]
                        fw.op(fw.pool, lambda e: e.tensor_copy(out=ZW[:, :, 136:144], in_=nxt[:, :, 0:8]),
                              reads=[r_zpt[(i + 1) % 3]], writes=[r_pm])
                    else:
                        fw.op(fw.pool, lambda e: e.memset(ZW[:, :, 136:144], 0.0), writes=[r_pm])
                    P = fw.pool
                    fw.op(P, lambda e: e.tensor_tensor(out=za[:, :, 0:143], in0=ZW[:, :, 0:143], in1=ZW[:, :, 1:144], op=ALU.add),
                          reads=[r_pm], writes=[r_pm])
                    fw.op(P, lambda e: e.tensor_tensor(out=zb[:, :, 0:141], in0=za[:, :, 0:141], in1=za[:, :, 2:143], op=ALU.add),
                          reads=[r_pm], writes=[r_pm])
                    fw.op(P, lambda e: e.tensor_tensor(out=zc[:, :, 0:137], in0=zb[:, :, 0:137], in1=zb[:, :, 4:141], op=ALU.add),
                          reads=[r_pm], writes=[r_pm])
                    fw.op(P, lambda e: e.tensor_tensor(out=zd[:, :, 0:129], in0=zc[:, :, 0:129], in1=zc[:, :, 8:137], op=ALU.add),
                          reads=[r_pm], writes=[r_pm])
                    kind = 0 if i == 0 else (2 if i == nt - 1 else 1)
                    srcs = [za[:, 0, 7:135], zb[:, 1, 6:134], zc[:, 2, 4:132], zd[:, 3, 0:128]]
                    for g in range(4):
                        fw.op(P, lambda e, g=g: e.tensor_tensor(out=pw[:, g, :], in0=srcs[g], in1=invc[:, kind, g, :], op=ALU.mult),
                              reads=[r_pm, r_c], writes=[r_pm])
                    fw.op(P, lambda e: e.tensor_tensor(out=pooled[:], in0=pw[:], in1=cur[:], op=ALU.subtract),
                          reads=[r_pm, r_zpt[i % 3]], writes=[r_pooled])

                    def mm_pool(e):
                        ins = None
                        for g in range(4):
                            ins = e.matmul(ps_po[:, g, :], Wp[:, g, :], pooled[:, g, :], start=True, stop=True)
                        return ins
                    fw.op(fw.pe, mm_pool, reads=[r_pooled, r_Wp], writes=[r_PLa])
                    sl = i % 2
                    fw.op(fw.act, lambda e: e.activation(out=pst[sl][:], in_=ps_po, func=AF.Copy),
                          reads=[r_PLa], writes=[r_pst[sl]])
                    g_idx = g0 + i // 4
                    fw.dma(fw.sp, pd[g_idx, :, i % 4, :, :], pst[sl][:], r_pst[sl], reads=[r_pst[sl]])

                def stageA_load(i):
                    sl = i % 2
                    fw.dma(fw.sp, xt[sl][:], x[tok0 + i * 128: tok0 + (i + 1) * 128, :], r_xt[sl], writes=[r_xt[sl]])

                def stageA(i):
                    sl = i % 2
                    fw.op(fw.act, lambda e: e.activation(out=junk[:], in_=xt[sl][:], func=AF.Square, accum_out=ss[sl][:]),
                          reads=[r_xt[sl]], writes=[r_junk, r_ss[sl]])
                    rstd_chain(ss[sl][:], ss[sl][:], [r_ss[sl]], 1.0 / D)
                    fw.op(fw.act, lambda e: e.activation(out=xs[sl][:], in_=xt[sl][:], func=AF.Copy, scale=ss[sl][:]),
                          reads=[r_xt[sl], r_ss[sl]], writes=[r_xs[sl]])

                    def tr1(e):
                        ins = None
                        for c in range(8):
                            ins = e.transpose(psT[:, c, :], xs[sl][:, c * 128:(c + 1) * 128], ident_bf[:])
                        return ins
                    fw.op(fw.pe, tr1, reads=[r_xs[sl], r_c], writes=[r_POa])
                    fw.op(fw.dve, lambda e: e.tensor_copy(out=xnT[sl][:], in_=psT), reads=[r_POa], writes=[r_xnT[sl]])

                def stageB(i):
                    sl = i % 2

                    def mm_qkv(e):
                        ins = None
                        for j, dst in enumerate((S0[:, 0, :], S0[:, 1, :], ps_v)):
                            for c in range(8):
                                ins = e.matmul(dst, xnT[sl][:, c, :], Win[:, c, 512 + j * 512: 1024 + j * 512],
                                               start=(c == 0), stop=(c == 7))
                        for g in range(4):
                            for c in range(8):
                                ins = e.matmul(ps_p[:, g, :], Win[:, c, g * 128:(g + 1) * 128], xnT[sl][:, c, :],
                                               start=(c == 0), stop=(c == 7))
                        return ins
                    fw.op(fw.pe, mm_qkv, reads=[r_xnT[sl], r_Win], writes=[r_S0a, r_S0b, r_S1a, r_S1b])
                    fw.op(fw.act, lambda e: e.activation(out=V[:, i, :], in_=ps_v, func=AF.Copy), reads=[r_S1a], writes=[r_V])
                    fw.op(fw.act, lambda e: e.activation(out=zpt[i % 3][:], in_=ps_p, func=AF.Copy),
                          reads=[r_S1b], writes=[r_zpt[i % 3]])
                    cosb = cs_tab[:, i, 0, :].unsqueeze(1).broadcast_to([128, 16, 32])
                    sinb = cs_tab[:, i, 1, :].unsqueeze(1).broadcast_to([128, 16, 32])
                    x1 = ps_qk4[:, :, 0, :]
                    x2 = ps_qk4[:, :, 1, :]
                    fw.op(fw.dve, lambda e: e.tensor_tensor(out=rp[0][:], in0=x1, in1=cosb, op=ALU.mult), reads=[r_S0a, r_S0b, r_cs], writes=[r_rp])
                    fw.op(fw.dve, lambda e: e.tensor_tensor(out=rp[1][:], in0=x2, in1=sinb, op=ALU.mult), reads=[r_S0a, r_S0b, r_cs], writes=[r_rp])
                    fw.op(fw.dve, lambda e: e.tensor_tensor(out=rp[2][:], in0=x2, in1=cosb, op=ALU.mult), reads=[r_S0a, r_S0b, r_cs], writes=[r_rp])
                    fw.op(fw.dve, lambda e: e.tensor_tensor(out=rp[3][:], in0=x1, in1=sinb, op=ALU.mult), reads=[r_S0a, r_S0b, r_cs], writes=[r_rp])
                    fw.op(fw.dve, lambda e: e.tensor_tensor(out=qkr[:, :, 0, :], in0=rp[0][:], in1=rp[1][:], op=ALU.subtract), reads=[r_rp], writes=[r_qkr])
                    fw.op(fw.dve, lambda e: e.tensor_tensor(out=qkr[:, :, 1, :], in0=rp[2][:], in1=rp[3][:], op=ALU.add), reads=[r_rp], writes=[r_qkr])

                def stageB2(i):
                    sl = i % 2
                    qkr_f = qkr[:].rearrange("p a h d -> p (a h d)")

                    def tr2(e):
                        ins = None
                        for c in range(8):
                            ins = e.transpose(psT2[:, c, :], qkr_f[:, c * 128:(c + 1) * 128], ident_bf[:])
                        return ins
                    fw.op(fw.pe, tr2, reads=[r_qkr, r_c], writes=[r_POb])
                    fw.op(fw.dve, lambda e: e.tensor_copy(out=kT[:, :, i * 128:(i + 1) * 128], in_=psT2[:, 4:8, :]),
                          reads=[r_POb], writes=[r_kT])
                    fw.op(fw.dve, lambda e: e.tensor_copy(out=qst[sl][:], in_=psT2[:, 0:4, :]), reads=[r_POb], writes=[r_qst[sl]])
                    fw.dma(fw.sp, qd[g0 + i // 4, :, :, (i % 4) * 128:(i % 4 + 1) * 128], qst[sl][:], r_qst[sl], reads=[r_qst[sl]])

                stageA_load(0)
                stageA_load(1)
                stageA(0)
                stageA(1)
                for i in range(nt):
                    if i + 2 < nt:
                        stageA_load(i + 2)
                    stageB(i)
                    if i + 2 < nt:
                        stageA(i + 2)
                    stageB2(i)
                    if i >= 1:
                        pool_stage(i - 1)
                pool_stage(nt - 1)
                r_qd = R("qd_seq")
                fw.barrier()

                st1.close()
                st2 = contextlib.ExitStack()
                qpad = sb(st2, "qpad", [128, 4, 2, 512], BF16)
                r_qpad = R("qpad")
                poolT = sb(st2, "poolT", [128, 4, 4, 128], BF16)
                r_poolT = R("poolT")
                pT = [sb(st2, f"pT{i}", [128, 2, 512], BF16) for i in range(4)]
                r_pT = [R(f"pT{i}") for i in range(4)]
                pTs = [sb(st2, f"pTs{i}", [128, 2, 512], BF16) for i in range(2)]
                r_pTs = [R(f"pTs{i}") for i in range(2)]
                rl = sb(st2, "rl", [128, 2, 512], F32)
                on = rl
                od = sb(st2, "od", [128, 512], F32)
                osq = sb(st2, "osq", [128, 512], F32)
                rn = sb(st2, "rn", [128, 512], F32)
                r_ep = R("epi")
                attnT = sb(st2, "attnT", [128, 4, 512], BF16)
                r_attnT = R("attnT")
                xr = [sb(st2, f"xr{i}", [128, D], F32) for i in range(2)]
                r_xr = [R(f"xr{i}") for i in range(2)]
                ht = [sb(st2, f"ht{i}", [128, D], F32) for i in range(2)]
                r_ht = [R(f"ht{i}") for i in range(2)]
                hnf = sb(st2, "hnf", [128, D], F32)
                r_hnf = R("hnf")
                hnTf = sb(st2, "hnTf", [128, 8, 128], F32)
                r_hnTf = R("hnTf")
                hnTb = [sb(st2, f"hnTb{i}", [128, D], BF16) for i in range(2)]
                r_hnTb = [R(f"hnTb{i}") for i in range(2)]
                ss2 = sb(st2, "ss2", [128, 1], F32)
                r_ss2 = R("ss2")

                fw.op(fw.pool, lambda e: e.memset(qpad[:], 0.0), writes=[r_qpad])
                nk = nt
                for j in range(ng if _DBG not in ("p1",) else 0):
                    gi = g0 + j
                    for m in range(2):
                        dst = qpad[m * 64:(m + 1) * 64, :, m, :]
                        fw.dma(fw.sp, dst, qd[gi, m * 64:(m + 1) * 64, :, :], r_qpad, writes=[r_qpad])
                    fw.dma(fw.sp, poolT[:], pd[gi], r_poolT, writes=[r_poolT])
                    units = [(hh, kk) for hh in range(4) for kk in range(nk)]

                    def emit_qk(ui):
                        hh, kk = units[ui]
                        bb = ui % 2
                        Sb = S0 if bb == 0 else S1
                        r_Sb = [r_S0a, r_S0b] if bb == 0 else [r_S1a, r_S1b]

                        def mm_s(e):
                            e.matmul(Sb[:, 0, :], kT[:, hh, kk * 128:(kk + 1) * 128], qpad[:, hh, 0, :], start=True, stop=True)
                            return e.matmul(Sb[:, 1, :], kT[:, hh, kk * 128:(kk + 1) * 128], qpad[:, hh, 1, :], start=True, stop=True)
                        fw.op(fw.pe, mm_s, reads=[r_kT, r_qpad], writes=r_Sb)

                    def emit_exp_pv(ui):
                        hh, kk = units[ui]
                        bb = ui % 2
                        p4 = ui % 4
                        Sb = S0 if bb == 0 else S1
                        r_Sb = [r_S0a, r_S0b] if bb == 0 else [r_S1a, r_S1b]
                        fw.op(fw.act, lambda e: e.activation(out=pT[p4][:], in_=Sb[:], func=AF.Exp, scale=0.125),
                              reads=r_Sb, writes=[r_pT[p4]])
                        if ui + 2 < len(units):
                            emit_qk(ui + 2)

                        def mm_pv(e):
                            ins = None
                            for m in range(2):
                                ins = e.matmul(PO[:, m, :], V[:, kk, hh * 128:(hh + 1) * 128], pT[p4][:, m, :],
                                               start=(kk == 0), stop=(kk == nk - 1))
                            return ins
                        fw.op(fw.pe, mm_pv, reads=[r_V, r_pT[p4]], writes=[r_POa, r_POb])
                        def mm_l_for(ps2_, kk_):
                            def mm_l(e):
                                ins = None
                                for m in range(2):
                                    ins = e.matmul(PL[:, m, :], ones_bf[:], pTs[ps2_][:, m, :],
                                                   start=(kk_ == 1), stop=(kk_ == nk - 1))
                                return ins
                            fw.op(fw.pe, mm_l, reads=[r_pTs[ps2_], r_ones], writes=[r_PLa, r_PLb])
                        if kk % 2 == 0 and pend_l:
                            mm_l_for(*pend_l.pop())
                        if kk % 2 == 1:
                            ps2 = (ui // 2) % 2
                            pprev = (ui - 1) % 4
                            fw.op(fw.dve, lambda e: e.tensor_tensor(out=pTs[ps2][:], in0=pT[pprev][:], in1=pT[p4][:], op=ALU.add),
                                  reads=[r_pT[pprev], r_pT[p4]], writes=[r_pTs[ps2]])
                            if kk == nk - 1:
                                mm_l_for(ps2, kk)
                            else:
                                pend_l.append((ps2, kk))

                    pend_l = []
                    emit_qk(0)
                    if len(units) > 1:
                        emit_qk(1)
                    for ui in range(len(units)):
                        h, kc = units[ui]
                        emit_exp_pv(ui)
                        if kc != nk - 1:
                            continue
                        fw.op(fw.dve, lambda e: e.reciprocal(out=rl[:], in_=PL[:]), reads=[r_PLa, r_PLb], writes=[r_ep])
                        fw.op(fw.dve, lambda e: e.tensor_tensor(out=on[:], in0=PO[:], in1=rl[:], op=ALU.mult),
                              reads=[r_POa, r_POb, r_ep], writes=[r_ep])
                        fw.op(fw.dve, lambda e: e.scalar_tensor_tensor(out=od[:], in0=on[:, 1, :], scalar=neglam[:], in1=on[:, 0, :],
                                                                       op0=ALU.mult, op1=ALU.add),
                              reads=[r_ep, r_lam], writes=[r_ep])
                        fw.op(fw.act, lambda e: e.activation(out=osq[:], in_=od[:], func=AF.Square), reads=[r_ep], writes=[r_ep])
                        fw.op(fw.pe, lambda e: e.matmul(PL[:, 0, :], ones_f[:], osq[:], start=True, stop=True),
                              reads=[r_ep, r_ones], writes=[r_PLa])
                        fw.op(fw.dve, lambda e: e.tensor_scalar(out=rn[:], in0=PL[:, 0, :], scalar1=1.0 / 128, scalar2=EPS,
                                                                op0=ALU.mult, op1=ALU.add), reads=[r_PLa], writes=[r_ep])
                        fw.op(fw.act, lambda e: e.activation(out=rn[:], in_=rn[:], func=AF.Ln), reads=[r_ep], writes=[r_ep])
                        fw.op(fw.act, lambda e: e.activation(out=rn[:], in_=rn[:], func=AF.Exp, scale=-0.5), reads=[r_ep], writes=[r_ep])
                        fw.op(fw.dve, lambda e, h=h: e.scalar_tensor_tensor(out=attnT[:, h, :], in0=od[:], scalar=1.0 - LAM_INIT, in1=rn[:],
                                                                            op0=ALU.mult, op1=ALU.mult),
                              reads=[r_ep], writes=[r_attnT])
                    def stage2A(ti):
                        tg = (tok0 // 128) + j * 4 + ti
                        sl = ti % 2
                        Hb = S0 if sl == 0 else S1
                        r_Hb = [r_S0a, r_S0b] if sl == 0 else [r_S1a, r_S1b]
                        fw.dma(fw.sp, xr[sl][:], x[tg * 128:(tg + 1) * 128, :], r_xr[sl], writes=[r_xr[sl]])

                        def mm_o(e, ti=ti, Hb=Hb):
                            ins = None
                            for cc in range(2):
                                for c in range(8):
                                    lhsT = poolT[:, ti, c, :] if c < 4 else attnT[:, c - 4, ti * 128:(ti + 1) * 128]
                                    ins = e.matmul(Hb[:, cc, :], lhsT, Wout[:, c, cc * 512:(cc + 1) * 512],
                                                   start=(c == 0), stop=(c == 7))
                            return ins
                        fw.op(fw.pe, mm_o, reads=[r_poolT, r_attnT, r_Wout], writes=r_Hb)
                        Hf = Hb[:].rearrange("p a b -> p (a b)")
                        fw.op(fw.dve, lambda e, Hf=Hf, sl=sl: e.tensor_tensor(out=ht[sl][:], in0=Hf, in1=xr[sl][:], op=ALU.add),
                              reads=r_Hb + [r_xr[sl]], writes=[r_ht[sl]])
                        fw.dma(fw.act, h_d[tg], ht[sl][:], r_ht[sl], reads=[r_ht[sl]])
                        fw.op(fw.act, lambda e, sl=sl: e.activation(out=hnTb[sl][:], in_=ht[sl][:], func=AF.Square, accum_out=ss2[:]),
                              reads=[r_ht[sl]], writes=[r_hnTb[sl], r_ss2])
                        rstd_chain(ss2[:], ss2[:], [r_ss2], 1.0 / D)
                        fw.op(fw.dve, lambda e, sl=sl: e.scalar_tensor_tensor(out=hnf[:], in0=ht[sl][:], scalar=ss2[:], in1=gffn[:],
                                                                              op0=ALU.mult, op1=ALU.mult),
                              reads=[r_ht[sl], r_ss2, r_c], writes=[r_hnf])
                        psTf = PO[:].rearrange("p a (c t) -> p (a c) t", t=128)

                        def tr3(e):
                            ins = None
                            for c in range(8):
                                ins = e.matmul(psTf[:, c, :], hnf[:, c * 128:(c + 1) * 128], ident_f[:], start=True, stop=True)
                            return ins
                        fw.op(fw.pe, tr3, reads=[r_hnf, r_c], writes=[r_POa, r_POb])
                        fw.op(fw.dve, lambda e: e.tensor_copy(out=hnTf[:], in_=psTf), reads=[r_POa, r_POb], writes=[r_hnTf])
                        fw.op(fw.pool, lambda e, sl=sl: e.tensor_copy(out=hnTb[sl][:], in_=hnf[:]),
                              reads=[r_hnf], writes=[r_hnTb[sl]])
                        fw.dma(fw.pool, hn_d[tg], hnTb[sl][:], r_hnTb[sl], reads=[r_hnTb[sl]])
                        ps_r = PL[:, 1, (ti % 2) * 64:(ti % 2) * 64 + 36]

                        def mm_r(e):
                            ins = None
                            for c in range(8):
                                ins = e.matmul(ps_r, hnTf[:, c, :], Wr[:, c, :], start=(c == 0), stop=(c == 7))
                            return ins
                        fw.op(fw.pe, mm_r, reads=[r_hnTf, r_Wr], writes=[r_psr[ti % 2], r_PLb])

                    def stage2B(ti):
                        tg = (tok0 // 128) + j * 4 + ti
                        ps_r = PL[:, 1, (ti % 2) * 64:(ti % 2) * 64 + 36]
                        fw.op(fw.dve, lambda e: e.tensor_tensor(out=lg_all[:, tg, :], in0=ps_r, in1=biasb[:], op=ALU.add),
                              reads=[r_psr[ti % 2], r_PLb, r_c], writes=[r_gates[tg]])

                    stage2A(0)
                    for ti in range(4):
                        if ti + 1 < 4:
                            stage2A(ti + 1)
                        stage2B(ti)
                tok0 += N
                g0 += ng
                fw.barrier()
                st2.close()
        fw.barrier()
        I32 = mybir.dt.int32
        with contextlib.ExitStack() as st:
            OH1 = sb(st, "OH1", [128, NT, 32], BF16)
            OH2 = sb(st, "OH2", [128, NT, 32], BF16)
            wts = sb(st, "wts", [128, NT, 2], F32)
            d1i = sb(st, "d1i", [128, NT], I32)
            d2i = sb(st, "d2i", [128, NT], I32)
            idxw = sb(st, "idxw", [128, NB], I32)
            r_rt3 = R("route3")
            TA = st.enter_context(nc.psum_tensor("TA", [128, 512], F32))
            TB = st.enter_context(nc.psum_tensor("TB", [128, 512], F32))
            G0 = st.enter_context(nc.psum_tensor("G0", [128, 2, 512], F32))
            G1 = st.enter_context(nc.psum_tensor("G1", [128, 2, 512], F32))
            Y0 = st.enter_context(nc.psum_tensor("Y0", [128, 2, 512], F32))
            r_TA, r_TB, r_G0, r_G1, r_Y0 = R("TA"), R("TB"), R("G0p"), R("G1p"), R("Y0p")
            with contextlib.ExitStack() as s0:
                r_r0 = R("router0")
                gq = sb(s0, "gq", [128, 8, NT], F32)
                mg = sb(s0, "mg", [128, NT, 4], F32)
                eg = sb(s0, "eg", [128, NT, 4], F32)
                t48 = sb(s0, "t48", [128, NT, 4, 8], F32)
                les = sb(s0, "les", [128, NT, 8], F32)
                les2 = sb(s0, "les2", [128, NT, 8], F32)
                k1 = sb(s0, "k1", [128, NT, 8], F32)
                k2 = sb(s0, "k2", [128, NT, 8], F32)
                gmax, gsum, m1, m2, ex, w1, w2 = (gq[:, i, :] for i in range(7))
                lg = lg_all[:, :, 0:4]
                le4 = lg_all[:, :, 4:36].rearrange("p t (g j) -> p t g j", g=4)
                rr0 = [r_r0]

                def d0(fn, extra=()):
                    fw.op(fw.dve, fn, reads=rr0 + list(extra), writes=rr0)

                def bc3(ap2, k):
                    return ap2.unsqueeze(2).broadcast_to([128, NT, k])
                d0(lambda e: e.tensor_reduce(out=gmax, in_=lg, axis=AX.X, op=ALU.max), r_gates)
                d0(lambda e: e.tensor_tensor(out=mg[:], in0=lg, in1=bc3(gmax, 4), op=ALU.is_equal), r_gates)
                d0(lambda e: e.tensor_tensor(out=eg[:], in0=lg, in1=bc3(gmax, 4), op=ALU.subtract), r_gates)
                fw.op(fw.act, lambda e: e.activation(out=eg[:], in_=eg[:], func=AF.Exp), reads=rr0, writes=rr0)
                d0(lambda e: e.tensor_reduce(out=gsum, in_=eg[:], axis=AX.X, op=ALU.add))
                d0(lambda e: e.reciprocal(out=gsum, in_=gsum))
                d0(lambda e: e.tensor_tensor(out=t48[:], in0=le4, in1=mg[:].unsqueeze(3).broadcast_to([128, NT, 4, 8]), op=ALU.mult), r_gates)
                d0(lambda e: e.tensor_reduce(out=les[:], in_=t48[:].rearrange("p t g j -> p t j g"), axis=AX.X, op=ALU.add))
                d0(lambda e: e.tensor_reduce(out=m1, in_=les[:], axis=AX.X, op=ALU.max))
                d0(lambda e: e.tensor_tensor(out=k1[:], in0=les[:], in1=bc3(m1, 8), op=ALU.is_equal))
                d0(lambda e: e.scalar_tensor_tensor(out=les2[:], in0=k1[:], scalar=-1e30, in1=les[:], op0=ALU.mult, op1=ALU.add))
                d0(lambda e: e.tensor_reduce(out=m2, in_=les2[:], axis=AX.X, op=ALU.max))
                d0(lambda e: e.tensor_tensor(out=k2[:], in0=les2[:], in1=bc3(m2, 8), op=ALU.is_equal))
                d0(lambda e: e.tensor_tensor(out=ex, in0=m2, in1=m1, op=ALU.subtract))
                fw.op(fw.act, lambda e: e.activation(out=ex, in_=ex, func=AF.Exp), reads=rr0, writes=rr0)
                d0(lambda e: e.tensor_scalar(out=w1, in0=ex, scalar1=1.0, scalar2=None, op0=ALU.add))
                d0(lambda e: e.reciprocal(out=w1, in_=w1))
                d0(lambda e: e.tensor_tensor(out=w1, in0=w1, in1=gsum, op=ALU.mult))
                d0(lambda e: e.tensor_tensor(out=w2, in0=w1, in1=ex, op=ALU.mult))
                mgb4 = mg[:].unsqueeze(3).broadcast_to([128, NT, 4, 8])
                o1v = OH1[:].rearrange("p t (g j) -> p t g j", g=4)
                o2v = OH2[:].rearrange("p t (g j) -> p t g j", g=4)
                fw.op(fw.dve, lambda e: e.tensor_tensor(out=o1v, in0=k1[:].unsqueeze(2).broadcast_to([128, NT, 4, 8]), in1=mgb4, op=ALU.mult),
                      reads=rr0, writes=r_gates)
                fw.op(fw.dve, lambda e: e.tensor_tensor(out=o2v, in0=k2[:].unsqueeze(2).broadcast_to([128, NT, 4, 8]), in1=mgb4, op=ALU.mult),
                      reads=rr0, writes=r_gates)
                fw.op(fw.dve, lambda e: e.tensor_copy(out=wts[:, :, 0], in_=w1), reads=rr0, writes=r_gates)
                fw.op(fw.dve, lambda e: e.tensor_copy(out=wts[:, :, 1], in_=w2), reads=rr0, writes=r_gates)
                fw.barrier()
            with contextlib.ExitStack() as sa:
                tri = sb(sa, "tri", [128, 128], BF16)
                bstart = sb(sa, "bstart", [128, NB], F32)
                pidx = sb(sa, "pidx", [128, 1], F32)
                r_ca = R("constA")
                fw.dma(fw.sp, tri[:], tri_d, r_ca, writes=[r_ca])
                fw.dma(fw.sp, bstart[:], bstart_d, r_ca, writes=[r_ca])
                fw.dma(fw.sp, pidx[:], pidx_d, r_ca, writes=[r_ca])
                NC3 = NT * 32
                Mb = sb(sa, "Mb", [128, NC3], BF16)
                rank = sb(sa, "rank", [128, NT, 32], F32)
                cnt = sb(sa, "cnt", [128, NT, 32], F32)
                sA = sb(sa, "sA", [128, NT, 32], F32)
                sB = sb(sa, "sB", [128, NT, 32], F32)
                cmp = sb(sa, "cmp", [128, NB, 32], F32)
                sm = sb(sa, "sm", [128, 8, 32], F32)
                ebf = sb(sa, "ebf", [128, NB], F32)
                eb2 = sb(sa, "eb2", [128, NB], F32)
                dfl = sb(sa, "dfl", [128, 2, NT], F32)
                Dv = fw.dve
                rr = [r_rt3]

                def dv(fn, extra=()):
                    fw.op(Dv, fn, reads=rr + list(extra), writes=rr)
                OH1f = OH1[:].rearrange("p t e -> p (t e)")
                OH2f = OH2[:].rearrange("p t e -> p (t e)")
                dv(lambda e: e.tensor_tensor(out=Mb[:], in0=OH1f, in1=OH2f, op=ALU.add), r_gates)
                rankf = rank[:].rearrange("p t e -> p (t e)")
                cntf = cnt[:].rearrange("p t e -> p (t e)")
                nch = (NC3 + 511) // 512
                for ch in range(nch):
                    c0 = ch * 512
                    c1 = min(NC3, c0 + 512)
                    w_ = c1 - c0
                    fw.op(fw.pe, lambda e: e.matmul(TA[:, 0:w_], tri[:], Mb[:, c0:c1], start=True, stop=True),
                          reads=[r_rt3, r_ca], writes=[r_TA])
                    fw.op(fw.pe, lambda e: e.matmul(TB[:, 0:w_], ones_bf[:], Mb[:, c0:c1], start=True, stop=True),
                          reads=[r_rt3, r_ones], writes=[r_TB])
                    fw.op(Dv, lambda e: e.tensor_copy(out=rankf[:, c0:c1], in_=TA[:, 0:w_]), reads=[r_TA], writes=rr)
                    fw.op(Dv, lambda e: e.tensor_copy(out=cntf[:, c0:c1], in_=TB[:, 0:w_]), reads=[r_TB], writes=rr)
                dv(lambda e: e.tensor_copy(out=sA[:], in_=cnt[:]))
                src, dst = sA, sB
                sft = 1
                while sft < NT:
                    dv(lambda e: e.tensor_tensor(out=dst[:, sft:, :], in0=src[:, sft:, :], in1=src[:, 0:NT - sft, :], op=ALU.add))
                    dv(lambda e: e.tensor_copy(out=dst[:, 0:sft, :], in_=src[:, 0:sft, :]))
                    src, dst = dst, src
                    sft *= 2
                incl = src
                other = dst
                tot = sm[:, 0, :]
                xm = sm[:, 1, :]
                padded = sm[:, 2, :]
                pA = sm[:, 3, :]
                pB = sm[:, 4, :]
                pstart = sm[:, 5, :]
                dv(lambda e: e.tensor_copy(out=tot, in_=incl[:, NT - 1, :]))
                dv(lambda e: e.tensor_scalar(out=xm, in0=tot, scalar1=1.0 / 128, scalar2=0.49609375, op0=ALU.mult, op1=ALU.add))
                dv(lambda e: e.tensor_scalar(out=xm, in0=xm, scalar1=8388608.0, scalar2=None, op0=ALU.add))
                dv(lambda e: e.tensor_scalar(out=xm, in0=xm, scalar1=-8388608.0, scalar2=None, op0=ALU.add))
                dv(lambda e: e.tensor_scalar(out=padded, in0=xm, scalar1=128.0, scalar2=None, op0=ALU.mult))
                dv(lambda e: e.tensor_copy(out=pA, in_=padded))
                ps_, pd_ = pA, pB
                sft = 1
                while sft < 32:
                    dv(lambda e: e.tensor_tensor(out=pd_[:, sft:], in0=ps_[:, sft:], in1=ps_[:, 0:32 - sft], op=ALU.add))
                    dv(lambda e: e.tensor_copy(out=pd_[:, 0:sft], in_=ps_[:, 0:sft]))
                    ps_, pd_ = pd_, ps_
                    sft *= 2
                pend = ps_
                dv(lambda e: e.tensor_tensor(out=pstart, in0=pend, in1=padded, op=ALU.subtract))
                dv(lambda e: e.tensor_tensor(out=other[:], in0=incl[:], in1=cnt[:], op=ALU.subtract))
                dv(lambda e: e.tensor_tensor(out=other[:], in0=other[:], in1=rank[:], op=ALU.add))
                dv(lambda e: e.tensor_tensor(out=other[:], in0=other[:], in1=pstart.unsqueeze(1).broadcast_to([128, NT, 32]), op=ALU.add))
                dest = other
                dv(lambda e: e.tensor_tensor(out=incl[:], in0=dest[:], in1=OH1[:], op=ALU.mult), r_gates)
                dv(lambda e: e.tensor_reduce(out=dfl[:, 0, :], in_=incl[:], axis=AX.X, op=ALU.add))
                dv(lambda e: e.tensor_tensor(out=incl[:], in0=dest[:], in1=OH2[:], op=ALU.mult), r_gates)
                dv(lambda e: e.tensor_reduce(out=dfl[:, 1, :], in_=incl[:], axis=AX.X, op=ALU.add))
                dv(lambda e: e.tensor_copy(out=d1i[:], in_=dfl[:, 0, :]))
                dv(lambda e: e.tensor_copy(out=d2i[:], in_=dfl[:, 1, :]))
                dv(lambda e: e.tensor_tensor(out=cmp[:], in0=pend.unsqueeze(1).broadcast_to([128, NB, 32]),
                                             in1=bstart[:].unsqueeze(2).broadcast_to([128, NB, 32]), op=ALU.is_le), [r_ca])
                dv(lambda e: e.tensor_reduce(out=ebf[:], in_=cmp[:], axis=AX.X, op=ALU.add))
                dv(lambda e: e.tensor_scalar(out=ebf[:], in0=ebf[:], scalar1=float(NE - 1), scalar2=None, op0=ALU.min))
                BIG = 1.0e6
                dv(lambda e: e.memset(eb2[:], 1.0))
                dv(lambda e: e.tensor_tensor(out=eb2[:, 2:], in0=ebf[:, 2:], in1=ebf[:, 0:NB - 2], op=ALU.not_equal))
                dv(lambda e: e.tensor_scalar(out=ebf[:], in0=ebf[:], scalar1=128.0, scalar2=pidx[:], op0=ALU.mult, op1=ALU.add), [r_ca])
                dv(lambda e: e.tensor_scalar(out=ebf[:], in0=ebf[:], scalar1=-BIG, scalar2=None, op0=ALU.add))
                dv(lambda e: e.tensor_tensor(out=ebf[:], in0=ebf[:], in1=eb2[:], op=ALU.mult))
                dv(lambda e: e.tensor_scalar(out=ebf[:], in0=ebf[:], scalar1=BIG, scalar2=None, op0=ALU.add))
                dv(lambda e: e.tensor_copy(out=idxw[:], in_=ebf[:]))
                fw.barrier()

            _bregs = {}

            def _breg(bound):
                if bound not in _bregs:
                    rg = nc.gpsimd.alloc_register(f"bnd{len(_bregs)}")
                    nc.gpsimd.reg_mov(rg, int(bound))
                    _bregs[bound] = rg
                return _bregs[bound]

            def indirect(out, out_off, in_, in_off, bound, sb_res, reads, writes):
                deps = []
                for r_ in reads:
                    if r_.last_w is not None:
                        deps.append(r_.last_w)
                for w_r in writes:
                    if w_r.last_w is not None:
                        deps.append(w_r.last_w)
                    deps.extend(w_r.readers)
                fw.pool.wait_tokens(deps)
                ent = fw.dma_reg.setdefault(sb_res.name, [None, 0])
                if ent[0] is None:
                    ent[0] = fw.new_sem(f"dma_{sb_res.name}")
                ins = nc.gpsimd.indirect_dma_start(
                    out=out, out_offset=(bass.IndirectOffsetOnAxis(ap=out_off, axis=0) if out_off is not None else None),
                    in_=in_, in_offset=(bass.IndirectOffsetOnAxis(ap=in_off, axis=0) if in_off is not None else None),
                    bounds_check=_breg(bound), oob_is_err=False)
                ent[1] += 16
                ins.then_inc(fw.sem_by_key[ent[0]], 16)
                tok = (ent[0], ent[1])
                for r_ in reads:
                    r_.readers.append(tok)
                for w_r in writes:
                    w_r.last_w = tok
                    w_r.readers = []

            with contextlib.ExitStack() as sbk:
                hb = [sb(sbk, f"hb{i}", [128, D], BF16) for i in range(3)]
                r_hb = [R(f"hb{i}") for i in range(3)]
                for i in range(NT):
                    s3 = i % 3
                    fw.dma(fw.sp, hb[s3][:], hn_d[i], r_hb[s3], writes=[r_hb[s3]])
                    indirect(xg[:, :], d1i[:, i:i + 1], hb[s3][:, :], None, PR - 1, r_hb[s3], [r_hb[s3], r_rt3], [])
                    indirect(xg[:, :], d2i[:, i:i + 1], hb[s3][:, :], None, PR - 1, r_hb[s3], [r_hb[s3], r_rt3], [])
                fw.barrier()
            wgv = w_gate[:, :]
            wuv = w_up[:, :]
            wdv = w_down[:, :]
            with contextlib.ExitStack() as sc:
                identb = sb(sc, "identb", [128, 128], BF16)
                r_idb = R("identb")
                fw.dma(fw.sp, identb[:], ident_bf_d, r_idb, writes=[r_idb])
                wg = [sb(sc, f"wg{i}", [128, 8 * DFF], BF16) for i in range(2)]
                wu = [sb(sc, f"wu{i}", [128, 8 * DFF], BF16) for i in range(2)]
                wd = [sb(sc, f"wd{i}", [128, 4 * D], BF16) for i in range(2)]
                r_wg = [R("wg0"), R("wg1")]
                r_wu = [R("wu0"), R("wu1")]
                r_wd = [R("wd0"), R("wd1")]
                xb = [sb(sc, f"xb{i}", [128, D], BF16) for i in range(2)]
                r_xb = [R("xb0"), R("xb1")]
                xgT = [sb(sc, f"xgT{i}", [128, 8, 128], BF16) for i in range(2)]
                r_xgT = [R("xgT0"), R("xgT1")]
                sgl = sb(sc, "sgl", [128, 512], F32)
                r_sgl = R("sgl")
                hidT = [sb(sc, f"hidT{i}", [128, 4, 128], BF16) for i in range(2)]
                r_hid = [R("hid0"), R("hid1")]
                yb = [sb(sc, f"yb{i}", [128, D], F32) for i in range(2)]
                r_yb = [R("yb0"), R("yb1")]
                Ts = [TA, TB]
                r_Ts = [r_TA, r_TB]
                Gs = [G0, G1]
                r_Gs = [r_G0, r_G1]
                for ws in range(2):
                    pass
                hidtok = [sb(sc, f"hidtok{i}", [128, DFF], BF16) for i in range(2)]
                r_hidtok = [R("hidtok0"), R("hidtok1")]
                sgl2 = [sgl, sb(sc, "sglb", [128, 512], F32)]
                r_sgl2 = [r_sgl, R("sglb")]
                xb3 = xb + [sb(sc, "xb2", [128, D], BF16)]
                r_xb3 = r_xb + [R("xb2")]
                xgT3 = xgT + [sb(sc, "xgT2", [128, 8, 128], BF16)]
                r_xgT3 = r_xgT + [R("xgT2")]
                psT3 = TA[:].bitcast(BF16).rearrange("p (c t) -> p c t", c=8)
                psH = TB[:, 0:256].bitcast(BF16).rearrange("p (c t) -> p c t", c=4)

                def gath_gu(b):
                    ws = b % 2
                    indirect(wg[ws][:, :], None, wgv, idxw[:, b:b + 1], NE * 128 - 1, r_wg[ws], [r_rt3], [r_wg[ws]])
                    indirect(wu[ws][:, :], None, wuv, idxw[:, b:b + 1], NE * 128 - 1, r_wu[ws], [r_rt3], [r_wu[ws]])

                def gath_d(b):
                    ws = b % 2
                    indirect(wd[ws][:, :], None, wdv, idxw[:, b:b + 1], NE * 128 - 1, r_wd[ws], [r_rt3], [r_wd[ws]])

                def preC(b):
                    s3 = b % 3
                    fw.dma(fw.sp, xb3[s3][:], xg[b * 128:(b + 1) * 128, :], r_xb3[s3], writes=[r_xb3[s3]])
                    xbv = xb3[s3][:].rearrange("p (q c) -> p c q", c=8)

                    def tr4(e):
                        ins = None
                        for c in range(8):
                            ins = e.transpose(psT3[:, c, :], xbv[:, c, :], identb[:])
                        return ins
                    fw.op(fw.pe, tr4, reads=[r_xb3[s3], r_idb], writes=[r_TA])
                    fw.op(fw.dve, lambda e: e.tensor_copy(out=xgT3[s3][:], in_=psT3), reads=[r_TA], writes=[r_xgT3[s3]])

                def stageG(b):
                    ws = b % 2
                    s3 = b % 3
                    Gb = Gs[ws]
                    wgs = wg[ws][:].rearrange("p (c f) -> p c f", c=8)
                    wus = wu[ws][:].rearrange("p (c f) -> p c f", c=8)

                    def mm_gu(e):
                        ins = None
                        for which, wv in ((0, wgs), (1, wus)):
                            for c in range(8):
                                ins = e.matmul(Gb[:, which, :], xgT3[s3][:, c, :], wv[:, c, :], start=(c == 0), stop=(c == 7))
                        return ins
                    fw.op(fw.pe, mm_gu, reads=[r_xgT3[s3], r_wg[ws], r_wu[ws]], writes=[r_Gs[ws]])
                    fw.op(fw.act, lambda e: e.activation(out=sgl2[ws][:], in_=Gb[:, 0, :], func=AF.Silu), reads=[r_Gs[ws]], writes=[r_sgl2[ws]])
                    fw.op(fw.dve, lambda e: e.tensor_tensor(out=hidtok[ws][:], in0=sgl2[ws][:], in1=Gb[:, 1, :], op=ALU.mult),
                          reads=[r_sgl2[ws], r_Gs[ws]], writes=[r_hidtok[ws]])

                def stageH(b):
                    ws = b % 2
                    wds = wd[ws][:].rearrange("p (c d) -> p c d", c=4)
                    hv = hidtok[ws][:].rearrange("p (q c) -> p c q", c=4)

                    def trH(e):
                        ins = None
                        for c in range(4):
                            ins = e.transpose(psH[:, c, :], hv[:, c, :], identb[:])
                        return ins
                    fw.op(fw.pe, trH, reads=[r_hidtok[ws], r_idb], writes=[r_TB])
                    fw.op(fw.dve, lambda e: e.tensor_copy(out=hidT[ws][:], in_=psH), reads=[r_TB], writes=[r_hid[ws]])

                def stageH2(b):
                    ws = b % 2
                    wds = wd[ws][:].rearrange("p (c d) -> p c d", c=4)

                    def mm_d(e):
                        ins = None
                        for cc in range(2):
                            for fc in range(4):
                                ins = e.matmul(Y0[:, cc, :], hidT[ws][:, fc, :], wds[:, fc, cc * 512:(cc + 1) * 512],
                                               start=(fc == 0), stop=(fc == 3))
                        return ins
                    fw.op(fw.pe, mm_d, reads=[r_hid[ws], r_wd[ws]], writes=[r_Y0])
                    fw.op(fw.act, lambda e: e.activation(out=yb[ws][:], in_=Y0[:].rearrange("p a b -> p (a b)"), func=AF.Copy),
                          reads=[r_Y0], writes=[r_yb[ws]])
                    fw.dma(fw.act, yg[b * 128:(b + 1) * 128, :], yb[ws][:], r_yb[ws], reads=[r_yb[ws]])

                for bb in (0, 1):
                    if bb < NB:
                        gath_gu(bb)
                        gath_d(bb)
                        preC(bb)
                stageG(0)
                for b in range(NB):
                    stageH(b)
                    if b + 2 < NB:
                        gath_gu(b + 2)
                        preC(b + 2)
                    stageH2(b)
                    if b + 1 < NB:
                        stageG(b + 1)
                    if b + 2 < NB:
                        gath_d(b + 2)
                fw.barrier()
            with contextlib.ExitStack() as sd:
                gfin = sb(sd, "gfin", [128, D], F32)
                fw.dma(fw.sp, gfin[:], gfin_b, r_gfin, writes=[r_gfin])
                y1 = [sb(sd, f"y1{i}", [128, D], F32) for i in range(2)]
                y2 = [sb(sd, f"y2{i}", [128, D], F32) for i in range(2)]
                r_y1 = [R("y10"), R("y11")]
                r_y2 = [R("y20"), R("y21")]
                hr = [sb(sd, f"hr{i}", [128, D], F32) for i in range(2)]
                r_hr = [R("hr0"), R("hr1")]
                yt = [sb(sd, f"yt{i}", [128, D], F32) for i in range(2)]
                r_yt = [R("yt0"), R("yt1")]
                junk3 = sb(sd, "junk3", [128, D], BF16)
                r_junk3 = R("junk3")
                ss3 = [sb(sd, f"ss3{i}", [128, 1], F32) for i in range(2)]
                r_ss3 = [R("ss30"), R("ss31")]
                def loadD(tg):
                    sl = tg % 2
                    indirect(y1[sl][:, :], None, yg[:, :], d1i[:, tg:tg + 1], PR - 1, r_y1[sl], [r_rt3], [r_y1[sl]])
                    indirect(y2[sl][:, :], None, yg[:, :], d2i[:, tg:tg + 1], PR - 1, r_y2[sl], [r_rt3], [r_y2[sl]])
                    fw.dma(fw.sp, hr[sl][:], h_d[tg], r_hr[sl], writes=[r_hr[sl]])

                def compD(tg):
                    sl = tg % 2
                    fw.op(fw.dve, lambda e: e.scalar_tensor_tensor(out=hr[sl][:], in0=y1[sl][:], scalar=wts[:, tg, 0:1], in1=hr[sl][:],
                                                                   op0=ALU.mult, op1=ALU.add),
                          reads=[r_y1[sl], r_hr[sl], r_gates[tg]], writes=[r_hr[sl]])
                    fw.op(fw.dve, lambda e: e.scalar_tensor_tensor(out=hr[sl][:], in0=y2[sl][:], scalar=wts[:, tg, 1:2], in1=hr[sl][:],
                                                                   op0=ALU.mult, op1=ALU.add),
                          reads=[r_y2[sl], r_hr[sl], r_gates[tg]], writes=[r_hr[sl]])
                    fw.op(fw.act, lambda e: e.activation(out=junk3[:], in_=hr[sl][:], func=AF.Square, accum_out=ss3[sl][:]),
                          reads=[r_hr[sl]], writes=[r_junk3, r_ss3[sl]])
                    fw.op(fw.dve, lambda e: e.tensor_scalar(out=ss3[sl][:], in0=ss3[sl][:], scalar1=1.0 / D, scalar2=EPS, op0=ALU.mult, op1=ALU.add),
                          reads=[r_ss3[sl]], writes=[r_ss3[sl]])
                    fw.op(fw.act, lambda e: e.activation(out=ss3[sl][:], in_=ss3[sl][:], func=AF.Ln), reads=[r_ss3[sl]], writes=[r_ss3[sl]])
                    fw.op(fw.act, lambda e: e.activation(out=ss3[sl][:], in_=ss3[sl][:], func=AF.Exp, scale=-0.5), reads=[r_ss3[sl]], writes=[r_ss3[sl]])
                    fw.op(fw.dve, lambda e: e.scalar_tensor_tensor(out=yt[sl][:], in0=hr[sl][:], scalar=ss3[sl][:], in1=gfin[:],
                                                                   op0=ALU.mult, op1=ALU.mult),
                          reads=[r_hr[sl], r_ss3[sl], r_gfin], writes=[r_yt[sl]])
                    fw.dma(fw.act, y[tg * 128:(tg + 1) * 128, :], yt[sl][:], r_yt[sl], reads=[r_yt[sl]])

                loadD(0)
                for tg in range(NT):
                    if tg + 1 < NT:
                        loadD(tg + 1)
                    compD(tg)
                fw.barrier()
    return nc


def _const_tables(max_n):
    max_nt = max_n // 128
    inv_freq = (np.float32(10000.0) ** (-np.arange(0, 64, 2, dtype=np.float32) / np.float32(64))).astype(np.float32)
    pos = np.arange(max_n, dtype=np.float32)
    ang = (pos[:, None] * inv_freq[None, :]).astype(np.float32)
    cos = np.cos(ang).astype(np.float32).reshape(max_nt, 128, 32)
    sin = np.sin(ang).astype(np.float32).reshape(max_nt, 128, 32)
    cs = np.stack([cos, sin], axis=2)
    cs = np.ascontiguousarray(cs.transpose(1, 0, 2, 3))
    invc = np.zeros((3, 4, 128), np.float32)
    for g, w in enumerate((2, 4, 8, 16)):
        half = w // 2
        t = np.arange(128)
        invc[0, g] = 1.0 / (np.minimum(t + half, 10 ** 9) - np.maximum(t - half, 0))
        invc[1, g] = 1.0 / w
        invc[2, g] = 1.0 / (np.minimum(t + half, 128) - (t - half))
    invc = np.ascontiguousarray(np.broadcast_to(invc[None], (128, 3, 4, 128))).astype(np.float32)
    return cs, invc


_PROG_CACHE = {}


def run_cores(core_seqs, weights, n_cores, sg_tok):
    seq_lens = tuple(int(s.shape[0]) for s in core_seqs[0])
    key = (seq_lens, sg_tok)
    if key not in _PROG_CACHE:
        _PROG_CACHE[key] = build_program(list(seq_lens), sg_tok)
    nc = _PROG_CACHE[key]
    f32 = np.float32
    W = {k: np.asarray(v, dtype=f32) for k, v in weights.items()}
    cs, invc = _const_tables(max(seq_lens))
    NBh = (2 * sum(seq_lens)) // 128 + NE
    shared = {
        "w_in": np.ascontiguousarray(W["w_in"][0]),
        "w_out": np.ascontiguousarray(W["w_out"][0]),
        "w_pool": np.ascontiguousarray(W["w_pool"][0]),
        "w_gate": np.ascontiguousarray(W["w_gate"][0]).reshape(NE * 128, 8 * DFF),
        "w_up": np.ascontiguousarray(W["w_up"][0]).reshape(NE * 128, 8 * DFF),
        "w_down": np.ascontiguousarray(W["w_down"][0]).reshape(NE * 128, 4 * D),
        "w_r": np.ascontiguousarray(np.concatenate([W["w_router_group"][0], W["w_router_expert"][0]], axis=1)),
        "gmix_t": np.ascontiguousarray(W["g_mix"][0].reshape(8, 128).T),
        "wos_t": np.ascontiguousarray(np.concatenate([W["pool_scale"][0].reshape(4, 128).T,
                                                      np.repeat(W["subln_g"][0][:, None], 4, axis=1)], axis=1)),
        "gffn_b": np.ascontiguousarray(np.broadcast_to(W["g_ffn"][0][None, :], (128, D))),
        "gfin_b": np.ascontiguousarray(np.broadcast_to(W["g_final"][None, :], (128, D))),
        "bias_b": np.ascontiguousarray(np.broadcast_to(
            np.concatenate([W["b_router_group"][0], W["b_router_expert"][0]])[None, :], (128, 36))),
        "lam_b": np.ascontiguousarray(np.broadcast_to(
            np.stack([W["lambda_q1"][0], W["lambda_k1"][0], W["lambda_q2"][0], W["lambda_k2"][0]])[None], (128, 4, 64))),
        "ident_bf": np.eye(128, dtype=f32).astype(ml_dtypes.bfloat16),
        "ident_f": np.eye(128, dtype=f32),
        "cs_tab": cs,
        "invc": invc,
        "tri_bf": np.triu(np.ones((128, 128), f32), k=1).astype(ml_dtypes.bfloat16),
        "bstart": np.ascontiguousarray(np.broadcast_to((np.arange(NBh, dtype=f32) * 128.0)[None, :], (128, NBh))),
        "pidx": np.arange(128, dtype=f32).reshape(128, 1),
    }
    in_maps = []
    for c in range(n_cores):
        m = dict(shared)
        m["x"] = np.ascontiguousarray(np.concatenate([np.asarray(s, dtype=f32) for s in core_seqs[c]], axis=0))
        in_maps.append(m)
    res = run_bass_kernel_spmd(nc, in_maps, core_ids=list(range(n_cores)))
    outs = []
    for c in range(n_cores):
        yc = np.asarray(res.results[c]["y"])
        o = []
        off = 0
        for n in seq_lens:
            o.append(yc[off:off + n])
            off += n
        outs.append(o)
    return outs


def kernel(x_prompt, x_sample, **weights):
    n_cores = 8
    xp = np.asarray(x_prompt)
    xsm = np.asarray(x_sample)
    pp = xp.shape[0] // n_cores
    ps = xsm.shape[0] // n_cores
    core_seqs = []
    for c in range(n_cores):
        core_seqs.append([xp[c * pp + i] for i in range(pp)] + [xsm[c * ps + i] for i in range(ps)])
    outs = run_cores(core_seqs, weights, n_cores, sg_tok=2048)
    yp = np.empty(xp.shape, np.float32)
    ys = np.empty(xsm.shape, np.float32)
    for c in range(n_cores):
        for i in range(pp):
            yp[c * pp + i] = outs[c][i]
        for i in range(ps):
            ys[c * ps + i] = outs[c][pp + i]
    return (yp, ys)
```

```python
import contextlib
import numpy as np
import ml_dtypes
import concourse.bass as bass
import concourse.mybir as mybir
from concourse.bass_utils import run_bass_kernel_spmd

F32 = mybir.dt.float32
BF16 = mybir.dt.bfloat16
ALU = mybir.AluOpType
AF = mybir.ActivationFunctionType
AX = mybir.AxisListType

D = 1024
NE = 32
DFF = 512
EPS = 1e-6
SEM_WRAP = 30000
LAM_INIT = 0.2


class Res:
    __slots__ = ("name", "last_w", "readers", "dma_sem", "dma_cnt")

    def __init__(self, name):
        self.name = name
        self.last_w = None
        self.readers = []
        self.dma_sem = None
        self.dma_cnt = 0


class Eng:
    def __init__(self, fw, name, obj, is_pe=False):
        self.fw = fw
        self.name = name
        self.obj = obj
        self.is_pe = is_pe
        self.n = 0
        self.sems = []
        self.waited = {}

    def cur_sem(self):
        idx = self.n // SEM_WRAP
        while len(self.sems) <= idx:
            self.sems.append(self.fw.new_sem(f"tl_{self.name}_{len(self.sems)}"))
        return self.sems[idx]

    def wait_tokens(self, toks):
        best = {}
        for (sk, v) in toks:
            if v > best.get(sk, 0):
                best[sk] = v
        for sk, v in best.items():
            if self.waited.get(sk, 0) >= v:
                continue
            self.waited[sk] = v
            self.obj.wait_ge(self.fw.sem_by_key[sk], v)


class FW:
    def __init__(self, nc, stack):
        self.nc = nc
        self.sem_by_key = {}
        self._stack = stack
        self.all_res = []
        self.dma_reg = {}
        self.pe = Eng(self, "pe", nc.tensor, is_pe=True)
        self.act = Eng(self, "act", nc.scalar)
        self.dve = Eng(self, "dve", nc.vector)
        self.pool = Eng(self, "pool", nc.gpsimd)
        self.sp = Eng(self, "sp", nc.sync)
        self.engs = [self.pe, self.act, self.dve, self.pool, self.sp]

    def res(self, name):
        r = Res(name)
        self.all_res.append(r)
        return r

    def new_sem(self, name):
        h = self._stack.enter_context(self.nc.semaphore(name))
        key = len(self.sem_by_key)
        self.sem_by_key[key] = h
        return key

    def op(self, eng, fn, reads=(), writes=()):
        deps = []
        own = set(eng.sems)
        for r in reads:
            if r.last_w is not None:
                deps.append(r.last_w)
        for w in writes:
            if w.last_w is not None:
                deps.append(w.last_w)
            for t in w.readers:
                if t[0] in own:
                    continue
                deps.append(t)
        if eng.is_pe:
            deps = [t for t in deps if t[0] not in own]
        eng.wait_tokens(deps)
        ins = fn(eng.obj)
        sk = eng.cur_sem()
        val = (eng.n % SEM_WRAP) + 1
        eng.n += 1
        ins.then_inc(self.sem_by_key[sk], 1)
        tok = (sk, val)
        for r in reads:
            r.readers.append(tok)
        for w in writes:
            w.last_w = tok
            w.readers = []
        return tok

    def dma(self, q, out, in_, sb_res, reads=(), writes=()):
        deps = []
        for r in reads:
            if r.last_w is not None:
                deps.append(r.last_w)
        for w in writes:
            if w.last_w is not None:
                deps.append(w.last_w)
            deps.extend(w.readers)
        q.wait_tokens(deps)
        ent = self.dma_reg.setdefault(sb_res.name, [None, 0])
        if ent[0] is None:
            ent[0] = self.new_sem(f"dma_{sb_res.name}")
        ins = q.obj.dma_start(out=out, in_=in_)
        ent[1] += 16
        ins.then_inc(self.sem_by_key[ent[0]], 16)
        tok = (ent[0], ent[1])
        for r in reads:
            r.readers.append(tok)
        for w in writes:
            w.last_w = tok
            w.readers = []
        return tok

    def all_tokens(self):
        toks = []
        for r in self.all_res:
            if r.last_w is not None:
                toks.append(r.last_w)
            toks.extend(r.readers)
        for e in self.engs:
            if e.n > 0:
                idx = (e.n - 1) // SEM_WRAP
                toks.append((e.sems[idx], ((e.n - 1) % SEM_WRAP) + 1))
        return toks

    def barrier(self):
        toks = self.all_tokens()
        for e in self.engs:
            e.wait_tokens(toks)


import os as _os
_DBG = _os.environ.get("KDBG", "full")


def build_program(seq_lens, sg_tok):
    T = sum(seq_lens)
    NT = T // 128
    NG = T // 512
    max_nt = max(seq_lens) // 128
    assert all(n % 512 == 0 for n in seq_lens)
    assert T % sg_tok == 0 and sg_tok % 512 == 0
    nc = bass.Bass("TRN2", target_bir_lowering=False)

    def din(name, shape, dt=F32):
        return nc.dram_tensor(name, list(shape), dt, kind="ExternalInput").ap()

    x = din("x", [T, D])
    w_in = din("w_in", [D, 2048])
    w_out = din("w_out", [D, D])
    w_pool = din("w_pool", [4, 128, 128])
    w_gate = din("w_gate", [NE * 128, 8 * DFF])
    w_up = din("w_up", [NE * 128, 8 * DFF])
    w_down = din("w_down", [NE * 128, 4 * D])
    w_r = din("w_r", [D, 36])
    gmix_t = din("gmix_t", [128, 8])
    wos_t = din("wos_t", [128, 8])
    gffn_b = din("gffn_b", [128, D])
    gfin_b = din("gfin_b", [128, D])
    bias_b = din("bias_b", [128, 36])
    lam_b = din("lam_b", [128, 4, 64])
    ident_bf_d = din("ident_bf", [128, 128], BF16)
    ident_f_d = din("ident_f", [128, 128])
    cs_tab_d = din("cs_tab", [128, max_nt, 2, 32])
    invc_d = din("invc", [128, 3, 4, 128])
    y = nc.dram_tensor("y", [T, D], F32, kind="ExternalOutput").ap()
    qd = nc.dram_tensor("qd", [NG, 128, 4, 512], BF16, kind="Internal").ap()
    pd = nc.dram_tensor("pd", [NG, 128, 4, 4, 128], BF16, kind="Internal").ap()
    h_d = nc.dram_tensor("h_d", [NT, 128, D], F32, kind="Internal").ap()
    hn_d = nc.dram_tensor("hn_d", [NT, 128, D], BF16, kind="Internal").ap()
    NB = (2 * T) // 128 + NE
    PR = NB * 128
    xg = nc.dram_tensor("xg", [PR, D], BF16, kind="Internal").ap()
    yg = nc.dram_tensor("yg", [PR, D], F32, kind="Internal").ap()
    tri_d = din("tri_bf", [128, 128], BF16)
    bstart_d = din("bstart", [128, NB])
    pidx_d = din("pidx", [128, 1])

    with contextlib.ExitStack() as st0:
        fw = FW(nc, st0)
        R = fw.res

        sfx = [""]

        def sb(st, name, shape, dt):
            return st.enter_context(nc.sbuf_tensor("s_" + name + sfx[0], list(shape), dt))

        lg_all = sb(st0, "lg_all", [128, NT, 36], F32)
        ones_bf = sb(st0, "ones_bf", [128, 128], BF16)
        ones_f = sb(st0, "ones_f", [128, 128], F32)
        r_gates = [R(f"gates{i}") for i in range(NT)]
        r_gfin = R("gfin")
        r_ones = R("ones")
        eps_t = sb(st0, "eps_t", [128, 1], F32)
        fw.op(fw.dve, lambda e: e.memset(eps_t[:], EPS), writes=[r_ones])
        fw.op(fw.dve, lambda e: e.memset(ones_f[:], 1.0), writes=[r_ones])
        fw.op(fw.dve, lambda e: e.tensor_copy(out=ones_bf[:], in_=ones_f[:]), reads=[r_ones], writes=[r_ones])

        with contextlib.ExitStack() as st:
            S0 = st.enter_context(nc.psum_tensor("S0", [128, 2, 512], F32))
            S1 = st.enter_context(nc.psum_tensor("S1", [128, 2, 512], F32))
            PO = st.enter_context(nc.psum_tensor("PO", [128, 2, 512], F32))
            PL = st.enter_context(nc.psum_tensor("PL", [128, 2, 512], F32))
            r_S0, r_S1, r_PO, r_PL = R("S0"), R("S1"), R("PO"), R("PL")
            r_S0a, r_S0b, r_S1a, r_S1b = R("S0a"), R("S0b"), R("S1a"), R("S1b")
            r_POa, r_POb, r_PLa, r_PLb = R("POa"), R("POb"), R("PLa"), R("PLb")
            r_psr = [R("psr0"), R("psr1")]

            Win = sb(st, "Win", [128, 8, 2048], BF16)
            Wout = sb(st, "Wout", [128, 8, 1024], BF16)
            Wp = sb(st, "Wp", [128, 4, 128], BF16)
            Wr = sb(st, "Wr", [128, 8, 36], F32)
            gmix = sb(st, "gmix", [128, 8], F32)
            wos = sb(st, "wos", [128, 8], F32)
            gffn = sb(st, "gffn", [128, D], F32)
            biasb = sb(st, "biasb", [128, 36], F32)
            lamt = sb(st, "lamt", [128, 4, 64], F32)
            lamp = sb(st, "lamp", [128, 2, 64], F32)
            lams = sb(st, "lams", [128, 2], F32)
            neglam = sb(st, "neglam", [128, 1], F32)
            ident_bf = sb(st, "ident_bf", [128, 128], BF16)
            ident_f = sb(st, "ident_f", [128, 128], F32)
            invc = sb(st, "invc", [128, 3, 4, 128], F32)
            st_w = contextlib.ExitStack()
            wstage = sb(st_w, "wstage", [128, 2048], F32)
            r_Win, r_Wout, r_Wp, r_Wr, r_wstage = R("Win"), R("Wout"), R("Wp"), R("Wr"), R("wstage")
            r_c = R("consts")
            r_lam = R("lam")
            for (t_sb, t_dr) in ((gmix, gmix_t), (wos, wos_t), (gffn, gffn_b), (biasb, bias_b),
                                 (ident_bf, ident_bf_d), (ident_f, ident_f_d), (invc, invc_d)):
                fw.dma(fw.sp, t_sb[:], t_dr, r_c, writes=[r_c])
            fw.dma(fw.sp, lamt[:], lam_b, r_lam, writes=[r_lam])
            fw.dma(fw.sp, Wr[:], w_r.rearrange("(c p) j -> p c j", p=128), r_Wr, writes=[r_Wr])
            fw.dma(fw.pool, Wp[:], w_pool.rearrange("g c e -> c g e"), r_Wp, writes=[r_Wp])
            for c in range(8):
                fw.dma(fw.sp, wstage[:], w_in[c * 128:(c + 1) * 128, :], r_wstage, writes=[r_wstage])
                fw.op(fw.act, lambda e, c=c: e.activation(out=Win[:, c, :], in_=wstage[:], func=AF.Copy,
                                                          scale=gmix[:, c:c + 1]),
                      reads=[r_wstage, r_c], writes=[r_Win])
            for c in range(8):
                fw.dma(fw.sp, wstage[:, 0:1024], w_out[c * 128:(c + 1) * 128, :], r_wstage, writes=[r_wstage])
                fw.op(fw.act, lambda e, c=c: e.activation(out=Wout[:, c, :], in_=wstage[:, 0:1024], func=AF.Copy,
                                                          scale=wos[:, c:c + 1]),
                      reads=[r_wstage, r_c], writes=[r_Wout])
            lam4 = lamt[:].rearrange("p (a b) d -> p a b d", b=2)
            fw.op(fw.dve, lambda e: e.tensor_tensor(out=lamp[:], in0=lam4[:, :, 0, :], in1=lam4[:, :, 1, :], op=ALU.mult),
                  reads=[r_lam], writes=[r_lam])
            fw.op(fw.dve, lambda e: e.tensor_reduce(out=lams[:], in_=lamp[:], axis=AX.X, op=ALU.add),
                  reads=[r_lam], writes=[r_lam])
            fw.op(fw.act, lambda e: e.activation(out=lams[:], in_=lams[:], func=AF.Exp), reads=[r_lam], writes=[r_lam])
            fw.op(fw.dve, lambda e: e.tensor_tensor(out=neglam[:], in0=lams[:, 1:2], in1=lams[:, 0:1], op=ALU.subtract),
                  reads=[r_lam], writes=[r_lam])
            fw.op(fw.dve, lambda e: e.tensor_scalar(out=neglam[:], in0=neglam[:], scalar1=-LAM_INIT, scalar2=None,
                                                    op0=ALU.add), reads=[r_lam], writes=[r_lam])

            fw.barrier()
            st_w.close()
            max_n = max(seq_lens)
            kT = sb(st, "kT", [128, 4, max_n], BF16)
            V = sb(st, "V", [128, max_n // 128, 512], BF16)
            r_kT, r_V = R("kT"), R("V")


            def rstd_chain(src, dst, res_list, inv_n):
                fw.op(fw.act, lambda e: e.activation(out=dst, in_=src, func=AF.Ln, bias=eps_t[:], scale=inv_n),
                      reads=res_list + [r_ones], writes=res_list)
                fw.op(fw.act, lambda e: e.activation(out=dst, in_=dst, func=AF.Exp, scale=-0.5),
                      reads=res_list, writes=res_list)

            psT = PO[:, 0, :].bitcast(BF16).rearrange("p (c t) -> p c t", c=8)
            psT2 = PO[:, 1, :].bitcast(BF16).rearrange("p (c t) -> p c t", c=8)
            ps_qk = S0[:].rearrange("p a (b d) -> p (a b) d", d=32)
            ps_qk4 = S0[:].rearrange("p a (b h d) -> p (a b) h d", h=2, d=32)
            ps_v = S1[:, 0, :]
            ps_p = S1[:, 1, :].rearrange("p (g t) -> p g t", g=4)
            ps_po = PL[:, 0, :].rearrange("p (g t) -> p g t", g=4)

            tok0 = 0
            g0 = 0
            for si, N in enumerate(seq_lens):
                nt = N // 128
                ng = N // 512

                sfx[0] = f"_s{si}"
                st1 = contextlib.ExitStack()
                cs_tab = sb(st1, "cs_tab", [128, max_nt, 2, 32], F32)
                r_cs = R("cs_tab")
                fw.dma(fw.sp, cs_tab[:], cs_tab_d, r_cs, writes=[r_cs])
                xt = [sb(st1, f"xt{i}", [128, D], F32) for i in range(2)]
                r_xt = [R(f"xt{i}") for i in range(2)]
                junk = sb(st1, "junk", [128, D], BF16)
                r_junk = R("junk")
                ss = [sb(st1, f"ss{i}", [128, 1], F32) for i in range(2)]
                r_ss = [R(f"ss{i}") for i in range(2)]
                xs = [sb(st1, f"xs{i}", [128, D], BF16) for i in range(2)]
                r_xs = [R(f"xs{i}") for i in range(2)]
                xnT = [sb(st1, f"xnT{i}", [128, 8, 128], BF16) for i in range(2)]
                r_xnT = [R(f"xnT{i}") for i in range(2)]
                rp = [sb(st1, f"rp{i}", [128, 16, 32], F32) for i in range(4)]
                r_rp = R("rp")
                qkr = sb(st1, "qkr", [128, 16, 2, 32], BF16)
                r_qkr = R("qkr")
                qst = [sb(st1, f"qst{i}", [128, 4, 128], BF16) for i in range(2)]
                r_qst = [R(f"qst{i}") for i in range(2)]
                pst = [sb(st1, f"pst{i}", [128, 4, 128], BF16) for i in range(2)]
                r_pst = [R(f"pst{i}") for i in range(2)]
                zpt = [sb(st1, f"zpt{i}", [128, 4, 128], F32) for i in range(3)]
                r_zpt = [R(f"zpt{i}") for i in range(3)]
                ZW = sb(st1, "ZW", [128, 4, 144], F32)
                za = sb(st1, "za", [128, 4, 144], F32)
                zb = sb(st1, "zb", [128, 4, 144], F32)
                zc = sb(st1, "zc", [128, 4, 144], F32)
                zd = sb(st1, "zd", [128, 4, 144], F32)
                pw = sb(st1, "pw", [128, 4, 128], F32)
                pooled = sb(st1, "pooled", [128, 4, 128], BF16)
                r_pm = R("poolmix")
                r_pooled = R("pooled")

                def pool_stage(i):
                    cur = zpt[i % 3]
                    deps_r = [r_zpt[i % 3]]
                    if i > 0:
                        prev = zpt[(i - 1) % 3]
                        deps_r.append(r_zpt[(i - 1) % 3])
                        fw.op(fw.pool, lambda e: e.tensor_copy(out=ZW[:, :, 0:8], in_=prev[:, :, 120:128]),
                              reads=deps_r, writes=[r_pm])
                    else:
                        fw.op(fw.pool, lambda e: e.memset(ZW[:, :, 0:8], 0.0), writes=[r_pm])
                    fw.op(fw.pool, lambda e: e.tensor_copy(out=ZW[:, :, 8:136], in_=cur[:]), reads=deps_r, writes=[r_pm])
                    if i < nt - 1:
                        nxt = zpt[(i + 1) % 3]
                        fw.op(fw.pool, lambda e: e.tensor_copy(out=ZW[:, :, 136:144], in_=nxt[:, :, 0:8]),
                              reads=[r_zpt[(i + 1) % 3]], writes=[r_pm])
                    else:
                        fw.op(fw.pool, lambda e: e.memset(ZW[:, :, 136:144], 0.0), writes=[r_pm])
                    P = fw.pool
                    fw.op(P, lambda e: e.tensor_tensor(out=za[:, :, 0:143], in0=ZW[:, :, 0:143], in1=ZW[:, :, 1:144], op=ALU.add),
                          reads=[r_pm], writes=[r_pm])
                    fw.op(P, lambda e: e.tensor_tensor(out=zb[:, :, 0:141], in0=za[:, :, 0:141], in1=za[:, :, 2:143], op=ALU.add),
                          reads=[r_pm], writes=[r_pm])
                    fw.op(P, lambda e: e.tensor_tensor(out=zc[:, :, 0:137], in0=zb[:, :, 0:137], in1=zb[:, :, 4:141], op=ALU.add),
                          reads=[r_pm], writes=[r_pm])
                    fw.op(P, lambda e: e.tensor_tensor(out=zd[:, :, 0:129], in0=zc[:, :, 0:129], in1=zc[:, :, 8:137], op=ALU.add),
                          reads=[r_pm], writes=[r_pm])
                    kind = 0 if i == 0 else (2 if i == nt - 1 else 1)
                    srcs = [za[:, 0, 7:135], zb[:, 1, 6:134], zc[:, 2, 4:132], zd[:, 3, 0:128]]
                    for g in range(4):
                        fw.op(P, lambda e, g=g: e.tensor_tensor(out=pw[:, g, :], in0=srcs[g], in1=invc[:, kind, g, :], op=ALU.mult),
                              reads=[r_pm, r_c], writes=[r_pm])
                    fw.op(P, lambda e: e.tensor_tensor(out=pooled[:], in0=pw[:], in1=cur[:], op=ALU.subtract),
                          reads=[r_pm, r_zpt[i % 3]], writes=[r_pooled])

                    def mm_pool(e):
                        ins = None
                        for g in range(4):
                            ins = e.matmul(ps_po[:, g, :], Wp[:, g, :], pooled[:, g, :], start=True, stop=True)
                        return ins
                    fw.op(fw.pe, mm_pool, reads=[r_pooled, r_Wp], writes=[r_PLa])
                    sl = i % 2
                    fw.op(fw.act, lambda e: e.activation(out=pst[sl][:], in_=ps_po, func=AF.Copy),
                          reads=[r_PLa], writes=[r_pst[sl]])
                    g_idx = g0 + i // 4
                    fw.dma(fw.sp, pd[g_idx, :, i % 4, :, :], pst[sl][:], r_pst[sl], reads=[r_pst[sl]])

                def stageA_load(i):
                    sl = i % 2
                    fw.dma(fw.sp, xt[sl][:], x[tok0 + i * 128: tok0 + (i + 1) * 128, :], r_xt[sl], writes=[r_xt[sl]])

                def stageA(i):
                    sl = i % 2
                    fw.op(fw.act, lambda e: e.activation(out=junk[:], in_=xt[sl][:], func=AF.Square, accum_out=ss[sl][:]),
                          reads=[r_xt[sl]], writes=[r_junk, r_ss[sl]])
                    rstd_chain(ss[sl][:], ss[sl][:], [r_ss[sl]], 1.0 / D)
                    fw.op(fw.act, lambda e: e.activation(out=xs[sl][:], in_=xt[sl][:], func=AF.Copy, scale=ss[sl][:]),
                          reads=[r_xt[sl], r_ss[sl]], writes=[r_xs[sl]])

                    def tr1(e):
                        ins = None
                        for c in range(8):
                            ins = e.transpose(psT[:, c, :], xs[sl][:, c * 128:(c + 1) * 128], ident_bf[:])
                        return ins
                    fw.op(fw.pe, tr1, reads=[r_xs[sl], r_c], writes=[r_POa])
                    fw.op(fw.dve, lambda e: e.tensor_copy(out=xnT[sl][:], in_=psT), reads=[r_POa], writes=[r_xnT[sl]])

                def stageB(i):
                    sl = i % 2

                    def mm_qkv(e):
                        ins = None
                        for j, dst in enumerate((S0[:, 0, :], S0[:, 1, :], ps_v)):
                            for c in range(8):
                                ins = e.matmul(dst, xnT[sl][:, c, :], Win[:, c, 512 + j * 512: 1024 + j * 512],
                                               start=(c == 0), stop=(c == 7))
                        for g in range(4):
                            for c in range(8):
                                ins = e.matmul(ps_p[:, g, :], Win[:, c, g * 128:(g + 1) * 128], xnT[sl][:, c, :],
                                               start=(c == 0), stop=(c == 7))
                        return ins
                    fw.op(fw.pe, mm_qkv, reads=[r_xnT[sl], r_Win], writes=[r_S0a, r_S0b, r_S1a, r_S1b])
                    fw.op(fw.act, lambda e: e.activation(out=V[:, i, :], in_=ps_v, func=AF.Copy), reads=[r_S1a], writes=[r_V])
                    fw.op(fw.act, lambda e: e.activation(out=zpt[i % 3][:], in_=ps_p, func=AF.Copy),
                          reads=[r_S1b], writes=[r_zpt[i % 3]])
                    cosb = cs_tab[:, i, 0, :].unsqueeze(1).broadcast_to([128, 16, 32])
                    sinb = cs_tab[:, i, 1, :].unsqueeze(1).broadcast_to([128, 16, 32])
                    x1 = ps_qk4[:, :, 0, :]
                    x2 = ps_qk4[:, :, 1, :]
                    fw.op(fw.dve, lambda e: e.tensor_tensor(out=rp[0][:], in0=x1, in1=cosb, op=ALU.mult), reads=[r_S0a, r_S0b, r_cs], writes=[r_rp])
                    fw.op(fw.dve, lambda e: e.tensor_tensor(out=rp[1][:], in0=x2, in1=sinb, op=ALU.mult), reads=[r_S0a, r_S0b, r_cs], writes=[r_rp])
                    fw.op(fw.dve, lambda e: e.tensor_tensor(out=rp[2][:], in0=x2, in1=cosb, op=ALU.mult), reads=[r_S0a, r_S0b, r_cs], writes=[r_rp])
                    fw.op(fw.dve, lambda e: e.tensor_tensor(out=rp[3][:], in0=x1, in1=sinb, op=ALU.mult), reads=[r_S0a, r_S0b, r_cs], writes=[r_rp])
                    fw.op(fw.dve, lambda e: e.tensor_tensor(out=qkr[:, :, 0, :], in0=rp[0][:], in1=rp[1][:], op=ALU.subtract), reads=[r_rp], writes=[r_qkr])
                    fw.op(fw.dve, lambda e: e.tensor_tensor(out=qkr[:, :, 1, :], in0=rp[2][:], in1=rp[3][:], op=ALU.add), reads=[r_rp], writes=[r_qkr])

                def stageB2(i):
                    sl = i % 2
                    qkr_f = qkr[:].rearrange("p a h d -> p (a h d)")

                    def tr2(e):
                        ins = None
                        for c in range(8):
                            ins = e.transpose(psT2[:, c, :], qkr_f[:, c * 128:(c + 1) * 128], ident_bf[:])
                        return ins
                    fw.op(fw.pe, tr2, reads=[r_qkr, r_c], writes=[r_POb])
                    fw.op(fw.dve, lambda e: e.tensor_copy(out=kT[:, :, i * 128:(i + 1) * 128], in_=psT2[:, 4:8, :]),
                          reads=[r_POb], writes=[r_kT])
                    fw.op(fw.dve, lambda e: e.tensor_copy(out=qst[sl][:], in_=psT2[:, 0:4, :]), reads=[r_POb], writes=[r_qst[sl]])
                    fw.dma(fw.sp, qd[g0 + i // 4, :, :, (i % 4) * 128:(i % 4 + 1) * 128], qst[sl][:], r_qst[sl], reads=[r_qst[sl]])

                stageA_load(0)
                stageA_load(1)
                stageA(0)
                stageA(1)
                for i in range(nt):
                    if i + 2 < nt:
                        stageA_load(i + 2)
                    stageB(i)
                    if i + 2 < nt:
                        stageA(i + 2)
                    stageB2(i)
                    if i >= 1:
                        pool_stage(i - 1)
                pool_stage(nt - 1)
                r_qd = R("qd_seq")
                fw.barrier()

                st1.close()
                st2 = contextlib.ExitStack()
                qpad = sb(st2, "qpad", [128, 4, 2, 512], BF16)
                r_qpad = R("qpad")
                poolT = sb(st2, "poolT", [128, 4, 4, 128], BF16)
                r_poolT = R("poolT")
                pT = [sb(st2, f"pT{i}", [128, 2, 512], BF16) for i in range(4)]
                r_pT = [R(f"pT{i}") for i in range(4)]
                pTs = [sb(st2, f"pTs{i}", [128, 2, 512], BF16) for i in range(2)]
                r_pTs = [R(f"pTs{i}") for i in range(2)]
                rl = sb(st2, "rl", [128, 2, 512], F32)
                on = rl
                od = sb(st2, "od", [128, 512], F32)
                osq = sb(st2, "osq", [128, 512], F32)
                rn = sb(st2, "rn", [128, 512], F32)
                r_ep = R("epi")
                attnT = sb(st2, "attnT", [128, 4, 512], BF16)
                r_attnT = R("attnT")
                xr = [sb(st2, f"xr{i}", [128, D], F32) for i in range(2)]
                r_xr = [R(f"xr{i}") for i in range(2)]
                ht = [sb(st2, f"ht{i}", [128, D], F32) for i in range(2)]
                r_ht = [R(f"ht{i}") for i in range(2)]
                hnf = sb(st2, "hnf", [128, D], F32)
                r_hnf = R("hnf")
                hnTf = sb(st2, "hnTf", [128, 8, 128], F32)
                r_hnTf = R("hnTf")
                hnTb = [sb(st2, f"hnTb{i}", [128, D], BF16) for i in range(2)]
                r_hnTb = [R(f"hnTb{i}") for i in range(2)]
                ss2 = sb(st2, "ss2", [128, 1], F32)
                r_ss2 = R("ss2")

                fw.op(fw.pool, lambda e: e.memset(qpad[:], 0.0), writes=[r_qpad])
                nk = nt
                for j in range(ng if _DBG not in ("p1",) else 0):
                    gi = g0 + j
                    for m in range(2):
                        dst = qpad[m * 64:(m + 1) * 64, :, m, :]
                        fw.dma(fw.sp, dst, qd[gi, m * 64:(m + 1) * 64, :, :], r_qpad, writes=[r_qpad])
                    fw.dma(fw.sp, poolT[:], pd[gi], r_poolT, writes=[r_poolT])
                    units = [(hh, kk) for hh in range(4) for kk in range(nk)]

                    def emit_qk(ui):
                        hh, kk = units[ui]
                        bb = ui % 2
                        Sb = S0 if bb == 0 else S1
                        r_Sb = [r_S0a, r_S0b] if bb == 0 else [r_S1a, r_S1b]

                        def mm_s(e):
                            e.matmul(Sb[:, 0, :], kT[:, hh, kk * 128:(kk + 1) * 128], qpad[:, hh, 0, :], start=True, stop=True)
                            return e.matmul(Sb[:, 1, :], kT[:, hh, kk * 128:(kk + 1) * 128], qpad[:, hh, 1, :], start=True, stop=True)
                        fw.op(fw.pe, mm_s, reads=[r_kT, r_qpad], writes=r_Sb)

                    def emit_exp_pv(ui):
                        hh, kk = units[ui]
                        bb = ui % 2
                        p4 = ui % 4
                        Sb = S0 if bb == 0 else S1
                        r_Sb = [r_S0a, r_S0b] if bb == 0 else [r_S1a, r_S1b]
                        fw.op(fw.act, lambda e: e.activation(out=pT[p4][:], in_=Sb[:], func=AF.Exp, scale=0.125),
                              reads=r_Sb, writes=[r_pT[p4]])
                        if ui + 2 < len(units):
                            emit_qk(ui + 2)

                        def mm_pv(e):
                            ins = None
                            for m in range(2):
                                ins = e.matmul(PO[:, m, :], V[:, kk, hh * 128:(hh + 1) * 128], pT[p4][:, m, :],
                                               start=(kk == 0), stop=(kk == nk - 1))
                            return ins
                        fw.op(fw.pe, mm_pv, reads=[r_V, r_pT[p4]], writes=[r_POa, r_POb])
                        def mm_l_for(ps2_, kk_):
                            def mm_l(e):
                                ins = None
                                for m in range(2):
                                    ins = e.matmul(PL[:, m, :], ones_bf[:], pTs[ps2_][:, m, :],
                                                   start=(kk_ == 1), stop=(kk_ == nk - 1))
                                return ins
                            fw.op(fw.pe, mm_l, reads=[r_pTs[ps2_], r_ones], writes=[r_PLa, r_PLb])
                        if kk % 2 == 0 and pend_l:
                            mm_l_for(*pend_l.pop())
                        if kk % 2 == 1:
                            ps2 = (ui // 2) % 2
                            pprev = (ui - 1) % 4
                            fw.op(fw.dve, lambda e: e.tensor_tensor(out=pTs[ps2][:], in0=pT[pprev][:], in1=pT[p4][:], op=ALU.add),
                                  reads=[r_pT[pprev], r_pT[p4]], writes=[r_pTs[ps2]])
                            if kk == nk - 1:
                                mm_l_for(ps2, kk)
                            else:
                                pend_l.append((ps2, kk))

                    pend_l = []
                    emit_qk(0)
                    if len(units) > 1:
                        emit_qk(1)
                    for ui in range(len(units)):
                        h, kc = units[ui]
                        emit_exp_pv(ui)
                        if kc != nk - 1:
                            continue
                        fw.op(fw.dve, lambda e: e.reciprocal(out=rl[:], in_=PL[:]), reads=[r_PLa, r_PLb], writes=[r_ep])
                        fw.op(fw.dve, lambda e: e.tensor_tensor(out=on[:], in0=PO[:], in1=rl[:], op=ALU.mult),
                              reads=[r_POa, r_POb, r_ep], writes=[r_ep])
                        fw.op(fw.dve, lambda e: e.scalar_tensor_tensor(out=od[:], in0=on[:, 1, :], scalar=neglam[:], in1=on[:, 0, :],
                                                                       op0=ALU.mult, op1=ALU.add),
                              reads=[r_ep, r_lam], writes=[r_ep])
                        fw.op(fw.act, lambda e: e.activation(out=osq[:], in_=od[:], func=AF.Square), reads=[r_ep], writes=[r_ep])
                        fw.op(fw.pe, lambda e: e.matmul(PL[:, 0, :], ones_f[:], osq[:], start=True, stop=True),
                              reads=[r_ep, r_ones], writes=[r_PLa])
                        fw.op(fw.act, lambda e: e.activation(out=rn[:], in_=PL[:, 0, :], func=AF.Ln, bias=eps_t[:], scale=1.0 / 128),
                              reads=[r_PLa, r_ones], writes=[r_ep])
                        fw.op(fw.act, lambda e: e.activation(out=rn[:], in_=rn[:], func=AF.Exp, scale=-0.5), reads=[r_ep], writes=[r_ep])
                        fw.op(fw.dve, lambda e, h=h: e.scalar_tensor_tensor(out=attnT[:, h, :], in0=od[:], scalar=1.0 - LAM_INIT, in1=rn[:],
                                                                            op0=ALU.mult, op1=ALU.mult),
                              reads=[r_ep], writes=[r_attnT])
                    def stage2A(ti):
                        tg = (tok0 // 128) + j * 4 + ti
                        sl = ti % 2
                        Hb = S0 if sl == 0 else S1
                        r_Hb = [r_S0a, r_S0b] if sl == 0 else [r_S1a, r_S1b]
                        fw.dma(fw.sp, xr[sl][:], x[tg * 128:(tg + 1) * 128, :], r_xr[sl], writes=[r_xr[sl]])

                        def mm_o(e, ti=ti, Hb=Hb):
                            ins = None
                            for cc in range(2):
                                for c in range(8):
                                    lhsT = poolT[:, ti, c, :] if c < 4 else attnT[:, c - 4, ti * 128:(ti + 1) * 128]
                                    ins = e.matmul(Hb[:, cc, :], lhsT, Wout[:, c, cc * 512:(cc + 1) * 512],
                                                   start=(c == 0), stop=(c == 7))
                            return ins
                        fw.op(fw.pe, mm_o, reads=[r_poolT, r_attnT, r_Wout], writes=r_Hb)
                        Hf = Hb[:].rearrange("p a b -> p (a b)")
                        fw.op(fw.dve, lambda e, Hf=Hf, sl=sl: e.tensor_tensor(out=ht[sl][:], in0=Hf, in1=xr[sl][:], op=ALU.add),
                              reads=r_Hb + [r_xr[sl]], writes=[r_ht[sl]])
                        fw.dma(fw.act, h_d[tg], ht[sl][:], r_ht[sl], reads=[r_ht[sl]])
                        fw.op(fw.act, lambda e, sl=sl: e.activation(out=hnTb[sl][:], in_=ht[sl][:], func=AF.Square, accum_out=ss2[:]),
                              reads=[r_ht[sl]], writes=[r_hnTb[sl], r_ss2])
                        rstd_chain(ss2[:], ss2[:], [r_ss2], 1.0 / D)
                        fw.op(fw.dve, lambda e, sl=sl: e.scalar_tensor_tensor(out=hnf[:], in0=ht[sl][:], scalar=ss2[:], in1=gffn[:],
                                                                              op0=ALU.mult, op1=ALU.mult),
                              reads=[r_ht[sl], r_ss2, r_c], writes=[r_hnf])
                        psTf = PO[:].rearrange("p a (c t) -> p (a c) t", t=128)

                        def tr3(e):
                            ins = None
                            for c in range(8):
                                ins = e.matmul(psTf[:, c, :], hnf[:, c * 128:(c + 1) * 128], ident_f[:], start=True, stop=True)
                            return ins
                        fw.op(fw.pe, tr3, reads=[r_hnf, r_c], writes=[r_POa, r_POb])
                        fw.op(fw.dve, lambda e: e.tensor_copy(out=hnTf[:], in_=psTf), reads=[r_POa, r_POb], writes=[r_hnTf])
                        fw.op(fw.pool, lambda e, sl=sl: e.tensor_copy(out=hnTb[sl][:], in_=hnf[:]),
                              reads=[r_hnf], writes=[r_hnTb[sl]])
                        fw.dma(fw.pool, hn_d[tg], hnTb[sl][:], r_hnTb[sl], reads=[r_hnTb[sl]])
                        ps_r = PL[:, 1, (ti % 2) * 64:(ti % 2) * 64 + 36]

                        def mm_r(e):
                            ins = None
                            for c in range(8):
                                ins = e.matmul(ps_r, hnTf[:, c, :], Wr[:, c, :], start=(c == 0), stop=(c == 7))
                            return ins
                        fw.op(fw.pe, mm_r, reads=[r_hnTf, r_Wr], writes=[r_psr[ti % 2], r_PLb])

                    def stage2B(ti):
                        tg = (tok0 // 128) + j * 4 + ti
                        ps_r = PL[:, 1, (ti % 2) * 64:(ti % 2) * 64 + 36]
                        fw.op(fw.dve, lambda e: e.tensor_tensor(out=lg_all[:, tg, :], in0=ps_r, in1=biasb[:], op=ALU.add),
                              reads=[r_psr[ti % 2], r_PLb, r_c], writes=[r_gates[tg]])

                    stage2A(0)
                    for ti in range(4):
                        if ti + 1 < 4:
                            stage2A(ti + 1)
                        stage2B(ti)
                tok0 += N
                g0 += ng
                fw.barrier()
                st2.close()
        fw.barrier()
        I32 = mybir.dt.int32
        with contextlib.ExitStack() as st:
            OH1 = sb(st, "OH1", [128, NT, 32], BF16)
            OH2 = sb(st, "OH2", [128, NT, 32], BF16)
            wts = sb(st, "wts", [128, NT, 2], F32)
            d1i = sb(st, "d1i", [128, NT], I32)
            d2i = sb(st, "d2i", [128, NT], I32)
            idxw = sb(st, "idxw", [128, NB], I32)
            r_rt3 = R("route3")
            TA = st.enter_context(nc.psum_tensor("TA", [128, 512], F32))
            TB = st.enter_context(nc.psum_tensor("TB", [128, 512], F32))
            G0 = st.enter_context(nc.psum_tensor("G0", [128, 2, 512], F32))
            G1 = st.enter_context(nc.psum_tensor("G1", [128, 2, 512], F32))
            Y0 = st.enter_context(nc.psum_tensor("Y0", [128, 2, 512], F32))
            r_TA, r_TB, r_G0, r_G1, r_Y0 = R("TA"), R("TB"), R("G0p"), R("G1p"), R("Y0p")
            with contextlib.ExitStack() as s0:
                r_r0 = R("router0")
                gq = sb(s0, "gq", [128, 8, NT], F32)
                mg = sb(s0, "mg", [128, NT, 4], F32)
                eg = sb(s0, "eg", [128, NT, 4], F32)
                t48 = sb(s0, "t48", [128, NT, 4, 8], F32)
                les = sb(s0, "les", [128, NT, 8], F32)
                les2 = sb(s0, "les2", [128, NT, 8], F32)
                k1 = sb(s0, "k1", [128, NT, 8], F32)
                k2 = sb(s0, "k2", [128, NT, 8], F32)
                gmax, gsum, m1, m2, ex, w1, w2 = (gq[:, i, :] for i in range(7))
                lg = lg_all[:, :, 0:4]
                le4 = lg_all[:, :, 4:36].rearrange("p t (g j) -> p t g j", g=4)
                rr0 = [r_r0]

                def d0(fn, extra=()):
                    fw.op(fw.dve, fn, reads=rr0 + list(extra), writes=rr0)

                def bc3(ap2, k):
                    return ap2.unsqueeze(2).broadcast_to([128, NT, k])
                d0(lambda e: e.tensor_reduce(out=gmax, in_=lg, axis=AX.X, op=ALU.max), r_gates)
                d0(lambda e: e.tensor_tensor(out=mg[:], in0=lg, in1=bc3(gmax, 4), op=ALU.is_equal), r_gates)
                d0(lambda e: e.tensor_tensor(out=eg[:], in0=lg, in1=bc3(gmax, 4), op=ALU.subtract), r_gates)
                fw.op(fw.act, lambda e: e.activation(out=eg[:], in_=eg[:], func=AF.Exp), reads=rr0, writes=rr0)
                d0(lambda e: e.tensor_reduce(out=gsum, in_=eg[:], axis=AX.X, op=ALU.add))
                d0(lambda e: e.reciprocal(out=gsum, in_=gsum))
                d0(lambda e: e.tensor_tensor(out=t48[:], in0=le4, in1=mg[:].unsqueeze(3).broadcast_to([128, NT, 4, 8]), op=ALU.mult), r_gates)
                d0(lambda e: e.tensor_reduce(out=les[:], in_=t48[:].rearrange("p t g j -> p t j g"), axis=AX.X, op=ALU.add))
                d0(lambda e: e.tensor_reduce(out=m1, in_=les[:], axis=AX.X, op=ALU.max))
                d0(lambda e: e.tensor_tensor(out=k1[:], in0=les[:], in1=bc3(m1, 8), op=ALU.is_equal))
                d0(lambda e: e.scalar_tensor_tensor(out=les2[:], in0=k1[:], scalar=-1e30, in1=les[:], op0=ALU.mult, op1=ALU.add))
                d0(lambda e: e.tensor_reduce(out=m2, in_=les2[:], axis=AX.X, op=ALU.max))
                d0(lambda e: e.tensor_tensor(out=k2[:], in0=les2[:], in1=bc3(m2, 8), op=ALU.is_equal))
                d0(lambda e: e.tensor_tensor(out=ex, in0=m2, in1=m1, op=ALU.subtract))
                fw.op(fw.act, lambda e: e.activation(out=ex, in_=ex, func=AF.Exp), reads=rr0, writes=rr0)
                d0(lambda e: e.tensor_scalar(out=w1, in0=ex, scalar1=1.0, scalar2=None, op0=ALU.add))
                d0(lambda e: e.reciprocal(out=w1, in_=w1))
                d0(lambda e: e.tensor_tensor(out=w1, in0=w1, in1=gsum, op=ALU.mult))
                d0(lambda e: e.tensor_tensor(out=w2, in0=w1, in1=ex, op=ALU.mult))
                mgb4 = mg[:].unsqueeze(3).broadcast_to([128, NT, 4, 8])
                o1v = OH1[:].rearrange("p t (g j) -> p t g j", g=4)
                o2v = OH2[:].rearrange("p t (g j) -> p t g j", g=4)
                fw.op(fw.dve, lambda e: e.tensor_tensor(out=o1v, in0=k1[:].unsqueeze(2).broadcast_to([128, NT, 4, 8]), in1=mgb4, op=ALU.mult),
                      reads=rr0, writes=r_gates)
                fw.op(fw.dve, lambda e: e.tensor_tensor(out=o2v, in0=k2[:].unsqueeze(2).broadcast_to([128, NT, 4, 8]), in1=mgb4, op=ALU.mult),
                      reads=rr0, writes=r_gates)
                fw.op(fw.dve, lambda e: e.tensor_copy(out=wts[:, :, 0], in_=w1), reads=rr0, writes=r_gates)
                fw.op(fw.dve, lambda e: e.tensor_copy(out=wts[:, :, 1], in_=w2), reads=rr0, writes=r_gates)
                fw.barrier()
            with contextlib.ExitStack() as sa:
                tri = sb(sa, "tri", [128, 128], BF16)
                bstart = sb(sa, "bstart", [128, NB], F32)
                pidx = sb(sa, "pidx", [128, 1], F32)
                r_ca = R("constA")
                fw.dma(fw.sp, tri[:], tri_d, r_ca, writes=[r_ca])
                fw.dma(fw.sp, bstart[:], bstart_d, r_ca, writes=[r_ca])
                fw.dma(fw.sp, pidx[:], pidx_d, r_ca, writes=[r_ca])
                NC3 = NT * 32
                Mb = sb(sa, "Mb", [128, NC3], BF16)
                rank = sb(sa, "rank", [128, NT, 32], F32)
                cnt = sb(sa, "cnt", [128, NT, 32], F32)
                sA = sb(sa, "sA", [128, NT, 32], F32)
                sB = sb(sa, "sB", [128, NT, 32], F32)
                cmp = sb(sa, "cmp", [128, NB, 32], F32)
                sm = sb(sa, "sm", [128, 8, 32], F32)
                ebf = sb(sa, "ebf", [128, NB], F32)
                eb2 = sb(sa, "eb2", [128, NB], F32)
                dfl = sb(sa, "dfl", [128, 2, NT], F32)
                Dv = fw.dve
                rr = [r_rt3]

                def dv(fn, extra=()):
                    fw.op(Dv, fn, reads=rr + list(extra), writes=rr)
                OH1f = OH1[:].rearrange("p t e -> p (t e)")
                OH2f = OH2[:].rearrange("p t e -> p (t e)")
                dv(lambda e: e.tensor_tensor(out=Mb[:], in0=OH1f, in1=OH2f, op=ALU.add), r_gates)
                rankf = rank[:].rearrange("p t e -> p (t e)")
                cntf = cnt[:].rearrange("p t e -> p (t e)")
                nch = (NC3 + 511) // 512
                for ch in range(nch):
                    c0 = ch * 512
                    c1 = min(NC3, c0 + 512)
                    w_ = c1 - c0
                    fw.op(fw.pe, lambda e: e.matmul(TA[:, 0:w_], tri[:], Mb[:, c0:c1], start=True, stop=True),
                          reads=[r_rt3, r_ca], writes=[r_TA])
                    fw.op(fw.pe, lambda e: e.matmul(TB[:, 0:w_], ones_bf[:], Mb[:, c0:c1], start=True, stop=True),
                          reads=[r_rt3, r_ones], writes=[r_TB])
                    fw.op(Dv, lambda e: e.tensor_copy(out=rankf[:, c0:c1], in_=TA[:, 0:w_]), reads=[r_TA], writes=rr)
                    fw.op(Dv, lambda e: e.tensor_copy(out=cntf[:, c0:c1], in_=TB[:, 0:w_]), reads=[r_TB], writes=rr)
                dv(lambda e: e.tensor_copy(out=sA[:], in_=cnt[:]))
                src, dst = sA, sB
                sft = 1
                while sft < NT:
                    dv(lambda e: e.tensor_tensor(out=dst[:, sft:, :], in0=src[:, sft:, :], in1=src[:, 0:NT - sft, :], op=ALU.add))
                    dv(lambda e: e.tensor_copy(out=dst[:, 0:sft, :], in_=src[:, 0:sft, :]))
                    src, dst = dst, src
                    sft *= 2
                incl = src
                other = dst
                tot = sm[:, 0, :]
                xm = sm[:, 1, :]
                padded = sm[:, 2, :]
                pA = sm[:, 3, :]
                pB = sm[:, 4, :]
                pstart = sm[:, 5, :]
                dv(lambda e: e.tensor_copy(out=tot, in_=incl[:, NT - 1, :]))
                dv(lambda e: e.tensor_scalar(out=xm, in0=tot, scalar1=1.0 / 128, scalar2=0.49609375, op0=ALU.mult, op1=ALU.add))
                dv(lambda e: e.tensor_scalar(out=xm, in0=xm, scalar1=8388608.0, scalar2=None, op0=ALU.add))
                dv(lambda e: e.tensor_scalar(out=xm, in0=xm, scalar1=-8388608.0, scalar2=None, op0=ALU.add))
                dv(lambda e: e.tensor_scalar(out=padded, in0=xm, scalar1=128.0, scalar2=None, op0=ALU.mult))
                dv(lambda e: e.tensor_copy(out=pA, in_=padded))
                ps_, pd_ = pA, pB
                sft = 1
                while sft < 32:
                    dv(lambda e: e.tensor_tensor(out=pd_[:, sft:], in0=ps_[:, sft:], in1=ps_[:, 0:32 - sft], op=ALU.add))
                    dv(lambda e: e.tensor_copy(out=pd_[:, 0:sft], in_=ps_[:, 0:sft]))
                    ps_, pd_ = pd_, ps_
                    sft *= 2
                pend = ps_
                dv(lambda e: e.tensor_tensor(out=pstart, in0=pend, in1=padded, op=ALU.subtract))
                dv(lambda e: e.tensor_tensor(out=other[:], in0=incl[:], in1=cnt[:], op=ALU.subtract))
                dv(lambda e: e.tensor_tensor(out=other[:], in0=other[:], in1=rank[:], op=ALU.add))
                dv(lambda e: e.tensor_tensor(out=other[:], in0=other[:], in1=pstart.unsqueeze(1).broadcast_to([128, NT, 32]), op=ALU.add))
                dest = other
                dv(lambda e: e.tensor_tensor(out=incl[:], in0=dest[:], in1=OH1[:], op=ALU.mult), r_gates)
                dv(lambda e: e.tensor_reduce(out=dfl[:, 0, :], in_=incl[:], axis=AX.X, op=ALU.add))
                dv(lambda e: e.tensor_tensor(out=incl[:], in0=dest[:], in1=OH2[:], op=ALU.mult), r_gates)
                dv(lambda e: e.tensor_reduce(out=dfl[:, 1, :], in_=incl[:], axis=AX.X, op=ALU.add))
                dv(lambda e: e.tensor_copy(out=d1i[:], in_=dfl[:, 0, :]))
                dv(lambda e: e.tensor_copy(out=d2i[:], in_=dfl[:, 1, :]))
                dv(lambda e: e.tensor_tensor(out=cmp[:], in0=pend.unsqueeze(1).broadcast_to([128, NB, 32]),
                                             in1=bstart[:].unsqueeze(2).broadcast_to([128, NB, 32]), op=ALU.is_le), [r_ca])
                dv(lambda e: e.tensor_reduce(out=ebf[:], in_=cmp[:], axis=AX.X, op=ALU.add))
                dv(lambda e: e.tensor_scalar(out=ebf[:], in0=ebf[:], scalar1=float(NE - 1), scalar2=None, op0=ALU.min))
                BIG = 1.0e6
                dv(lambda e: e.memset(eb2[:], 1.0))
                dv(lambda e: e.tensor_tensor(out=eb2[:, 2:], in0=ebf[:, 2:], in1=ebf[:, 0:NB - 2], op=ALU.not_equal))
                dv(lambda e: e.tensor_scalar(out=ebf[:], in0=ebf[:], scalar1=128.0, scalar2=pidx[:], op0=ALU.mult, op1=ALU.add), [r_ca])
                dv(lambda e: e.tensor_scalar(out=ebf[:], in0=ebf[:], scalar1=-BIG, scalar2=None, op0=ALU.add))
                dv(lambda e: e.tensor_tensor(out=ebf[:], in0=ebf[:], in1=eb2[:], op=ALU.mult))
                dv(lambda e: e.tensor_scalar(out=ebf[:], in0=ebf[:], scalar1=BIG, scalar2=None, op0=ALU.add))
                dv(lambda e: e.tensor_copy(out=idxw[:], in_=ebf[:]))
                fw.barrier()

            _bregs = {}

            def _breg(bound):
                if bound not in _bregs:
                    rg = nc.gpsimd.alloc_register(f"bnd{len(_bregs)}")
                    nc.gpsimd.reg_mov(rg, int(bound))
                    _bregs[bound] = rg
                return _bregs[bound]

            def indirect(out, out_off, in_, in_off, bound, sb_res, reads, writes):
                deps = []
                for r_ in reads:
                    if r_.last_w is not None:
                        deps.append(r_.last_w)
                for w_r in writes:
                    if w_r.last_w is not None:
                        deps.append(w_r.last_w)
                    deps.extend(w_r.readers)
                fw.pool.wait_tokens(deps)
                ent = fw.dma_reg.setdefault(sb_res.name, [None, 0])
                if ent[0] is None:
                    ent[0] = fw.new_sem(f"dma_{sb_res.name}")
                ins = nc.gpsimd.indirect_dma_start(
                    out=out, out_offset=(bass.IndirectOffsetOnAxis(ap=out_off, axis=0) if out_off is not None else None),
                    in_=in_, in_offset=(bass.IndirectOffsetOnAxis(ap=in_off, axis=0) if in_off is not None else None),
                    bounds_check=_breg(bound), oob_is_err=False)
                ent[1] += 16
                ins.then_inc(fw.sem_by_key[ent[0]], 16)
                tok = (ent[0], ent[1])
                for r_ in reads:
                    r_.readers.append(tok)
                for w_r in writes:
                    w_r.last_w = tok
                    w_r.readers = []

            with contextlib.ExitStack() as sbk:
                hb = [sb(sbk, f"hb{i}", [128, D], BF16) for i in range(3)]
                r_hb = [R(f"hb{i}") for i in range(3)]
                for i in range(NT):
                    s3 = i % 3
                    fw.dma(fw.sp, hb[s3][:], hn_d[i], r_hb[s3], writes=[r_hb[s3]])
                    indirect(xg[:, :], d1i[:, i:i + 1], hb[s3][:, :], None, PR - 1, r_hb[s3], [r_hb[s3], r_rt3], [])
                    indirect(xg[:, :], d2i[:, i:i + 1], hb[s3][:, :], None, PR - 1, r_hb[s3], [r_hb[s3], r_rt3], [])
                fw.barrier()
            wgv = w_gate[:, :]
            wuv = w_up[:, :]
            wdv = w_down[:, :]
            with contextlib.ExitStack() as sc:
                identb = sb(sc, "identb", [128, 128], BF16)
                r_idb = R("identb")
                fw.dma(fw.sp, identb[:], ident_bf_d, r_idb, writes=[r_idb])
                wg = [sb(sc, f"wg{i}", [128, 8 * DFF], BF16) for i in range(2)]
                wu = [sb(sc, f"wu{i}", [128, 8 * DFF], BF16) for i in range(2)]
                wd = [sb(sc, f"wd{i}", [128, 4 * D], BF16) for i in range(2)]
                r_wg = [R("wg0"), R("wg1")]
                r_wu = [R("wu0"), R("wu1")]
                r_wd = [R("wd0"), R("wd1")]
                xb = [sb(sc, f"xb{i}", [128, D], BF16) for i in range(2)]
                r_xb = [R("xb0"), R("xb1")]
                xgT = [sb(sc, f"xgT{i}", [128, 8, 128], BF16) for i in range(2)]
                r_xgT = [R("xgT0"), R("xgT1")]
                sgl = sb(sc, "sgl", [128, 512], F32)
                r_sgl = R("sgl")
                hidT = [sb(sc, f"hidT{i}", [128, 4, 128], BF16) for i in range(2)]
                r_hid = [R("hid0"), R("hid1")]
                yb = [sb(sc, f"yb{i}", [128, D], F32) for i in range(2)]
                r_yb = [R("yb0"), R("yb1")]
                Ts = [TA, TB]
                r_Ts = [r_TA, r_TB]
                Gs = [G0, G1]
                r_Gs = [r_G0, r_G1]
                for ws in range(2):
                    pass
                hidtok = [sb(sc, f"hidtok{i}", [128, DFF], BF16) for i in range(2)]
                r_hidtok = [R("hidtok0"), R("hidtok1")]
                sgl2 = [sgl, sb(sc, "sglb", [128, 512], F32)]
                r_sgl2 = [r_sgl, R("sglb")]
                xb3 = xb + [sb(sc, "xb2", [128, D], BF16)]
                r_xb3 = r_xb + [R("xb2")]
                xgT3 = xgT + [sb(sc, "xgT2", [128, 8, 128], BF16)]
                r_xgT3 = r_xgT + [R("xgT2")]
                psT3 = TA[:].bitcast(BF16).rearrange("p (c t) -> p c t", c=8)
                psH = TB[:, 0:256].bitcast(BF16).rearrange("p (c t) -> p c t", c=4)

                def gath_gu(b):
                    ws = b % 2
                    indirect(wg[ws][:, :], None, wgv, idxw[:, b:b + 1], NE * 128 - 1, r_wg[ws], [r_rt3], [r_wg[ws]])
                    indirect(wu[ws][:, :], None, wuv, idxw[:, b:b + 1], NE * 128 - 1, r_wu[ws], [r_rt3], [r_wu[ws]])

                def gath_d(b):
                    ws = b % 2
                    indirect(wd[ws][:, :], None, wdv, idxw[:, b:b + 1], NE * 128 - 1, r_wd[ws], [r_rt3], [r_wd[ws]])

                def preC(b):
                    s3 = b % 3
                    fw.dma(fw.sp, xb3[s3][:], xg[b * 128:(b + 1) * 128, :], r_xb3[s3], writes=[r_xb3[s3]])
                    xbv = xb3[s3][:].rearrange("p (q c) -> p c q", c=8)

                    def tr4(e):
                        ins = None
                        for c in range(8):
                            ins = e.transpose(psT3[:, c, :], xbv[:, c, :], identb[:])
                        return ins
                    fw.op(fw.pe, tr4, reads=[r_xb3[s3], r_idb], writes=[r_TA])
                    fw.op(fw.dve, lambda e: e.tensor_copy(out=xgT3[s3][:], in_=psT3), reads=[r_TA], writes=[r_xgT3[s3]])

                def stageG(b):
                    ws = b % 2
                    s3 = b % 3
                    Gb = Gs[ws]
                    wgs = wg[ws][:].rearrange("p (c f) -> p c f", c=8)
                    wus = wu[ws][:].rearrange("p (c f) -> p c f", c=8)

                    def mm_gu(e):
                        ins = None
                        for which, wv in ((0, wgs), (1, wus)):
                            for c in range(8):
                                ins = e.matmul(Gb[:, which, :], xgT3[s3][:, c, :], wv[:, c, :], start=(c == 0), stop=(c == 7))
                        return ins
                    fw.op(fw.pe, mm_gu, reads=[r_xgT3[s3], r_wg[ws], r_wu[ws]], writes=[r_Gs[ws]])
                    fw.op(fw.act, lambda e: e.activation(out=sgl2[ws][:], in_=Gb[:, 0, :], func=AF.Silu), reads=[r_Gs[ws]], writes=[r_sgl2[ws]])
                    fw.op(fw.dve, lambda e: e.tensor_tensor(out=hidtok[ws][:], in0=sgl2[ws][:], in1=Gb[:, 1, :], op=ALU.mult),
                          reads=[r_sgl2[ws], r_Gs[ws]], writes=[r_hidtok[ws]])

                def stageH(b):
                    ws = b % 2
                    wds = wd[ws][:].rearrange("p (c d) -> p c d", c=4)
                    hv = hidtok[ws][:].rearrange("p (q c) -> p c q", c=4)

                    def trH(e):
                        ins = None
                        for c in range(4):
                            ins = e.transpose(psH[:, c, :], hv[:, c, :], identb[:])
                        return ins
                    fw.op(fw.pe, trH, reads=[r_hidtok[ws], r_idb], writes=[r_TB])
                    fw.op(fw.dve, lambda e: e.tensor_copy(out=hidT[ws][:], in_=psH), reads=[r_TB], writes=[r_hid[ws]])

                def stageH2(b):
                    ws = b % 2
                    wds = wd[ws][:].rearrange("p (c d) -> p c d", c=4)

                    def mm_d(e):
                        ins = None
                        for cc in range(2):
                            for fc in range(4):
                                ins = e.matmul(Y0[:, cc, :], hidT[ws][:, fc, :], wds[:, fc, cc * 512:(cc + 1) * 512],
                                               start=(fc == 0), stop=(fc == 3))
                        return ins
                    fw.op(fw.pe, mm_d, reads=[r_hid[ws], r_wd[ws]], writes=[r_Y0])
                    fw.op(fw.act, lambda e: e.activation(out=yb[ws][:], in_=Y0[:].rearrange("p a b -> p (a b)"), func=AF.Copy),
                          reads=[r_Y0], writes=[r_yb[ws]])
                    fw.dma(fw.act, yg[b * 128:(b + 1) * 128, :], yb[ws][:], r_yb[ws], reads=[r_yb[ws]])

                for bb in (0, 1):
                    if bb < NB:
                        gath_gu(bb)
                        gath_d(bb)
                        preC(bb)
                stageG(0)
                for b in range(NB):
                    stageH(b)
                    if b + 2 < NB:
                        gath_gu(b + 2)
                        preC(b + 2)
                    stageH2(b)
                    if b + 1 < NB:
                        stageG(b + 1)
                    if b + 2 < NB:
                        gath_d(b + 2)
                fw.barrier()
            with contextlib.ExitStack() as sd:
                gfin = sb(sd, "gfin", [128, D], F32)
                fw.dma(fw.sp, gfin[:], gfin_b, r_gfin, writes=[r_gfin])
                y1 = [sb(sd, f"y1{i}", [128, D], F32) for i in range(2)]
                y2 = [sb(sd, f"y2{i}", [128, D], F32) for i in range(2)]
                r_y1 = [R("y10"), R("y11")]
                r_y2 = [R("y20"), R("y21")]
                hr = [sb(sd, f"hr{i}", [128, D], F32) for i in range(2)]
                r_hr = [R("hr0"), R("hr1")]
                yt = [sb(sd, f"yt{i}", [128, D], F32) for i in range(2)]
                r_yt = [R("yt0"), R("yt1")]
                junk3 = sb(sd, "junk3", [128, D], BF16)
                r_junk3 = R("junk3")
                ss3 = [sb(sd, f"ss3{i}", [128, 1], F32) for i in range(2)]
                r_ss3 = [R("ss30"), R("ss31")]
                def loadD(tg):
                    sl = tg % 2
                    indirect(y1[sl][:, :], None, yg[:, :], d1i[:, tg:tg + 1], PR - 1, r_y1[sl], [r_rt3], [r_y1[sl]])
                    indirect(y2[sl][:, :], None, yg[:, :], d2i[:, tg:tg + 1], PR - 1, r_y2[sl], [r_rt3], [r_y2[sl]])
                    fw.dma(fw.sp, hr[sl][:], h_d[tg], r_hr[sl], writes=[r_hr[sl]])

                def compD(tg):
                    sl = tg % 2
                    fw.op(fw.dve, lambda e: e.scalar_tensor_tensor(out=hr[sl][:], in0=y1[sl][:], scalar=wts[:, tg, 0:1], in1=hr[sl][:],
                                                                   op0=ALU.mult, op1=ALU.add),
                          reads=[r_y1[sl], r_hr[sl], r_gates[tg]], writes=[r_hr[sl]])
                    fw.op(fw.dve, lambda e: e.scalar_tensor_tensor(out=hr[sl][:], in0=y2[sl][:], scalar=wts[:, tg, 1:2], in1=hr[sl][:],
                                                                   op0=ALU.mult, op1=ALU.add),
                          reads=[r_y2[sl], r_hr[sl], r_gates[tg]], writes=[r_hr[sl]])
                    fw.op(fw.act, lambda e: e.activation(out=junk3[:], in_=hr[sl][:], func=AF.Square, accum_out=ss3[sl][:]),
                          reads=[r_hr[sl]], writes=[r_junk3, r_ss3[sl]])
                    fw.op(fw.act, lambda e: e.activation(out=ss3[sl][:], in_=ss3[sl][:], func=AF.Ln, bias=eps_t[:], scale=1.0 / D),
                          reads=[r_ss3[sl], r_ones], writes=[r_ss3[sl]])
                    fw.op(fw.act, lambda e: e.activation(out=ss3[sl][:], in_=ss3[sl][:], func=AF.Exp, scale=-0.5), reads=[r_ss3[sl]], writes=[r_ss3[sl]])
                    fw.op(fw.dve, lambda e: e.scalar_tensor_tensor(out=yt[sl][:], in0=hr[sl][:], scalar=ss3[sl][:], in1=gfin[:],
                                                                   op0=ALU.mult, op1=ALU.mult),
                          reads=[r_hr[sl], r_ss3[sl], r_gfin], writes=[r_yt[sl]])
                    fw.dma(fw.act, y[tg * 128:(tg + 1) * 128, :], yt[sl][:], r_yt[sl], reads=[r_yt[sl]])

                loadD(0)
                for tg in range(NT):
                    if tg + 1 < NT:
                        loadD(tg + 1)
                    compD(tg)
                fw.barrier()
    return nc


def _const_tables(max_n):
    max_nt = max_n // 128
    inv_freq = (np.float32(10000.0) ** (-np.arange(0, 64, 2, dtype=np.float32) / np.float32(64))).astype(np.float32)
    pos = np.arange(max_n, dtype=np.float32)
    ang = (pos[:, None] * inv_freq[None, :]).astype(np.float32)
    cos = np.cos(ang).astype(np.float32).reshape(max_nt, 128, 32)
    sin = np.sin(ang).astype(np.float32).reshape(max_nt, 128, 32)
    cs = np.stack([cos, sin], axis=2)
    cs = np.ascontiguousarray(cs.transpose(1, 0, 2, 3))
    invc = np.zeros((3, 4, 128), np.float32)
    for g, w in enumerate((2, 4, 8, 16)):
        half = w // 2
        t = np.arange(128)
        invc[0, g] = 1.0 / (np.minimum(t + half, 10 ** 9) - np.maximum(t - half, 0))
        invc[1, g] = 1.0 / w
        invc[2, g] = 1.0 / (np.minimum(t + half, 128) - (t - half))
    invc = np.ascontiguousarray(np.broadcast_to(invc[None], (128, 3, 4, 128))).astype(np.float32)
    return cs, invc


_PROG_CACHE = {}


def run_cores(core_seqs, weights, n_cores, sg_tok):
    seq_lens = tuple(int(s.shape[0]) for s in core_seqs[0])
    key = (seq_lens, sg_tok)
    if key not in _PROG_CACHE:
        _PROG_CACHE[key] = build_program(list(seq_lens), sg_tok)
    nc = _PROG_CACHE[key]
    f32 = np.float32
    W = {k: np.asarray(v, dtype=f32) for k, v in weights.items()}
    cs, invc = _const_tables(max(seq_lens))
    NBh = (2 * sum(seq_lens)) // 128 + NE
    shared = {
        "w_in": np.ascontiguousarray(W["w_in"][0]),
        "w_out": np.ascontiguousarray(W["w_out"][0]),
        "w_pool": np.ascontiguousarray(W["w_pool"][0]),
        "w_gate": np.ascontiguousarray(W["w_gate"][0]).reshape(NE * 128, 8 * DFF),
        "w_up": np.ascontiguousarray(W["w_up"][0]).reshape(NE * 128, 8 * DFF),
        "w_down": np.ascontiguousarray(W["w_down"][0]).reshape(NE * 128, 4 * D),
        "w_r": np.ascontiguousarray(np.concatenate([W["w_router_group"][0], W["w_router_expert"][0]], axis=1)),
        "gmix_t": np.ascontiguousarray(W["g_mix"][0].reshape(8, 128).T),
        "wos_t": np.ascontiguousarray(np.concatenate([W["pool_scale"][0].reshape(4, 128).T,
                                                      np.repeat(W["subln_g"][0][:, None], 4, axis=1)], axis=1)),
        "gffn_b": np.ascontiguousarray(np.broadcast_to(W["g_ffn"][0][None, :], (128, D))),
        "gfin_b": np.ascontiguousarray(np.broadcast_to(W["g_final"][None, :], (128, D))),
        "bias_b": np.ascontiguousarray(np.broadcast_to(
            np.concatenate([W["b_router_group"][0], W["b_router_expert"][0]])[None, :], (128, 36))),
        "lam_b": np.ascontiguousarray(np.broadcast_to(
            np.stack([W["lambda_q1"][0], W["lambda_k1"][0], W["lambda_q2"][0], W["lambda_k2"][0]])[None], (128, 4, 64))),
        "ident_bf": np.eye(128, dtype=f32).astype(ml_dtypes.bfloat16),
        "ident_f": np.eye(128, dtype=f32),
        "cs_tab": cs,
        "invc": invc,
        "tri_bf": np.triu(np.ones((128, 128), f32), k=1).astype(ml_dtypes.bfloat16),
        "bstart": np.ascontiguousarray(np.broadcast_to((np.arange(NBh, dtype=f32) * 128.0)[None, :], (128, NBh))),
        "pidx": np.arange(128, dtype=f32).reshape(128, 1),
    }
    in_maps = []
    for c in range(n_cores):
        m = dict(shared)
        m["x"] = np.ascontiguousarray(np.concatenate([np.asarray(s, dtype=f32) for s in core_seqs[c]], axis=0))
        in_maps.append(m)
    res = run_bass_kernel_spmd(nc, in_maps, core_ids=list(range(n_cores)))
    outs = []
    for c in range(n_cores):
        yc = np.asarray(res.results[c]["y"])
        o = []
        off = 0
        for n in seq_lens:
            o.append(yc[off:off + n])
            off += n
        outs.append(o)
    return outs


def kernel(x_prompt, x_sample, **weights):
    n_cores = 8
    xp = np.asarray(x_prompt)
    xsm = np.asarray(x_sample)
    pp = xp.shape[0] // n_cores
    ps = xsm.shape[0] // n_cores
    core_seqs = []
    for c in range(n_cores):
        core_seqs.append([xp[c * pp + i] for i in range(pp)] + [xsm[c * ps + i] for i in range(ps)])
    outs = run_cores(core_seqs, weights, n_cores, sg_tok=2048)
    yp = np.empty(xp.shape, np.float32)
    ys = np.empty(xsm.shape, np.float32)
    for c in range(n_cores):
        for i in range(pp):
            yp[c * pp + i] = outs[c][i]
        for i in range(ps):
            ys[c * ps + i] = outs[c][pp + i]
    return (yp, ys)
```

```python
import contextlib
import numpy as np
import ml_dtypes
import concourse.bass as bass
import concourse.mybir as mybir
from concourse.bass_utils import run_bass_kernel_spmd

F32 = mybir.dt.float32
BF16 = mybir.dt.bfloat16
ALU = mybir.AluOpType
AF = mybir.ActivationFunctionType
AX = mybir.AxisListType

D = 1024
NE = 32
DFF = 512
EPS = 1e-6
SEM_WRAP = 30000
LAM_INIT = 0.2


class Res:
    __slots__ = ("name", "last_w", "readers", "dma_sem", "dma_cnt")

    def __init__(self, name):
        self.name = name
        self.last_w = None
        self.readers = []
        self.dma_sem = None
        self.dma_cnt = 0


class Eng:
    def __init__(self, fw, name, obj, is_pe=False):
        self.fw = fw
        self.name = name
        self.obj = obj
        self.is_pe = is_pe
        self.n = 0
        self.sems = []
        self.waited = {}

    def cur_sem(self):
        idx = self.n // SEM_WRAP
        while len(self.sems) <= idx:
            self.sems.append(self.fw.new_sem(f"tl_{self.name}_{len(self.sems)}"))
        return self.sems[idx]

    def wait_tokens(self, toks):
        best = {}
        for (sk, v) in toks:
            if v > best.get(sk, 0):
                best[sk] = v
        for sk, v in best.items():
            if self.waited.get(sk, 0) >= v:
                continue
            self.waited[sk] = v
            self.obj.wait_ge(self.fw.sem_by_key[sk], v)


class FW:
    def __init__(self, nc, stack):
        self.nc = nc
        self.sem_by_key = {}
        self._stack = stack
        self.all_res = []
        self.dma_reg = {}
        self.pe = Eng(self, "pe", nc.tensor, is_pe=True)
        self.act = Eng(self, "act", nc.scalar)
        self.dve = Eng(self, "dve", nc.vector)
        self.pool = Eng(self, "pool", nc.gpsimd)
        self.sp = Eng(self, "sp", nc.sync)
        self.engs = [self.pe, self.act, self.dve, self.pool, self.sp]

    def res(self, name):
        r = Res(name)
        self.all_res.append(r)
        return r

    def new_sem(self, name):
        h = self._stack.enter_context(self.nc.semaphore(name))
        key = len(self.sem_by_key)
        self.sem_by_key[key] = h
        return key

    def op(self, eng, fn, reads=(), writes=()):
        deps = []
        own = set(eng.sems)
        for r in reads:
            if r.last_w is not None:
                deps.append(r.last_w)
        for w in writes:
            if w.last_w is not None:
                deps.append(w.last_w)
            for t in w.readers:
                if t[0] in own:
                    continue
                deps.append(t)
        if eng.is_pe:
            deps = [t for t in deps if t[0] not in own]
        eng.wait_tokens(deps)
        ins = fn(eng.obj)
        sk = eng.cur_sem()
        val = (eng.n % SEM_WRAP) + 1
        eng.n += 1
        ins.then_inc(self.sem_by_key[sk], 1)
        tok = (sk, val)
        for r in reads:
            r.readers.append(tok)
        for w in writes:
            w.last_w = tok
            w.readers = []
        return tok

    def dma(self, q, out, in_, sb_res, reads=(), writes=()):
        deps = []
        for r in reads:
            if r.last_w is not None:
                deps.append(r.last_w)
        for w in writes:
            if w.last_w is not None:
                deps.append(w.last_w)
            deps.extend(w.readers)
        q.wait_tokens(deps)
        ent = self.dma_reg.setdefault(sb_res.name, [None, 0])
        if ent[0] is None:
            ent[0] = self.new_sem(f"dma_{sb_res.name}")
        ins = q.obj.dma_start(out=out, in_=in_)
        ent[1] += 16
        ins.then_inc(self.sem_by_key[ent[0]], 16)
        tok = (ent[0], ent[1])
        for r in reads:
            r.readers.append(tok)
        for w in writes:
            w.last_w = tok
            w.readers = []
        return tok

    def all_tokens(self):
        toks = []
        for r in self.all_res:
            if r.last_w is not None:
                toks.append(r.last_w)
            toks.extend(r.readers)
        for e in self.engs:
            if e.n > 0:
                idx = (e.n - 1) // SEM_WRAP
                toks.append((e.sems[idx], ((e.n - 1) % SEM_WRAP) + 1))
        return toks

    def barrier(self):
        toks = self.all_tokens()
        for e in self.engs:
            e.wait_tokens(toks)


import os as _os
_DBG = _os.environ.get("KDBG", "full")


def build_program(seq_lens, sg_tok):
    T = sum(seq_lens)
    NT = T // 128
    NG = T // 512
    max_nt = max(seq_lens) // 128
    assert all(n % 512 == 0 for n in seq_lens)
    assert T % sg_tok == 0 and sg_tok % 512 == 0
    nc = bass.Bass("TRN2", target_bir_lowering=False)

    def din(name, shape, dt=F32):
        return nc.dram_tensor(name, list(shape), dt, kind="ExternalInput").ap()

    x = din("x", [T, D])
    w_in = din("w_in", [D, 2048])
    w_out = din("w_out", [D, D])
    w_pool = din("w_pool", [4, 128, 128])
    w_gate = din("w_gate", [NE * 128, 8 * DFF])
    w_up = din("w_up", [NE * 128, 8 * DFF])
    w_down = din("w_down", [NE * 128, 4 * D])
    w_r = din("w_r", [D, 36])
    gmix_t = din("gmix_t", [128, 8])
    wos_t = din("wos_t", [128, 8])
    gffn_b = din("gffn_b", [128, D])
    gfin_b = din("gfin_b", [128, D])
    bias_b = din("bias_b", [128, 36])
    lam_b = din("lam_b", [128, 4, 64])
    ident_bf_d = din("ident_bf", [128, 128], BF16)
    ident_f_d = din("ident_f", [128, 128])
    cs_tab_d = din("cs_tab", [128, max_nt, 2, 32])
    invc_d = din("invc", [128, 3, 4, 128])
    y = nc.dram_tensor("y", [T, D], F32, kind="ExternalOutput").ap()
    qd = nc.dram_tensor("qd", [NG, 128, 4, 512], BF16, kind="Internal").ap()
    pd = nc.dram_tensor("pd", [NG, 128, 4, 4, 128], BF16, kind="Internal").ap()
    h_d = nc.dram_tensor("h_d", [NT, 128, D], F32, kind="Internal").ap()
    hn_d = nc.dram_tensor("hn_d", [NT, 128, D], BF16, kind="Internal").ap()
    NB = (2 * T) // 128 + NE
    PR = NB * 128
    xg = nc.dram_tensor("xg", [PR, D], BF16, kind="Internal").ap()
    yg = nc.dram_tensor("yg", [PR, D], F32, kind="Internal").ap()
    tri_d = din("tri_bf", [128, 128], BF16)
    bstart_d = din("bstart", [128, NB])
    pidx_d = din("pidx", [128, 1])

    with contextlib.ExitStack() as st0:
        fw = FW(nc, st0)
        R = fw.res

        sfx = [""]

        def sb(st, name, shape, dt):
            return st.enter_context(nc.sbuf_tensor("s_" + name + sfx[0], list(shape), dt))

        lg_all = sb(st0, "lg_all", [128, NT, 36], F32)
        ones_bf = sb(st0, "ones_bf", [128, 128], BF16)
        ones_f = sb(st0, "ones_f", [128, 128], F32)
        r_gates = [R(f"gates{i}") for i in range(NT)]
        r_gfin = R("gfin")
        r_ones = R("ones")
        eps_t = sb(st0, "eps_t", [128, 1], F32)
        fw.op(fw.dve, lambda e: e.memset(eps_t[:], EPS), writes=[r_ones])
        fw.op(fw.dve, lambda e: e.memset(ones_f[:], 1.0), writes=[r_ones])
        fw.op(fw.dve, lambda e: e.tensor_copy(out=ones_bf[:], in_=ones_f[:]), reads=[r_ones], writes=[r_ones])

        with contextlib.ExitStack() as st:
            S0 = st.enter_context(nc.psum_tensor("S0", [128, 2, 512], F32))
            S1 = st.enter_context(nc.psum_tensor("S1", [128, 2, 512], F32))
            PO = st.enter_context(nc.psum_tensor("PO", [128, 2, 512], F32))
            PL = st.enter_context(nc.psum_tensor("PL", [128, 2, 512], F32))
            r_S0, r_S1, r_PO, r_PL = R("S0"), R("S1"), R("PO"), R("PL")
            r_S0a, r_S0b, r_S1a, r_S1b = R("S0a"), R("S0b"), R("S1a"), R("S1b")
            r_POa, r_POb, r_PLa, r_PLb = R("POa"), R("POb"), R("PLa"), R("PLb")
            r_psr = [R("psr0"), R("psr1")]

            Win = sb(st, "Win", [128, 8, 2048], BF16)
            Wout = sb(st, "Wout", [128, 8, 1024], BF16)
            Wp = sb(st, "Wp", [128, 4, 128], BF16)
            Wr = sb(st, "Wr", [128, 8, 36], F32)
            gmix = sb(st, "gmix", [128, 8], F32)
            wos = sb(st, "wos", [128, 8], F32)
            gffn = sb(st, "gffn", [128, D], F32)
            biasb = sb(st, "biasb", [128, 36], F32)
            lamt = sb(st, "lamt", [128, 4, 64], F32)
            lamp = sb(st, "lamp", [128, 2, 64], F32)
            lams = sb(st, "lams", [128, 2], F32)
            neglam = sb(st, "neglam", [128, 1], F32)
            ident_bf = sb(st, "ident_bf", [128, 128], BF16)
            ident_f = sb(st, "ident_f", [128, 128], F32)
            invc = sb(st, "invc", [128, 3, 4, 128], F32)
            st_w = contextlib.ExitStack()
            wstage = sb(st_w, "wstage", [128, 2048], F32)
            r_Win, r_Wout, r_Wp, r_Wr, r_wstage = R("Win"), R("Wout"), R("Wp"), R("Wr"), R("wstage")
            r_c = R("consts")
            r_lam = R("lam")
            for (t_sb, t_dr) in ((gmix, gmix_t), (wos, wos_t), (gffn, gffn_b), (biasb, bias_b),
                                 (ident_bf, ident_bf_d), (ident_f, ident_f_d), (invc, invc_d)):
                fw.dma(fw.sp, t_sb[:], t_dr, r_c, writes=[r_c])
            fw.dma(fw.sp, lamt[:], lam_b, r_lam, writes=[r_lam])
            fw.dma(fw.sp, Wr[:], w_r.rearrange("(c p) j -> p c j", p=128), r_Wr, writes=[r_Wr])
            fw.dma(fw.pool, Wp[:], w_pool.rearrange("g c e -> c g e"), r_Wp, writes=[r_Wp])
            for c in range(8):
                fw.dma(fw.sp, wstage[:], w_in[c * 128:(c + 1) * 128, :], r_wstage, writes=[r_wstage])
                fw.op(fw.act, lambda e, c=c: e.activation(out=Win[:, c, :], in_=wstage[:], func=AF.Copy,
                                                          scale=gmix[:, c:c + 1]),
                      reads=[r_wstage, r_c], writes=[r_Win])
            for c in range(8):
                fw.dma(fw.sp, wstage[:, 0:1024], w_out[c * 128:(c + 1) * 128, :], r_wstage, writes=[r_wstage])
                fw.op(fw.act, lambda e, c=c: e.activation(out=Wout[:, c, :], in_=wstage[:, 0:1024], func=AF.Copy,
                                                          scale=wos[:, c:c + 1]),
                      reads=[r_wstage, r_c], writes=[r_Wout])
            lam4 = lamt[:].rearrange("p (a b) d -> p a b d", b=2)
            fw.op(fw.dve, lambda e: e.tensor_tensor(out=lamp[:], in0=lam4[:, :, 0, :], in1=lam4[:, :, 1, :], op=ALU.mult),
                  reads=[r_lam], writes=[r_lam])
            fw.op(fw.dve, lambda e: e.tensor_reduce(out=lams[:], in_=lamp[:], axis=AX.X, op=ALU.add),
                  reads=[r_lam], writes=[r_lam])
            fw.op(fw.act, lambda e: e.activation(out=lams[:], in_=lams[:], func=AF.Exp), reads=[r_lam], writes=[r_lam])
            fw.op(fw.dve, lambda e: e.tensor_tensor(out=neglam[:], in0=lams[:, 1:2], in1=lams[:, 0:1], op=ALU.subtract),
                  reads=[r_lam], writes=[r_lam])
            fw.op(fw.dve, lambda e: e.tensor_scalar(out=neglam[:], in0=neglam[:], scalar1=-LAM_INIT, scalar2=None,
                                                    op0=ALU.add), reads=[r_lam], writes=[r_lam])

            fw.barrier()
            st_w.close()
            max_n = max(seq_lens)
            kT = sb(st, "kT", [128, 4, max_n], BF16)
            V = sb(st, "V", [128, max_n // 128, 512], BF16)
            r_kT, r_V = R("kT"), R("V")


            def rstd_chain(src, dst, res_list, inv_n):
                fw.op(fw.act, lambda e: e.activation(out=dst, in_=src, func=AF.Ln, bias=eps_t[:], scale=inv_n),
                      reads=res_list + [r_ones], writes=res_list)
                fw.op(fw.act, lambda e: e.activation(out=dst, in_=dst, func=AF.Exp, scale=-0.5),
                      reads=res_list, writes=res_list)

            psT = PO[:, 0, :].bitcast(BF16).rearrange("p (c t) -> p c t", c=8)
            psT2 = PO[:, 1, :].bitcast(BF16).rearrange("p (c t) -> p c t", c=8)
            ps_qk = S0[:].rearrange("p a (b d) -> p (a b) d", d=32)
            ps_qk4 = S0[:].rearrange("p a (b h d) -> p (a b) h d", h=2, d=32)
            ps_v = S1[:, 0, :]
            ps_p = S1[:, 1, :].rearrange("p (g t) -> p g t", g=4)
            ps_po = PL[:, 0, :].rearrange("p (g t) -> p g t", g=4)

            tok0 = 0
            g0 = 0
            for si, N in enumerate(seq_lens):
                nt = N // 128
                ng = N // 512

                sfx[0] = f"_s{si}"
                st1 = contextlib.ExitStack()
                cs_tab = sb(st1, "cs_tab", [128, max_nt, 2, 32], F32)
                r_cs = R("cs_tab")
                fw.dma(fw.sp, cs_tab[:], cs_tab_d, r_cs, writes=[r_cs])
                xt = [sb(st1, f"xt{i}", [128, D], F32) for i in range(2)]
                r_xt = [R(f"xt{i}") for i in range(2)]
                junk = sb(st1, "junk", [128, D], BF16)
                r_junk = R("junk")
                ss = [sb(st1, f"ss{i}", [128, 1], F32) for i in range(2)]
                r_ss = [R(f"ss{i}") for i in range(2)]
                xs = [sb(st1, f"xs{i}", [128, D], BF16) for i in range(2)]
                r_xs = [R(f"xs{i}") for i in range(2)]
                xnT = [sb(st1, f"xnT{i}", [128, 8, 128], BF16) for i in range(2)]
                r_xnT = [R(f"xnT{i}") for i in range(2)]
                rp = [sb(st1, f"rp{i}", [128, 16, 32], F32) for i in range(4)]
                r_rp = R("rp")
                qkr = sb(st1, "qkr", [128, 16, 2, 32], BF16)
                r_qkr = R("qkr")
                qst = [sb(st1, f"qst{i}", [128, 4, 128], BF16) for i in range(2)]
                r_qst = [R(f"qst{i}") for i in range(2)]
                pst = [sb(st1, f"pst{i}", [128, 4, 128], BF16) for i in range(2)]
                r_pst = [R(f"pst{i}") for i in range(2)]
                zpt = [sb(st1, f"zpt{i}", [128, 4, 128], F32) for i in range(3)]
                r_zpt = [R(f"zpt{i}") for i in range(3)]
                ZW = sb(st1, "ZW", [128, 4, 144], F32)
                za = sb(st1, "za", [128, 4, 144], F32)
                zb = sb(st1, "zb", [128, 4, 144], F32)
                zc = sb(st1, "zc", [128, 4, 144], F32)
                zd = sb(st1, "zd", [128, 4, 144], F32)
                pw = sb(st1, "pw", [128, 4, 128], F32)
                pooled2 = [sb(st1, f"pooled{k}", [128, 4, 128], BF16) for k in range(2)]
                r_pm = R("poolmix")
                r_pooled2 = [R("pooled0"), R("pooled1")]

                def pool_stage(i):
                    cur = zpt[i % 3]
                    deps_r = [r_zpt[i % 3]]
                    if i > 0:
                        prev = zpt[(i - 1) % 3]
                        deps_r.append(r_zpt[(i - 1) % 3])
                        fw.op(fw.pool, lambda e: e.tensor_copy(out=ZW[:, :, 0:8], in_=prev[:, :, 120:128]),
                              reads=deps_r, writes=[r_pm])
                    else:
                        fw.op(fw.pool, lambda e: e.memset(ZW[:, :, 0:8], 0.0), writes=[r_pm])
                    fw.op(fw.pool, lambda e: e.tensor_copy(out=ZW[:, :, 8:136], in_=cur[:]), reads=deps_r, writes=[r_pm])
                    if i < nt - 1:
                        nxt = zpt[(i + 1) % 3]
                        fw.op(fw.pool, lambda e: e.tensor_copy(out=ZW[:, :, 136:144], in_=nxt[:, :, 0:8]),
                              reads=[r_zpt[(i + 1) % 3]], writes=[r_pm])
                    else:
                        fw.op(fw.pool, lambda e: e.memset(ZW[:, :, 136:144], 0.0), writes=[r_pm])
                    P = fw.pool
                    fw.op(P, lambda e: e.tensor_tensor(out=za[:, :, 0:143], in0=ZW[:, :, 0:143], in1=ZW[:, :, 1:144], op=ALU.add),
                          reads=[r_pm], writes=[r_pm])
                    fw.op(P, lambda e: e.tensor_tensor(out=zb[:, :, 0:141], in0=za[:, :, 0:141], in1=za[:, :, 2:143], op=ALU.add),
                          reads=[r_pm], writes=[r_pm])
                    fw.op(P, lambda e: e.tensor_tensor(out=zc[:, :, 0:137], in0=zb[:, :, 0:137], in1=zb[:, :, 4:141], op=ALU.add),
                          reads=[r_pm], writes=[r_pm])
                    fw.op(P, lambda e: e.tensor_tensor(out=zd[:, :, 0:129], in0=zc[:, :, 0:129], in1=zc[:, :, 8:137], op=ALU.add),
                          reads=[r_pm], writes=[r_pm])
                    kind = 0 if i == 0 else (2 if i == nt - 1 else 1)
                    srcs = [za[:, 0, 7:135], zb[:, 1, 6:134], zc[:, 2, 4:132], zd[:, 3, 0:128]]
                    for g in range(4):
                        fw.op(P, lambda e, g=g: e.tensor_tensor(out=pw[:, g, :], in0=srcs[g], in1=invc[:, kind, g, :], op=ALU.mult),
                              reads=[r_pm, r_c], writes=[r_pm])
                    fw.op(P, lambda e: e.tensor_tensor(out=pooled2[i % 2][:], in0=pw[:], in1=cur[:], op=ALU.subtract),
                          reads=[r_pm, r_zpt[i % 3]], writes=[r_pooled2[i % 2]])

                def pool_mm(i):
                    pooled = pooled2[i % 2]
                    r_pooled = r_pooled2[i % 2]

                    def mm_pool(e):
                        ins = None
                        for g in range(4):
                            ins = e.matmul(ps_po[:, g, :], Wp[:, g, :], pooled[:, g, :], start=True, stop=True)
                        return ins
                    fw.op(fw.pe, mm_pool, reads=[r_pooled, r_Wp], writes=[r_PLa])
                    sl = i % 2
                    fw.op(fw.act, lambda e: e.activation(out=pst[sl][:], in_=ps_po, func=AF.Copy),
                          reads=[r_PLa], writes=[r_pst[sl]])
                    g_idx = g0 + i // 4
                    fw.dma(fw.sp, pd[g_idx, :, i % 4, :, :], pst[sl][:], r_pst[sl], reads=[r_pst[sl]])

                def stageA_load(i):
                    sl = i % 2
                    fw.dma(fw.sp, xt[sl][:], x[tok0 + i * 128: tok0 + (i + 1) * 128, :], r_xt[sl], writes=[r_xt[sl]])

                def stageA(i):
                    sl = i % 2
                    fw.op(fw.act, lambda e: e.activation(out=junk[:], in_=xt[sl][:], func=AF.Square, accum_out=ss[sl][:]),
                          reads=[r_xt[sl]], writes=[r_junk, r_ss[sl]])
                    rstd_chain(ss[sl][:], ss[sl][:], [r_ss[sl]], 1.0 / D)
                    fw.op(fw.act, lambda e: e.activation(out=xs[sl][:], in_=xt[sl][:], func=AF.Copy, scale=ss[sl][:]),
                          reads=[r_xt[sl], r_ss[sl]], writes=[r_xs[sl]])

                    def tr1(e):
                        ins = None
                        for c in range(8):
                            ins = e.transpose(psT[:, c, :], xs[sl][:, c * 128:(c + 1) * 128], ident_bf[:])
                        return ins
                    fw.op(fw.pe, tr1, reads=[r_xs[sl], r_c], writes=[r_POa])
                    fw.op(fw.dve, lambda e: e.tensor_copy(out=xnT[sl][:], in_=psT), reads=[r_POa], writes=[r_xnT[sl]])

                def stageB(i):
                    sl = i % 2

                    def mm_qkv(e):
                        ins = None
                        for j, dst in enumerate((S0[:, 0, :], S0[:, 1, :], ps_v)):
                            for c in range(8):
                                ins = e.matmul(dst, xnT[sl][:, c, :], Win[:, c, 512 + j * 512: 1024 + j * 512],
                                               start=(c == 0), stop=(c == 7))
                        for g in range(4):
                            for c in range(8):
                                ins = e.matmul(ps_p[:, g, :], Win[:, c, g * 128:(g + 1) * 128], xnT[sl][:, c, :],
                                               start=(c == 0), stop=(c == 7))
                        return ins
                    fw.op(fw.pe, mm_qkv, reads=[r_xnT[sl], r_Win], writes=[r_S0a, r_S0b, r_S1a, r_S1b])
                    fw.op(fw.act, lambda e: e.activation(out=V[:, i, :], in_=ps_v, func=AF.Copy), reads=[r_S1a], writes=[r_V])
                    fw.op(fw.act, lambda e: e.activation(out=zpt[i % 3][:], in_=ps_p, func=AF.Copy),
                          reads=[r_S1b], writes=[r_zpt[i % 3]])
                    cosb = cs_tab[:, i, 0, :].unsqueeze(1).broadcast_to([128, 16, 32])
                    sinb = cs_tab[:, i, 1, :].unsqueeze(1).broadcast_to([128, 16, 32])
                    x1 = ps_qk4[:, :, 0, :]
                    x2 = ps_qk4[:, :, 1, :]
                    fw.op(fw.dve, lambda e: e.tensor_tensor(out=rp[0][:], in0=x1, in1=cosb, op=ALU.mult), reads=[r_S0a, r_S0b, r_cs], writes=[r_rp])
                    fw.op(fw.dve, lambda e: e.tensor_tensor(out=rp[1][:], in0=x2, in1=sinb, op=ALU.mult), reads=[r_S0a, r_S0b, r_cs], writes=[r_rp])
                    fw.op(fw.dve, lambda e: e.tensor_tensor(out=rp[2][:], in0=x2, in1=cosb, op=ALU.mult), reads=[r_S0a, r_S0b, r_cs], writes=[r_rp])
                    fw.op(fw.dve, lambda e: e.tensor_tensor(out=rp[3][:], in0=x1, in1=sinb, op=ALU.mult), reads=[r_S0a, r_S0b, r_cs], writes=[r_rp])
                    fw.op(fw.dve, lambda e: e.tensor_tensor(out=qkr[:, :, 0, :], in0=rp[0][:], in1=rp[1][:], op=ALU.subtract), reads=[r_rp], writes=[r_qkr])
                    fw.op(fw.dve, lambda e: e.tensor_tensor(out=qkr[:, :, 1, :], in0=rp[2][:], in1=rp[3][:], op=ALU.add), reads=[r_rp], writes=[r_qkr])

                def stageB2(i):
                    sl = i % 2
                    qkr_f = qkr[:].rearrange("p a h d -> p (a h d)")

                    def tr2(e):
                        ins = None
                        for c in range(8):
                            ins = e.transpose(psT2[:, c, :], qkr_f[:, c * 128:(c + 1) * 128], ident_bf[:])
                        return ins
                    fw.op(fw.pe, tr2, reads=[r_qkr, r_c], writes=[r_POb])
                    fw.op(fw.dve, lambda e: e.tensor_copy(out=kT[:, :, i * 128:(i + 1) * 128], in_=psT2[:, 4:8, :]),
                          reads=[r_POb], writes=[r_kT])
                    fw.op(fw.dve, lambda e: e.tensor_copy(out=qst[sl][:], in_=psT2[:, 0:4, :]), reads=[r_POb], writes=[r_qst[sl]])
                    fw.dma(fw.sp, qd[g0 + i // 4, :, :, (i % 4) * 128:(i % 4 + 1) * 128], qst[sl][:], r_qst[sl], reads=[r_qst[sl]])

                stageA_load(0)
                stageA_load(1)
                stageA(0)
                stageA(1)
                for i in range(nt):
                    if i + 2 < nt:
                        stageA_load(i + 2)
                    stageB(i)
                    if i + 2 < nt:
                        stageA(i + 2)
                    stageB2(i)
                    if i >= 1:
                        pool_stage(i - 1)
                    if i >= 2:
                        pool_mm(i - 2)
                pool_stage(nt - 1)
                pool_mm(nt - 2)
                pool_mm(nt - 1)
                r_qd = R("qd_seq")
                fw.barrier()

                st1.close()
                st2 = contextlib.ExitStack()
                qpad = sb(st2, "qpad", [128, 4, 2, 512], BF16)
                r_qpad = R("qpad")
                poolT = sb(st2, "poolT", [128, 4, 4, 128], BF16)
                r_poolT = R("poolT")
                pT = [sb(st2, f"pT{i}", [128, 2, 512], BF16) for i in range(4)]
                r_pT = [R(f"pT{i}") for i in range(4)]
                pTs = [sb(st2, f"pTs{i}", [128, 2, 512], BF16) for i in range(2)]
                r_pTs = [R(f"pTs{i}") for i in range(2)]
                rl = sb(st2, "rl", [128, 2, 512], F32)
                on = rl
                od = sb(st2, "od", [128, 512], F32)
                osq = sb(st2, "osq", [128, 512], F32)
                rn = sb(st2, "rn", [128, 512], F32)
                r_ep = R("epi")
                attnT = sb(st2, "attnT", [128, 4, 512], BF16)
                r_attnT = R("attnT")
                xr = [sb(st2, f"xr{i}", [128, D], F32) for i in range(2)]
                r_xr = [R(f"xr{i}") for i in range(2)]
                ht = [sb(st2, f"ht{i}", [128, D], F32) for i in range(2)]
                r_ht = [R(f"ht{i}") for i in range(2)]
                hnf = sb(st2, "hnf", [128, D], F32)
                r_hnf = R("hnf")
                hnTf = sb(st2, "hnTf", [128, 8, 128], F32)
                r_hnTf = R("hnTf")
                hnTb = [sb(st2, f"hnTb{i}", [128, D], BF16) for i in range(2)]
                r_hnTb = [R(f"hnTb{i}") for i in range(2)]
                ss2 = sb(st2, "ss2", [128, 1], F32)
                r_ss2 = R("ss2")

                fw.op(fw.pool, lambda e: e.memset(qpad[:], 0.0), writes=[r_qpad])
                nk = nt
                for j in range(ng if _DBG not in ("p1",) else 0):
                    gi = g0 + j
                    for m in range(2):
                        dst = qpad[m * 64:(m + 1) * 64, :, m, :]
                        fw.dma(fw.sp, dst, qd[gi, m * 64:(m + 1) * 64, :, :], r_qpad, writes=[r_qpad])
                    fw.dma(fw.sp, poolT[:], pd[gi], r_poolT, writes=[r_poolT])
                    units = [(hh, kk) for hh in range(4) for kk in range(nk)]

                    def emit_qk(ui):
                        hh, kk = units[ui]
                        bb = ui % 2
                        Sb = S0 if bb == 0 else S1
                        r_Sb = [r_S0a, r_S0b] if bb == 0 else [r_S1a, r_S1b]

                        def mm_s(e):
                            e.matmul(Sb[:, 0, :], kT[:, hh, kk * 128:(kk + 1) * 128], qpad[:, hh, 0, :], start=True, stop=True)
                            return e.matmul(Sb[:, 1, :], kT[:, hh, kk * 128:(kk + 1) * 128], qpad[:, hh, 1, :], start=True, stop=True)
                        fw.op(fw.pe, mm_s, reads=[r_kT, r_qpad], writes=r_Sb)

                    def emit_exp_pv(ui):
                        hh, kk = units[ui]
                        bb = ui % 2
                        p4 = ui % 4
                        Sb = S0 if bb == 0 else S1
                        r_Sb = [r_S0a, r_S0b] if bb == 0 else [r_S1a, r_S1b]
                        fw.op(fw.act, lambda e: e.activation(out=pT[p4][:], in_=Sb[:], func=AF.Exp, scale=0.125),
                              reads=r_Sb, writes=[r_pT[p4]])
                        if ui + 2 < len(units):
                            emit_qk(ui + 2)

                        def mm_pv(e):
                            ins = None
                            for m in range(2):
                                ins = e.matmul(PO[:, m, :], V[:, kk, hh * 128:(hh + 1) * 128], pT[p4][:, m, :],
                                               start=(kk == 0), stop=(kk == nk - 1))
                            return ins
                        fw.op(fw.pe, mm_pv, reads=[r_V, r_pT[p4]], writes=[r_POa, r_POb])
                        def mm_l_for(ps2_, kk_):
                            def mm_l(e):
                                ins = None
                                for m in range(2):
                                    ins = e.matmul(PL[:, m, :], ones_bf[:], pTs[ps2_][:, m, :],
                                                   start=(kk_ == 1), stop=(kk_ == nk - 1))
                                return ins
                            fw.op(fw.pe, mm_l, reads=[r_pTs[ps2_], r_ones], writes=[r_PLa, r_PLb])
                        if kk % 2 == 0 and pend_l:
                            mm_l_for(*pend_l.pop())
                        if kk % 2 == 1:
                            ps2 = (ui // 2) % 2
                            pprev = (ui - 1) % 4
                            fw.op(fw.dve, lambda e: e.tensor_tensor(out=pTs[ps2][:], in0=pT[pprev][:], in1=pT[p4][:], op=ALU.add),
                                  reads=[r_pT[pprev], r_pT[p4]], writes=[r_pTs[ps2]])
                            if kk == nk - 1:
                                mm_l_for(ps2, kk)
                            else:
                                pend_l.append((ps2, kk))

                    pend_l = []
                    emit_qk(0)
                    if len(units) > 1:
                        emit_qk(1)
                    for ui in range(len(units)):
                        h, kc = units[ui]
                        emit_exp_pv(ui)
                        if kc != nk - 1:
                            continue
                        fw.op(fw.dve, lambda e: e.reciprocal(out=rl[:], in_=PL[:]), reads=[r_PLa, r_PLb], writes=[r_ep])
                        fw.op(fw.dve, lambda e: e.tensor_tensor(out=on[:], in0=PO[:], in1=rl[:], op=ALU.mult),
                              reads=[r_POa, r_POb, r_ep], writes=[r_ep])
                        fw.op(fw.dve, lambda e: e.scalar_tensor_tensor(out=od[:], in0=on[:, 1, :], scalar=neglam[:], in1=on[:, 0, :],
                                                                       op0=ALU.mult, op1=ALU.add),
                              reads=[r_ep, r_lam], writes=[r_ep])
                        fw.op(fw.act, lambda e: e.activation(out=osq[:], in_=od[:], func=AF.Square), reads=[r_ep], writes=[r_ep])
                        fw.op(fw.pe, lambda e: e.matmul(PL[:, 0, :], ones_f[:], osq[:], start=True, stop=True),
                              reads=[r_ep, r_ones], writes=[r_PLa])
                        fw.op(fw.act, lambda e: e.activation(out=rn[:], in_=PL[:, 0, :], func=AF.Ln, bias=eps_t[:], scale=1.0 / 128),
                              reads=[r_PLa, r_ones], writes=[r_ep])
                        fw.op(fw.act, lambda e: e.activation(out=rn[:], in_=rn[:], func=AF.Exp, scale=-0.5), reads=[r_ep], writes=[r_ep])
                        fw.op(fw.dve, lambda e, h=h: e.scalar_tensor_tensor(out=attnT[:, h, :], in0=od[:], scalar=1.0 - LAM_INIT, in1=rn[:],
                                                                            op0=ALU.mult, op1=ALU.mult),
                              reads=[r_ep], writes=[r_attnT])
                    def stage2A(ti):
                        tg = (tok0 // 128) + j * 4 + ti
                        sl = ti % 2
                        Hb = S0 if sl == 0 else S1
                        r_Hb = [r_S0a, r_S0b] if sl == 0 else [r_S1a, r_S1b]
                        fw.dma(fw.sp, xr[sl][:], x[tg * 128:(tg + 1) * 128, :], r_xr[sl], writes=[r_xr[sl]])

                        def mm_o(e, ti=ti, Hb=Hb):
                            ins = None
                            for cc in range(2):
                                for c in range(8):
                                    lhsT = poolT[:, ti, c, :] if c < 4 else attnT[:, c - 4, ti * 128:(ti + 1) * 128]
                                    ins = e.matmul(Hb[:, cc, :], lhsT, Wout[:, c, cc * 512:(cc + 1) * 512],
                                                   start=(c == 0), stop=(c == 7))
                            return ins
                        fw.op(fw.pe, mm_o, reads=[r_poolT, r_attnT, r_Wout], writes=r_Hb)
                        Hf = Hb[:].rearrange("p a b -> p (a b)")
                        fw.op(fw.dve, lambda e, Hf=Hf, sl=sl: e.tensor_tensor(out=ht[sl][:], in0=Hf, in1=xr[sl][:], op=ALU.add),
                              reads=r_Hb + [r_xr[sl]], writes=[r_ht[sl]])
                        fw.dma(fw.act, h_d[tg], ht[sl][:], r_ht[sl], reads=[r_ht[sl]])
                        fw.op(fw.act, lambda e, sl=sl: e.activation(out=hnTb[sl][:], in_=ht[sl][:], func=AF.Square, accum_out=ss2[:]),
                              reads=[r_ht[sl]], writes=[r_hnTb[sl], r_ss2])
                        rstd_chain(ss2[:], ss2[:], [r_ss2], 1.0 / D)
                        fw.op(fw.dve, lambda e, sl=sl: e.scalar_tensor_tensor(out=hnf[:], in0=ht[sl][:], scalar=ss2[:], in1=gffn[:],
                                                                              op0=ALU.mult, op1=ALU.mult),
                              reads=[r_ht[sl], r_ss2, r_c], writes=[r_hnf])
                        psTf = PO[:].rearrange("p a (c t) -> p (a c) t", t=128)

                        def tr3(e):
                            ins = None
                            for c in range(8):
                                ins = e.matmul(psTf[:, c, :], hnf[:, c * 128:(c + 1) * 128], ident_f[:], start=True, stop=True)
                            return ins
                        fw.op(fw.pe, tr3, reads=[r_hnf, r_c], writes=[r_POa, r_POb])
                        fw.op(fw.dve, lambda e: e.tensor_copy(out=hnTf[:], in_=psTf), reads=[r_POa, r_POb], writes=[r_hnTf])
                        fw.op(fw.pool, lambda e, sl=sl: e.tensor_copy(out=hnTb[sl][:], in_=hnf[:]),
                              reads=[r_hnf], writes=[r_hnTb[sl]])
                        fw.dma(fw.pool, hn_d[tg], hnTb[sl][:], r_hnTb[sl], reads=[r_hnTb[sl]])
                        ps_r = PL[:, 1, (ti % 2) * 64:(ti % 2) * 64 + 36]

                        def mm_r(e):
                            ins = None
                            for c in range(8):
                                ins = e.matmul(ps_r, hnTf[:, c, :], Wr[:, c, :], start=(c == 0), stop=(c == 7))
                            return ins
                        fw.op(fw.pe, mm_r, reads=[r_hnTf, r_Wr], writes=[r_psr[ti % 2], r_PLb])

                    def stage2B(ti):
                        tg = (tok0 // 128) + j * 4 + ti
                        ps_r = PL[:, 1, (ti % 2) * 64:(ti % 2) * 64 + 36]
                        fw.op(fw.dve, lambda e: e.tensor_tensor(out=lg_all[:, tg, :], in0=ps_r, in1=biasb[:], op=ALU.add),
                              reads=[r_psr[ti % 2], r_PLb, r_c], writes=[r_gates[tg]])

                    stage2A(0)
                    for ti in range(4):
                        if ti + 1 < 4:
                            stage2A(ti + 1)
                        stage2B(ti)
                tok0 += N
                g0 += ng
                fw.barrier()
                st2.close()
        fw.barrier()
        I32 = mybir.dt.int32
        with contextlib.ExitStack() as st:
            OH1 = sb(st, "OH1", [128, NT, 32], BF16)
            OH2 = sb(st, "OH2", [128, NT, 32], BF16)
            wts = sb(st, "wts", [128, NT, 2], F32)
            d1i = sb(st, "d1i", [128, NT], I32)
            d2i = sb(st, "d2i", [128, NT], I32)
            idxw = sb(st, "idxw", [128, NB], I32)
            r_rt3 = R("route3")
            TA = st.enter_context(nc.psum_tensor("TA", [128, 512], F32))
            TB = st.enter_context(nc.psum_tensor("TB", [128, 512], F32))
            G0 = st.enter_context(nc.psum_tensor("G0", [128, 2, 512], F32))
            G1 = st.enter_context(nc.psum_tensor("G1", [128, 2, 512], F32))
            Y0 = st.enter_context(nc.psum_tensor("Y0", [128, 2, 512], F32))
            r_TA, r_TB, r_G0, r_G1, r_Y0 = R("TA"), R("TB"), R("G0p"), R("G1p"), R("Y0p")
            with contextlib.ExitStack() as s0:
                r_r0 = R("router0")
                gq = sb(s0, "gq", [128, 8, NT], F32)
                mg = sb(s0, "mg", [128, NT, 4], F32)
                eg = sb(s0, "eg", [128, NT, 4], F32)
                t48 = sb(s0, "t48", [128, NT, 4, 8], F32)
                les = sb(s0, "les", [128, NT, 8], F32)
                les2 = sb(s0, "les2", [128, NT, 8], F32)
                k1 = sb(s0, "k1", [128, NT, 8], F32)
                k2 = sb(s0, "k2", [128, NT, 8], F32)
                gmax, gsum, m1, m2, ex, w1, w2 = (gq[:, i, :] for i in range(7))
                lg = lg_all[:, :, 0:4]
                le4 = lg_all[:, :, 4:36].rearrange("p t (g j) -> p t g j", g=4)
                rr0 = [r_r0]

                def d0(fn, extra=()):
                    fw.op(fw.dve, fn, reads=rr0 + list(extra), writes=rr0)

                def bc3(ap2, k):
                    return ap2.unsqueeze(2).broadcast_to([128, NT, k])
                d0(lambda e: e.tensor_reduce(out=gmax, in_=lg, axis=AX.X, op=ALU.max), r_gates)
                d0(lambda e: e.tensor_tensor(out=mg[:], in0=lg, in1=bc3(gmax, 4), op=ALU.is_equal), r_gates)
                d0(lambda e: e.tensor_tensor(out=eg[:], in0=lg, in1=bc3(gmax, 4), op=ALU.subtract), r_gates)
                fw.op(fw.act, lambda e: e.activation(out=eg[:], in_=eg[:], func=AF.Exp), reads=rr0, writes=rr0)
                d0(lambda e: e.tensor_reduce(out=gsum, in_=eg[:], axis=AX.X, op=ALU.add))
                d0(lambda e: e.reciprocal(out=gsum, in_=gsum))
                d0(lambda e: e.tensor_tensor(out=t48[:], in0=le4, in1=mg[:].unsqueeze(3).broadcast_to([128, NT, 4, 8]), op=ALU.mult), r_gates)
                d0(lambda e: e.tensor_reduce(out=les[:], in_=t48[:].rearrange("p t g j -> p t j g"), axis=AX.X, op=ALU.add))
                d0(lambda e: e.tensor_reduce(out=m1, in_=les[:], axis=AX.X, op=ALU.max))
                d0(lambda e: e.tensor_tensor(out=k1[:], in0=les[:], in1=bc3(m1, 8), op=ALU.is_equal))
                d0(lambda e: e.scalar_tensor_tensor(out=les2[:], in0=k1[:], scalar=-1e30, in1=les[:], op0=ALU.mult, op1=ALU.add))
                d0(lambda e: e.tensor_reduce(out=m2, in_=les2[:], axis=AX.X, op=ALU.max))
                d0(lambda e: e.tensor_tensor(out=k2[:], in0=les2[:], in1=bc3(m2, 8), op=ALU.is_equal))
                d0(lambda e: e.tensor_tensor(out=ex, in0=m2, in1=m1, op=ALU.subtract))
                fw.op(fw.act, lambda e: e.activation(out=ex, in_=ex, func=AF.Exp), reads=rr0, writes=rr0)
                d0(lambda e: e.tensor_scalar(out=w1, in0=ex, scalar1=1.0, scalar2=None, op0=ALU.add))
                d0(lambda e: e.reciprocal(out=w1, in_=w1))
                d0(lambda e: e.tensor_tensor(out=w1, in0=w1, in1=gsum, op=ALU.mult))
                d0(lambda e: e.tensor_tensor(out=w2, in0=w1, in1=ex, op=ALU.mult))
                mgb4 = mg[:].unsqueeze(3).broadcast_to([128, NT, 4, 8])
                o1v = OH1[:].rearrange("p t (g j) -> p t g j", g=4)
                o2v = OH2[:].rearrange("p t (g j) -> p t g j", g=4)
                fw.op(fw.dve, lambda e: e.tensor_tensor(out=o1v, in0=k1[:].unsqueeze(2).broadcast_to([128, NT, 4, 8]), in1=mgb4, op=ALU.mult),
                      reads=rr0, writes=r_gates)
                fw.op(fw.dve, lambda e: e.tensor_tensor(out=o2v, in0=k2[:].unsqueeze(2).broadcast_to([128, NT, 4, 8]), in1=mgb4, op=ALU.mult),
                      reads=rr0, writes=r_gates)
                fw.op(fw.dve, lambda e: e.tensor_copy(out=wts[:, :, 0], in_=w1), reads=rr0, writes=r_gates)
                fw.op(fw.dve, lambda e: e.tensor_copy(out=wts[:, :, 1], in_=w2), reads=rr0, writes=r_gates)
                fw.barrier()
            with contextlib.ExitStack() as sa:
                tri = sb(sa, "tri", [128, 128], BF16)
                bstart = sb(sa, "bstart", [128, NB], F32)
                pidx = sb(sa, "pidx", [128, 1], F32)
                r_ca = R("constA")
                fw.dma(fw.sp, tri[:], tri_d, r_ca, writes=[r_ca])
                fw.dma(fw.sp, bstart[:], bstart_d, r_ca, writes=[r_ca])
                fw.dma(fw.sp, pidx[:], pidx_d, r_ca, writes=[r_ca])
                NC3 = NT * 32
                Mb = sb(sa, "Mb", [128, NC3], BF16)
                rank = sb(sa, "rank", [128, NT, 32], F32)
                cnt = sb(sa, "cnt", [128, NT, 32], F32)
                sA = sb(sa, "sA", [128, NT, 32], F32)
                sB = sb(sa, "sB", [128, NT, 32], F32)
                cmp = sb(sa, "cmp", [128, NB, 32], F32)
                sm = sb(sa, "sm", [128, 8, 32], F32)
                ebf = sb(sa, "ebf", [128, NB], F32)
                eb2 = sb(sa, "eb2", [128, NB], F32)
                dfl = sb(sa, "dfl", [128, 2, NT], F32)
                Dv = fw.dve
                rr = [r_rt3]

                def dv(fn, extra=()):
                    fw.op(Dv, fn, reads=rr + list(extra), writes=rr)
                OH1f = OH1[:].rearrange("p t e -> p (t e)")
                OH2f = OH2[:].rearrange("p t e -> p (t e)")
                dv(lambda e: e.tensor_tensor(out=Mb[:], in0=OH1f, in1=OH2f, op=ALU.add), r_gates)
                rankf = rank[:].rearrange("p t e -> p (t e)")
                cntf = cnt[:].rearrange("p t e -> p (t e)")
                nch = (NC3 + 511) // 512
                for ch in range(nch):
                    c0 = ch * 512
                    c1 = min(NC3, c0 + 512)
                    w_ = c1 - c0
                    fw.op(fw.pe, lambda e: e.matmul(TA[:, 0:w_], tri[:], Mb[:, c0:c1], start=True, stop=True),
                          reads=[r_rt3, r_ca], writes=[r_TA])
                    fw.op(fw.pe, lambda e: e.matmul(TB[:, 0:w_], ones_bf[:], Mb[:, c0:c1], start=True, stop=True),
                          reads=[r_rt3, r_ones], writes=[r_TB])
                    fw.op(Dv, lambda e: e.tensor_copy(out=rankf[:, c0:c1], in_=TA[:, 0:w_]), reads=[r_TA], writes=rr)
                    fw.op(Dv, lambda e: e.tensor_copy(out=cntf[:, c0:c1], in_=TB[:, 0:w_]), reads=[r_TB], writes=rr)
                dv(lambda e: e.tensor_copy(out=sA[:], in_=cnt[:]))
                src, dst = sA, sB
                sft = 1
                while sft < NT:
                    dv(lambda e: e.tensor_tensor(out=dst[:, sft:, :], in0=src[:, sft:, :], in1=src[:, 0:NT - sft, :], op=ALU.add))
                    dv(lambda e: e.tensor_copy(out=dst[:, 0:sft, :], in_=src[:, 0:sft, :]))
                    src, dst = dst, src
                    sft *= 2
                incl = src
                other = dst
                tot = sm[:, 0, :]
                xm = sm[:, 1, :]
                padded = sm[:, 2, :]
                pA = sm[:, 3, :]
                pB = sm[:, 4, :]
                pstart = sm[:, 5, :]
                dv(lambda e: e.tensor_copy(out=tot, in_=incl[:, NT - 1, :]))
                dv(lambda e: e.tensor_scalar(out=xm, in0=tot, scalar1=1.0 / 128, scalar2=0.49609375, op0=ALU.mult, op1=ALU.add))
                dv(lambda e: e.tensor_scalar(out=xm, in0=xm, scalar1=8388608.0, scalar2=None, op0=ALU.add))
                dv(lambda e: e.tensor_scalar(out=xm, in0=xm, scalar1=-8388608.0, scalar2=None, op0=ALU.add))
                dv(lambda e: e.tensor_scalar(out=padded, in0=xm, scalar1=128.0, scalar2=None, op0=ALU.mult))
                dv(lambda e: e.tensor_copy(out=pA, in_=padded))
                ps_, pd_ = pA, pB
                sft = 1
                while sft < 32:
                    dv(lambda e: e.tensor_tensor(out=pd_[:, sft:], in0=ps_[:, sft:], in1=ps_[:, 0:32 - sft], op=ALU.add))
                    dv(lambda e: e.tensor_copy(out=pd_[:, 0:sft], in_=ps_[:, 0:sft]))
                    ps_, pd_ = pd_, ps_
                    sft *= 2
                pend = ps_
                dv(lambda e: e.tensor_tensor(out=pstart, in0=pend, in1=padded, op=ALU.subtract))
                dv(lambda e: e.tensor_tensor(out=other[:], in0=incl[:], in1=cnt[:], op=ALU.subtract))
                dv(lambda e: e.tensor_tensor(out=other[:], in0=other[:], in1=rank[:], op=ALU.add))
                dv(lambda e: e.tensor_tensor(out=other[:], in0=other[:], in1=pstart.unsqueeze(1).broadcast_to([128, NT, 32]), op=ALU.add))
                dest = other
                dv(lambda e: e.tensor_tensor(out=incl[:], in0=dest[:], in1=OH1[:], op=ALU.mult), r_gates)
                dv(lambda e: e.tensor_reduce(out=dfl[:, 0, :], in_=incl[:], axis=AX.X, op=ALU.add))
                dv(lambda e: e.tensor_tensor(out=incl[:], in0=dest[:], in1=OH2[:], op=ALU.mult), r_gates)
                dv(lambda e: e.tensor_reduce(out=dfl[:, 1, :], in_=incl[:], axis=AX.X, op=ALU.add))
                dv(lambda e: e.tensor_copy(out=d1i[:], in_=dfl[:, 0, :]))
                dv(lambda e: e.tensor_copy(out=d2i[:], in_=dfl[:, 1, :]))
                dv(lambda e: e.tensor_tensor(out=cmp[:], in0=pend.unsqueeze(1).broadcast_to([128, NB, 32]),
                                             in1=bstart[:].unsqueeze(2).broadcast_to([128, NB, 32]), op=ALU.is_le), [r_ca])
                dv(lambda e: e.tensor_reduce(out=ebf[:], in_=cmp[:], axis=AX.X, op=ALU.add))
                dv(lambda e: e.tensor_scalar(out=ebf[:], in0=ebf[:], scalar1=float(NE - 1), scalar2=None, op0=ALU.min))
                BIG = 1.0e6
                dv(lambda e: e.memset(eb2[:], 1.0))
                dv(lambda e: e.tensor_tensor(out=eb2[:, 2:], in0=ebf[:, 2:], in1=ebf[:, 0:NB - 2], op=ALU.not_equal))
                dv(lambda e: e.tensor_scalar(out=ebf[:], in0=ebf[:], scalar1=128.0, scalar2=pidx[:], op0=ALU.mult, op1=ALU.add), [r_ca])
                dv(lambda e: e.tensor_scalar(out=ebf[:], in0=ebf[:], scalar1=-BIG, scalar2=None, op0=ALU.add))
                dv(lambda e: e.tensor_tensor(out=ebf[:], in0=ebf[:], in1=eb2[:], op=ALU.mult))
                dv(lambda e: e.tensor_scalar(out=ebf[:], in0=ebf[:], scalar1=BIG, scalar2=None, op0=ALU.add))
                dv(lambda e: e.tensor_copy(out=idxw[:], in_=ebf[:]))
                fw.barrier()

            _bregs = {}

            def _breg(bound):
                if bound not in _bregs:
                    rg = nc.gpsimd.alloc_register(f"bnd{len(_bregs)}")
                    nc.gpsimd.reg_mov(rg, int(bound))
                    _bregs[bound] = rg
                return _bregs[bound]

            def indirect(out, out_off, in_, in_off, bound, sb_res, reads, writes):
                deps = []
                for r_ in reads:
                    if r_.last_w is not None:
                        deps.append(r_.last_w)
                for w_r in writes:
                    if w_r.last_w is not None:
                        deps.append(w_r.last_w)
                    deps.extend(w_r.readers)
                fw.pool.wait_tokens(deps)
                ent = fw.dma_reg.setdefault(sb_res.name, [None, 0])
                if ent[0] is None:
                    ent[0] = fw.new_sem(f"dma_{sb_res.name}")
                ins = nc.gpsimd.indirect_dma_start(
                    out=out, out_offset=(bass.IndirectOffsetOnAxis(ap=out_off, axis=0) if out_off is not None else None),
                    in_=in_, in_offset=(bass.IndirectOffsetOnAxis(ap=in_off, axis=0) if in_off is not None else None),
                    bounds_check=_breg(bound), oob_is_err=False)
                ent[1] += 16
                ins.then_inc(fw.sem_by_key[ent[0]], 16)
                tok = (ent[0], ent[1])
                for r_ in reads:
                    r_.readers.append(tok)
                for w_r in writes:
                    w_r.last_w = tok
                    w_r.readers = []

            with contextlib.ExitStack() as sbk:
                hb = [sb(sbk, f"hb{i}", [128, D], BF16) for i in range(3)]
                r_hb = [R(f"hb{i}") for i in range(3)]
                for i in range(NT):
                    s3 = i % 3
                    fw.dma(fw.sp, hb[s3][:], hn_d[i], r_hb[s3], writes=[r_hb[s3]])
                    indirect(xg[:, :], d1i[:, i:i + 1], hb[s3][:, :], None, PR - 1, r_hb[s3], [r_hb[s3], r_rt3], [])
                    indirect(xg[:, :], d2i[:, i:i + 1], hb[s3][:, :], None, PR - 1, r_hb[s3], [r_hb[s3], r_rt3], [])
                fw.barrier()
            wgv = w_gate[:, :]
            wuv = w_up[:, :]
            wdv = w_down[:, :]
            with contextlib.ExitStack() as sc:
                identb = sb(sc, "identb", [128, 128], BF16)
                r_idb = R("identb")
                fw.dma(fw.sp, identb[:], ident_bf_d, r_idb, writes=[r_idb])
                wg = [sb(sc, f"wg{i}", [128, 8 * DFF], BF16) for i in range(2)]
                wu = [sb(sc, f"wu{i}", [128, 8 * DFF], BF16) for i in range(2)]
                wd = [sb(sc, f"wd{i}", [128, 4 * D], BF16) for i in range(2)]
                r_wg = [R("wg0"), R("wg1")]
                r_wu = [R("wu0"), R("wu1")]
                r_wd = [R("wd0"), R("wd1")]
                xb = [sb(sc, f"xb{i}", [128, D], BF16) for i in range(2)]
                r_xb = [R("xb0"), R("xb1")]
                xgT = [sb(sc, f"xgT{i}", [128, 8, 128], BF16) for i in range(2)]
                r_xgT = [R("xgT0"), R("xgT1")]
                sgl = sb(sc, "sgl", [128, 512], F32)
                r_sgl = R("sgl")
                hidT = [sb(sc, f"hidT{i}", [128, 4, 128], BF16) for i in range(2)]
                r_hid = [R("hid0"), R("hid1")]
                yb = [sb(sc, f"yb{i}", [128, D], F32) for i in range(2)]
                r_yb = [R("yb0"), R("yb1")]
                Ts = [TA, TB]
                r_Ts = [r_TA, r_TB]
                Gs = [G0, G1]
                r_Gs = [r_G0, r_G1]
                for ws in range(2):
                    pass
                hidtok = [sb(sc, f"hidtok{i}", [128, DFF], BF16) for i in range(2)]
                r_hidtok = [R("hidtok0"), R("hidtok1")]
                sgl2 = [sgl, sb(sc, "sglb", [128, 512], F32)]
                r_sgl2 = [r_sgl, R("sglb")]
                xb3 = xb + [sb(sc, "xb2", [128, D], BF16)]
                r_xb3 = r_xb + [R("xb2")]
                xgT3 = xgT + [sb(sc, "xgT2", [128, 8, 128], BF16)]
                r_xgT3 = r_xgT + [R("xgT2")]
                psT3 = TA[:].bitcast(BF16).rearrange("p (c t) -> p c t", c=8)
                psH = TB[:, 0:256].bitcast(BF16).rearrange("p (c t) -> p c t", c=4)

                def gath_gu(b):
                    ws = b % 2
                    indirect(wg[ws][:, :], None, wgv, idxw[:, b:b + 1], NE * 128 - 1, r_wg[ws], [r_rt3], [r_wg[ws]])
                    indirect(wu[ws][:, :], None, wuv, idxw[:, b:b + 1], NE * 128 - 1, r_wu[ws], [r_rt3], [r_wu[ws]])

                def gath_d(b):
                    ws = b % 2
                    indirect(wd[ws][:, :], None, wdv, idxw[:, b:b + 1], NE * 128 - 1, r_wd[ws], [r_rt3], [r_wd[ws]])

                def preC(b):
                    s3 = b % 3
                    fw.dma(fw.sp, xb3[s3][:], xg[b * 128:(b + 1) * 128, :], r_xb3[s3], writes=[r_xb3[s3]])
                    xbv = xb3[s3][:].rearrange("p (q c) -> p c q", c=8)

                    def tr4(e):
                        ins = None
                        for c in range(8):
                            ins = e.transpose(psT3[:, c, :], xbv[:, c, :], identb[:])
                        return ins
                    fw.op(fw.pe, tr4, reads=[r_xb3[s3], r_idb], writes=[r_TA])
                    fw.op(fw.dve, lambda e: e.tensor_copy(out=xgT3[s3][:], in_=psT3), reads=[r_TA], writes=[r_xgT3[s3]])

                def stageG(b):
                    ws = b % 2
                    s3 = b % 3
                    Gb = Gs[ws]
                    wgs = wg[ws][:].rearrange("p (c f) -> p c f", c=8)
                    wus = wu[ws][:].rearrange("p (c f) -> p c f", c=8)

                    def mm_gu(e):
                        ins = None
                        for which, wv in ((0, wgs), (1, wus)):
                            for c in range(8):
                                ins = e.matmul(Gb[:, which, :], xgT3[s3][:, c, :], wv[:, c, :], start=(c == 0), stop=(c == 7))
                        return ins
                    fw.op(fw.pe, mm_gu, reads=[r_xgT3[s3], r_wg[ws], r_wu[ws]], writes=[r_Gs[ws]])
                    fw.op(fw.act, lambda e: e.activation(out=sgl2[ws][:], in_=Gb[:, 0, :], func=AF.Silu), reads=[r_Gs[ws]], writes=[r_sgl2[ws]])
                    fw.op(fw.dve, lambda e: e.tensor_tensor(out=hidtok[ws][:], in0=sgl2[ws][:], in1=Gb[:, 1, :], op=ALU.mult),
                          reads=[r_sgl2[ws], r_Gs[ws]], writes=[r_hidtok[ws]])

                def stageH(b):
                    ws = b % 2
                    wds = wd[ws][:].rearrange("p (c d) -> p c d", c=4)
                    hv = hidtok[ws][:].rearrange("p (q c) -> p c q", c=4)

                    def trH(e):
                        ins = None
                        for c in range(4):
                            ins = e.transpose(psH[:, c, :], hv[:, c, :], identb[:])
                        return ins
                    fw.op(fw.pe, trH, reads=[r_hidtok[ws], r_idb], writes=[r_TB])
                    fw.op(fw.dve, lambda e: e.tensor_copy(out=hidT[ws][:], in_=psH), reads=[r_TB], writes=[r_hid[ws]])

                def stageH2(b):
                    ws = b % 2
                    wds = wd[ws][:].rearrange("p (c d) -> p c d", c=4)

                    def mm_d(e):
                        ins = None
                        for cc in range(2):
                            for fc in range(4):
                                ins = e.matmul(Y0[:, cc, :], hidT[ws][:, fc, :], wds[:, fc, cc * 512:(cc + 1) * 512],
                                               start=(fc == 0), stop=(fc == 3))
                        return ins
                    fw.op(fw.pe, mm_d, reads=[r_hid[ws], r_wd[ws]], writes=[r_Y0])
                    fw.op(fw.act, lambda e: e.activation(out=yb[ws][:], in_=Y0[:].rearrange("p a b -> p (a b)"), func=AF.Copy),
                          reads=[r_Y0], writes=[r_yb[ws]])
                    fw.dma(fw.act, yg[b * 128:(b + 1) * 128, :], yb[ws][:], r_yb[ws], reads=[r_yb[ws]])

                for bb in (0, 1):
                    if bb < NB:
                        gath_gu(bb)
                        gath_d(bb)
                        preC(bb)
                stageG(0)
                for b in range(NB):
                    stageH(b)
                    if b + 2 < NB:
                        gath_gu(b + 2)
                        preC(b + 2)
                    stageH2(b)
                    if b + 1 < NB:
                        stageG(b + 1)
                    if b + 2 < NB:
                        gath_d(b + 2)
                fw.barrier()
            with contextlib.ExitStack() as sd:
                gfin = sb(sd, "gfin", [128, D], F32)
                fw.dma(fw.sp, gfin[:], gfin_b, r_gfin, writes=[r_gfin])
                y1 = [sb(sd, f"y1{i}", [128, D], F32) for i in range(2)]
                y2 = [sb(sd, f"y2{i}", [128, D], F32) for i in range(2)]
                r_y1 = [R("y10"), R("y11")]
                r_y2 = [R("y20"), R("y21")]
                hr = [sb(sd, f"hr{i}", [128, D], F32) for i in range(2)]
                r_hr = [R("hr0"), R("hr1")]
                yt = [sb(sd, f"yt{i}", [128, D], F32) for i in range(2)]
                r_yt = [R("yt0"), R("yt1")]
                junk3 = sb(sd, "junk3", [128, D], BF16)
                r_junk3 = R("junk3")
                ss3 = [sb(sd, f"ss3{i}", [128, 1], F32) for i in range(2)]
                r_ss3 = [R("ss30"), R("ss31")]
                def loadD(tg):
                    sl = tg % 2
                    indirect(y1[sl][:, :], None, yg[:, :], d1i[:, tg:tg + 1], PR - 1, r_y1[sl], [r_rt3], [r_y1[sl]])
                    indirect(y2[sl][:, :], None, yg[:, :], d2i[:, tg:tg + 1], PR - 1, r_y2[sl], [r_rt3], [r_y2[sl]])
                    fw.dma(fw.sp, hr[sl][:], h_d[tg], r_hr[sl], writes=[r_hr[sl]])

                def compD(tg):
                    sl = tg % 2
                    fw.op(fw.dve, lambda e: e.scalar_tensor_tensor(out=hr[sl][:], in0=y1[sl][:], scalar=wts[:, tg, 0:1], in1=hr[sl][:],
                                                                   op0=ALU.mult, op1=ALU.add),
                          reads=[r_y1[sl], r_hr[sl], r_gates[tg]], writes=[r_hr[sl]])
                    fw.op(fw.dve, lambda e: e.scalar_tensor_tensor(out=hr[sl][:], in0=y2[sl][:], scalar=wts[:, tg, 1:2], in1=hr[sl][:],
                                                                   op0=ALU.mult, op1=ALU.add),
                          reads=[r_y2[sl], r_hr[sl], r_gates[tg]], writes=[r_hr[sl]])
                    fw.op(fw.act, lambda e: e.activation(out=junk3[:], in_=hr[sl][:], func=AF.Square, accum_out=ss3[sl][:]),
                          reads=[r_hr[sl]], writes=[r_junk3, r_ss3[sl]])
                    fw.op(fw.act, lambda e: e.activation(out=ss3[sl][:], in_=ss3[sl][:], func=AF.Ln, bias=eps_t[:], scale=1.0 / D),
                          reads=[r_ss3[sl], r_ones], writes=[r_ss3[sl]])
                    fw.op(fw.act, lambda e: e.activation(out=ss3[sl][:], in_=ss3[sl][:], func=AF.Exp, scale=-0.5), reads=[r_ss3[sl]], writes=[r_ss3[sl]])
                    fw.op(fw.dve, lambda e: e.scalar_tensor_tensor(out=yt[sl][:], in0=hr[sl][:], scalar=ss3[sl][:], in1=gfin[:],
                                                                   op0=ALU.mult, op1=ALU.mult),
                          reads=[r_hr[sl], r_ss3[sl], r_gfin], writes=[r_yt[sl]])
                    fw.dma(fw.act, y[tg * 128:(tg + 1) * 128, :], yt[sl][:], r_yt[sl], reads=[r_yt[sl]])

                loadD(0)
                for tg in range(NT):
                    if tg + 1 < NT:
                        loadD(tg + 1)
                    compD(tg)
                fw.barrier()
    return nc


def _const_tables(max_n):
    max_nt = max_n // 128
    inv_freq = (np.float32(10000.0) ** (-np.arange(0, 64, 2, dtype=np.float32) / np.float32(64))).astype(np.float32)
    pos = np.arange(max_n, dtype=np.float32)
    ang = (pos[:, None] * inv_freq[None, :]).astype(np.float32)
    cos = np.cos(ang).astype(np.float32).reshape(max_nt, 128, 32)
    sin = np.sin(ang).astype(np.float32).reshape(max_nt, 128, 32)
    cs = np.stack([cos, sin], axis=2)
    cs = np.ascontiguousarray(cs.transpose(1, 0, 2, 3))
    invc = np.zeros((3, 4, 128), np.float32)
    for g, w in enumerate((2, 4, 8, 16)):
        half = w // 2
        t = np.arange(128)
        invc[0, g] = 1.0 / (np.minimum(t + half, 10 ** 9) - np.maximum(t - half, 0))
        invc[1, g] = 1.0 / w
        invc[2, g] = 1.0 / (np.minimum(t + half, 128) - (t - half))
    invc = np.ascontiguousarray(np.broadcast_to(invc[None], (128, 3, 4, 128))).astype(np.float32)
    return cs, invc


_PROG_CACHE = {}


def run_cores(core_seqs, weights, n_cores, sg_tok):
    seq_lens = tuple(int(s.shape[0]) for s in core_seqs[0])
    key = (seq_lens, sg_tok)
    if key not in _PROG_CACHE:
        _PROG_CACHE[key] = build_program(list(seq_lens), sg_tok)
    nc = _PROG_CACHE[key]
    f32 = np.float32
    W = {k: np.asarray(v, dtype=f32) for k, v in weights.items()}
    cs, invc = _const_tables(max(seq_lens))
    NBh = (2 * sum(seq_lens)) // 128 + NE
    shared = {
        "w_in": np.ascontiguousarray(W["w_in"][0]),
        "w_out": np.ascontiguousarray(W["w_out"][0]),
        "w_pool": np.ascontiguousarray(W["w_pool"][0]),
        "w_gate": np.ascontiguousarray(W["w_gate"][0]).reshape(NE * 128, 8 * DFF),
        "w_up": np.ascontiguousarray(W["w_up"][0]).reshape(NE * 128, 8 * DFF),
        "w_down": np.ascontiguousarray(W["w_down"][0]).reshape(NE * 128, 4 * D),
        "w_r": np.ascontiguousarray(np.concatenate([W["w_router_group"][0], W["w_router_expert"][0]], axis=1)),
        "gmix_t": np.ascontiguousarray(W["g_mix"][0].reshape(8, 128).T),
        "wos_t": np.ascontiguousarray(np.concatenate([W["pool_scale"][0].reshape(4, 128).T,
                                                      np.repeat(W["subln_g"][0][:, None], 4, axis=1)], axis=1)),
        "gffn_b": np.ascontiguousarray(np.broadcast_to(W["g_ffn"][0][None, :], (128, D))),
        "gfin_b": np.ascontiguousarray(np.broadcast_to(W["g_final"][None, :], (128, D))),
        "bias_b": np.ascontiguousarray(np.broadcast_to(
            np.concatenate([W["b_router_group"][0], W["b_router_expert"][0]])[None, :], (128, 36))),
        "lam_b": np.ascontiguousarray(np.broadcast_to(
            np.stack([W["lambda_q1"][0], W["lambda_k1"][0], W["lambda_q2"][0], W["lambda_k2"][0]])[None], (128, 4, 64))),
        "ident_bf": np.eye(128, dtype=f32).astype(ml_dtypes.bfloat16),
        "ident_f": np.eye(128, dtype=f32),
        "cs_tab": cs,
        "invc": invc,
        "tri_bf": np.triu(np.ones((128, 128), f32), k=1).astype(ml_dtypes.bfloat16),
        "bstart": np.ascontiguousarray(np.broadcast_to((np.arange(NBh, dtype=f32) * 128.0)[None, :], (128, NBh))),
        "pidx": np.arange(128, dtype=f32).reshape(128, 1),
    }
    in_maps = []
    for c in range(n_cores):
        m = dict(shared)
        m["x"] = np.ascontiguousarray(np.concatenate([np.asarray(s, dtype=f32) for s in core_seqs[c]], axis=0))
        in_maps.append(m)
    res = run_bass_kernel_spmd(nc, in_maps, core_ids=list(range(n_cores)))
    outs = []
    for c in range(n_cores):
        yc = np.asarray(res.results[c]["y"])
        o = []
        off = 0
        for n in seq_lens:
            o.append(yc[off:off + n])
            off += n
        outs.append(o)
    return outs


def kernel(x_prompt, x_sample, **weights):
    n_cores = 8
    xp = np.asarray(x_prompt)
    xsm = np.asarray(x_sample)
    pp = xp.shape[0] // n_cores
    ps = xsm.shape[0] // n_cores
    core_seqs = []
    for c in range(n_cores):
        core_seqs.append([xp[c * pp + i] for i in range(pp)] + [xsm[c * ps + i] for i in range(ps)])
    outs = run_cores(core_seqs, weights, n_cores, sg_tok=2048)
    yp = np.empty(xp.shape, np.float32)
    ys = np.empty(xsm.shape, np.float32)
    for c in range(n_cores):
        for i in range(pp):
            yp[c * pp + i] = outs[c][i]
        for i in range(ps):
            ys[c * ps + i] = outs[c][pp + i]
    return (yp, ys)
```

```python
import contextlib
import numpy as np
import ml_dtypes
import concourse.bass as bass
import concourse.mybir as mybir
from concourse.bass_utils import run_bass_kernel_spmd

F32 = mybir.dt.float32
BF16 = mybir.dt.bfloat16
ALU = mybir.AluOpType
AF = mybir.ActivationFunctionType
AX = mybir.AxisListType

D = 1024
NE = 32
DFF = 512
EPS = 1e-6
SEM_WRAP = 30000
LAM_INIT = 0.2


class Res:
    __slots__ = ("name", "last_w", "readers", "dma_sem", "dma_cnt")

    def __init__(self, name):
        self.name = name
        self.last_w = None
        self.readers = []
        self.dma_sem = None
        self.dma_cnt = 0


class Eng:
    def __init__(self, fw, name, obj, is_pe=False):
        self.fw = fw
        self.name = name
        self.obj = obj
        self.is_pe = is_pe
        self.n = 0
        self.sems = []
        self.waited = {}

    def cur_sem(self):
        idx = self.n // SEM_WRAP
        while len(self.sems) <= idx:
            self.sems.append(self.fw.new_sem(f"tl_{self.name}_{len(self.sems)}"))
        return self.sems[idx]

    def wait_tokens(self, toks):
        best = {}
        for (sk, v) in toks:
            if v > best.get(sk, 0):
                best[sk] = v
        for sk, v in best.items():
            if self.waited.get(sk, 0) >= v:
                continue
            self.waited[sk] = v
            self.obj.wait_ge(self.fw.sem_by_key[sk], v)


class FW:
    def __init__(self, nc, stack):
        self.nc = nc
        self.sem_by_key = {}
        self._stack = stack
        self.all_res = []
        self.dma_reg = {}
        self.pe = Eng(self, "pe", nc.tensor, is_pe=True)
        self.act = Eng(self, "act", nc.scalar)
        self.dve = Eng(self, "dve", nc.vector)
        self.pool = Eng(self, "pool", nc.gpsimd)
        self.sp = Eng(self, "sp", nc.sync)
        self.engs = [self.pe, self.act, self.dve, self.pool, self.sp]

    def res(self, name):
        r = Res(name)
        self.all_res.append(r)
        return r

    def new_sem(self, name):
        h = self._stack.enter_context(self.nc.semaphore(name))
        key = len(self.sem_by_key)
        self.sem_by_key[key] = h
        return key

    def op(self, eng, fn, reads=(), writes=()):
        deps = []
        own = set(eng.sems)
        for r in reads:
            if r.last_w is not None:
                deps.append(r.last_w)
        for w in writes:
            if w.last_w is not None:
                deps.append(w.last_w)
            for t in w.readers:
                if t[0] in own:
                    continue
                deps.append(t)
        if eng.is_pe:
            deps = [t for t in deps if t[0] not in own]
        eng.wait_tokens(deps)
        ins = fn(eng.obj)
        sk = eng.cur_sem()
        val = (eng.n % SEM_WRAP) + 1
        eng.n += 1
        ins.then_inc(self.sem_by_key[sk], 1)
        tok = (sk, val)
        for r in reads:
            r.readers.append(tok)
        for w in writes:
            w.last_w = tok
            w.readers = []
        return tok

    def dma(self, q, out, in_, sb_res, reads=(), writes=()):
        deps = []
        for r in reads:
            if r.last_w is not None:
                deps.append(r.last_w)
        for w in writes:
            if w.last_w is not None:
                deps.append(w.last_w)
            deps.extend(w.readers)
        q.wait_tokens(deps)
        ent = self.dma_reg.setdefault(sb_res.name, [None, 0])
        if ent[0] is None:
            ent[0] = self.new_sem(f"dma_{sb_res.name}")
        ins = q.obj.dma_start(out=out, in_=in_)
        ent[1] += 16
        ins.then_inc(self.sem_by_key[ent[0]], 16)
        tok = (ent[0], ent[1])
        for r in reads:
            r.readers.append(tok)
        for w in writes:
            w.last_w = tok
            w.readers = []
        return tok

    def all_tokens(self):
        toks = []
        for r in self.all_res:
            if r.last_w is not None:
                toks.append(r.last_w)
            toks.extend(r.readers)
        for e in self.engs:
            if e.n > 0:
                idx = (e.n - 1) // SEM_WRAP
                toks.append((e.sems[idx], ((e.n - 1) % SEM_WRAP) + 1))
        return toks

    def barrier(self):
        toks = self.all_tokens()
        for e in self.engs:
            e.wait_tokens(toks)


import os as _os
_DBG = _os.environ.get("KDBG", "full")


def build_program(seq_lens, sg_tok):
    T = sum(seq_lens)
    NT = T // 128
    NG = T // 512
    max_nt = max(seq_lens) // 128
    assert all(n % 512 == 0 for n in seq_lens)
    assert T % sg_tok == 0 and sg_tok % 512 == 0
    nc = bass.Bass("TRN2", target_bir_lowering=False)

    def din(name, shape, dt=F32):
        return nc.dram_tensor(name, list(shape), dt, kind="ExternalInput").ap()

    x = din("x", [T, D])
    w_in = din("w_in", [D, 2048])
    w_out = din("w_out", [D, D])
    w_pool = din("w_pool", [4, 128, 128])
    w_gate = din("w_gate", [NE * 128, 8 * DFF])
    w_up = din("w_up", [NE * 128, 8 * DFF])
    w_down = din("w_down", [NE * 128, 4 * D])
    w_r = din("w_r", [D, 36])
    gmix_t = din("gmix_t", [128, 8])
    wos_t = din("wos_t", [128, 8])
    gffn_b = din("gffn_b", [128, D])
    gfin_b = din("gfin_b", [128, D])
    bias_b = din("bias_b", [128, 36])
    lam_b = din("lam_b", [128, 4, 64])
    ident_bf_d = din("ident_bf", [128, 128], BF16)
    ident_f_d = din("ident_f", [128, 128])
    cs_tab_d = din("cs_tab", [128, max_nt, 2, 32])
    invc_d = din("invc", [128, 3, 4, 128])
    y = nc.dram_tensor("y", [T, D], F32, kind="ExternalOutput").ap()
    qd = nc.dram_tensor("qd", [NG, 128, 4, 512], BF16, kind="Internal").ap()
    pd = nc.dram_tensor("pd", [NG, 128, 4, 4, 128], BF16, kind="Internal").ap()
    h_d = nc.dram_tensor("h_d", [NT, 128, D], F32, kind="Internal").ap()
    hn_d = nc.dram_tensor("hn_d", [NT, 128, D], BF16, kind="Internal").ap()
    NB = (2 * T) // 128 + NE
    PR = NB * 128
    xg = nc.dram_tensor("xg", [PR, D], BF16, kind="Internal").ap()
    yg = nc.dram_tensor("yg", [PR, D], F32, kind="Internal").ap()
    tri_d = din("tri_bf", [128, 128], BF16)
    bstart_d = din("bstart", [128, NB])
    pidx_d = din("pidx", [128, 1])

    with contextlib.ExitStack() as st0:
        fw = FW(nc, st0)
        R = fw.res

        sfx = [""]

        def sb(st, name, shape, dt):
            return st.enter_context(nc.sbuf_tensor("s_" + name + sfx[0], list(shape), dt))

        lg_all = sb(st0, "lg_all", [128, NT, 36], F32)
        ones_bf = sb(st0, "ones_bf", [128, 128], BF16)
        ones_f = sb(st0, "ones_f", [128, 128], F32)
        r_gates = [R(f"gates{i}") for i in range(NT)]
        r_gfin = R("gfin")
        r_ones = R("ones")
        eps_t = sb(st0, "eps_t", [128, 1], F32)
        fw.op(fw.dve, lambda e: e.memset(eps_t[:], EPS), writes=[r_ones])
        fw.op(fw.dve, lambda e: e.memset(ones_f[:], 1.0), writes=[r_ones])
        fw.op(fw.dve, lambda e: e.tensor_copy(out=ones_bf[:], in_=ones_f[:]), reads=[r_ones], writes=[r_ones])

        with contextlib.ExitStack() as st:
            S0 = st.enter_context(nc.psum_tensor("S0", [128, 2, 512], F32))
            S1 = st.enter_context(nc.psum_tensor("S1", [128, 2, 512], F32))
            PO = st.enter_context(nc.psum_tensor("PO", [128, 2, 512], F32))
            PL = st.enter_context(nc.psum_tensor("PL", [128, 2, 512], F32))
            r_S0, r_S1, r_PO, r_PL = R("S0"), R("S1"), R("PO"), R("PL")
            r_S0a, r_S0b, r_S1a, r_S1b = R("S0a"), R("S0b"), R("S1a"), R("S1b")
            r_POa, r_POb, r_PLa, r_PLb = R("POa"), R("POb"), R("PLa"), R("PLb")
            r_psr = [R("psr0"), R("psr1")]

            Win = sb(st, "Win", [128, 8, 2048], BF16)
            Wout = sb(st, "Wout", [128, 8, 1024], BF16)
            Wp = sb(st, "Wp", [128, 4, 128], BF16)
            Wr = sb(st, "Wr", [128, 8, 36], F32)
            gmix = sb(st, "gmix", [128, 8], F32)
            wos = sb(st, "wos", [128, 8], F32)
            gffn = sb(st, "gffn", [128, D], F32)
            biasb = sb(st, "biasb", [128, 36], F32)
            lamt = sb(st, "lamt", [128, 4, 64], F32)
            lamp = sb(st, "lamp", [128, 2, 64], F32)
            lams = sb(st, "lams", [128, 2], F32)
            neglam = sb(st, "neglam", [128, 1], F32)
            ident_bf = sb(st, "ident_bf", [128, 128], BF16)
            ident_f = sb(st, "ident_f", [128, 128], F32)
            invc = sb(st, "invc", [128, 3, 4, 128], F32)
            st_w = contextlib.ExitStack()
            wstage = sb(st_w, "wstage", [128, 2048], F32)
            r_Win, r_Wout, r_Wp, r_Wr, r_wstage = R("Win"), R("Wout"), R("Wp"), R("Wr"), R("wstage")
            r_c = R("consts")
            r_lam = R("lam")
            for (t_sb, t_dr) in ((gmix, gmix_t), (wos, wos_t), (gffn, gffn_b), (biasb, bias_b),
                                 (ident_bf, ident_bf_d), (ident_f, ident_f_d), (invc, invc_d)):
                fw.dma(fw.sp, t_sb[:], t_dr, r_c, writes=[r_c])
            fw.dma(fw.sp, lamt[:], lam_b, r_lam, writes=[r_lam])
            fw.dma(fw.sp, Wr[:], w_r.rearrange("(c p) j -> p c j", p=128), r_Wr, writes=[r_Wr])
            fw.dma(fw.pool, Wp[:], w_pool.rearrange("g c e -> c g e"), r_Wp, writes=[r_Wp])
            for c in range(8):
                fw.dma(fw.sp, wstage[:], w_in[c * 128:(c + 1) * 128, :], r_wstage, writes=[r_wstage])
                fw.op(fw.act, lambda e, c=c: e.activation(out=Win[:, c, :], in_=wstage[:], func=AF.Copy,
                                                          scale=gmix[:, c:c + 1]),
                      reads=[r_wstage, r_c], writes=[r_Win])
            for c in range(8):
                fw.dma(fw.sp, wstage[:, 0:1024], w_out[c * 128:(c + 1) * 128, :], r_wstage, writes=[r_wstage])
                fw.op(fw.act, lambda e, c=c: e.activation(out=Wout[:, c, :], in_=wstage[:, 0:1024], func=AF.Copy,
                                                          scale=wos[:, c:c + 1]),
                      reads=[r_wstage, r_c], writes=[r_Wout])
            lam4 = lamt[:].rearrange("p (a b) d -> p a b d", b=2)
            fw.op(fw.dve, lambda e: e.tensor_tensor(out=lamp[:], in0=lam4[:, :, 0, :], in1=lam4[:, :, 1, :], op=ALU.mult),
                  reads=[r_lam], writes=[r_lam])
            fw.op(fw.dve, lambda e: e.tensor_reduce(out=lams[:], in_=lamp[:], axis=AX.X, op=ALU.add),
                  reads=[r_lam], writes=[r_lam])
            fw.op(fw.act, lambda e: e.activation(out=lams[:], in_=lams[:], func=AF.Exp), reads=[r_lam], writes=[r_lam])
            fw.op(fw.dve, lambda e: e.tensor_tensor(out=neglam[:], in0=lams[:, 1:2], in1=lams[:, 0:1], op=ALU.subtract),
                  reads=[r_lam], writes=[r_lam])
            fw.op(fw.dve, lambda e: e.tensor_scalar(out=neglam[:], in0=neglam[:], scalar1=-LAM_INIT, scalar2=None,
                                                    op0=ALU.add), reads=[r_lam], writes=[r_lam])

            fw.barrier()
            st_w.close()
            max_n = max(seq_lens)
            kT = sb(st, "kT", [128, 4, max_n], BF16)
            V = sb(st, "V", [128, max_n // 128, 512], BF16)
            r_kT, r_V = R("kT"), R("V")


            def rstd_chain(src, dst, res_list, inv_n):
                fw.op(fw.act, lambda e: e.activation(out=dst, in_=src, func=AF.Ln, bias=eps_t[:], scale=inv_n),
                      reads=res_list + [r_ones], writes=res_list)
                fw.op(fw.act, lambda e: e.activation(out=dst, in_=dst, func=AF.Exp, scale=-0.5),
                      reads=res_list, writes=res_list)

            psT = PO[:, 0, :].bitcast(BF16).rearrange("p (c t) -> p c t", c=8)
            psT2 = PO[:, 1, :].bitcast(BF16).rearrange("p (c t) -> p c t", c=8)
            ps_qk = S0[:].rearrange("p a (b d) -> p (a b) d", d=32)
            ps_qk4 = S0[:].rearrange("p a (b h d) -> p (a b) h d", h=2, d=32)
            ps_v = S1[:, 0, :]
            ps_p = S1[:, 1, :].rearrange("p (g t) -> p g t", g=4)
            ps_po = PL[:, 0, :].rearrange("p (g t) -> p g t", g=4)

            tok0 = 0
            g0 = 0
            for si, N in enumerate(seq_lens):
                nt = N // 128
                ng = N // 512

                sfx[0] = f"_s{si}"
                st1 = contextlib.ExitStack()
                cs_tab = sb(st1, "cs_tab", [128, max_nt, 2, 32], F32)
                r_cs = R("cs_tab")
                fw.dma(fw.sp, cs_tab[:], cs_tab_d, r_cs, writes=[r_cs])
                xt = [sb(st1, f"xt{i}", [128, D], F32) for i in range(2)]
                r_xt = [R(f"xt{i}") for i in range(2)]
                junk = sb(st1, "junk", [128, D], BF16)
                r_junk = R("junk")
                ss = [sb(st1, f"ss{i}", [128, 1], F32) for i in range(2)]
                r_ss = [R(f"ss{i}") for i in range(2)]
                xs = [sb(st1, f"xs{i}", [128, D], BF16) for i in range(2)]
                r_xs = [R(f"xs{i}") for i in range(2)]
                xnT = [sb(st1, f"xnT{i}", [128, 8, 128], BF16) for i in range(2)]
                r_xnT = [R(f"xnT{i}") for i in range(2)]
                rp = [sb(st1, f"rp{i}", [128, 16, 32], F32) for i in range(4)]
                r_rp = R("rp")
                qkr = sb(st1, "qkr", [128, 16, 2, 32], BF16)
                r_qkr = R("qkr")
                qst = [sb(st1, f"qst{i}", [128, 4, 128], BF16) for i in range(2)]
                r_qst = [R(f"qst{i}") for i in range(2)]
                pst = [sb(st1, f"pst{i}", [128, 4, 128], BF16) for i in range(2)]
                r_pst = [R(f"pst{i}") for i in range(2)]
                zpt = [sb(st1, f"zpt{i}", [128, 4, 128], F32) for i in range(3)]
                r_zpt = [R(f"zpt{i}") for i in range(3)]
                ZW = sb(st1, "ZW", [128, 4, 144], F32)
                za = sb(st1, "za", [128, 4, 144], F32)
                zb = sb(st1, "zb", [128, 4, 144], F32)
                zc = sb(st1, "zc", [128, 4, 144], F32)
                zd = sb(st1, "zd", [128, 4, 144], F32)
                pw = sb(st1, "pw", [128, 4, 128], F32)
                pooled2 = [sb(st1, f"pooled{k}", [128, 4, 128], BF16) for k in range(2)]
                r_pm = R("poolmix")
                r_pooled2 = [R("pooled0"), R("pooled1")]

                def pool_stage(i):
                    cur = zpt[i % 3]
                    deps_r = [r_zpt[i % 3]]
                    if i > 0:
                        prev = zpt[(i - 1) % 3]
                        deps_r.append(r_zpt[(i - 1) % 3])
                        fw.op(fw.pool, lambda e: e.tensor_copy(out=ZW[:, :, 0:8], in_=prev[:, :, 120:128]),
                              reads=deps_r, writes=[r_pm])
                    else:
                        fw.op(fw.pool, lambda e: e.memset(ZW[:, :, 0:8], 0.0), writes=[r_pm])
                    fw.op(fw.pool, lambda e: e.tensor_copy(out=ZW[:, :, 8:136], in_=cur[:]), reads=deps_r, writes=[r_pm])
                    if i < nt - 1:
                        nxt = zpt[(i + 1) % 3]
                        fw.op(fw.pool, lambda e: e.tensor_copy(out=ZW[:, :, 136:144], in_=nxt[:, :, 0:8]),
                              reads=[r_zpt[(i + 1) % 3]], writes=[r_pm])
                    else:
                        fw.op(fw.pool, lambda e: e.memset(ZW[:, :, 136:144], 0.0), writes=[r_pm])
                    P = fw.pool
                    fw.op(P, lambda e: e.tensor_tensor(out=za[:, :, 0:143], in0=ZW[:, :, 0:143], in1=ZW[:, :, 1:144], op=ALU.add),
                          reads=[r_pm], writes=[r_pm])
                    fw.op(P, lambda e: e.tensor_tensor(out=zb[:, :, 0:141], in0=za[:, :, 0:141], in1=za[:, :, 2:143], op=ALU.add),
                          reads=[r_pm], writes=[r_pm])
                    fw.op(P, lambda e: e.tensor_tensor(out=zc[:, :, 0:137], in0=zb[:, :, 0:137], in1=zb[:, :, 4:141], op=ALU.add),
                          reads=[r_pm], writes=[r_pm])
                    fw.op(P, lambda e: e.tensor_tensor(out=zd[:, :, 0:129], in0=zc[:, :, 0:129], in1=zc[:, :, 8:137], op=ALU.add),
                          reads=[r_pm], writes=[r_pm])
                    kind = 0 if i == 0 else (2 if i == nt - 1 else 1)
                    srcs = [za[:, 0, 7:135], zb[:, 1, 6:134], zc[:, 2, 4:132], zd[:, 3, 0:128]]
                    for g in range(4):
                        fw.op(P, lambda e, g=g: e.tensor_tensor(out=pw[:, g, :], in0=srcs[g], in1=invc[:, kind, g, :], op=ALU.mult),
                              reads=[r_pm, r_c], writes=[r_pm])
                    fw.op(P, lambda e: e.tensor_tensor(out=pooled2[i % 2][:], in0=pw[:], in1=cur[:], op=ALU.subtract),
                          reads=[r_pm, r_zpt[i % 3]], writes=[r_pooled2[i % 2]])

                def pool_mm(i):
                    pooled = pooled2[i % 2]
                    r_pooled = r_pooled2[i % 2]

                    def mm_pool(e):
                        ins = None
                        for g in range(4):
                            ins = e.matmul(ps_po[:, g, :], Wp[:, g, :], pooled[:, g, :], start=True, stop=True)
                        return ins
                    fw.op(fw.pe, mm_pool, reads=[r_pooled, r_Wp], writes=[r_PLa])
                    sl = i % 2
                    fw.op(fw.act, lambda e: e.activation(out=pst[sl][:], in_=ps_po, func=AF.Copy),
                          reads=[r_PLa], writes=[r_pst[sl]])
                    g_idx = g0 + i // 4
                    fw.dma(fw.sp, pd[g_idx, :, i % 4, :, :], pst[sl][:], r_pst[sl], reads=[r_pst[sl]])

                def stageA_load(i):
                    sl = i % 2
                    fw.dma(fw.sp, xt[sl][:], x[tok0 + i * 128: tok0 + (i + 1) * 128, :], r_xt[sl], writes=[r_xt[sl]])

                def stageA(i):
                    sl = i % 2
                    fw.op(fw.act, lambda e: e.activation(out=junk[:], in_=xt[sl][:], func=AF.Square, accum_out=ss[sl][:]),
                          reads=[r_xt[sl]], writes=[r_junk, r_ss[sl]])
                    rstd_chain(ss[sl][:], ss[sl][:], [r_ss[sl]], 1.0 / D)
                    fw.op(fw.act, lambda e: e.activation(out=xs[sl][:], in_=xt[sl][:], func=AF.Copy, scale=ss[sl][:]),
                          reads=[r_xt[sl], r_ss[sl]], writes=[r_xs[sl]])

                    def tr1(e):
                        ins = None
                        for c in range(8):
                            ins = e.transpose(psT[:, c, :], xs[sl][:, c * 128:(c + 1) * 128], ident_bf[:])
                        return ins
                    fw.op(fw.pe, tr1, reads=[r_xs[sl], r_c], writes=[r_POa])
                    fw.op(fw.dve, lambda e: e.tensor_copy(out=xnT[sl][:], in_=psT), reads=[r_POa], writes=[r_xnT[sl]])

                def stageB(i):
                    sl = i % 2

                    def mm_qkv(e):
                        ins = None
                        for j, dst in enumerate((S0[:, 0, :], S0[:, 1, :], ps_v)):
                            for c in range(8):
                                ins = e.matmul(dst, xnT[sl][:, c, :], Win[:, c, 512 + j * 512: 1024 + j * 512],
                                               start=(c == 0), stop=(c == 7))
                        for g in range(4):
                            for c in range(8):
                                ins = e.matmul(ps_p[:, g, :], Win[:, c, g * 128:(g + 1) * 128], xnT[sl][:, c, :],
                                               start=(c == 0), stop=(c == 7))
                        return ins
                    fw.op(fw.pe, mm_qkv, reads=[r_xnT[sl], r_Win], writes=[r_S0a, r_S0b, r_S1a, r_S1b])
                    fw.op(fw.act, lambda e: e.activation(out=V[:, i, :], in_=ps_v, func=AF.Copy), reads=[r_S1a], writes=[r_V])
                    fw.op(fw.act, lambda e: e.activation(out=zpt[i % 3][:], in_=ps_p, func=AF.Copy),
                          reads=[r_S1b], writes=[r_zpt[i % 3]])
                    cosb = cs_tab[:, i, 0, :].unsqueeze(1).broadcast_to([128, 16, 32])
                    sinb = cs_tab[:, i, 1, :].unsqueeze(1).broadcast_to([128, 16, 32])
                    x1 = ps_qk4[:, :, 0, :]
                    x2 = ps_qk4[:, :, 1, :]
                    fw.op(fw.dve, lambda e: e.tensor_tensor(out=rp[0][:], in0=x1, in1=cosb, op=ALU.mult), reads=[r_S0a, r_S0b, r_cs], writes=[r_rp])
                    fw.op(fw.dve, lambda e: e.tensor_tensor(out=rp[1][:], in0=x2, in1=sinb, op=ALU.mult), reads=[r_S0a, r_S0b, r_cs], writes=[r_rp])
                    fw.op(fw.dve, lambda e: e.tensor_tensor(out=rp[2][:], in0=x2, in1=cosb, op=ALU.mult), reads=[r_S0a, r_S0b, r_cs], writes=[r_rp])
                    fw.op(fw.dve, lambda e: e.tensor_tensor(out=rp[3][:], in0=x1, in1=sinb, op=ALU.mult), reads=[r_S0a, r_S0b, r_cs], writes=[r_rp])
                    fw.op(fw.dve, lambda e: e.tensor_tensor(out=qkr[:, :, 0, :], in0=rp[0][:], in1=rp[1][:], op=ALU.subtract), reads=[r_rp], writes=[r_qkr])
                    fw.op(fw.dve, lambda e: e.tensor_tensor(out=qkr[:, :, 1, :], in0=rp[2][:], in1=rp[3][:], op=ALU.add), reads=[r_rp], writes=[r_qkr])

                def stageB2(i):
                    sl = i % 2
                    qkr_f = qkr[:].rearrange("p a h d -> p (a h d)")

                    def tr2(e):
                        ins = None
                        for c in range(8):
                            ins = e.transpose(psT2[:, c, :], qkr_f[:, c * 128:(c + 1) * 128], ident_bf[:])
                        return ins
                    fw.op(fw.pe, tr2, reads=[r_qkr, r_c], writes=[r_POb])
                    fw.op(fw.dve, lambda e: e.tensor_copy(out=kT[:, :, i * 128:(i + 1) * 128], in_=psT2[:, 4:8, :]),
                          reads=[r_POb], writes=[r_kT])
                    fw.op(fw.dve, lambda e: e.tensor_copy(out=qst[sl][:], in_=psT2[:, 0:4, :]), reads=[r_POb], writes=[r_qst[sl]])
                    fw.dma(fw.sp, qd[g0 + i // 4, :, :, (i % 4) * 128:(i % 4 + 1) * 128], qst[sl][:], r_qst[sl], reads=[r_qst[sl]])

                stageA_load(0)
                stageA_load(1)
                stageA(0)
                stageA(1)
                for i in range(nt):
                    if i + 2 < nt:
                        stageA_load(i + 2)
                    stageB(i)
                    if i + 2 < nt:
                        stageA(i + 2)
                    stageB2(i)
                    if i >= 1:
                        pool_stage(i - 1)
                    if i >= 2:
                        pool_mm(i - 2)
                pool_stage(nt - 1)
                pool_mm(nt - 2)
                pool_mm(nt - 1)
                r_qd = R("qd_seq")
                fw.barrier()

                st1.close()
                st2 = contextlib.ExitStack()
                qpad = sb(st2, "qpad", [128, 4, 2, 512], BF16)
                r_qpad = R("qpad")
                poolT = sb(st2, "poolT", [128, 4, 4, 128], BF16)
                r_poolT = R("poolT")
                pT = [sb(st2, f"pT{i}", [128, 2, 512], BF16) for i in range(4)]
                r_pT = [R(f"pT{i}") for i in range(4)]
                pTs = [sb(st2, f"pTs{i}", [128, 2, 512], BF16) for i in range(2)]
                r_pTs = [R(f"pTs{i}") for i in range(2)]
                rl = sb(st2, "rl", [128, 2, 512], F32)
                on = rl
                od = sb(st2, "od", [128, 512], F32)
                osq = rl[:, 0, :]
                rn = rl[:, 1, :]
                r_ep = R("epi")
                attnT = sb(st2, "attnT", [128, 4, 512], BF16)
                r_attnT = R("attnT")
                xr = [sb(st2, f"xr{i}", [128, D], F32) for i in range(2)]
                r_xr = [R(f"xr{i}") for i in range(2)]
                ht = [sb(st2, f"ht{i}", [128, D], F32) for i in range(2)]
                r_ht = [R(f"ht{i}") for i in range(2)]
                hnf2 = [sb(st2, f"hnf{k}", [128, D], F32) for k in range(2)]
                r_hnf2 = [R("hnf0"), R("hnf1")]
                hnTf = sb(st2, "hnTf", [128, 8, 128], F32)
                r_hnTf = R("hnTf")
                hnTb = [sb(st2, f"hnTb{i}", [128, D], BF16) for i in range(2)]
                r_hnTb = [R(f"hnTb{i}") for i in range(2)]
                ss2 = sb(st2, "ss2", [128, 1], F32)
                r_ss2 = R("ss2")

                fw.op(fw.pool, lambda e: e.memset(qpad[:], 0.0), writes=[r_qpad])
                nk = nt
                for j in range(ng if _DBG not in ("p1",) else 0):
                    gi = g0 + j
                    for m in range(2):
                        dst = qpad[m * 64:(m + 1) * 64, :, m, :]
                        fw.dma(fw.sp, dst, qd[gi, m * 64:(m + 1) * 64, :, :], r_qpad, writes=[r_qpad])
                    fw.dma(fw.sp, poolT[:], pd[gi], r_poolT, writes=[r_poolT])
                    units = [(hh, kk) for hh in range(4) for kk in range(nk)]

                    def emit_qk(ui):
                        hh, kk = units[ui]
                        bb = ui % 2
                        Sb = S0 if bb == 0 else S1
                        r_Sb = [r_S0a, r_S0b] if bb == 0 else [r_S1a, r_S1b]

                        def mm_s(e):
                            e.matmul(Sb[:, 0, :], kT[:, hh, kk * 128:(kk + 1) * 128], qpad[:, hh, 0, :], start=True, stop=True)
                            return e.matmul(Sb[:, 1, :], kT[:, hh, kk * 128:(kk + 1) * 128], qpad[:, hh, 1, :], start=True, stop=True)
                        fw.op(fw.pe, mm_s, reads=[r_kT, r_qpad], writes=r_Sb)

                    def emit_exp_pv(ui):
                        hh, kk = units[ui]
                        bb = ui % 2
                        p4 = ui % 4
                        Sb = S0 if bb == 0 else S1
                        r_Sb = [r_S0a, r_S0b] if bb == 0 else [r_S1a, r_S1b]
                        fw.op(fw.act, lambda e: e.activation(out=pT[p4][:], in_=Sb[:], func=AF.Exp, scale=0.125),
                              reads=r_Sb, writes=[r_pT[p4]])
                        if ui + 2 < len(units):
                            emit_qk(ui + 2)

                        def mm_pv(e):
                            ins = None
                            for m in range(2):
                                ins = e.matmul(PO[:, m, :], V[:, kk, hh * 128:(hh + 1) * 128], pT[p4][:, m, :],
                                               start=(kk == 0), stop=(kk == nk - 1))
                            return ins
                        fw.op(fw.pe, mm_pv, reads=[r_V, r_pT[p4]], writes=[r_POa, r_POb])
                        def mm_l_for(ps2_, kk_):
                            def mm_l(e):
                                ins = None
                                for m in range(2):
                                    ins = e.matmul(PL[:, m, :], ones_bf[:], pTs[ps2_][:, m, :],
                                                   start=(kk_ == 1), stop=(kk_ == nk - 1))
                                return ins
                            fw.op(fw.pe, mm_l, reads=[r_pTs[ps2_], r_ones], writes=[r_PLa, r_PLb])
                        if kk % 2 == 0 and pend_l:
                            mm_l_for(*pend_l.pop())
                        if kk % 2 == 1:
                            ps2 = (ui // 2) % 2
                            pprev = (ui - 1) % 4
                            fw.op(fw.dve, lambda e: e.tensor_tensor(out=pTs[ps2][:], in0=pT[pprev][:], in1=pT[p4][:], op=ALU.add),
                                  reads=[r_pT[pprev], r_pT[p4]], writes=[r_pTs[ps2]])
                            if kk == nk - 1:
                                mm_l_for(ps2, kk)
                            else:
                                pend_l.append((ps2, kk))

                    pend_l = []
                    emit_qk(0)
                    if len(units) > 1:
                        emit_qk(1)
                    for ui in range(len(units)):
                        h, kc = units[ui]
                        emit_exp_pv(ui)
                        if kc != nk - 1:
                            continue
                        fw.op(fw.dve, lambda e: e.reciprocal(out=rl[:], in_=PL[:]), reads=[r_PLa, r_PLb], writes=[r_ep])
                        fw.op(fw.dve, lambda e: e.tensor_tensor(out=on[:], in0=PO[:], in1=rl[:], op=ALU.mult),
                              reads=[r_POa, r_POb, r_ep], writes=[r_ep])
                        fw.op(fw.dve, lambda e: e.scalar_tensor_tensor(out=od[:], in0=on[:, 1, :], scalar=neglam[:], in1=on[:, 0, :],
                                                                       op0=ALU.mult, op1=ALU.add),
                              reads=[r_ep, r_lam], writes=[r_ep])
                        fw.op(fw.act, lambda e: e.activation(out=osq[:], in_=od[:], func=AF.Square), reads=[r_ep], writes=[r_ep])
                        fw.op(fw.pe, lambda e: e.matmul(PL[:, 0, :], ones_f[:], osq[:], start=True, stop=True),
                              reads=[r_ep, r_ones], writes=[r_PLa])
                        fw.op(fw.act, lambda e: e.activation(out=rn[:], in_=PL[:, 0, :], func=AF.Ln, bias=eps_t[:], scale=1.0 / 128),
                              reads=[r_PLa, r_ones], writes=[r_ep])
                        fw.op(fw.act, lambda e: e.activation(out=rn[:], in_=rn[:], func=AF.Exp, scale=-0.5), reads=[r_ep], writes=[r_ep])
                        fw.op(fw.dve, lambda e, h=h: e.scalar_tensor_tensor(out=attnT[:, h, :], in0=od[:], scalar=1.0 - LAM_INIT, in1=rn[:],
                                                                            op0=ALU.mult, op1=ALU.mult),
                              reads=[r_ep], writes=[r_attnT])
                    def stage2A(ti):
                        tg = (tok0 // 128) + j * 4 + ti
                        sl = ti % 2
                        Hb = S0 if sl == 0 else S1
                        r_Hb = [r_S0a, r_S0b] if sl == 0 else [r_S1a, r_S1b]
                        fw.dma(fw.sp, xr[sl][:], x[tg * 128:(tg + 1) * 128, :], r_xr[sl], writes=[r_xr[sl]])

                        def mm_o(e, ti=ti, Hb=Hb):
                            ins = None
                            for cc in range(2):
                                for c in range(8):
                                    lhsT = poolT[:, ti, c, :] if c < 4 else attnT[:, c - 4, ti * 128:(ti + 1) * 128]
                                    ins = e.matmul(Hb[:, cc, :], lhsT, Wout[:, c, cc * 512:(cc + 1) * 512],
                                                   start=(c == 0), stop=(c == 7))
                            return ins
                        fw.op(fw.pe, mm_o, reads=[r_poolT, r_attnT, r_Wout], writes=r_Hb)
                        Hf = Hb[:].rearrange("p a b -> p (a b)")
                        fw.op(fw.dve, lambda e, Hf=Hf, sl=sl: e.tensor_tensor(out=ht[sl][:], in0=Hf, in1=xr[sl][:], op=ALU.add),
                              reads=r_Hb + [r_xr[sl]], writes=[r_ht[sl]])
                        fw.dma(fw.act, h_d[tg], ht[sl][:], r_ht[sl], reads=[r_ht[sl]])
                        fw.op(fw.act, lambda e, sl=sl: e.activation(out=hnTb[sl][:], in_=ht[sl][:], func=AF.Square, accum_out=ss2[:]),
                              reads=[r_ht[sl]], writes=[r_hnTb[sl], r_ss2])
                        rstd_chain(ss2[:], ss2[:], [r_ss2], 1.0 / D)
                        hnf = hnf2[sl]
                        r_hnf = r_hnf2[sl]
                        fw.op(fw.dve, lambda e, sl=sl: e.scalar_tensor_tensor(out=hnf[:], in0=ht[sl][:], scalar=ss2[:], in1=gffn[:],
                                                                              op0=ALU.mult, op1=ALU.mult),
                              reads=[r_ht[sl], r_ss2, r_c], writes=[r_hnf])
                        fw.op(fw.pool, lambda e, sl=sl: e.tensor_copy(out=hnTb[sl][:], in_=hnf[:]),
                              reads=[r_hnf], writes=[r_hnTb[sl]])
                        fw.dma(fw.pool, hn_d[tg], hnTb[sl][:], r_hnTb[sl], reads=[r_hnTb[sl]])

                    def stage2A2(ti):
                        sl = ti % 2
                        hnf = hnf2[sl]
                        r_hnf = r_hnf2[sl]
                        psTf = PO[:].rearrange("p a (c t) -> p (a c) t", t=128)

                        def tr3(e):
                            ins = None
                            for c in range(8):
                                ins = e.matmul(psTf[:, c, :], hnf[:, c * 128:(c + 1) * 128], ident_f[:], start=True, stop=True)
                            return ins
                        fw.op(fw.pe, tr3, reads=[r_hnf, r_c], writes=[r_POa, r_POb])
                        fw.op(fw.dve, lambda e: e.tensor_copy(out=hnTf[:], in_=psTf), reads=[r_POa, r_POb], writes=[r_hnTf])
                        ps_r = PL[:, 1, (ti % 2) * 64:(ti % 2) * 64 + 36]

                        def mm_r(e):
                            ins = None
                            for c in range(8):
                                ins = e.matmul(ps_r, hnTf[:, c, :], Wr[:, c, :], start=(c == 0), stop=(c == 7))
                            return ins
                        fw.op(fw.pe, mm_r, reads=[r_hnTf, r_Wr], writes=[r_psr[ti % 2], r_PLb])

                    def stage2B(ti):
                        tg = (tok0 // 128) + j * 4 + ti
                        ps_r = PL[:, 1, (ti % 2) * 64:(ti % 2) * 64 + 36]
                        fw.op(fw.dve, lambda e: e.tensor_tensor(out=lg_all[:, tg, :], in0=ps_r, in1=biasb[:], op=ALU.add),
                              reads=[r_psr[ti % 2], r_PLb, r_c], writes=[r_gates[tg]])

                    stage2A(0)
                    for ti in range(4):
                        if ti + 1 < 4:
                            stage2A(ti + 1)
                        stage2A2(ti)
                        stage2B(ti)
                tok0 += N
                g0 += ng
                fw.barrier()
                st2.close()
        fw.barrier()
        I32 = mybir.dt.int32
        with contextlib.ExitStack() as st:
            OH1 = sb(st, "OH1", [128, NT, 32], BF16)
            OH2 = sb(st, "OH2", [128, NT, 32], BF16)
            wts = sb(st, "wts", [128, NT, 2], F32)
            d1i = sb(st, "d1i", [128, NT], I32)
            d2i = sb(st, "d2i", [128, NT], I32)
            idxw = sb(st, "idxw", [128, NB], I32)
            r_rt3 = R("route3")
            TA = st.enter_context(nc.psum_tensor("TA", [128, 512], F32))
            TB = st.enter_context(nc.psum_tensor("TB", [128, 512], F32))
            G0 = st.enter_context(nc.psum_tensor("G0", [128, 2, 512], F32))
            G1 = st.enter_context(nc.psum_tensor("G1", [128, 2, 512], F32))
            Y0 = st.enter_context(nc.psum_tensor("Y0", [128, 2, 512], F32))
            r_TA, r_TB, r_G0, r_G1, r_Y0 = R("TA"), R("TB"), R("G0p"), R("G1p"), R("Y0p")
            with contextlib.ExitStack() as s0:
                r_r0 = R("router0")
                gq = sb(s0, "gq", [128, 8, NT], F32)
                mg = sb(s0, "mg", [128, NT, 4], F32)
                eg = sb(s0, "eg", [128, NT, 4], F32)
                t48 = sb(s0, "t48", [128, NT, 4, 8], F32)
                les = sb(s0, "les", [128, NT, 8], F32)
                les2 = sb(s0, "les2", [128, NT, 8], F32)
                k1 = sb(s0, "k1", [128, NT, 8], F32)
                k2 = sb(s0, "k2", [128, NT, 8], F32)
                gmax, gsum, m1, m2, ex, w1, w2 = (gq[:, i, :] for i in range(7))
                lg = lg_all[:, :, 0:4]
                le4 = lg_all[:, :, 4:36].rearrange("p t (g j) -> p t g j", g=4)
                rr0 = [r_r0]

                def d0(fn, extra=()):
                    fw.op(fw.dve, fn, reads=rr0 + list(extra), writes=rr0)

                def bc3(ap2, k):
                    return ap2.unsqueeze(2).broadcast_to([128, NT, k])
                d0(lambda e: e.tensor_reduce(out=gmax, in_=lg, axis=AX.X, op=ALU.max), r_gates)
                d0(lambda e: e.tensor_tensor(out=mg[:], in0=lg, in1=bc3(gmax, 4), op=ALU.is_equal), r_gates)
                d0(lambda e: e.tensor_tensor(out=eg[:], in0=lg, in1=bc3(gmax, 4), op=ALU.subtract), r_gates)
                fw.op(fw.act, lambda e: e.activation(out=eg[:], in_=eg[:], func=AF.Exp), reads=rr0, writes=rr0)
                d0(lambda e: e.tensor_reduce(out=gsum, in_=eg[:], axis=AX.X, op=ALU.add))
                d0(lambda e: e.reciprocal(out=gsum, in_=gsum))
                d0(lambda e: e.tensor_tensor(out=t48[:], in0=le4, in1=mg[:].unsqueeze(3).broadcast_to([128, NT, 4, 8]), op=ALU.mult), r_gates)
                d0(lambda e: e.tensor_reduce(out=les[:], in_=t48[:].rearrange("p t g j -> p t j g"), axis=AX.X, op=ALU.add))
                d0(lambda e: e.tensor_reduce(out=m1, in_=les[:], axis=AX.X, op=ALU.max))
                d0(lambda e: e.tensor_tensor(out=k1[:], in0=les[:], in1=bc3(m1, 8), op=ALU.is_equal))
                d0(lambda e: e.scalar_tensor_tensor(out=les2[:], in0=k1[:], scalar=-1e30, in1=les[:], op0=ALU.mult, op1=ALU.add))
                d0(lambda e: e.tensor_reduce(out=m2, in_=les2[:], axis=AX.X, op=ALU.max))
                d0(lambda e: e.tensor_tensor(out=k2[:], in0=les2[:], in1=bc3(m2, 8), op=ALU.is_equal))
                d0(lambda e: e.tensor_tensor(out=ex, in0=m2, in1=m1, op=ALU.subtract))
                fw.op(fw.act, lambda e: e.activation(out=ex, in_=ex, func=AF.Exp), reads=rr0, writes=rr0)
                d0(lambda e: e.tensor_scalar(out=w1, in0=ex, scalar1=1.0, scalar2=None, op0=ALU.add))
                d0(lambda e: e.reciprocal(out=w1, in_=w1))
                d0(lambda e: e.tensor_tensor(out=w1, in0=w1, in1=gsum, op=ALU.mult))
                d0(lambda e: e.tensor_tensor(out=w2, in0=w1, in1=ex, op=ALU.mult))
                mgb4 = mg[:].unsqueeze(3).broadcast_to([128, NT, 4, 8])
                o1v = OH1[:].rearrange("p t (g j) -> p t g j", g=4)
                o2v = OH2[:].rearrange("p t (g j) -> p t g j", g=4)
                fw.op(fw.dve, lambda e: e.tensor_tensor(out=o1v, in0=k1[:].unsqueeze(2).broadcast_to([128, NT, 4, 8]), in1=mgb4, op=ALU.mult),
                      reads=rr0, writes=r_gates)
                fw.op(fw.dve, lambda e: e.tensor_tensor(out=o2v, in0=k2[:].unsqueeze(2).broadcast_to([128, NT, 4, 8]), in1=mgb4, op=ALU.mult),
                      reads=rr0, writes=r_gates)
                fw.op(fw.dve, lambda e: e.tensor_copy(out=wts[:, :, 0], in_=w1), reads=rr0, writes=r_gates)
                fw.op(fw.dve, lambda e: e.tensor_copy(out=wts[:, :, 1], in_=w2), reads=rr0, writes=r_gates)
                fw.barrier()
            with contextlib.ExitStack() as sa:
                tri = sb(sa, "tri", [128, 128], BF16)
                bstart = sb(sa, "bstart", [128, NB], F32)
                pidx = sb(sa, "pidx", [128, 1], F32)
                r_ca = R("constA")
                fw.dma(fw.sp, tri[:], tri_d, r_ca, writes=[r_ca])
                fw.dma(fw.sp, bstart[:], bstart_d, r_ca, writes=[r_ca])
                fw.dma(fw.sp, pidx[:], pidx_d, r_ca, writes=[r_ca])
                NC3 = NT * 32
                Mb = sb(sa, "Mb", [128, NC3], BF16)
                rank = sb(sa, "rank", [128, NT, 32], F32)
                cnt = sb(sa, "cnt", [128, NT, 32], F32)
                sA = sb(sa, "sA", [128, NT, 32], F32)
                sB = sb(sa, "sB", [128, NT, 32], F32)
                cmp = sb(sa, "cmp", [128, NB, 32], F32)
                sm = sb(sa, "sm", [128, 8, 32], F32)
                ebf = sb(sa, "ebf", [128, NB], F32)
                eb2 = sb(sa, "eb2", [128, NB], F32)
                dfl = sb(sa, "dfl", [128, 2, NT], F32)
                Dv = fw.dve
                rr = [r_rt3]

                def dv(fn, extra=()):
                    fw.op(Dv, fn, reads=rr + list(extra), writes=rr)
                OH1f = OH1[:].rearrange("p t e -> p (t e)")
                OH2f = OH2[:].rearrange("p t e -> p (t e)")
                dv(lambda e: e.tensor_tensor(out=Mb[:], in0=OH1f, in1=OH2f, op=ALU.add), r_gates)
                rankf = rank[:].rearrange("p t e -> p (t e)")
                cntf = cnt[:].rearrange("p t e -> p (t e)")
                nch = (NC3 + 511) // 512
                for ch in range(nch):
                    c0 = ch * 512
                    c1 = min(NC3, c0 + 512)
                    w_ = c1 - c0
                    fw.op(fw.pe, lambda e: e.matmul(TA[:, 0:w_], tri[:], Mb[:, c0:c1], start=True, stop=True),
                          reads=[r_rt3, r_ca], writes=[r_TA])
                    fw.op(fw.pe, lambda e: e.matmul(TB[:, 0:w_], ones_bf[:], Mb[:, c0:c1], start=True, stop=True),
                          reads=[r_rt3, r_ones], writes=[r_TB])
                    fw.op(Dv, lambda e: e.tensor_copy(out=rankf[:, c0:c1], in_=TA[:, 0:w_]), reads=[r_TA], writes=rr)
                    fw.op(Dv, lambda e: e.tensor_copy(out=cntf[:, c0:c1], in_=TB[:, 0:w_]), reads=[r_TB], writes=rr)
                dv(lambda e: e.tensor_copy(out=sA[:], in_=cnt[:]))
                src, dst = sA, sB
                sft = 1
                while sft < NT:
                    dv(lambda e: e.tensor_tensor(out=dst[:, sft:, :], in0=src[:, sft:, :], in1=src[:, 0:NT - sft, :], op=ALU.add))
                    dv(lambda e: e.tensor_copy(out=dst[:, 0:sft, :], in_=src[:, 0:sft, :]))
                    src, dst = dst, src
                    sft *= 2
                incl = src
                other = dst
                tot = sm[:, 0, :]
                xm = sm[:, 1, :]
                padded = sm[:, 2, :]
                pA = sm[:, 3, :]
                pB = sm[:, 4, :]
                pstart = sm[:, 5, :]
                dv(lambda e: e.tensor_copy(out=tot, in_=incl[:, NT - 1, :]))
                dv(lambda e: e.tensor_scalar(out=xm, in0=tot, scalar1=1.0 / 128, scalar2=0.49609375, op0=ALU.mult, op1=ALU.add))
                dv(lambda e: e.tensor_scalar(out=xm, in0=xm, scalar1=8388608.0, scalar2=None, op0=ALU.add))
                dv(lambda e: e.tensor_scalar(out=xm, in0=xm, scalar1=-8388608.0, scalar2=None, op0=ALU.add))
                dv(lambda e: e.tensor_scalar(out=padded, in0=xm, scalar1=128.0, scalar2=None, op0=ALU.mult))
                dv(lambda e: e.tensor_copy(out=pA, in_=padded))
                ps_, pd_ = pA, pB
                sft = 1
                while sft < 32:
                    dv(lambda e: e.tensor_tensor(out=pd_[:, sft:], in0=ps_[:, sft:], in1=ps_[:, 0:32 - sft], op=ALU.add))
                    dv(lambda e: e.tensor_copy(out=pd_[:, 0:sft], in_=ps_[:, 0:sft]))
                    ps_, pd_ = pd_, ps_
                    sft *= 2
                pend = ps_
                dv(lambda e: e.tensor_tensor(out=pstart, in0=pend, in1=padded, op=ALU.subtract))
                dv(lambda e: e.tensor_tensor(out=other[:], in0=incl[:], in1=cnt[:], op=ALU.subtract))
                dv(lambda e: e.tensor_tensor(out=other[:], in0=other[:], in1=rank[:], op=ALU.add))
                dv(lambda e: e.tensor_tensor(out=other[:], in0=other[:], in1=pstart.unsqueeze(1).broadcast_to([128, NT, 32]), op=ALU.add))
                dest = other
                dv(lambda e: e.tensor_tensor(out=incl[:], in0=dest[:], in1=OH1[:], op=ALU.mult), r_gates)
                dv(lambda e: e.tensor_reduce(out=dfl[:, 0, :], in_=incl[:], axis=AX.X, op=ALU.add))
                dv(lambda e: e.tensor_tensor(out=incl[:], in0=dest[:], in1=OH2[:], op=ALU.mult), r_gates)
                dv(lambda e: e.tensor_reduce(out=dfl[:, 1, :], in_=incl[:], axis=AX.X, op=ALU.add))
                dv(lambda e: e.tensor_copy(out=d1i[:], in_=dfl[:, 0, :]))
                dv(lambda e: e.tensor_copy(out=d2i[:], in_=dfl[:, 1, :]))
                dv(lambda e: e.tensor_tensor(out=cmp[:], in0=pend.unsqueeze(1).broadcast_to([128, NB, 32]),
                                             in1=bstart[:].unsqueeze(2).broadcast_to([128, NB, 32]), op=ALU.is_le), [r_ca])
                dv(lambda e: e.tensor_reduce(out=ebf[:], in_=cmp[:], axis=AX.X, op=ALU.add))
                dv(lambda e: e.tensor_scalar(out=ebf[:], in0=ebf[:], scalar1=float(NE - 1), scalar2=None, op0=ALU.min))
                BIG = 1.0e6
                dv(lambda e: e.memset(eb2[:], 1.0))
                dv(lambda e: e.tensor_tensor(out=eb2[:, 2:], in0=ebf[:, 2:], in1=ebf[:, 0:NB - 2], op=ALU.not_equal))
                dv(lambda e: e.tensor_scalar(out=ebf[:], in0=ebf[:], scalar1=128.0, scalar2=pidx[:], op0=ALU.mult, op1=ALU.add), [r_ca])
                dv(lambda e: e.tensor_scalar(out=ebf[:], in0=ebf[:], scalar1=-BIG, scalar2=None, op0=ALU.add))
                dv(lambda e: e.tensor_tensor(out=ebf[:], in0=ebf[:], in1=eb2[:], op=ALU.mult))
                dv(lambda e: e.tensor_scalar(out=ebf[:], in0=ebf[:], scalar1=BIG, scalar2=None, op0=ALU.add))
                dv(lambda e: e.tensor_copy(out=idxw[:], in_=ebf[:]))
                fw.barrier()

            _bregs = {}

            def _breg(bound):
                if bound not in _bregs:
                    rg = nc.gpsimd.alloc_register(f"bnd{len(_bregs)}")
                    nc.gpsimd.reg_mov(rg, int(bound))
                    _bregs[bound] = rg
                return _bregs[bound]

            def indirect(out, out_off, in_, in_off, bound, sb_res, reads, writes):
                deps = []
                for r_ in reads:
                    if r_.last_w is not None:
                        deps.append(r_.last_w)
                for w_r in writes:
                    if w_r.last_w is not None:
                        deps.append(w_r.last_w)
                    deps.extend(w_r.readers)
                fw.pool.wait_tokens(deps)
                ent = fw.dma_reg.setdefault(sb_res.name, [None, 0])
                if ent[0] is None:
                    ent[0] = fw.new_sem(f"dma_{sb_res.name}")
                ins = nc.gpsimd.indirect_dma_start(
                    out=out, out_offset=(bass.IndirectOffsetOnAxis(ap=out_off, axis=0) if out_off is not None else None),
                    in_=in_, in_offset=(bass.IndirectOffsetOnAxis(ap=in_off, axis=0) if in_off is not None else None),
                    bounds_check=_breg(bound), oob_is_err=False)
                ent[1] += 16
                ins.then_inc(fw.sem_by_key[ent[0]], 16)
                tok = (ent[0], ent[1])
                for r_ in reads:
                    r_.readers.append(tok)
                for w_r in writes:
                    w_r.last_w = tok
                    w_r.readers = []

            with contextlib.ExitStack() as sbk:
                hb = [sb(sbk, f"hb{i}", [128, D], BF16) for i in range(3)]
                r_hb = [R(f"hb{i}") for i in range(3)]
                for i in range(NT):
                    s3 = i % 3
                    fw.dma(fw.sp, hb[s3][:], hn_d[i], r_hb[s3], writes=[r_hb[s3]])
                    indirect(xg[:, :], d1i[:, i:i + 1], hb[s3][:, :], None, PR - 1, r_hb[s3], [r_hb[s3], r_rt3], [])
                    indirect(xg[:, :], d2i[:, i:i + 1], hb[s3][:, :], None, PR - 1, r_hb[s3], [r_hb[s3], r_rt3], [])
                fw.barrier()
            wgv = w_gate[:, :]
            wuv = w_up[:, :]
            wdv = w_down[:, :]
            with contextlib.ExitStack() as sc:
                identb = sb(sc, "identb", [128, 128], BF16)
                r_idb = R("identb")
                fw.dma(fw.sp, identb[:], ident_bf_d, r_idb, writes=[r_idb])
                wg = [sb(sc, f"wg{i}", [128, 8 * DFF], BF16) for i in range(2)]
                wu = [sb(sc, f"wu{i}", [128, 8 * DFF], BF16) for i in range(2)]
                wd = [sb(sc, f"wd{i}", [128, 4 * D], BF16) for i in range(2)]
                r_wg = [R("wg0"), R("wg1")]
                r_wu = [R("wu0"), R("wu1")]
                r_wd = [R("wd0"), R("wd1")]
                xb = [sb(sc, f"xb{i}", [128, D], BF16) for i in range(2)]
                r_xb = [R("xb0"), R("xb1")]
                xgT = [sb(sc, f"xgT{i}", [128, 8, 128], BF16) for i in range(2)]
                r_xgT = [R("xgT0"), R("xgT1")]
                sgl = sb(sc, "sgl", [128, 512], F32)
                r_sgl = R("sgl")
                hidT = [sb(sc, f"hidT{i}", [128, 4, 128], BF16) for i in range(2)]
                r_hid = [R("hid0"), R("hid1")]
                yb = [sb(sc, f"yb{i}", [128, D], F32) for i in range(2)]
                r_yb = [R("yb0"), R("yb1")]
                Ts = [TA, TB]
                r_Ts = [r_TA, r_TB]
                Gs = [G0, G1]
                r_Gs = [r_G0, r_G1]
                for ws in range(2):
                    pass
                hidtok = [sb(sc, f"hidtok{i}", [128, DFF], BF16) for i in range(2)]
                r_hidtok = [R("hidtok0"), R("hidtok1")]
                sgl2 = [sgl, sb(sc, "sglb", [128, 512], F32)]
                r_sgl2 = [r_sgl, R("sglb")]
                xb3 = xb + [sb(sc, "xb2", [128, D], BF16)]
                r_xb3 = r_xb + [R("xb2")]
                xgT3 = xgT + [sb(sc, "xgT2", [128, 8, 128], BF16)]
                r_xgT3 = r_xgT + [R("xgT2")]
                psT3 = TA[:].bitcast(BF16).rearrange("p (c t) -> p c t", c=8)
                psH = TB[:, 0:256].bitcast(BF16).rearrange("p (c t) -> p c t", c=4)

                def gath_gu(b):
                    ws = b % 2
                    indirect(wg[ws][:, :], None, wgv, idxw[:, b:b + 1], NE * 128 - 1, r_wg[ws], [r_rt3], [r_wg[ws]])
                    indirect(wu[ws][:, :], None, wuv, idxw[:, b:b + 1], NE * 128 - 1, r_wu[ws], [r_rt3], [r_wu[ws]])

                def gath_d(b):
                    ws = b % 2
                    indirect(wd[ws][:, :], None, wdv, idxw[:, b:b + 1], NE * 128 - 1, r_wd[ws], [r_rt3], [r_wd[ws]])

                def preC(b):
                    s3 = b % 3
                    fw.dma(fw.sp, xb3[s3][:], xg[b * 128:(b + 1) * 128, :], r_xb3[s3], writes=[r_xb3[s3]])
                    xbv = xb3[s3][:].rearrange("p (q c) -> p c q", c=8)

                    def tr4(e):
                        ins = None
                        for c in range(8):
                            ins = e.transpose(psT3[:, c, :], xbv[:, c, :], identb[:])
                        return ins
                    fw.op(fw.pe, tr4, reads=[r_xb3[s3], r_idb], writes=[r_TA])
                    fw.op(fw.dve, lambda e: e.tensor_copy(out=xgT3[s3][:], in_=psT3), reads=[r_TA], writes=[r_xgT3[s3]])

                def stageG(b):
                    ws = b % 2
                    s3 = b % 3
                    Gb = Gs[ws]
                    wgs = wg[ws][:].rearrange("p (c f) -> p c f", c=8)
                    wus = wu[ws][:].rearrange("p (c f) -> p c f", c=8)

                    def mm_gu(e):
                        ins = None
                        for which, wv in ((0, wgs), (1, wus)):
                            for c in range(8):
                                ins = e.matmul(Gb[:, which, :], xgT3[s3][:, c, :], wv[:, c, :], start=(c == 0), stop=(c == 7))
                        return ins
                    fw.op(fw.pe, mm_gu, reads=[r_xgT3[s3], r_wg[ws], r_wu[ws]], writes=[r_Gs[ws]])
                    fw.op(fw.act, lambda e: e.activation(out=sgl2[ws][:], in_=Gb[:, 0, :], func=AF.Silu), reads=[r_Gs[ws]], writes=[r_sgl2[ws]])
                    fw.op(fw.dve, lambda e: e.tensor_tensor(out=hidtok[ws][:], in0=sgl2[ws][:], in1=Gb[:, 1, :], op=ALU.mult),
                          reads=[r_sgl2[ws], r_Gs[ws]], writes=[r_hidtok[ws]])

                def stageH(b):
                    ws = b % 2
                    wds = wd[ws][:].rearrange("p (c d) -> p c d", c=4)
                    hv = hidtok[ws][:].rearrange("p (q c) -> p c q", c=4)

                    def trH(e):
                        ins = None
                        for c in range(4):
                            ins = e.transpose(psH[:, c, :], hv[:, c, :], identb[:])
                        return ins
                    fw.op(fw.pe, trH, reads=[r_hidtok[ws], r_idb], writes=[r_TB])
                    fw.op(fw.dve, lambda e: e.tensor_copy(out=hidT[ws][:], in_=psH), reads=[r_TB], writes=[r_hid[ws]])

                def stageH2(b):
                    ws = b % 2
                    wds = wd[ws][:].rearrange("p (c d) -> p c d", c=4)

                    def mm_d(e):
                        ins = None
                        for cc in range(2):
                            for fc in range(4):
                                ins = e.matmul(Y0[:, cc, :], hidT[ws][:, fc, :], wds[:, fc, cc * 512:(cc + 1) * 512],
                                               start=(fc == 0), stop=(fc == 3))
                        return ins
                    fw.op(fw.pe, mm_d, reads=[r_hid[ws], r_wd[ws]], writes=[r_Y0])
                    fw.op(fw.act, lambda e: e.activation(out=yb[ws][:], in_=Y0[:].rearrange("p a b -> p (a b)"), func=AF.Copy),
                          reads=[r_Y0], writes=[r_yb[ws]])
                    fw.dma(fw.act, yg[b * 128:(b + 1) * 128, :], yb[ws][:], r_yb[ws], reads=[r_yb[ws]])

                for bb in (0, 1):
                    if bb < NB:
                        gath_gu(bb)
                        gath_d(bb)
                        preC(bb)
                stageG(0)
                for b in range(NB):
                    stageH(b)
                    if b + 2 < NB:
                        gath_gu(b + 2)
                        preC(b + 2)
                    stageH2(b)
                    if b + 1 < NB:
                        stageG(b + 1)
                    if b + 2 < NB:
                        gath_d(b + 2)
                fw.barrier()
            with contextlib.ExitStack() as sd:
                gfin = sb(sd, "gfin", [128, D], F32)
                fw.dma(fw.sp, gfin[:], gfin_b, r_gfin, writes=[r_gfin])
                y1 = [sb(sd, f"y1{i}", [128, D], F32) for i in range(2)]
                y2 = [sb(sd, f"y2{i}", [128, D], F32) for i in range(2)]
                r_y1 = [R("y10"), R("y11")]
                r_y2 = [R("y20"), R("y21")]
                hr = [sb(sd, f"hr{i}", [128, D], F32) for i in range(2)]
                r_hr = [R("hr0"), R("hr1")]
                yt = [sb(sd, f"yt{i}", [128, D], F32) for i in range(2)]
                r_yt = [R("yt0"), R("yt1")]
                junk3 = sb(sd, "junk3", [128, D], BF16)
                r_junk3 = R("junk3")
                ss3 = [sb(sd, f"ss3{i}", [128, 1], F32) for i in range(2)]
                r_ss3 = [R("ss30"), R("ss31")]
                def loadD(tg):
                    sl = tg % 2
                    indirect(y1[sl][:, :], None, yg[:, :], d1i[:, tg:tg + 1], PR - 1, r_y1[sl], [r_rt3], [r_y1[sl]])
                    indirect(y2[sl][:, :], None, yg[:, :], d2i[:, tg:tg + 1], PR - 1, r_y2[sl], [r_rt3], [r_y2[sl]])
                    fw.dma(fw.sp, hr[sl][:], h_d[tg], r_hr[sl], writes=[r_hr[sl]])

                def compD(tg):
                    sl = tg % 2
                    fw.op(fw.dve, lambda e: e.scalar_tensor_tensor(out=hr[sl][:], in0=y1[sl][:], scalar=wts[:, tg, 0:1], in1=hr[sl][:],
                                                                   op0=ALU.mult, op1=ALU.add),
                          reads=[r_y1[sl], r_hr[sl], r_gates[tg]], writes=[r_hr[sl]])
                    fw.op(fw.dve, lambda e: e.scalar_tensor_tensor(out=hr[sl][:], in0=y2[sl][:], scalar=wts[:, tg, 1:2], in1=hr[sl][:],
                                                                   op0=ALU.mult, op1=ALU.add),
                          reads=[r_y2[sl], r_hr[sl], r_gates[tg]], writes=[r_hr[sl]])
                    fw.op(fw.act, lambda e: e.activation(out=junk3[:], in_=hr[sl][:], func=AF.Square, accum_out=ss3[sl][:]),
                          reads=[r_hr[sl]], writes=[r_junk3, r_ss3[sl]])
                    fw.op(fw.act, lambda e: e.activation(out=ss3[sl][:], in_=ss3[sl][:], func=AF.Ln, bias=eps_t[:], scale=1.0 / D),
                          reads=[r_ss3[sl], r_ones], writes=[r_ss3[sl]])
                    fw.op(fw.act, lambda e: e.activation(out=ss3[sl][:], in_=ss3[sl][:], func=AF.Exp, scale=-0.5), reads=[r_ss3[sl]], writes=[r_ss3[sl]])
                    fw.op(fw.dve, lambda e: e.scalar_tensor_tensor(out=yt[sl][:], in0=hr[sl][:], scalar=ss3[sl][:], in1=gfin[:],
                                                                   op0=ALU.mult, op1=ALU.mult),
                          reads=[r_hr[sl], r_ss3[sl], r_gfin], writes=[r_yt[sl]])
                    fw.dma(fw.act, y[tg * 128:(tg + 1) * 128, :], yt[sl][:], r_yt[sl], reads=[r_yt[sl]])

                loadD(0)
                for tg in range(NT):
                    if tg + 1 < NT:
                        loadD(tg + 1)
                    compD(tg)
                fw.barrier()
    return nc


def _const_tables(max_n):
    max_nt = max_n // 128
    inv_freq = (np.float32(10000.0) ** (-np.arange(0, 64, 2, dtype=np.float32) / np.float32(64))).astype(np.float32)
    pos = np.arange(max_n, dtype=np.float32)
    ang = (pos[:, None] * inv_freq[None, :]).astype(np.float32)
    cos = np.cos(ang).astype(np.float32).reshape(max_nt, 128, 32)
    sin = np.sin(ang).astype(np.float32).reshape(max_nt, 128, 32)
    cs = np.stack([cos, sin], axis=2)
    cs = np.ascontiguousarray(cs.transpose(1, 0, 2, 3))
    invc = np.zeros((3, 4, 128), np.float32)
    for g, w in enumerate((2, 4, 8, 16)):
        half = w // 2
        t = np.arange(128)
        invc[0, g] = 1.0 / (np.minimum(t + half, 10 ** 9) - np.maximum(t - half, 0))
        invc[1, g] = 1.0 / w
        invc[2, g] = 1.0 / (np.minimum(t + half, 128) - (t - half))
    invc = np.ascontiguousarray(np.broadcast_to(invc[None], (128, 3, 4, 128))).astype(np.float32)
    return cs, invc


_PROG_CACHE = {}


def run_cores(core_seqs, weights, n_cores, sg_tok):
    seq_lens = tuple(int(s.shape[0]) for s in core_seqs[0])
    key = (seq_lens, sg_tok)
    if key not in _PROG_CACHE:
        _PROG_CACHE[key] = build_program(list(seq_lens), sg_tok)
    nc = _PROG_CACHE[key]
    f32 = np.float32
    W = {k: np.asarray(v, dtype=f32) for k, v in weights.items()}
    cs, invc = _const_tables(max(seq_lens))
    NBh = (2 * sum(seq_lens)) // 128 + NE
    shared = {
        "w_in": np.ascontiguousarray(W["w_in"][0]),
        "w_out": np.ascontiguousarray(W["w_out"][0]),
        "w_pool": np.ascontiguousarray(W["w_pool"][0]),
        "w_gate": np.ascontiguousarray(W["w_gate"][0]).reshape(NE * 128, 8 * DFF),
        "w_up": np.ascontiguousarray(W["w_up"][0]).reshape(NE * 128, 8 * DFF),
        "w_down": np.ascontiguousarray(W["w_down"][0]).reshape(NE * 128, 4 * D),
        "w_r": np.ascontiguousarray(np.concatenate([W["w_router_group"][0], W["w_router_expert"][0]], axis=1)),
        "gmix_t": np.ascontiguousarray(W["g_mix"][0].reshape(8, 128).T),
        "wos_t": np.ascontiguousarray(np.concatenate([W["pool_scale"][0].reshape(4, 128).T,
                                                      np.repeat(W["subln_g"][0][:, None], 4, axis=1)], axis=1)),
        "gffn_b": np.ascontiguousarray(np.broadcast_to(W["g_ffn"][0][None, :], (128, D))),
        "gfin_b": np.ascontiguousarray(np.broadcast_to(W["g_final"][None, :], (128, D))),
        "bias_b": np.ascontiguousarray(np.broadcast_to(
            np.concatenate([W["b_router_group"][0], W["b_router_expert"][0]])[None, :], (128, 36))),
        "lam_b": np.ascontiguousarray(np.broadcast_to(
            np.stack([W["lambda_q1"][0], W["lambda_k1"][0], W["lambda_q2"][0], W["lambda_k2"][0]])[None], (128, 4, 64))),
        "ident_bf": np.eye(128, dtype=f32).astype(ml_dtypes.bfloat16),
        "ident_f": np.eye(128, dtype=f32),
        "cs_tab": cs,
        "invc": invc,
        "tri_bf": np.triu(np.ones((128, 128), f32), k=1).astype(ml_dtypes.bfloat16),
        "bstart": np.ascontiguousarray(np.broadcast_to((np.arange(NBh, dtype=f32) * 128.0)[None, :], (128, NBh))),
        "pidx": np.arange(128, dtype=f32).reshape(128, 1),
    }
    in_maps = []
    for c in range(n_cores):
        m = dict(shared)
        m["x"] = np.ascontiguousarray(np.concatenate([np.asarray(s, dtype=f32) for s in core_seqs[c]], axis=0))
        in_maps.append(m)
    res = run_bass_kernel_spmd(nc, in_maps, core_ids=list(range(n_cores)))
    outs = []
    for c in range(n_cores):
        yc = np.asarray(res.results[c]["y"])
        o = []
        off = 0
        for n in seq_lens:
            o.append(yc[off:off + n])
            off += n
        outs.append(o)
    return outs


def kernel(x_prompt, x_sample, **weights):
    n_cores = 8
    xp = np.asarray(x_prompt)
    xsm = np.asarray(x_sample)
    pp = xp.shape[0] // n_cores
    ps = xsm.shape[0] // n_cores
    core_seqs = []
    for c in range(n_cores):
        core_seqs.append([xp[c * pp + i] for i in range(pp)] + [xsm[c * ps + i] for i in range(ps)])
    outs = run_cores(core_seqs, weights, n_cores, sg_tok=2048)
    yp = np.empty(xp.shape, np.float32)
    ys = np.empty(xsm.shape, np.float32)
    for c in range(n_cores):
        for i in range(pp):
            yp[c * pp + i] = outs[c][i]
        for i in range(ps):
            ys[c * ps + i] = outs[c][pp + i]
    return (yp, ys)
```
